# Optimizing a Trainium2 kernel written in Bass

```python
import math
import jax, jax.numpy as jnp
from jax import lax
import numpy as np

D_MODEL = 2048
BATCH = 4
SEQ = 2048
DEPTH = 4

MEM_LEN = 256
GRID_W = 64
N_MIXERS = 2
EPS = 1e-6
D_MIX = D_MODEL
D_XA = D_MIX // 4
XA_HEADS = 4
XA_DH = D_XA // XA_HEADS
D_SSD = D_MIX - D_XA
SSD_HEADDIM = 64
SSD_HEADS = D_SSD // SSD_HEADDIM
SSD_GROUPS = 4
SSD_STATE = 128
SSD_CONV = 5
SSD_CHUNK = 128
SSD_GN = SSD_GROUPS * SSD_STATE
SSD_CONV_DIM = D_SSD + 2 * SSD_GN
SSD_IN = D_SSD + SSD_CONV_DIM + 2 * SSD_HEADS + D_XA
D_NA = D_MIX - D_XA
NA_DH = 128
NA_HEADS = D_NA // NA_DH
NA_KR = 8
NA_KC = 16
NA_QCB = 16
NA_KBAND = 2 * NA_KC
NA_IN = 3 * D_NA + D_XA
N_EXPERTS = 16
EC_FACTOR = 2
D_EXPERT = D_MODEL // 2

kernel_name = "hybrid_ssd_natten_ecmoe_encoder"


def rmsnorm(x, g):
    xf = x.astype(jnp.float32)
    y = xf * lax.rsqrt(jnp.mean(xf * xf, axis=-1, keepdims=True) + EPS)
    return (y * g.astype(jnp.float32)).astype(x.dtype)


def depthwise_conv_centred(u, w, b):
    K, C = w.shape
    out = lax.conv_general_dilated(u, w[:, None, :].astype(u.dtype), window_strides=(1,),
                                   padding=[(K // 2, K // 2)],
                                   dimension_numbers=("NWC", "WIO", "NWC"),
                                   feature_group_count=C)
    return out + b.astype(u.dtype)


def memory_attention(q, mem_k, mem_v):
    s = jnp.einsum("bthd,bmhd->bhtm", q, mem_k).astype(jnp.float32) * (XA_DH ** -0.5)
    p = jax.nn.softmax(s, axis=-1).astype(mem_v.dtype)
    o = jnp.einsum("bhtm,bmhd->bthd", p, mem_v)
    return o.reshape(q.shape[0], q.shape[1], D_XA)


def ssd_chunked(x, dt, A, Bm, Cm):
    B_, T, H, P = x.shape
    G, N = Bm.shape[2], Bm.shape[3]
    Hg = H // G
    L = SSD_CHUNK
    nc = T // L
    xdt = (x * dt[..., None].astype(x.dtype)).reshape(B_, nc, L, G, Hg, P)
    dA = (dt * A).reshape(B_, nc, L, G, Hg)
    Bc = Bm.reshape(B_, nc, L, G, N)
    Cc = Cm.reshape(B_, nc, L, G, N)
    A_cum = jnp.cumsum(dA, axis=2)
    mask = np.tril(np.ones((L, L), dtype=bool))[None, None, :, :, None, None]
    seg = A_cum[:, :, :, None] - A_cum[:, :, None, :]
    Lmat = jnp.exp(jnp.where(mask, seg, -jnp.inf))
    CB = jnp.einsum("bclgn,bcsgn->bclsg", Cc, Bc)
    y_diag = jnp.einsum("bclsg,bclsgh,bcsghp->bclghp", CB, Lmat, xdt)
    decay_states = jnp.exp(A_cum[:, :, -1:] - A_cum)
    states = jnp.einsum("bclgn,bclgh,bclghp->bcghpn", Bc, decay_states, xdt)
    chunk_decay = jnp.exp(A_cum[:, :, -1])

    def step(h, inp):
        st, dec = inp
        return h * dec[..., None, None].astype(h.dtype) + st, h

    h0 = jnp.zeros((B_, G, Hg, P, N), dtype=states.dtype)
    _, prev = lax.scan(step, h0, (jnp.moveaxis(states, 1, 0), jnp.moveaxis(chunk_decay, 1, 0)))
    prev = jnp.moveaxis(prev, 0, 1)
    y_off = jnp.einsum("bclgn,bcghpn,bclgh->bclghp", Cc, prev, jnp.exp(A_cum))
    return (y_diag + y_off).reshape(B_, T, H, P)


def ssd_mixer(h, mem_k, mem_v, w_in, conv_w, conv_b, dt_bias, a_log, d_skip, gate_norm_g, w_out):
    B_, T, _ = h.shape
    proj = jnp.einsum("btd,de->bte", h, w_in)
    z, xbc, dt_raw, xq = jnp.split(
        proj, [D_SSD, D_SSD + SSD_CONV_DIM, D_SSD + SSD_CONV_DIM + 2 * SSD_HEADS], axis=-1)
    xbc = jax.nn.silu(depthwise_conv_centred(xbc, conv_w, conv_b))
    xs, Bm, Cm = jnp.split(xbc, [D_SSD, D_SSD + SSD_GN], axis=-1)
    xs = xs.reshape(B_, T, SSD_HEADS, SSD_HEADDIM)
    Bm = Bm.reshape(B_, T, SSD_GROUPS, SSD_STATE)
    Cm = Cm.reshape(B_, T, SSD_GROUPS, SSD_STATE)
    dt = jax.nn.softplus(dt_raw.astype(jnp.float32).reshape(B_, T, 2, SSD_HEADS)
                         + dt_bias.astype(jnp.float32))
    A = -jnp.exp(a_log.astype(jnp.float32))
    y_f = ssd_chunked(xs, dt[:, :, 0], A[0], Bm, Cm)
    y_b = ssd_chunked(xs[:, ::-1], dt[:, ::-1, 1], A[1], Bm[:, ::-1], Cm[:, ::-1])[:, ::-1]
    y = y_f + y_b + d_skip[:, None].astype(xs.dtype) * xs
    y = (y.reshape(B_, T, D_SSD) * jax.nn.silu(z)).astype(h.dtype)
    y = rmsnorm(y.reshape(B_, T, SSD_GROUPS, D_SSD // SSD_GROUPS),
                gate_norm_g.reshape(SSD_GROUPS, D_SSD // SSD_GROUPS)).reshape(B_, T, D_SSD)
    o_x = memory_attention(xq.reshape(B_, T, XA_HEADS, XA_DH), mem_k, mem_v).astype(h.dtype)
    return jnp.einsum("bte,ed->btd", jnp.concatenate([y, o_x], axis=-1), w_out)


def neighbourhood_attention(q, k, v, rpb):
    B_, T, H, Dh = q.shape
    rows = T // GRID_W
    kr = min(NA_KR, rows)
    q = q.reshape(B_, rows, GRID_W, H, Dh)
    k = k.reshape(B_, rows, GRID_W, H, Dh)
    v = v.reshape(B_, rows, GRID_W, H, Dh)
    n_cb = GRID_W // NA_QCB
    band_start = np.clip(np.arange(n_cb) * NA_QCB - NA_KC // 2, 0, GRID_W - NA_KBAND)
    key_cols = band_start[:, None] + np.arange(NA_KBAND)
    q_cols = np.arange(GRID_W).reshape(n_cb, NA_QCB)
    win_start = np.clip(q_cols - NA_KC // 2, 0, GRID_W - NA_KC)
    kc = key_cols[:, None, :]
    col_mask = (kc >= win_start[..., None]) & (kc < win_start[..., None] + NA_KC)
    dc_idx = np.clip(kc - q_cols[..., None] + NA_KC - 1, 0, 2 * NA_KC - 2)
    scale = Dh ** -0.5

    def row_block(args):
        r, q_r = args
        rs = jnp.clip(r - kr // 2, 0, rows - kr)
        k_rows = lax.dynamic_slice_in_dim(k, rs, kr, axis=1)
        v_rows = lax.dynamic_slice_in_dim(v, rs, kr, axis=1)
        k_band = k_rows[:, :, key_cols]
        v_band = v_rows[:, :, key_cols]
        qb = q_r.reshape(B_, n_cb, NA_QCB, H, Dh)
        s = jnp.einsum("bjqhd,brjkhd->bhjqrk", qb, k_band).astype(jnp.float32) * scale
        dr_idx = rs + jnp.arange(kr) - r + NA_KR - 1
        bias = rpb[:, dr_idx[None, None, :, None], dc_idx[:, :, None, :]]
        s = s + bias[None].astype(jnp.float32)
        s = jnp.where(col_mask[None, None, :, :, None, :], s, -1e30)
        p = jax.nn.softmax(s.reshape(B_, H, n_cb, NA_QCB, kr * NA_KBAND), axis=-1)
        p = p.reshape(B_, H, n_cb, NA_QCB, kr, NA_KBAND).astype(v.dtype)
        o = jnp.einsum("bhjqrk,brjkhd->bjqhd", p, v_band)
        return o.reshape(B_, GRID_W, H, Dh)

    outs = lax.map(row_block, (jnp.arange(rows), jnp.moveaxis(q, 1, 0)))
    return jnp.moveaxis(outs, 0, 1).reshape(B_, T, H * Dh)


def na_mixer(h, mem_k, mem_v, w_in, rpb, w_out):
    B_, T, _ = h.shape
    proj = jnp.einsum("btd,de->bte", h, w_in)
    q, k, v, xq = jnp.split(proj, [D_NA, 2 * D_NA, 3 * D_NA], axis=-1)
    shp = (B_, T, NA_HEADS, NA_DH)
    o_na = neighbourhood_attention(q.reshape(shp), k.reshape(shp), v.reshape(shp), rpb).astype(h.dtype)
    o_x = memory_attention(xq.reshape(B_, T, XA_HEADS, XA_DH), mem_k, mem_v).astype(h.dtype)
    return jnp.einsum("bte,ed->btd", jnp.concatenate([o_na, o_x], axis=-1), w_out)


def ec_moe(h, w_router, w1, w3, w2):
    B_, T, _ = h.shape
    cap = EC_FACTOR * T // N_EXPERTS
    logits = jnp.einsum("btd,de->bte", h, w_router).astype(jnp.float32)
    aff = jax.nn.softmax(logits, axis=-1)
    gate, idx = lax.top_k(jnp.swapaxes(aff, 1, 2), cap)
    bidx = jnp.arange(B_)[:, None, None]
    xs = h[bidx, idx]
    a = jnp.einsum("becd,edf->becf", xs, w1)
    b = jnp.einsum("becd,edf->becf", xs, w3)
    y = jnp.einsum("becf,efd->becd", jax.nn.silu(a) * b, w2)
    y = y * gate[..., None].astype(y.dtype)
    return jnp.zeros_like(h).at[bidx, idx].add(y.astype(h.dtype))


def setup_inputs(seed: int = 0) -> dict:
    key = jax.random.key(seed)
    ks = jax.random.split(key, 24)
    n_ssd = (DEPTH + N_MIXERS - 1) // N_MIXERS
    n_na = DEPTH // N_MIXERS
    f32 = jnp.float32

    def nrm(k, shape, scale):
        return jax.random.normal(k, shape, dtype=f32) * scale

    dt0 = jnp.exp(jax.random.uniform(ks[9], (n_ssd, 2, SSD_HEADS), dtype=f32,
                                     minval=math.log(1e-3), maxval=math.log(1e-1)))
    return {
        "x": nrm(ks[0], (BATCH, SEQ, D_MODEL), 1.0),
        "mem": nrm(ks[1], (BATCH, MEM_LEN, D_MODEL), 1.0),
        "norm_mix_g": 1.0 + nrm(ks[2], (DEPTH, D_MODEL), 0.02),
        "norm_ffn_g": 1.0 + nrm(ks[3], (DEPTH, D_MODEL), 0.02),
        "norm_final_g": 1.0 + nrm(ks[4], (D_MODEL,), 0.02),
        "mem_norm_g": 1.0 + nrm(ks[5], (D_MODEL,), 0.02),
        "ssd_w_in": nrm(ks[6], (n_ssd, D_MODEL, SSD_IN), D_MODEL ** -0.5),
        "ssd_conv_w": nrm(ks[7], (n_ssd, SSD_CONV, SSD_CONV_DIM), SSD_CONV ** -0.5),
        "ssd_conv_b": nrm(ks[8], (n_ssd, SSD_CONV_DIM), 0.02),
        "ssd_dt_bias": dt0 + jnp.log(-jnp.expm1(-dt0)),
        "ssd_a_log": jnp.log(jax.random.uniform(ks[10], (n_ssd, 2, SSD_HEADS), dtype=f32,
                                                minval=1.0, maxval=16.0)),
        "ssd_d": 1.0 + nrm(ks[11], (n_ssd, SSD_HEADS), 0.02),
        "ssd_gate_norm_g": 1.0 + nrm(ks[12], (n_ssd, D_SSD), 0.02),
        "ssd_w_out": nrm(ks[13], (n_ssd, D_MIX, D_MODEL), D_MIX ** -0.5),
        "na_w_in": nrm(ks[14], (n_na, D_MODEL, NA_IN), D_MODEL ** -0.5),
        "na_rpb": nrm(ks[15], (n_na, NA_HEADS, 2 * NA_KR - 1, 2 * NA_KC - 1), 0.1),
        "na_w_out": nrm(ks[16], (n_na, D_MIX, D_MODEL), D_MIX ** -0.5),
        "xa_w_kv": nrm(ks[17], (DEPTH, D_MODEL, 2 * D_XA), D_MODEL ** -0.5),
        "moe_w_router": nrm(ks[18], (DEPTH, D_MODEL, N_EXPERTS), D_MODEL ** -0.5),
        "moe_w1": nrm(ks[19], (DEPTH, N_EXPERTS, D_MODEL, D_EXPERT), D_MODEL ** -0.5),
        "moe_w3": nrm(ks[20], (DEPTH, N_EXPERTS, D_MODEL, D_EXPERT), D_MODEL ** -0.5),
        "moe_w2": nrm(ks[21], (DEPTH, N_EXPERTS, D_EXPERT, D_MODEL), D_EXPERT ** -0.5),
    }


def reference(x, mem, norm_mix_g, norm_ffn_g, norm_final_g, mem_norm_g,
              ssd_w_in, ssd_conv_w, ssd_conv_b, ssd_dt_bias, ssd_a_log, ssd_d,
              ssd_gate_norm_g, ssd_w_out, na_w_in, na_rpb, na_w_out, xa_w_kv,
              moe_w_router, moe_w1, moe_w3, moe_w2):
    B_, M = mem.shape[0], mem.shape[1]
    mem_n = rmsnorm(mem, mem_norm_g)
    for i in range(DEPTH):
        j = i // N_MIXERS
        kv = jnp.einsum("bmd,de->bme", mem_n, xa_w_kv[i]).reshape(B_, M, 2, XA_HEADS, XA_DH)
        mem_k, mem_v = kv[:, :, 0], kv[:, :, 1]
        h = rmsnorm(x, norm_mix_g[i])
        if i % N_MIXERS == 0:
            mix = ssd_mixer(h, mem_k, mem_v, ssd_w_in[j], ssd_conv_w[j], ssd_conv_b[j],
                            ssd_dt_bias[j], ssd_a_log[j], ssd_d[j], ssd_gate_norm_g[j], ssd_w_out[j])
        else:
            mix = na_mixer(h, mem_k, mem_v, na_w_in[j], na_rpb[j], na_w_out[j])
        x = x + mix.astype(x.dtype)
        h = rmsnorm(x, norm_ffn_g[i])
        x = x + ec_moe(h, moe_w_router[i], moe_w1[i], moe_w3[i], moe_w2[i])
    return rmsnorm(x, norm_final_g)
```

```python
import contextlib
import numpy as np
import concourse.bass as bass
import concourse.mybir as mybir
from concourse.bass_utils import run_bass_kernel_spmd

F32 = mybir.dt.float32
BF16 = mybir.dt.bfloat16
I32 = mybir.dt.int32
AF = mybir.ActivationFunctionType
ALU = mybir.AluOpType
AX = mybir.AxisListType

T = 2048
D = 2048
NT = 16
ND = 16
DEPTH = 4
MEM = 256
EPS = 1e-6
D_XA = 512
D_SSD = 1536
SSD_H = 24
SSD_P = 64
SSD_G = 4
SSD_N = 128
SSD_CONV_DIM = 2560
SSD_IN = 4656
NA_H = 12
NA_IN = 5120
NE = 16
CAP = 256
DFF = 1024
NEG = -30000.0

ENGS = ("pe", "act", "dve", "pool", "sp")
ENGOBJ = {"pe": "tensor", "act": "scalar", "dve": "vector", "pool": "gpsimd", "sp": "sync"}
SEM_BLOCK = 30000


class Buf:
    __slots__ = ("name", "last_w", "readers", "sem", "base", "ndma")

    def __init__(self, name):
        self.name = name
        self.last_w = None
        self.readers = []
        self.sem = None
        self.base = 0
        self.ndma = 0


class Op:
    __slots__ = ("eng", "fn", "is_dma", "deps", "need_sig", "sig", "dst", "k")


class _Rec:
    def __init__(self):
        self.call = None

    def __getattr__(self, name):
        def f(*a, **kw):
            self.call = (name, a, kw)
            return self
        return f


class Prog:
    def __init__(self, nc, stack):
        self.nc = nc
        self.stack = stack
        self.q = {e: [] for e in ENGS}
        self.bufs = []
        self.eng_cnt = {e: 0 for e in ENGS}
        self.eng_sems = {}
        self.dma_pool = []
        self.n_sems = 0
        self.n_ops = 0

    def buf(self, name="b"):
        b = Buf(name)
        self.bufs.append(b)
        return b

    def _newsem(self, name):
        self.n_sems += 1
        return self.stack.enter_context(self.nc.semaphore(f"{name}_{self.n_sems}"))

    def op(self, eng, fn, reads=(), writes=(), dma=False):
        o = Op()
        rec = _Rec()
        fn(rec)
        assert rec.call is not None
        o.eng, o.fn, o.is_dma = eng, rec.call, dma
        o.need_sig, o.sig, o.dst, o.k = False, None, None, 0
        deps, seen = [], set()
        for r in reads:
            if r.last_w is not None:
                deps.append(r.last_w)
        for w in writes:
            if w.last_w is not None:
                deps.append(w.last_w)
            deps.extend(w.readers)
        dd = []
        for d in deps:
            if id(d) in seen or d is o:
                continue
            seen.add(id(d))
            if d.eng == "pe" and eng == "pe" and not d.is_dma and not dma:
                continue
            d.need_sig = True
            dd.append(d)
        o.deps = dd
        if dma:
            o.dst = writes[0]
            o.dst.ndma += 1
            o.k = o.dst.ndma
        for r in reads:
            r.readers.append(o)
        for w in writes:
            w.last_w = o
            w.readers = []
        self.q[eng].append(o)
        self.n_ops += 1
        return o

    def flush(self):
        nc = self.nc
        if not any(self.q[e] for e in ENGS):
            return
        lasts = []
        for e in ENGS:
            comp = [o for o in self.q[e] if not o.is_dma]
            if comp:
                comp[-1].need_sig = True
                lasts.append(comp[-1])
        dma_bufs = []
        for e in ENGS:
            for o in self.q[e]:
                if o.is_dma:
                    b = o.dst
                    if b.sem is None:
                        if self.dma_pool:
                            b.sem, b.base = self.dma_pool.pop(0)
                        else:
                            b.sem, b.base = self._newsem("d"), 0
                        dma_bufs.append(b)
                    o.sig = (b.sem, b.base + 16 * o.k)
                elif o.need_sig:
                    c = self.eng_cnt[e]
                    blk = c // SEM_BLOCK
                    key = (e, blk)
                    if key not in self.eng_sems:
                        self.eng_sems[key] = self._newsem(e)
                    o.sig = (self.eng_sems[key], c % SEM_BLOCK + 1)
                    self.eng_cnt[e] = c + 1
        finals = [o.sig for o in lasts] + [(b.sem, b.base + 16 * b.ndma) for b in dma_bufs]

        def run_queue(e, eng):
            waited = {}
            for o in self.q[e]:
                for d in o.deps:
                    sem, val = d.sig
                    if waited.get(id(sem), 0) >= val:
                        continue
                    waited[id(sem)] = val
                    eng.wait_ge(sem, val)
                nm, a_, kw_ = o.fn
                try:
                    ins = getattr(eng, nm)(*a_, **kw_)
                except Exception:
                    print("EMIT FAIL", e, nm, [getattr(x, "shape", x) for x in a_], {k_: getattr(v_, "shape", v_) for k_, v_ in kw_.items()}, flush=True)
                    for k_, v_ in kw_.items():
                        print("   ARG", k_, repr(v_)[:300], repr(getattr(v_, "ap", None))[:300], flush=True)
                    raise
                if o.sig is not None:
                    ins.then_inc(o.sig[0], 16 if o.is_dma else 1)
            for sem, val in finals:
                if waited.get(id(sem), 0) >= val:
                    continue
                eng.wait_ge(sem, val)

        with nc.Block() as block:
            for e in ENGS:
                def mk(e):
                    return lambda eng: run_queue(e, eng)
                getattr(block, ENGOBJ[e])(mk(e))
        for b in dma_bufs:
            cnt = b.base + 16 * b.ndma
            assert cnt < 32000, "dma semaphore count too large"
            self.dma_pool.append((b.sem, cnt))
        for b in self.bufs:
            b.last_w, b.readers, b.sem, b.base, b.ndma = None, [], None, 0, 0
        self.q = {e: [] for e in ENGS}


class Tl:
    __slots__ = ("t", "b")

    def __init__(self, t, b):
        self.t, self.b = t, b


class Scope:
    def __init__(self, k):
        self.k = k
        self.st = contextlib.ExitStack()

    def __enter__(self):
        self.st.__enter__()
        return self

    def __exit__(self, *a):
        self.k.P.flush()
        return self.st.__exit__(*a)

    def sb(self, name, shape, dt):
        self.k.uid += 1
        t = self.st.enter_context(self.k.nc.sbuf_tensor(f"{name}_{self.k.uid}", list(shape), dt))
        return Tl(t, self.k.P.buf(name))

    def ps(self, name, shape, dt=F32):
        self.k.uid += 1
        t = self.st.enter_context(self.k.nc.psum_tensor(f"{name}_{self.k.uid}", list(shape), dt))
        return Tl(t, self.k.P.buf(name))


class KB:
    def __init__(self, nlayers=DEPTH, dbg=None):
        self.nlayers = nlayers
        self.dbg = dbg
        self.uid = 0
        self.nc = bass.Bass("TRN2", target_bir_lowering=False)
        self.outer = contextlib.ExitStack()
        self.P = Prog(self.nc, self.outer)

    def din(self, name, shape, dt=F32):
        return self.nc.dram_tensor(name, list(shape), dt, kind="ExternalInput").ap()

    def dscr(self, name, shape, dt):
        return self.nc.dram_tensor(name, list(shape), dt).ap()

    def op(self, *a, **k):
        return self.P.op(*a, **k)

    def dma(self, q, out, in_, reads, writes, **kw):
        return self.P.op(q, lambda e: e.dma_start(out=out, in_=in_, **kw), reads=reads, writes=writes, dma=True)

    def bcast_load(self, q, dst, row_ap, nparts=128):
        return self.dma(q, dst.t[:], row_ap.partition_broadcast(nparts), [], [dst.b])

    def build(self):
        nc = self.nc
        L = self.nlayers
        n_ssd = (L + 1) // 2
        n_na = L // 2
        I = {}
        self.moeh = [self.dscr(f"moeh{cq}", [T, 512], F32) for cq in range(4)]
        self.hn = self.dscr("hn", [T, D], BF16)
        I["x"] = self.din("x", [T, D])
        I["mem"] = self.din("mem", [MEM, D])
        I["norm_mix_g"] = self.din("norm_mix_g", [L, D])
        I["norm_ffn_g"] = self.din("norm_ffn_g", [L, D])
        I["norm_final_g"] = self.din("norm_final_g", [1, D])
        I["mem_norm_g"] = self.din("mem_norm_g", [1, D])
        I["ssd_w_in"] = self.din("ssd_w_in", [n_ssd, D, SSD_IN])
        I["ssd_conv_w"] = self.din("ssd_conv_w", [n_ssd, 5, SSD_CONV_DIM])
        I["ssd_conv_b"] = self.din("ssd_conv_b", [n_ssd, SSD_CONV_DIM])
        I["ssd_dt_bias"] = self.din("ssd_dt_bias", [n_ssd, 48])
        I["ssd_a_log"] = self.din("ssd_a_log", [n_ssd, 48])
        I["ssd_d"] = self.din("ssd_d", [n_ssd, SSD_H])
        I["ssd_gate_norm_g"] = self.din("ssd_gate_norm_g", [n_ssd, D_SSD])
        I["ssd_w_out"] = self.din("ssd_w_out", [n_ssd, D, D])
        if n_na:
            I["na_w_in"] = self.din("na_w_in", [n_na, D, NA_IN])
            I["na_bias"] = self.din("na_bias", [n_na, NA_H, 128, 25, 128])
            I["na_w_out"] = self.din("na_w_out", [n_na, D, D])
        I["xa_w_kv"] = self.din("xa_w_kv", [L, D, 2 * D_XA])
        I["moe_w_router"] = self.din("moe_w_router", [L, D, NE])
        I["moe_w1"] = self.din("moe_w1", [L, NE, D, DFF])
        I["moe_w3"] = self.din("moe_w3", [L, NE, D, DFF])
        I["moe_w2"] = self.din("moe_w2", [L, NE, DFF, D])
        self.I = I
        self.out = nc.dram_tensor("out", [T, D], F32, kind="ExternalOutput").ap()
        self.xres = self.dscr("xres", [T, D], F32)
        self.ycat = self.dscr("ycat", [T, D], BF16)
        self.szd = self.dscr("szd", [T, D_SSD], BF16)
        self.Gd = self.dscr("Gd", [NT, 128, D_SSD], BF16)
        self.ABd = self.dscr("ABd", [NT, 2 * SSD_H * 128], F32)
        self.TOTd = self.dscr("TOTd", [1, NT * 48], F32)
        self.xsBd = self.dscr("xsBd", [T, 2048], BF16)
        self.BCTd = self.dscr("BCTd", [8, 128, T], BF16)
        self.TOKd = self.dscr("TOKd", [T, 4 * 48], F32)

        with self.outer:
            with Scope(self) as cs:
                self.consts(cs)
                for i in range(L):
                    if i % 2 == 0:
                        self.ssd_layer(cs, i)
                    else:
                        self.na_layer(cs, i)
                    if self.dbg == ("mix", i):
                        self.dump_xres(cs)
                        return self.nc
                    self.moe_layer(cs, i)
                    if self.dbg == ("moe", i):
                        self.dump_xres(cs)
                        return self.nc
                self.final_norm(cs)
        return self.nc

    def consts(self, cs):
        k = self
        self.identf = cs.sb("identf", [128, 128], F32)
        self.ident = cs.sb("ident", [128, 128], BF16)
        self.ones_bf = cs.sb("ones_bf", [128, 128], BF16)
        self.maskf = cs.sb("maskf", [128, 128], F32)
        self.maskb = cs.sb("maskb", [128, 128], F32)
        self.iota256 = cs.sb("iota256", [128, 256], F32)
        self.memT = cs.sb("memT", [128, ND, MEM], BF16)
        self.epsc = cs.sb("epsc", [128, 1], F32)
        idf, idb = self.identf, self.ident
        k.op("pool", lambda e: e.memset(idf.t[:], 0.0), writes=[idf.b])
        k.op("pool", lambda e: e.affine_select(out=idf.t[:], in_=idf.t[:], pattern=[[-1, 128]], compare_op=ALU.not_equal,
                                               fill=1.0, base=0, channel_multiplier=1), reads=[idf.b], writes=[idf.b])
        k.op("dve", lambda e: e.tensor_copy(out=idb.t[:], in_=idf.t[:]), reads=[idf.b], writes=[idb.b])
        k.op("pool", lambda e: e.memset(self.ones_bf.t[:], 1.0), writes=[self.ones_bf.b])
        k.op("pool", lambda e: e.memset(self.epsc.t[:], EPS), writes=[self.epsc.b])
        k.op("pool", lambda e: e.memset(self.maskf.t[:], 1.0), writes=[self.maskf.b])
        k.op("pool", lambda e: e.affine_select(out=self.maskf.t[:], in_=self.maskf.t[:], pattern=[[1, 128]], compare_op=ALU.is_ge,
                                               fill=0.0, base=0, channel_multiplier=-1), reads=[self.maskf.b], writes=[self.maskf.b])
        k.op("pool", lambda e: e.memset(self.maskb.t[:], 1.0), writes=[self.maskb.b])
        k.op("pool", lambda e: e.affine_select(out=self.maskb.t[:], in_=self.maskb.t[:], pattern=[[-1, 128]], compare_op=ALU.is_ge,
                                               fill=0.0, base=0, channel_multiplier=1), reads=[self.maskb.b], writes=[self.maskb.b])
        k.op("pool", lambda e: e.iota(self.iota256.t[:], pattern=[[1, 256]], base=0, channel_multiplier=0,
                                      allow_small_or_imprecise_dtypes=True), writes=[self.iota256.b])
        xb = self.P.buf("xres_all")
        k.dma("sp", self.xres[:, :], self.I["x"][:, :], [], [xb])
        self.P.flush()
        with Scope(self) as s:
            g = s.sb("g", [128, D], F32)
            k.bcast_load("sp", g, self.I["mem_norm_g"][0:1, :])
            for mt in range(2):
                self.norm_tile(s, self.I["mem"][mt * 128:(mt + 1) * 128, :], g, dstT=self.memT, tcol=mt * 128, tag=f"m{mt}")

    def norm_tile(self, s, src_ap, g, dstT=None, tcol=0, tag="", hn_dst=None, want_f32=None, q="sp"):
        k = self
        if not hasattr(s, "_nt"):
            s._nt = {}
            for j in range(2):
                s._nt[j] = dict(
                    xt=s.sb("n_xt", [128, D], F32), ss=s.sb("n_ss", [128, 1], F32),
                    hb=s.sb("n_hb", [128, D], BF16), pt=s.ps("n_pt", [128, 8, 128], BF16))
            s._ntc = 0
        R = s._nt[s._ntc % 2]
        s._ntc += 1
        xt, ss, hb, pt = R["xt"], R["ss"], R["hb"], R["pt"]
        k.dma(q, xt.t[:], src_ap, [], [xt.b])
        k.op("act", lambda e: e.activation(out=hb.t[:], in_=xt.t[:], func=AF.Square, accum_out=ss.t[:]), reads=[xt.b], writes=[hb.b, ss.b])
        k.op("act", lambda e: e.activation(out=ss.t[:], in_=ss.t[:], func=AF.Sqrt, scale=1.0 / D, bias=self.epsc.t[:]), reads=[ss.b, self.epsc.b], writes=[ss.b])
        k.op("dve", lambda e: e.reciprocal(out=ss.t[:], in_=ss.t[:]), reads=[ss.b], writes=[ss.b])
        hf = want_f32 if want_f32 is not None else xt
        k.op("dve", lambda e: e.scalar_tensor_tensor(out=hf.t[:], in0=xt.t[:], scalar=ss.t[:, 0:1], in1=g.t[:], op0=ALU.mult, op1=ALU.mult),
             reads=[xt.b, ss.b, g.b], writes=[hf.b])
        k.op("pool", lambda e: e.tensor_copy(out=hb.t[:], in_=hf.t[:]), reads=[hf.b], writes=[hb.b])
        if hn_dst is not None:
            k.dma("sp", hn_dst[0], hb.t[:], [hb.b], [hn_dst[1]])
        if dstT is not None:
            for half in range(2):
                for j in range(8):
                    kk = half * 8 + j
                    k.op("pe", lambda e, kk=kk, j=j: e.transpose(out=pt.t[:, j, :], in_=hb.t[:, kk * 128:(kk + 1) * 128], identity=self.ident.t[:]),
                         reads=[hb.b, self.ident.b], writes=[pt.b])
                eng = "dve" if half == 0 else "act"
                if eng == "dve":
                    k.op("dve", lambda e, half=half: e.tensor_copy(out=dstT.t[:, half * 8:(half + 1) * 8, tcol:tcol + 128], in_=pt.t[:]),
                         reads=[pt.b], writes=[dstT.b])
                else:
                    k.op("act", lambda e, half=half: e.activation(out=dstT.t[:, half * 8:(half + 1) * 8, tcol:tcol + 128], in_=pt.t[:], func=AF.Copy),
                         reads=[pt.b], writes=[dstT.b])

    def dump_xres(self, cs):
        self.P.flush()
        ob = self.P.buf("out")
        self.dma("sp", self.out[:, :], self.xres[:, :], [], [ob])
        self.P.flush()

    def final_norm(self, cs):
        k = self
        with Scope(self) as s:
            g = s.sb("g", [128, D], F32)
            k.bcast_load("sp", g, self.I["norm_final_g"][0:1, :])
            outs = [s.sb("fo", [128, D], F32) for _ in range(2)]
            ob = self.P.buf("out")
            for tt in range(NT):
                o = outs[tt % 2]
                self.norm_tile(s, self.xres[tt * 128:(tt + 1) * 128, :], g, want_f32=o, tag=f"f{tt}")
                k.dma("sp", self.out[tt * 128:(tt + 1) * 128, :], o.t[:], [o.b], [ob])

    def wblock(self, s, w_ap, c0, ncols, nk=ND):
        if not hasattr(s, "_wb"):
            s._wb = [s.sb("wblk", [128, ND, 512], BF16) for _ in range(2)]
            s._wbc = 0
        w = s._wb[s._wbc % 2]
        s._wbc += 1
        self.dma("pool", w.t[:, 0:nk, 0:ncols], w_ap[:, c0:c0 + ncols].rearrange("(k p) n -> p k n", p=128), [], [w.b])
        return w

    def proj_fm(self, s, hT, w, ncols, ps_bank_tiles, et):
        m0 = et * 128
        m = min(128, ncols - m0)
        for tc in range(4):
            pb = ps_bank_tiles[tc]
            for kk in range(ND):
                self.op("pe", lambda e, kk=kk, tc=tc, pb=pb: e.matmul(pb.t[0:m, :], lhsT=w.t[:, kk, m0:m0 + m], rhs=hT.t[:, kk, tc * 512:(tc + 1) * 512],
                                                                     start=(kk == 0), stop=(kk == ND - 1)),
                        reads=[w.b, hT.b], writes=[pb.b])

    def proj_tm(self, hT, w, ncols, pb, tt):
        for kk in range(ND):
            self.op("pe", lambda e, kk=kk: e.matmul(pb.t[:, 0:ncols], lhsT=hT.t[:, kk, tt * 128:(tt + 1) * 128], rhs=w.t[:, kk, 0:ncols],
                                                    start=(kk == 0), stop=(kk == ND - 1)),
                    reads=[w.b, hT.b], writes=[pb.b])

    def norm_to_hT(self, s, hT, gain_row):
        g = s.sb("g", [128, D], F32)
        self.bcast_load("sp", g, gain_row)
        for tt in range(NT):
            self.norm_tile(s, self.xres[tt * 128:(tt + 1) * 128, :], g, dstT=hT, tcol=tt * 128)

    def xa_block(self, s, li, hT, wq_ap, c0):
        k = self
        qT = s.sb("xa_qT", [128, 4, T], BF16)
        kT = s.sb("xa_kT", [128, 4, MEM], BF16)
        va = s.sb("xa_va", [128, 2, 4, 129], BF16)
        pbs = [s.ps("xa_pb", [128, 512]) for _ in range(4)]
        w = self.wblock(s, wq_ap, c0, 512)
        for et in range(4):
            self.proj_fm(s, hT, w, 512, pbs, et)
            for tc in range(4):
                eng = "act" if tc % 2 == 0 else "dve"
                if eng == "act":
                    k.op("act", lambda e, et=et, tc=tc: e.activation(out=qT.t[:, et, tc * 512:(tc + 1) * 512], in_=pbs[tc].t[:], func=AF.Copy),
                         reads=[pbs[tc].b], writes=[qT.b])
                else:
                    k.op("dve", lambda e, et=et, tc=tc: e.tensor_copy(out=qT.t[:, et, tc * 512:(tc + 1) * 512], in_=pbs[tc].t[:]),
                         reads=[pbs[tc].b], writes=[qT.b])
        wkv = self.I["xa_w_kv"][li]
        w = self.wblock(s, wkv, 0, 512)
        for et in range(4):
            pb = pbs[et]
            for kk in range(ND):
                k.op("pe", lambda e, kk=kk, et=et, pb=pb: e.matmul(pb.t[:, 0:MEM], lhsT=w.t[:, kk, et * 128:(et + 1) * 128], rhs=self.memT.t[:, kk, :],
                                                                  start=(kk == 0), stop=(kk == ND - 1)), reads=[w.b, self.memT.b], writes=[pb.b])
            k.op("act", lambda e, et=et, pb=pb: e.activation(out=kT.t[:, et, :], in_=pb.t[:, 0:MEM], func=AF.Copy), reads=[pb.b], writes=[kT.b])
        w = self.wblock(s, wkv, 512, 512)
        k.op("pool", lambda e: e.memset(va.t[:], 1.0), writes=[va.b])
        for mt in range(2):
            pb = pbs[mt]
            for kk in range(ND):
                k.op("pe", lambda e, kk=kk, mt=mt, pb=pb: e.matmul(pb.t[:, :], lhsT=self.memT.t[:, kk, mt * 128:(mt + 1) * 128], rhs=w.t[:, kk, :],
                                                                  start=(kk == 0), stop=(kk == ND - 1)), reads=[w.b, self.memT.b], writes=[pb.b])
            k.op("dve", lambda e, mt=mt, pb=pb: e.tensor_copy(out=va.t[:, mt, :, 0:128], in_=pb.t[:].rearrange("p (h d) -> p h d", h=4)),
                 reads=[pb.b], writes=[va.b])
        sps = [s.ps("xa_s", [128, 2, 128]) for _ in range(2)]
        ops_ = [s.ps("xa_o", [128, 4, 256]) for _ in range(1)]
        pT = [s.sb("xa_p", [128, 2, 128], BF16) for _ in range(2)]
        rc = [s.sb("xa_rc", [128, 4], F32) for _ in range(2)]
        ot = [s.sb("xa_ot", [128, 512], BF16) for _ in range(2)]
        yb = self.P.buf("ycat_xa")
        scale = 128.0 ** -0.5
        it = 0
        for tt in range(NT):
            o_ps, o_t, r_c = ops_[0], ot[tt % 2], rc[tt % 2]
            for h in range(4):
                sp_, p_ = sps[it % 2], pT[it % 2]
                it += 1
                for mt in range(2):
                    k.op("pe", lambda e, mt=mt, h=h, sp_=sp_: e.matmul(sp_.t[:, mt, :], lhsT=kT.t[:, h, mt * 128:(mt + 1) * 128], rhs=qT.t[:, h, tt * 128:(tt + 1) * 128],
                                                                      start=True, stop=True), reads=[kT.b, qT.b], writes=[sp_.b])
                k.op("act", lambda e, sp_=sp_, p_=p_: e.activation(out=p_.t[:], in_=sp_.t[:], func=AF.Exp, scale=scale), reads=[sp_.b], writes=[p_.b])
                for mt in range(2):
                    k.op("pe", lambda e, mt=mt, h=h, p_=p_, o_ps=o_ps: e.matmul(o_ps.t[:, h, 0:129], lhsT=p_.t[:, mt, :], rhs=va.t[:, mt, h, :],
                                                                               start=(mt == 0), stop=(mt == 1)), reads=[p_.b, va.b], writes=[o_ps.b])
            k.op("dve", lambda e, o_ps=o_ps, r_c=r_c: e.reciprocal(out=r_c.t[:], in_=o_ps.t[:, :, 128]), reads=[o_ps.b], writes=[r_c.b])
            k.op("dve", lambda e, o_ps=o_ps, r_c=r_c, o_t=o_t: e.tensor_tensor(out=o_t.t[:].rearrange("p (h d) -> p h d", h=4), in0=o_ps.t[:, :, 0:128],
                                                                               in1=r_c.t[:].unsqueeze(2).to_broadcast([128, 4, 128]), op=ALU.mult),
                 reads=[o_ps.b, r_c.b], writes=[o_t.b])
            k.dma("sp", self.ycat[tt * 128:(tt + 1) * 128, D_SSD:D], o_t.t[:], [o_t.b], [yb])

    def out_proj(self, w_ap):
        k = self
        with Scope(self) as s:
            W = s.sb("wout", [128, ND, D], BF16)
            for c in range(4):
                k.dma("pool", W.t[:, :, c * 512:(c + 1) * 512], w_ap[:, c * 512:(c + 1) * 512].rearrange("(k p) n -> p k n", p=128), [], [W.b])
            yt = [s.sb("op_y", [128, D], BF16) for _ in range(2)]
            yT = [s.sb("op_yT", [128, ND, 128], BF16) for _ in range(2)]
            pt = [s.ps("op_pt", [128, 8, 128], BF16) for _ in range(2)]
            po = [s.ps("op_po", [128, 512]) for _ in range(4)]
            mix = [s.sb("op_mix", [128, D], F32) for _ in range(2)]
            xb = self.P.buf("xres_w")
            for tt in range(NT):
                y_, yT_, mx = yt[tt % 2], yT[tt % 2], mix[tt % 2]
                k.dma("sp", y_.t[:], self.ycat[tt * 128:(tt + 1) * 128, :], [], [y_.b])
                for half in range(2):
                    p_ = pt[half]
                    for j in range(8):
                        kk = half * 8 + j
                        k.op("pe", lambda e, kk=kk, j=j, p_=p_, y_=y_: e.transpose(out=p_.t[:, j, :], in_=y_.t[:, kk * 128:(kk + 1) * 128], identity=self.ident.t[:]),
                             reads=[y_.b, self.ident.b], writes=[p_.b])
                    if half == 0:
                        k.op("dve", lambda e, p_=p_, yT_=yT_: e.tensor_copy(out=yT_.t[:, 0:8, :], in_=p_.t[:]), reads=[p_.b], writes=[yT_.b])
                    else:
                        k.op("act", lambda e, p_=p_, yT_=yT_: e.activation(out=yT_.t[:, 8:16, :], in_=p_.t[:], func=AF.Copy), reads=[p_.b], writes=[yT_.b])
                for dc in range(4):
                    pb = po[dc]
                    for kk in range(ND):
                        k.op("pe", lambda e, kk=kk, dc=dc, pb=pb, yT_=yT_: e.matmul(pb.t[:], lhsT=yT_.t[:, kk, :], rhs=W.t[:, kk, dc * 512:(dc + 1) * 512],
                                                                                   start=(kk == 0), stop=(kk == ND - 1)), reads=[yT_.b, W.b], writes=[pb.b])
                    if dc % 2 == 0:
                        k.op("act", lambda e, dc=dc, pb=pb, mx=mx: e.activation(out=mx.t[:, dc * 512:(dc + 1) * 512], in_=pb.t[:], func=AF.Copy), reads=[pb.b], writes=[mx.b])
                    else:
                        k.op("dve", lambda e, dc=dc, pb=pb, mx=mx: e.tensor_copy(out=mx.t[:, dc * 512:(dc + 1) * 512], in_=pb.t[:]), reads=[pb.b], writes=[mx.b])
                k.dma("pool", self.xres[tt * 128:(tt + 1) * 128, :], mx.t[:], [mx.b], [xb], accum_op=ALU.add)

    def ssd_layer(self, cs, li):
        j = li // 2
        Win = self.I["ssd_w_in"][j]
        with Scope(self) as AS:
            hT = AS.sb("hT", [128, ND, T], BF16)
            with Scope(self) as s:
                self.norm_to_hT(s, hT, self.I["norm_mix_g"][li:li + 1, :])
            with Scope(self) as s:
                self.ssd_proj_z(s, Win, hT)
            with Scope(self) as s:
                self.ssd_proj_xbc(s, j, Win, hT)
            with Scope(self) as s:
                self.ssd_proj_dt(s, j, Win, hT)
            with Scope(self) as s:
                self.xa_block(s, li, hT, Win, 4144)
        with Scope(self) as s:
            self.ssd_scan(s, j)
        self.out_proj(self.I["ssd_w_out"][j])

    def ssd_proj_z(self, s, Win, hT):
        k = self
        pbs = [s.ps("z_pb", [128, 512]) for _ in range(4)]
        zs = [s.sb("z_sb", [128, 512], BF16) for _ in range(4)]
        zb = self.P.buf("szd")
        it = 0
        for blk in range(3):
            w = self.wblock(s, Win, blk * 512, 512)
            for tt in range(NT):
                pb, z_ = pbs[it % 4], zs[it % 4]
                it += 1
                self.proj_tm(hT, w, 512, pb, tt)
                k.op("act", lambda e, pb=pb, z_=z_: e.activation(out=z_.t[:], in_=pb.t[:], func=AF.Silu), reads=[pb.b], writes=[z_.b])
                k.dma("sp", self.szd[tt * 128:(tt + 1) * 128, blk * 512:(blk + 1) * 512], z_.t[:], [z_.b], [zb])

    def ssd_proj_xbc(self, s, j, Win, hT):
        k = self
        raw = s.sb("cw_raw", [120, 128], F32)
        cwT = s.sb("cwT", [128, 120], F32)
        ptr = s.ps("cw_pt", [128, 120], F32)
        k.dma("sp", raw.t[0:100, :], self.I["ssd_conv_w"][j].rearrange("k (n p) -> (k n) p", p=128), [], [raw.b])
        k.dma("sp", raw.t[100:120, :], self.I["ssd_conv_b"][j:j + 1, :].rearrange("o (n p) -> (o n) p", p=128), [], [raw.b])
        k.op("pe", lambda e: e.transpose(out=ptr.t[:], in_=raw.t[:], identity=self.identf.t[0:120, 0:120]), reads=[raw.b, self.identf.b], writes=[ptr.b])
        k.op("dve", lambda e: e.tensor_copy(out=cwT.t[:], in_=ptr.t[:]), reads=[ptr.b], writes=[cwT.b])
        pbs = [s.ps("x_pb", [128, 512]) for _ in range(4)]
        pcs = [s.ps("x_pc", [128, 512]) for _ in range(2)]
        ptt = s.ps("x_ptt", [128, 8, 128], BF16)
        pre = [s.sb("x_pre", [128, T + 4], BF16) for _ in range(2)]
        post = [s.sb("x_post", [128, T], BF16) for _ in range(2)]
        dg = [s.sb("x_dg", [128, 5, 128], BF16) for _ in range(2)]
        tok = [s.sb("x_tok", [128, NT, 128], BF16) for _ in range(2)]
        xb = self.P.buf("xsBd")
        bb = self.P.buf("BCTd")
        for p_ in pre:
            k.op("pool", lambda e, p_=p_: e.memset(p_.t[:], 0.0), writes=[p_.b])
        cvi = 0
        for blk in range(5):
            w = self.wblock(s, Win, 1536 + blk * 512, 512)
            for et in range(4):
                ct = blk * 4 + et
                pr, po, dg_, tk = pre[ct % 2], post[ct % 2], dg[ct % 2], tok[ct % 2]
                for kk in range(5):
                    k.op("dve", lambda e, kk=kk, ct=ct, dg_=dg_: e.tensor_scalar(out=dg_.t[:, kk, :], in0=self.identf.t[:], scalar1=cwT.t[:, kk * 20 + ct:kk * 20 + ct + 1],
                                                                               scalar2=None, op0=ALU.mult), reads=[self.identf.b, cwT.b], writes=[dg_.b])
                self.proj_fm(s, hT, w, 512, pbs, et)
                for tc in range(4):
                    if tc % 2 == 0:
                        k.op("act", lambda e, tc=tc, pr=pr: e.activation(out=pr.t[:, 2 + tc * 512:2 + (tc + 1) * 512], in_=pbs[tc].t[:], func=AF.Copy), reads=[pbs[tc].b], writes=[pr.b])
                    else:
                        k.op("dve", lambda e, tc=tc, pr=pr: e.tensor_copy(out=pr.t[:, 2 + tc * 512:2 + (tc + 1) * 512], in_=pbs[tc].t[:]), reads=[pbs[tc].b], writes=[pr.b])
                for tc in range(4):
                    pc = pcs[cvi % 2]
                    cvi += 1
                    for kk in range(5):
                        k.op("pe", lambda e, kk=kk, tc=tc, pc=pc, dg_=dg_, pr=pr: e.matmul(pc.t[:], lhsT=dg_.t[:, kk, :], rhs=pr.t[:, tc * 512 + kk:tc * 512 + kk + 512],
                                                                                          start=(kk == 0), stop=(kk == 4)), reads=[dg_.b, pr.b], writes=[pc.b])
                    k.op("act", lambda e, tc=tc, pc=pc, po=po, ct=ct: e.activation(out=po.t[:, tc * 512:(tc + 1) * 512], in_=pc.t[:], func=AF.Silu, bias=cwT.t[:, 100 + ct:101 + ct]),
                         reads=[pc.b, cwT.b], writes=[po.b])
                if ct >= 12:
                    k.dma("sp", self.BCTd[ct - 12], po.t[:], [po.b], [bb])
                if ct < 16:
                    for half in range(2):
                        for jj in range(8):
                            tt = half * 8 + jj
                            k.op("pe", lambda e, jj=jj, tt=tt, po=po: e.transpose(out=ptt.t[:, jj, :], in_=po.t[:, tt * 128:(tt + 1) * 128], identity=self.ident.t[:]),
                                 reads=[po.b, self.ident.b], writes=[ptt.b])
                        k.op("dve", lambda e, half=half, tk=tk: e.tensor_copy(out=tk.t[:, half * 8:(half + 1) * 8, :], in_=ptt.t[:]), reads=[ptt.b], writes=[tk.b])
                    k.dma("sp", self.xsBd.rearrange("(n p) c -> p n c", p=128)[:, :, ct * 128:(ct + 1) * 128], tk.t[:], [tk.b], [xb])

    def ssd_proj_dt(self, s, j, Win, hT):
        k = self
        pbs = [s.ps("d_pb", [128, 512]) for _ in range(4)]
        ptk = s.ps("d_ptk", [128, 4, 48], F32)
        dtb = s.sb("d_dtb", [48, 1], F32)
        nA = s.sb("d_nA", [48, 1], F32)
        v = s.sb("d_v", [48, T], F32)
        a = s.sb("d_a", [48, T], F32)
        dt = s.sb("d_dt", [48, T], F32)
        dA = s.sb("d_dA", [48, T], F32)
        pre = s.sb("d_pre", [48, T], F32)
        suf = s.sb("d_suf", [48, T], F32)
        tk = [s.sb("d_tk", [128, 4, 48], F32) for _ in range(2)]
        k.dma("sp", dtb.t[:], self.I["ssd_dt_bias"][j:j + 1, :].rearrange("o h -> h o"), [], [dtb.b])
        k.dma("sp", nA.t[:], self.I["ssd_a_log"][j:j + 1, :].rearrange("o h -> h o"), [], [nA.b])
        k.op("act", lambda e: e.activation(out=nA.t[:], in_=nA.t[:], func=AF.Exp), reads=[nA.b], writes=[nA.b])
        k.op("dve", lambda e: e.tensor_scalar(out=nA.t[:], in0=nA.t[:], scalar1=-1.0, scalar2=None, op0=ALU.mult), reads=[nA.b], writes=[nA.b])
        w = self.wblock(s, Win, 4096, 48)
        self.proj_fm(s, hT, w, 48, pbs, 0)
        for tc in range(4):
            k.op("act", lambda e, tc=tc: e.activation(out=v.t[:, tc * 512:(tc + 1) * 512], in_=pbs[tc].t[0:48, :], func=AF.Identity, bias=dtb.t[:]),
                 reads=[pbs[tc].b, dtb.b], writes=[v.b])
        k.op("dve", lambda e: e.scalar_tensor_tensor(out=a.t[:], in0=v.t[:], scalar=-1.0, in1=v.t[:], op0=ALU.mult, op1=ALU.max), reads=[v.b], writes=[a.b])
        k.op("act", lambda e: e.activation(out=a.t[:], in_=a.t[:], func=AF.Exp, scale=-1.0), reads=[a.b], writes=[a.b])
        k.op("act", lambda e: e.activation(out=a.t[:], in_=a.t[:], func=AF.Ln, bias=1.0), reads=[a.b], writes=[a.b])
        k.op("dve", lambda e: e.scalar_tensor_tensor(out=dt.t[:], in0=v.t[:], scalar=0.0, in1=a.t[:], op0=ALU.max, op1=ALU.add), reads=[v.b, a.b], writes=[dt.b])
        k.op("dve", lambda e: e.tensor_scalar(out=dA.t[:], in0=dt.t[:], scalar1=nA.t[:, 0:1], scalar2=None, op0=ALU.mult), reads=[dt.b, nA.b], writes=[dA.b])
        for c in range(NT):
            sl = slice(c * 128, (c + 1) * 128)
            k.op("dve", lambda e, sl=sl: e.tensor_tensor_scan(out=pre.t[:, sl], data0=dA.t[:, sl], data1=dA.t[:, sl], initial=0.0, op0=ALU.add, op1=ALU.bypass),
                 reads=[dA.b], writes=[pre.b])
        for c in range(NT):
            sl = slice(c * 128, (c + 1) * 128)
            k.op("dve", lambda e, sl=sl, c=c: e.tensor_scalar(out=suf.t[:, sl], in0=pre.t[:, sl], scalar1=-1.0, scalar2=pre.t[:, c * 128 + 127:c * 128 + 128],
                                                             op0=ALU.mult, op1=ALU.add), reads=[pre.b], writes=[suf.b])
        k.op("dve", lambda e: e.tensor_tensor(out=suf.t[:], in0=suf.t[:], in1=dA.t[:], op=ALU.add), reads=[suf.b, dA.b], writes=[suf.b])
        ab = self.P.buf("ABd")
        tb = self.P.buf("TOTd")
        kb = self.P.buf("TOKd")
        ABv = self.ABd.rearrange("c (d h l) -> d h c l", d=2, h=SSD_H)
        k.dma("sp", ABv[0], pre.t[0:24, :].rearrange("h (c l) -> h c l", l=128), [pre.b], [ab])
        k.dma("sp", ABv[1], suf.t[24:48, :].rearrange("h (c l) -> h c l", l=128), [suf.b], [ab])
        k.dma("sp", self.TOTd.rearrange("o (c h) -> h (o c)", h=48), pre.t[:, 127::128], [pre.b], [tb], allow_slow_non_contiguous=True)
        srcs = [dt, pre, suf, dA]
        for c in range(NT):
            t_ = tk[c % 2]
            for qi, src in enumerate(srcs):
                k.op("pe", lambda e, qi=qi, src=src, c=c: e.transpose(out=ptk.t[:, qi, :], in_=src.t[:, c * 128:(c + 1) * 128], identity=self.identf.t[0:48, 0:48]),
                     reads=[src.b, self.identf.b], writes=[ptk.b])
            k.op("dve", lambda e, t_=t_: e.tensor_copy(out=t_.t[:], in_=ptk.t[:]), reads=[ptk.b], writes=[t_.b])
            k.dma("sp", self.TOKd[c * 128:(c + 1) * 128, :], t_.t[:].rearrange("p a h -> p (a h)"), [t_.b], [kb])

    def ssd_scan(self, s, j):
        k = self
        H, Pd = SSD_H, SSD_P
        Dful = s.sb("sc_D", [128, D_SSD], F32)
        d24 = s.sb("sc_d24", [128, H], F32)
        gn = s.sb("sc_gn", [128, D_SSD], F32)
        CD = s.sb("sc_CD", [128, NT, 48], F32)
        k.bcast_load("sp", d24, self.I["ssd_d"][j:j + 1, :])
        k.bcast_load("sp", gn, self.I["ssd_gate_norm_g"][j:j + 1, :])
        k.bcast_load("sp", CD_flat := Tl(CD.t, CD.b), self.TOTd[0:1, :]) if False else k.dma("sp", CD.t[:].rearrange("p c h -> p (c h)"), self.TOTd[0:1, :].partition_broadcast(128), [], [CD.b])
        k.op("act", lambda e: e.activation(out=CD.t[:], in_=CD.t[:], func=AF.Exp), reads=[CD.b], writes=[CD.b])
        k.op("dve", lambda e: e.tensor_copy(out=Dful.t[:].rearrange("p (h q) -> p h q", h=H), in_=d24.t[:].unsqueeze(2).to_broadcast([128, H, Pd])), reads=[d24.b], writes=[Dful.b])
        xs = [s.sb("sc_xs", [128, 2048], BF16) for _ in range(2)]
        tok = [s.sb("sc_tok", [128, 4, 48], F32) for _ in range(2)]
        Hs = s.sb("sc_H", [128, D_SSD], F32)
        Hbf = [s.sb("sc_Hbf", [128, D_SSD], BF16) for _ in range(2)]
        w24 = [s.sb("sc_w24", [128, H], F32) for _ in range(2)]
        xdd = [s.sb("sc_xdd", [128, D_SSD], BF16) for _ in range(2)]
        psS = [s.ps("sc_psS", [128, 512])] * 2
        gb = self.P.buf("Gd")
        xview = lambda t_: t_.t[:, 0:D_SSD].rearrange("p (h q) -> p h q", h=H)

        def load_chunk(c, bi):
            k.dma("sp", xs[bi].t[:], self.xsBd[c * 128:(c + 1) * 128, :], [], [xs[bi].b])
            k.dma("sp", tok[bi].t[:].rearrange("p a h -> p (a h)"), self.TOKd[c * 128:(c + 1) * 128, :], [], [tok[bi].b])

        def states(c, bi, d, Hacc, psl):
            ho = 24 * d
            x_, t_, w_, xd = xs[bi], tok[bi], w24[bi], xdd[bi]
            src = 2 if d == 0 else 1
            k.op("dve", lambda e: e.tensor_tensor(out=w_.t[:], in0=t_.t[:, src, ho:ho + 24], in1=t_.t[:, 3, ho:ho + 24], op=ALU.subtract), reads=[t_.b], writes=[w_.b])
            k.op("act", lambda e: e.activation(out=w_.t[:], in_=w_.t[:], func=AF.Exp), reads=[w_.b], writes=[w_.b])
            k.op("dve", lambda e: e.tensor_tensor(out=w_.t[:], in0=w_.t[:], in1=t_.t[:, 0, ho:ho + 24], op=ALU.mult), reads=[w_.b, t_.b], writes=[w_.b])
            k.op("pool", lambda e: e.tensor_tensor(out=xd.t[:].rearrange("p (h q) -> p h q", h=H), in0=xview(x_), in1=w_.t[:].unsqueeze(2).to_broadcast([128, H, Pd]), op=ALU.mult),
                 reads=[x_.b, w_.b], writes=[xd.b])
            for g in range(SSD_G):
                ps_ = psl[g % 2]
                k.op("pe", lambda e, g=g, ps_=ps_: e.matmul(ps_.t[:, 0:384], lhsT=x_.t[:, D_SSD + g * 128:D_SSD + (g + 1) * 128], rhs=xd.t[:, g * 384:(g + 1) * 384], start=True, stop=True),
                     reads=[x_.b, xd.b], writes=[ps_.b])
                hv = Hacc.t[:, g * 384:(g + 1) * 384]
                k.op("dve", lambda e, g=g, hv=hv: e.tensor_tensor(out=hv.rearrange("p (h q) -> p h q", h=6), in0=hv.rearrange("p (h q) -> p h q", h=6),
                                                                 in1=CD.t[:, c, ho + g * 6:ho + g * 6 + 6].unsqueeze(2).to_broadcast([128, 6, Pd]), op=ALU.mult),
                     reads=[Hacc.b, CD.b], writes=[Hacc.b])
                k.op("dve", lambda e, g=g, hv=hv, ps_=ps_: e.tensor_tensor(out=hv, in0=hv, in1=ps_.t[:, 0:384], op=ALU.add), reads=[Hacc.b, ps_.b], writes=[Hacc.b])

        k.op("pool", lambda e: e.memset(Hs.t[:], 0.0), writes=[Hs.b])
        load_chunk(NT - 1, (NT - 1) % 2)
        for c in range(NT - 1, -1, -1):
            bi = c % 2
            if c > 0:
                load_chunk(c - 1, (c - 1) % 2)
            hb_ = Hbf[bi]
            k.op("act", lambda e, hb_=hb_: e.activation(out=hb_.t[:], in_=Hs.t[:], func=AF.Copy), reads=[Hs.b], writes=[hb_.b])
            k.dma("sp", self.Gd[c], hb_.t[:], [hb_.b], [gb])
            if c > 0:
                states(c, bi, 1, Hs, psS)
        self.P.flush()
        bct = [s.sb("sc_bct", [128, 8, 128], BF16) for _ in range(2)]
        bc = [s.sb("sc_bc", [128, 2, H, 128], F32) for _ in range(2)]
        Gc = [s.sb("sc_G", [128, D_SSD], BF16) for _ in range(2)]
        sz = [s.sb("sc_sz", [128, D_SSD], BF16) for _ in range(2)]
        eA = [s.sb("sc_eA", [128, 48], F32) for _ in range(2)]
        xdt = [[s.sb("sc_xdt", [128, D_SSD], BF16) for _ in range(2)] for _ in range(2)]
        mcb = [[s.sb("sc_mcb", [128, 4, 128], F32) for _ in range(2)] for _ in range(2)]
        seg = [[s.sb("sc_seg", [128, 6, 128], F32) for _ in range(2)] for _ in range(2)]
        MT = [[s.sb("sc_MT", [128, 6, 128], BF16) for _ in range(2)] for _ in range(2)]
        t1 = [s.sb("sc_t1", [128, 384], F32) for _ in range(2)]
        t2 = [s.sb("sc_t2", [128, 384], F32) for _ in range(2)]
        yall = [s.sb("sc_yall", [128, D_SSD], F32) for _ in range(2)]
        junk = s.sb("sc_junk", [128, 384], BF16)
        ss = [s.sb("sc_ss", [128, 4], F32) for _ in range(2)]
        yn = [s.sb("sc_yn", [128, D_SSD], BF16) for _ in range(2)]
        psCB = s.ps("sc_psCB", [128, 4, 128])
        psY = [[s.ps("sc_psY", [128, 512]) for _ in range(3)] for _ in range(2)]
        yb_ = self.P.buf("ycat_ssd")

        def load2(c, bi):
            load_chunk(c, bi)
            k.dma("sp", bct[bi].t[:], self.BCTd.rearrange("g n t -> n g t")[:, :, c * 128:(c + 1) * 128], [], [bct[bi].b])
            k.dma("sp", bc[bi].t[:].rearrange("p d h l -> p (d h l)"), self.ABd[c:c + 1, :].partition_broadcast(128), [], [bc[bi].b])
            k.dma("sp", Gc[bi].t[:], self.Gd[c], [], [Gc[bi].b])
            k.dma("sp", sz[bi].t[:], self.szd[c * 128:(c + 1) * 128, :], [], [sz[bi].b])

        k.op("pool", lambda e: e.memset(Hs.t[:], 0.0), writes=[Hs.b])
        k.op("pool", lambda e: e.memset(Hbf[0].t[:], 0.0), writes=[Hbf[0].b])
        load2(0, 0)
        gi = 0
        for c in range(NT):
            bi = c % 2
            if c + 1 < NT:
                load2(c + 1, (c + 1) % 2)
            x_, t_, b_, bc_, G_, sz_, eA_, ya = xs[bi], tok[bi], bct[bi], bc[bi], Gc[bi], sz[bi], eA[bi], yall[bi]
            hf_ = Hbf[bi]
            k.op("act", lambda e, t_=t_, eA_=eA_: e.activation(out=eA_.t[:, 0:24], in_=t_.t[:, 1, 0:24], func=AF.Exp), reads=[t_.b], writes=[eA_.b])
            k.op("act", lambda e, t_=t_, eA_=eA_: e.activation(out=eA_.t[:, 24:48], in_=t_.t[:, 2, 24:48], func=AF.Exp), reads=[t_.b], writes=[eA_.b])
            for d in range(2):
                k.op("pool", lambda e, d=d, x_=x_, t_=t_: e.tensor_tensor(out=xdt[d][bi].t[:].rearrange("p (h q) -> p h q", h=H), in0=xview(x_),
                                                                         in1=t_.t[:, 0, 24 * d:24 * d + 24].unsqueeze(2).to_broadcast([128, H, Pd]), op=ALU.mult),
                     reads=[x_.b, t_.b], writes=[xdt[d][bi].b])
            for g in range(SSD_G):
                k.op("pe", lambda e, g=g, b_=b_: e.matmul(psCB.t[:, g, :], lhsT=b_.t[:, g, :], rhs=b_.t[:, 4 + g, :], start=True, stop=True), reads=[b_.b], writes=[psCB.b])
            k.op("dve", lambda e: e.tensor_tensor(out=mcb[0][bi].t[:], in0=psCB.t[:], in1=self.maskf.t[:].unsqueeze(1).to_broadcast([128, 4, 128]), op=ALU.mult),
                 reads=[psCB.b, self.maskf.b], writes=[mcb[0][bi].b])
            k.op("dve", lambda e: e.tensor_tensor(out=mcb[1][bi].t[:], in0=psCB.t[:], in1=self.maskb.t[:].unsqueeze(1).to_broadcast([128, 4, 128]), op=ALU.mult),
                 reads=[psCB.b, self.maskb.b], writes=[mcb[1][bi].b])
            for g in range(SSD_G):
                pY = psY[gi % 2]
                sgi = gi % 2
                gi += 1
                for d in range(2):
                    sg, mt_ = seg[d][sgi], MT[d][sgi]
                    col = 1 if d == 0 else 2
                    for jh in range(6):
                        h = g * 6 + jh
                        k.op("dve", lambda e, d=d, jh=jh, h=h, sg=sg, col=col: e.tensor_scalar(out=sg.t[:, jh, :], in0=bc_.t[:, d, h, :], scalar1=t_.t[:, col, 24 * d + h:24 * d + h + 1],
                                                                                              scalar2=0.0, op0=ALU.subtract, op1=ALU.min), reads=[bc_.b, t_.b], writes=[sg.b])
                    k.op("act", lambda e, sg=sg: e.activation(out=sg.t[:], in_=sg.t[:], func=AF.Exp), reads=[sg.b], writes=[sg.b])
                    k.op("pool", lambda e, d=d, g=g, sg=sg, mt_=mt_: e.tensor_tensor(out=mt_.t[:], in0=sg.t[:], in1=mcb[d][bi].t[:, g, :].unsqueeze(1).to_broadcast([128, 6, 128]), op=ALU.mult),
                         reads=[sg.b, mcb[d][bi].b], writes=[mt_.b])
                for jh in range(6):
                    h = g * 6 + jh
                    for d in range(2):
                        k.op("pe", lambda e, d=d, jh=jh, h=h, sgi=sgi, pY=pY: e.matmul(pY[0].t[:, jh * 64:(jh + 1) * 64], lhsT=MT[d][sgi].t[:, jh, :], rhs=xdt[d][bi].t[:, h * 64:(h + 1) * 64],
                                                                                      start=(d == 0), stop=(d == 1)), reads=[MT[d][sgi].b, xdt[d][bi].b], writes=[pY[0].b])
                k.op("pe", lambda e, g=g, pY=pY, hf_=hf_: e.matmul(pY[1].t[:, 0:384], lhsT=b_.t[:, 4 + g, :], rhs=hf_.t[:, g * 384:(g + 1) * 384], start=True, stop=True),
                     reads=[b_.b, hf_.b], writes=[pY[1].b])
                k.op("pe", lambda e, g=g, pY=pY: e.matmul(pY[2].t[:, 0:384], lhsT=b_.t[:, 4 + g, :], rhs=G_.t[:, g * 384:(g + 1) * 384], start=True, stop=True),
                     reads=[b_.b, G_.b], writes=[pY[2].b])
                a1, a2 = t1[sgi], t2[sgi]
                v3 = lambda ap: ap.rearrange("p (h q) -> p h q", h=6)
                k.op("dve", lambda e, g=g, pY=pY, a1=a1: e.tensor_tensor(out=v3(a1.t[:]), in0=v3(pY[1].t[:, 0:384]), in1=eA_.t[:, g * 6:g * 6 + 6].unsqueeze(2).to_broadcast([128, 6, Pd]), op=ALU.mult),
                     reads=[pY[1].b, eA_.b], writes=[a1.b])
                k.op("dve", lambda e, g=g, pY=pY, a2=a2: e.tensor_tensor(out=v3(a2.t[:]), in0=v3(pY[2].t[:, 0:384]), in1=eA_.t[:, 24 + g * 6:24 + g * 6 + 6].unsqueeze(2).to_broadcast([128, 6, Pd]), op=ALU.mult),
                     reads=[pY[2].b, eA_.b], writes=[a2.b])
                k.op("pool", lambda e, a1=a1, a2=a2: e.tensor_tensor(out=a1.t[:], in0=a1.t[:], in1=a2.t[:], op=ALU.add), reads=[a1.b, a2.b], writes=[a1.b])
                k.op("dve", lambda e, pY=pY, a1=a1: e.tensor_tensor(out=a1.t[:], in0=pY[0].t[:, 0:384], in1=a1.t[:], op=ALU.add), reads=[pY[0].b, a1.b], writes=[a1.b])
                k.op("pool", lambda e, g=g, a2=a2: e.tensor_tensor(out=a2.t[:], in0=x_.t[:, g * 384:(g + 1) * 384], in1=Dful.t[:, g * 384:(g + 1) * 384], op=ALU.mult),
                     reads=[x_.b, Dful.b], writes=[a2.b])
                k.op("pool", lambda e, g=g, a1=a1, a2=a2: e.tensor_tensor(out=ya.t[:, g * 384:(g + 1) * 384], in0=a1.t[:], in1=a2.t[:], op=ALU.add), reads=[a1.b, a2.b], writes=[ya.b])
            if c + 1 < NT:
                states(c, bi, 0, Hs, psS)
                hn_ = Hbf[(c + 1) % 2]
                k.op("act", lambda e, hn_=hn_: e.activation(out=hn_.t[:], in_=Hs.t[:], func=AF.Copy), reads=[Hs.b], writes=[hn_.b])
            s_, yn_ = ss[bi], yn[bi]
            k.op("dve", lambda e: e.tensor_tensor(out=ya.t[:], in0=ya.t[:], in1=sz_.t[:], op=ALU.mult), reads=[ya.b, sz_.b], writes=[ya.b])
            for g in range(SSD_G):
                k.op("act", lambda e, g=g: e.activation(out=junk.t[:], in_=ya.t[:, g * 384:(g + 1) * 384], func=AF.Square, accum_out=s_.t[:, g:g + 1]), reads=[ya.b], writes=[junk.b, s_.b])
            k.op("act", lambda e: e.activation(out=s_.t[:], in_=s_.t[:], func=AF.Sqrt, scale=1.0 / 384, bias=self.epsc.t[:]), reads=[s_.b, self.epsc.b], writes=[s_.b])
            k.op("dve", lambda e: e.reciprocal(out=s_.t[:], in_=s_.t[:]), reads=[s_.b], writes=[s_.b])
            k.op("dve", lambda e: e.tensor_tensor(out=ya.t[:].rearrange("p (g q) -> p g q", g=4), in0=ya.t[:].rearrange("p (g q) -> p g q", g=4),
                                                  in1=s_.t[:].unsqueeze(2).to_broadcast([128, 4, 384]), op=ALU.mult), reads=[ya.b, s_.b], writes=[ya.b])
            k.op("pool", lambda e: e.tensor_tensor(out=yn_.t[:], in0=ya.t[:], in1=gn.t[:], op=ALU.mult), reads=[ya.b, gn.b], writes=[yn_.b])
            k.dma("sp", self.ycat[c * 128:(c + 1) * 128, 0:D_SSD], yn_.t[:], [yn_.b], [yb_])

    def na_layer(self, cs, li):
        j = li // 2
        Win = self.I["na_w_in"][j]
        with Scope(self) as AS:
            hT = AS.sb("hT", [128, ND, T], BF16)
            with Scope(self) as s:
                self.norm_to_hT(s, hT, self.I["norm_mix_g"][li:li + 1, :])
            for hg in range(6):
                with Scope(self) as s:
                    self.na_group(s, j, hg, Win, hT)
            with Scope(self) as s:
                self.xa_block(s, li, hT, Win, 4608)
        self.out_proj(self.I["na_w_out"][j])

    def na_group(self, s, j, hg, Win, hT):
        k = self
        HG = 2
        W = HG * 128
        QT = s.sb("na_QT", [128, HG, T], BF16)
        KT = s.sb("na_KT", [128, HG, T], BF16)
        Va = s.sb("na_Va", [128, NT, HG, 129], BF16)
        oall = s.sb("na_oall", [128, NT, W], BF16)
        pbs = [s.ps("na_pb", [128, 512]) for _ in range(4)]
        k.op("pool", lambda e: e.memset(Va.t[:], 1.0), writes=[Va.b])
        for which, dst in ((0, QT), (1, KT)):
            w = self.wblock(s, Win, which * 1536 + hg * W, W)
            for et in range(HG):
                self.proj_fm(s, hT, w, W, pbs, et)
                for tc in range(4):
                    if tc % 2 == 0:
                        k.op("act", lambda e, et=et, tc=tc, dst=dst: e.activation(out=dst.t[:, et, tc * 512:(tc + 1) * 512], in_=pbs[tc].t[:], func=AF.Copy), reads=[pbs[tc].b], writes=[dst.b])
                    else:
                        k.op("dve", lambda e, et=et, tc=tc, dst=dst: e.tensor_copy(out=dst.t[:, et, tc * 512:(tc + 1) * 512], in_=pbs[tc].t[:]), reads=[pbs[tc].b], writes=[dst.b])
        w = self.wblock(s, Win, 3072 + hg * W, W)
        for tt in range(NT):
            pb = pbs[tt % 4]
            self.proj_tm(hT, w, W, pb, tt)
            k.op("act", lambda e, tt=tt, pb=pb: e.activation(out=Va.t[:, tt, :, 0:128], in_=pb.t[:, 0:W].rearrange("p (h d) -> p h d", h=HG), func=AF.Copy), reads=[pb.b], writes=[Va.b])
        self.P.flush()
        bias = [s.sb("na_bias", [128, 25, 128], F32) for _ in range(2)]
        psS = [s.ps("na_psS", [128, 5, 128]) for _ in range(1)]
        psO = [s.ps("na_psO", [128, 256]) for _ in range(2)]
        tmp = [s.sb("na_tmp", [128, 5, 128], F32) for _ in range(2)]
        Pm = [s.sb("na_P", [128, 5, 128], BF16) for _ in range(2)]
        rc = [s.sb("na_rc", [128, 1], F32) for _ in range(2)]
        scale = 128.0 ** -0.5
        it = 0
        for hh in range(HG):
            h = hg * HG + hh
            bt = bias[hh % 2]
            k.dma("sp", bt.t[:], self.I["na_bias"][j, h], [], [bt.b])
            for i in range(NT):
                kb = min(max(2 * i - 4, 0), 22)
                kt0 = kb // 2
                case = 0 if i == 0 else 1 if i == 1 else 3 if i == 14 else 4 if i == 15 else 2
                pS, pO, tm, P_, r_ = psS[0], psO[it % 2], tmp[it % 2], Pm[it % 2], rc[it % 2]
                it += 1
                for jt in range(5):
                    k.op("pe", lambda e, jt=jt, hh=hh, i=i, kt0=kt0, pS=pS: e.matmul(pS.t[:, jt, :], lhsT=KT.t[:, hh, (kt0 + jt) * 128:(kt0 + jt + 1) * 128], rhs=QT.t[:, hh, i * 128:(i + 1) * 128],
                                                                                  start=True, stop=True), reads=[KT.b, QT.b], writes=[pS.b])
                k.op("dve", lambda e, case=case, pS=pS, tm=tm, bt=bt: e.scalar_tensor_tensor(out=tm.t[:], in0=pS.t[:], scalar=scale, in1=bt.t[:, case * 5:(case + 1) * 5, :], op0=ALU.mult, op1=ALU.add),
                     reads=[pS.b, bt.b], writes=[tm.b])
                k.op("act", lambda e, tm=tm, P_=P_: e.activation(out=P_.t[:], in_=tm.t[:], func=AF.Exp), reads=[tm.b], writes=[P_.b])
                for jt in range(5):
                    k.op("pe", lambda e, jt=jt, hh=hh, kt0=kt0, pO=pO, P_=P_: e.matmul(pO.t[:, 0:129], lhsT=P_.t[:, jt, :], rhs=Va.t[:, kt0 + jt, hh, :], start=(jt == 0), stop=(jt == 4)),
                         reads=[P_.b, Va.b], writes=[pO.b])
                k.op("dve", lambda e, pO=pO, r_=r_: e.reciprocal(out=r_.t[:], in_=pO.t[:, 128:129]), reads=[pO.b], writes=[r_.b])
                k.op("act", lambda e, i=i, hh=hh, pO=pO, r_=r_: e.activation(out=oall.t[:, i, hh * 128:(hh + 1) * 128], in_=pO.t[:, 0:128], func=AF.Copy, scale=r_.t[:, 0:1]),
                     reads=[pO.b, r_.b], writes=[oall.b])
        yb = self.P.buf("ycat_na")
        k.dma("sp", self.ycat.rearrange("(n p) c -> p n c", p=128)[:, :, hg * W:(hg + 1) * W], oall.t[:], [oall.b], [yb])

    def moe_layer(self, cs, li):
        k = self
        with Scope(self) as MS:
            AFF = MS.sb("m_AFF", [128, NT, NE], F32)
            IDX = MS.sb("m_IDX", [128, 2 * NE], I32)
            GATE = MS.sb("m_GATE", [128, 2 * NE], F32)
            with Scope(self) as s:
                self.moe_router(s, li, AFF)
            with Scope(self) as s:
                self.moe_topk(s, AFF, IDX, GATE)
            with Scope(self) as s:
                self.moe_experts(s, li, IDX, GATE)

    def moe_router(self, s, li, AFF):
        k = self
        g = s.sb("g", [128, D], F32)
        k.bcast_load("sp", g, self.I["norm_ffn_g"][li:li + 1, :])
        Wr = s.sb("m_Wr", [128, ND, NE], F32)
        k.dma("sp", Wr.t[:], self.I["moe_w_router"][li].rearrange("(k p) e -> p k e", p=128), [], [Wr.b])
        hf = [s.sb("m_hf", [128, D], F32) for _ in range(2)]
        hTf = [s.sb("m_hTf", [128, ND, 128], F32) for _ in range(2)]
        ptf = [s.ps("m_ptf", [128, 4, 128], F32) for _ in range(4)]
        pl = [s.ps("m_pl", [128, NE], F32) for _ in range(2)]
        sm = [s.sb("m_sm", [128, 4], F32) for _ in range(2)]
        ex = [s.sb("m_ex", [128, NE], F32) for _ in range(2)]
        hb = self.P.buf("hn")
        for tt in range(NT):
            h_, hT_, pl_, sm_, ex_ = hf[tt % 2], hTf[tt % 2], pl[tt % 2], sm[tt % 2], ex[tt % 2]
            self.norm_tile(s, self.xres[tt * 128:(tt + 1) * 128, :], g, want_f32=h_, hn_dst=(self.hn[tt * 128:(tt + 1) * 128, :], hb))
            for q4 in range(4):
                p_ = ptf[q4]
                for jj in range(4):
                    kk = q4 * 4 + jj
                    k.op("pe", lambda e, kk=kk, jj=jj, p_=p_, h_=h_: e.transpose(out=p_.t[:, jj, :], in_=h_.t[:, kk * 128:(kk + 1) * 128], identity=self.identf.t[:]),
                         reads=[h_.b, self.identf.b], writes=[p_.b])
                if q4 % 2 == 0:
                    k.op("dve", lambda e, q4=q4, p_=p_, hT_=hT_: e.tensor_copy(out=hT_.t[:, q4 * 4:(q4 + 1) * 4, :], in_=p_.t[:]), reads=[p_.b], writes=[hT_.b])
                else:
                    k.op("act", lambda e, q4=q4, p_=p_, hT_=hT_: e.activation(out=hT_.t[:, q4 * 4:(q4 + 1) * 4, :], in_=p_.t[:], func=AF.Copy), reads=[p_.b], writes=[hT_.b])
            for kk in range(ND):
                k.op("pe", lambda e, kk=kk, hT_=hT_, pl_=pl_: e.matmul(pl_.t[:], lhsT=hT_.t[:, kk, :], rhs=Wr.t[:, kk, :], start=(kk == 0), stop=(kk == ND - 1)),
                     reads=[hT_.b, Wr.b], writes=[pl_.b])
            k.op("dve", lambda e, pl_=pl_, sm_=sm_: e.reduce_max(out=sm_.t[:, 0:1], in_=pl_.t[:], axis=AX.X), reads=[pl_.b], writes=[sm_.b])
            k.op("dve", lambda e, sm_=sm_: e.tensor_scalar(out=sm_.t[:, 1:2], in0=sm_.t[:, 0:1], scalar1=-1.0, scalar2=None, op0=ALU.mult), reads=[sm_.b], writes=[sm_.b])
            k.op("act", lambda e, pl_=pl_, sm_=sm_, ex_=ex_: e.activation(out=ex_.t[:], in_=pl_.t[:], func=AF.Exp, bias=sm_.t[:, 1:2], accum_out=sm_.t[:, 2:3]),
                 reads=[pl_.b, sm_.b], writes=[ex_.b, sm_.b])
            k.op("dve", lambda e, sm_=sm_: e.reciprocal(out=sm_.t[:, 3:4], in_=sm_.t[:, 2:3]), reads=[sm_.b], writes=[sm_.b])
            k.op("dve", lambda e, tt=tt, ex_=ex_, sm_=sm_: e.tensor_scalar(out=AFF.t[:, tt, :], in0=ex_.t[:], scalar1=sm_.t[:, 3:4], scalar2=None, op0=ALU.mult),
                 reads=[ex_.b, sm_.b], writes=[AFF.b])

    def moe_topk(self, s, AFF, IDX, GATE):
        k = self
        affT = s.sb("k_affT", [NE, T], F32)
        work = s.sb("k_work", [NE, T], F32)
        mx8 = s.sb("k_mx8", [NE, 8], F32)
        maskT = s.sb("k_mask", [NE, T], F32)
        pos = s.sb("k_pos", [NE, T], F32)
        POSM = s.sb("k_POSM", [128, NT, NE], F32)
        pA = [s.ps("k_pA", [128, 512]) for _ in range(4)]
        pP = s.ps("k_pP", [128, NT, NE], F32)
        for tt in range(NT):
            k.op("pe", lambda e, tt=tt: e.transpose(out=pA[tt // 4].t[0:NE, (tt % 4) * 128:(tt % 4 + 1) * 128], in_=AFF.t[:, tt, :], identity=self.identf.t[:]),
                 reads=[AFF.b, self.identf.b], writes=[pA[tt // 4].b])
        for q4 in range(4):
            k.op("dve", lambda e, q4=q4: e.tensor_copy(out=affT.t[:, q4 * 512:(q4 + 1) * 512], in_=pA[q4].t[0:NE, :]), reads=[pA[q4].b], writes=[affT.b])
        src = affT
        for it in range(CAP // 8):
            k.op("dve", lambda e, src=src: e.max(out=mx8.t[:], in_=src.t[:]), reads=[src.b], writes=[mx8.b])
            if it < CAP // 8 - 1:
                k.op("dve", lambda e, src=src: e.match_replace(out=work.t[:], in_to_replace=mx8.t[:], in_values=src.t[:], imm_value=-1.0), reads=[src.b, mx8.b], writes=[work.b])
                src = work
        k.op("dve", lambda e: e.tensor_scalar(out=maskT.t[:], in0=affT.t[:], scalar1=mx8.t[:, 7:8], scalar2=None, op0=ALU.is_ge), reads=[affT.b, mx8.b], writes=[maskT.b])
        k.op("dve", lambda e: e.tensor_tensor_scan(out=pos.t[:], data0=maskT.t[:], data1=maskT.t[:], initial=0.0, op0=ALU.add, op1=ALU.bypass), reads=[maskT.b], writes=[pos.b])
        k.op("dve", lambda e: e.tensor_tensor(out=pos.t[:], in0=pos.t[:], in1=maskT.t[:], op=ALU.mult), reads=[pos.b, maskT.b], writes=[pos.b])
        k.op("dve", lambda e: e.tensor_scalar(out=pos.t[:], in0=pos.t[:], scalar1=-1.0, scalar2=None, op0=ALU.add), reads=[pos.b], writes=[pos.b])
        for tt in range(NT):
            k.op("pe", lambda e, tt=tt: e.transpose(out=pP.t[:, tt, :], in_=pos.t[:, tt * 128:(tt + 1) * 128], identity=self.identf.t[0:NE, 0:NE]),
                 reads=[pos.b, self.identf.b], writes=[pP.b])
        k.op("dve", lambda e: e.tensor_copy(out=POSM.t[:], in_=pP.t[:]), reads=[pP.b], writes=[POSM.b])
        RV = s.sb("k_RV", [128, NT, NE, 5], BF16)
        r1 = s.sb("k_r1", [128, NT, NE], F32)
        gh = s.sb("k_gh", [128, NT, NE], BF16)
        pcol = s.sb("k_pcol", [128, 1], F32)
        k.op("pool", lambda e: e.iota(pcol.t[:], pattern=[[0, 1]], base=0, channel_multiplier=1, allow_small_or_imprecise_dtypes=True), writes=[pcol.b])
        for tt in range(NT):
            k.op("pool", lambda e, tt=tt: e.memset(RV.t[:, tt, :, 0], float(tt)), writes=[RV.b])
        k.op("dve", lambda e: e.tensor_copy(out=RV.t[:, :, :, 1].rearrange("p a b -> p (a b)"), in_=pcol.t[:].to_broadcast([128, NT * NE])), reads=[pcol.b], writes=[RV.b])
        k.op("dve", lambda e: e.tensor_copy(out=gh.t[:], in_=AFF.t[:]), reads=[AFF.b], writes=[gh.b])
        k.op("dve", lambda e: e.tensor_copy(out=RV.t[:, :, :, 2], in_=gh.t[:]), reads=[gh.b], writes=[RV.b])
        k.op("dve", lambda e: e.tensor_tensor(out=r1.t[:], in0=AFF.t[:], in1=gh.t[:], op=ALU.subtract), reads=[AFF.b, gh.b], writes=[r1.b])
        k.op("dve", lambda e: e.tensor_copy(out=gh.t[:], in_=r1.t[:]), reads=[r1.b], writes=[gh.b])
        k.op("dve", lambda e: e.tensor_copy(out=RV.t[:, :, :, 3], in_=gh.t[:]), reads=[gh.b], writes=[RV.b])
        k.op("dve", lambda e: e.tensor_tensor(out=r1.t[:], in0=r1.t[:], in1=gh.t[:], op=ALU.subtract), reads=[r1.b, gh.b], writes=[r1.b])
        k.op("dve", lambda e: e.tensor_copy(out=RV.t[:, :, :, 4], in_=r1.t[:]), reads=[r1.b], writes=[RV.b])
        O = [s.sb("k_O", [128, NT, CAP], BF16) for _ in range(2)]
        pz = [s.ps("k_pz", [128, 8], F32) for _ in range(2)]
        idf = s.sb("k_idf", [128, 2 * NE], F32)
        pzs = [s.sb("k_pzs", [128, 8], F32) for _ in range(2)]
        zi = 0
        for ex in range(NE):
            O_ = O[ex % 2]
            for tt in range(NT):
                k.op("dve" if tt % 2 == 0 else "pool", lambda e, tt=tt, ex=ex, O_=O_: e.tensor_scalar(out=O_.t[:, tt, :], in0=self.iota256.t[:], scalar1=POSM.t[:, tt, ex:ex + 1], scalar2=None, op0=ALU.is_equal),
                     reads=[self.iota256.b, POSM.b], writes=[O_.b])
            for half in range(2):
                pz_ = pz[zi % 2]
                zi += 1
                col = ex * 2 + half
                for tt in range(NT):
                    k.op("pe", lambda e, tt=tt, ex=ex, half=half, O_=O_, pz_=pz_: e.matmul(pz_.t[:, 0:5], lhsT=O_.t[:, tt, half * 128:(half + 1) * 128], rhs=RV.t[:, tt, ex, :], start=(tt == 0), stop=(tt == NT - 1)),
                         reads=[O_.b, RV.b], writes=[pz_.b])
                zs_ = pzs[zi % 2]
                k.op("dve", lambda e, pz_=pz_, zs_=zs_: e.tensor_copy(out=zs_.t[:, 0:5], in_=pz_.t[:, 0:5]), reads=[pz_.b], writes=[zs_.b])
                k.op("dve", lambda e, col=col, zs_=zs_: e.scalar_tensor_tensor(out=idf.t[:, col:col + 1], in0=zs_.t[:, 0:1], scalar=128.0, in1=zs_.t[:, 1:2], op0=ALU.mult, op1=ALU.add),
                     reads=[zs_.b], writes=[idf.b])
                k.op("dve", lambda e, col=col, zs_=zs_: e.tensor_reduce(out=GATE.t[:, col:col + 1], in_=zs_.t[:, 2:5], axis=AX.X, op=ALU.add), reads=[zs_.b], writes=[GATE.b])
        k.op("dve", lambda e: e.tensor_copy(out=IDX.t[:], in_=idf.t[:]), reads=[idf.b], writes=[IDX.b])

    def moe_experts(self, s, li, IDX, GATE):
        k = self
        NR = 6
        ring = [s.sb("e_ring", [128, 8192], BF16) for _ in range(NR)]
        xg = [s.sb("e_xg", [128, D], BF16) for _ in range(4)]
        xsT = [s.sb("e_xsT", [128, ND, CAP], BF16) for _ in range(2)]
        gT = [s.sb("e_gT", [128, 8, CAP], BF16) for _ in range(2)]
        sa = [s.sb("e_sa", [128, CAP], F32) for _ in range(2)]
        ysb = [s.sb("e_y", [128, D], F32) for _ in range(4)]
        ptr = [s.ps("e_ptr", [128, 8, 128], BF16) for _ in range(2)]
        pab = [s.ps("e_pab", [128, 512]) for _ in range(4)]
        py = [s.ps("e_py", [128, 512]) for _ in range(2)]
        xb = self.P.buf("xres_moe")
        hnb = self.P.buf("hn_r")
        ri = 0
        ti = ai = yi = 0
        for cq in range(4):
            k.dma("sp", self.moeh[cq][:, :], self.xres[:, cq * 512:(cq + 1) * 512], [], [xb])
        for ex in range(NE):
            xT, g_ = xsT[ex % 2], gT[ex % 2]
            for half in range(2):
                col = ex * 2 + half
                x_ = xg[col % 4]
                k.op("pool", lambda e, col=col, x_=x_: e.indirect_dma_start(out=x_.t[:, :], out_offset=None, in_=self.hn[:, :],
                                                                             in_offset=bass.IndirectOffsetOnAxis(ap=IDX.t[:, col:col + 1], axis=0),
                                                                             ), reads=[hnb, IDX.b], writes=[x_.b], dma=True)
                for kg in range(2):
                    p_ = ptr[ti % 2]
                    ti += 1
                    for jj in range(8):
                        kk = kg * 8 + jj
                        k.op("pe", lambda e, kk=kk, jj=jj, p_=p_, x_=x_: e.transpose(out=p_.t[:, jj, :], in_=x_.t[:, kk * 128:(kk + 1) * 128], identity=self.ident.t[:]),
                             reads=[x_.b, self.ident.b], writes=[p_.b])
                    if kg == 0:
                        k.op("dve", lambda e, kg=kg, half=half, p_=p_, xT=xT: e.tensor_copy(out=xT.t[:, kg * 8:(kg + 1) * 8, half * 128:(half + 1) * 128], in_=p_.t[:]), reads=[p_.b], writes=[xT.b])
                    else:
                        k.op("act", lambda e, kg=kg, half=half, p_=p_, xT=xT: e.activation(out=xT.t[:, kg * 8:(kg + 1) * 8, half * 128:(half + 1) * 128], in_=p_.t[:], func=AF.Copy), reads=[p_.b], writes=[xT.b])
            for fh in range(2):
                wc = []
                for wname in ("moe_w1", "moe_w3"):
                    r_ = ring[ri % NR]
                    ri += 1
                    k.dma("pool", r_.t[:].rearrange("p (k n) -> p k n", k=ND), self.I[wname][li, ex][:, fh * 512:(fh + 1) * 512].rearrange("(k p) n -> p k n", p=128), [], [r_.b])
                    wc.append(r_)
                for ft in range(4):
                    pa, pb = pab[ai % 4], pab[(ai + 1) % 4]
                    ai += 2
                    for pp, r_ in ((pa, wc[0]), (pb, wc[1])):
                        rv = r_.t[:].rearrange("p (k n) -> p k n", k=ND)
                        for kk in range(ND):
                            k.op("pe", lambda e, kk=kk, ft=ft, pp=pp, rv=rv, xT=xT: e.matmul(pp.t[:, 0:CAP], lhsT=rv[:, kk, ft * 128:(ft + 1) * 128], rhs=xT.t[:, kk, :], start=(kk == 0), stop=(kk == ND - 1)),
                                 reads=[r_.b, xT.b], writes=[pp.b])
                    s_ = sa[ft % 2]
                    k.op("act", lambda e, pa=pa, s_=s_: e.activation(out=s_.t[:], in_=pa.t[:, 0:CAP], func=AF.Silu), reads=[pa.b], writes=[s_.b])
                    k.op("dve", lambda e, fh=fh, ft=ft, pb=pb, s_=s_, g_=g_: e.tensor_tensor(out=g_.t[:, fh * 4 + ft, :], in0=s_.t[:], in1=pb.t[:, 0:CAP], op=ALU.mult), reads=[s_.b, pb.b], writes=[g_.b])
            ys = [ysb[(ex * 2) % 4], ysb[(ex * 2 + 1) % 4]]
            for dh in range(2):
                r_ = ring[ri % NR]
                ri += 1
                k.dma("pool", r_.t[:].rearrange("p (k n) -> p k n", k=8), self.I["moe_w2"][li, ex][:, dh * 1024:(dh + 1) * 1024].rearrange("(k p) n -> p k n", p=128), [], [r_.b])
                rv = r_.t[:].rearrange("p (k n) -> p k n", k=8)
                for half in range(2):
                    col = ex * 2 + half
                    for dc in range(2):
                        p_ = py[yi % 2]
                        yi += 1
                        for ft in range(8):
                            k.op("pe", lambda e, ft=ft, half=half, dc=dc, p_=p_, rv=rv, g_=g_: e.matmul(p_.t[:], lhsT=g_.t[:, ft, half * 128:(half + 1) * 128], rhs=rv[:, ft, dc * 512:(dc + 1) * 512], start=(ft == 0), stop=(ft == 7)),
                                 reads=[g_.b, r_.b], writes=[p_.b])
                        o0 = dh * 1024 + dc * 512
                        if dc == 0:
                            k.op("act", lambda e, o0=o0, col=col, p_=p_, y_=ys[half]: e.activation(out=y_.t[:, o0:o0 + 512], in_=p_.t[:], func=AF.Copy, scale=GATE.t[:, col:col + 1]),
                                 reads=[p_.b, GATE.b], writes=[ys[half].b])
                        else:
                            k.op("dve", lambda e, o0=o0, col=col, p_=p_, y_=ys[half]: e.tensor_scalar(out=y_.t[:, o0:o0 + 512], in0=p_.t[:], scalar1=GATE.t[:, col:col + 1], scalar2=None, op0=ALU.mult),
                                 reads=[p_.b, GATE.b], writes=[ys[half].b])
            for half in range(2):
                col = ex * 2 + half
                for cq in range(4):
                    k.op("pool", lambda e, col=col, cq=cq, y_=ys[half]: e.indirect_dma_start(out=self.moeh[cq][:, :], out_offset=bass.IndirectOffsetOnAxis(ap=IDX.t[:, col:col + 1], axis=0),
                                                                                             in_=y_.t[:, cq * 512:(cq + 1) * 512], in_offset=None, compute_op=ALU.add),
                         reads=[ys[half].b, IDX.b, xb], writes=[xb], dma=True)
        for cq in range(4):
            k.dma("sp", self.xres[:, cq * 512:(cq + 1) * 512], self.moeh[cq][:, :], [xb], [xb])


def _na_bias_tiles(rpb):
    n, Hh = rpb.shape[0], rpb.shape[1]
    out = np.full((n, Hh, 128, 25, 128), NEG, dtype=np.float32)
    a = np.arange(2)[:, None]; wk = np.arange(64)[None, :]
    for case, i in enumerate((0, 1, 2, 14, 15)):
        kb = min(max(2 * i - 4, 0), 22)
        for jt in range(5):
            for aa in range(2):
                kr = kb + 2 * jt + aa
                for bb in range(2):
                    r = 2 * i + bb
                    rs = min(max(r - 4, 0), 24)
                    if not (rs <= kr < rs + 8):
                        continue
                    dr = kr - r + 7
                    wq = np.arange(64)
                    ws = np.clip(wq - 8, 0, 48)
                    wkk = np.arange(64)[:, None]
                    valid = (wkk >= ws[None, :]) & (wkk < ws[None, :] + 16)
                    dc = np.clip(wkk - wq[None, :] + 15, 0, 30)
                    vals = rpb[:, :, dr, :][:, :, dc]
                    blk = out[:, :, aa * 64:(aa + 1) * 64, case * 5 + jt, bb * 64:(bb + 1) * 64]
                    blk[...] = np.where(valid[None, None], vals, NEG)
    return out


def _prep(inputs, nlayers=DEPTH):
    n_ssd = (nlayers + 1) // 2
    n_na = nlayers // 2
    f = lambda a: np.ascontiguousarray(a, dtype=np.float32)
    common = {
        "norm_mix_g": f(inputs["norm_mix_g"][:nlayers]), "norm_ffn_g": f(inputs["norm_ffn_g"][:nlayers]),
        "norm_final_g": f(inputs["norm_final_g"]).reshape(1, D), "mem_norm_g": f(inputs["mem_norm_g"]).reshape(1, D),
        "ssd_w_in": f(inputs["ssd_w_in"][:n_ssd]), "ssd_conv_w": f(inputs["ssd_conv_w"][:n_ssd]), "ssd_conv_b": f(inputs["ssd_conv_b"][:n_ssd]),
        "ssd_dt_bias": f(inputs["ssd_dt_bias"][:n_ssd]).reshape(n_ssd, 48), "ssd_a_log": f(inputs["ssd_a_log"][:n_ssd]).reshape(n_ssd, 48),
        "ssd_d": f(inputs["ssd_d"][:n_ssd]), "ssd_gate_norm_g": f(inputs["ssd_gate_norm_g"][:n_ssd]), "ssd_w_out": f(inputs["ssd_w_out"][:n_ssd]),
        "xa_w_kv": f(inputs["xa_w_kv"][:nlayers]), "moe_w_router": f(inputs["moe_w_router"][:nlayers]),
        "moe_w1": f(inputs["moe_w1"][:nlayers]), "moe_w3": f(inputs["moe_w3"][:nlayers]), "moe_w2": f(inputs["moe_w2"][:nlayers]),
    }
    if n_na:
        common["na_w_in"] = f(inputs["na_w_in"][:n_na])
        common["na_bias"] = _na_bias_tiles(f(inputs["na_rpb"][:n_na]))
        common["na_w_out"] = f(inputs["na_w_out"][:n_na])
    return common


_NC_CACHE = {}


def kernel(**inputs):
    x = np.asarray(inputs["x"], dtype=np.float32)
    mem = np.asarray(inputs["mem"], dtype=np.float32)
    common = _prep(inputs)
    if "nc" not in _NC_CACHE:
        _NC_CACHE["nc"] = KB().build()
    nc = _NC_CACHE["nc"]
    B = x.shape[0]
    in_maps = []
    for c in range(8):
        b = c % B
        m = dict(common)
        m["x"] = np.ascontiguousarray(x[b])
        m["mem"] = np.ascontiguousarray(mem[b])
        in_maps.append(m)
    res = run_bass_kernel_spmd(nc, in_maps, core_ids=list(range(8)))
    return np.stack([np.asarray(res.results[b]["out"], dtype=np.float32) for b in range(B)], axis=0)
```

```python
import contextlib
import numpy as np
import concourse.bass as bass
import concourse.mybir as mybir
from concourse.bass_utils import run_bass_kernel_spmd

F32 = mybir.dt.float32
BF16 = mybir.dt.bfloat16
I32 = mybir.dt.int32
AF = mybir.ActivationFunctionType
ALU = mybir.AluOpType
AX = mybir.AxisListType

T = 2048
D = 2048
NT = 16
ND = 16
DEPTH = 4
MEM = 256
EPS = 1e-6
D_XA = 512
D_SSD = 1536
SSD_H = 24
SSD_P = 64
SSD_G = 4
SSD_N = 128
SSD_CONV_DIM = 2560
SSD_IN = 4656
NA_H = 12
NA_IN = 5120
NE = 16
CAP = 256
DFF = 1024
NEG = -30000.0

ENGS = ("pe", "act", "dve", "pool", "sp")
ENGOBJ = {"pe": "tensor", "act": "scalar", "dve": "vector", "pool": "gpsimd", "sp": "sync"}
SEM_BLOCK = 30000


class Buf:
    __slots__ = ("name", "last_w", "readers", "sem", "base", "ndma")

    def __init__(self, name):
        self.name = name
        self.last_w = None
        self.readers = []
        self.sem = None
        self.base = 0
        self.ndma = 0


class Op:
    __slots__ = ("eng", "fn", "is_dma", "deps", "need_sig", "sig", "dst", "k")


class _Rec:
    def __init__(self):
        self.call = None

    def __getattr__(self, name):
        def f(*a, **kw):
            self.call = (name, a, kw)
            return self
        return f


class Prog:
    def __init__(self, nc, stack):
        self.nc = nc
        self.stack = stack
        self.q = {e: [] for e in ENGS}
        self.bufs = []
        self.eng_cnt = {e: 0 for e in ENGS}
        self.eng_sems = {}
        self.dma_pool = []
        self.n_sems = 0
        self.n_ops = 0

    def buf(self, name="b"):
        b = Buf(name)
        self.bufs.append(b)
        return b

    def _newsem(self, name):
        self.n_sems += 1
        return self.stack.enter_context(self.nc.semaphore(f"{name}_{self.n_sems}"))

    def op(self, eng, fn, reads=(), writes=(), dma=False):
        o = Op()
        rec = _Rec()
        fn(rec)
        assert rec.call is not None
        o.eng, o.fn, o.is_dma = eng, rec.call, dma
        o.need_sig, o.sig, o.dst, o.k = False, None, None, 0
        deps, seen = [], set()
        for r in reads:
            if r.last_w is not None:
                deps.append(r.last_w)
        for w in writes:
            if w.last_w is not None:
                deps.append(w.last_w)
            deps.extend(w.readers)
        dd = []
        for d in deps:
            if id(d) in seen or d is o:
                continue
            seen.add(id(d))
            if d.eng == "pe" and eng == "pe" and not d.is_dma and not dma:
                continue
            d.need_sig = True
            dd.append(d)
        o.deps = dd
        if dma:
            o.dst = writes[0]
            o.dst.ndma += 1
            o.k = o.dst.ndma
        for r in reads:
            r.readers.append(o)
        for w in writes:
            w.last_w = o
            w.readers = []
        self.q[eng].append(o)
        self.n_ops += 1
        return o

    def flush(self):
        nc = self.nc
        if not any(self.q[e] for e in ENGS):
            return
        lasts = []
        for e in ENGS:
            comp = [o for o in self.q[e] if not o.is_dma]
            if comp:
                comp[-1].need_sig = True
                lasts.append(comp[-1])
        dma_bufs = []
        for e in ENGS:
            for o in self.q[e]:
                if o.is_dma:
                    b = o.dst
                    if b.sem is None:
                        if self.dma_pool:
                            b.sem, b.base = self.dma_pool.pop(0)
                        else:
                            b.sem, b.base = self._newsem("d"), 0
                        dma_bufs.append(b)
                    o.sig = (b.sem, b.base + 16 * o.k)
                elif o.need_sig:
                    c = self.eng_cnt[e]
                    blk = c // SEM_BLOCK
                    key = (e, blk)
                    if key not in self.eng_sems:
                        self.eng_sems[key] = self._newsem(e)
                    o.sig = (self.eng_sems[key], c % SEM_BLOCK + 1)
                    self.eng_cnt[e] = c + 1
        finals = [o.sig for o in lasts] + [(b.sem, b.base + 16 * b.ndma) for b in dma_bufs]

        def run_queue(e, eng):
            waited = {}
            for o in self.q[e]:
                for d in o.deps:
                    sem, val = d.sig
                    if waited.get(id(sem), 0) >= val:
                        continue
                    waited[id(sem)] = val
                    eng.wait_ge(sem, val)
                nm, a_, kw_ = o.fn
                try:
                    ins = getattr(eng, nm)(*a_, **kw_)
                except Exception:
                    print("EMIT FAIL", e, nm, [getattr(x, "shape", x) for x in a_], {k_: getattr(v_, "shape", v_) for k_, v_ in kw_.items()}, flush=True)
                    for k_, v_ in kw_.items():
                        print("   ARG", k_, repr(v_)[:300], repr(getattr(v_, "ap", None))[:300], flush=True)
                    raise
                if o.sig is not None:
                    ins.then_inc(o.sig[0], 16 if o.is_dma else 1)
            for sem, val in finals:
                if waited.get(id(sem), 0) >= val:
                    continue
                eng.wait_ge(sem, val)

        with nc.Block() as block:
            for e in ENGS:
                def mk(e):
                    return lambda eng: run_queue(e, eng)
                getattr(block, ENGOBJ[e])(mk(e))
        for b in dma_bufs:
            cnt = b.base + 16 * b.ndma
            assert cnt < 32000, "dma semaphore count too large"
            self.dma_pool.append((b.sem, cnt))
        for b in self.bufs:
            b.last_w, b.readers, b.sem, b.base, b.ndma = None, [], None, 0, 0
        self.q = {e: [] for e in ENGS}


class Tl:
    __slots__ = ("t", "b")

    def __init__(self, t, b):
        self.t, self.b = t, b


class Scope:
    def __init__(self, k):
        self.k = k
        self.st = contextlib.ExitStack()

    def __enter__(self):
        self.st.__enter__()
        return self

    def __exit__(self, *a):
        self.k.P.flush()
        return self.st.__exit__(*a)

    def sb(self, name, shape, dt):
        self.k.uid += 1
        t = self.st.enter_context(self.k.nc.sbuf_tensor(f"{name}_{self.k.uid}", list(shape), dt))
        return Tl(t, self.k.P.buf(name))

    def ps(self, name, shape, dt=F32):
        self.k.uid += 1
        t = self.st.enter_context(self.k.nc.psum_tensor(f"{name}_{self.k.uid}", list(shape), dt))
        return Tl(t, self.k.P.buf(name))


class KB:
    def __init__(self, nlayers=DEPTH, dbg=None):
        self.nlayers = nlayers
        self.dbg = dbg
        self.uid = 0
        self.nc = bass.Bass("TRN2", target_bir_lowering=False)
        self.outer = contextlib.ExitStack()
        self.P = Prog(self.nc, self.outer)

    def din(self, name, shape, dt=F32):
        return self.nc.dram_tensor(name, list(shape), dt, kind="ExternalInput").ap()

    def dscr(self, name, shape, dt):
        return self.nc.dram_tensor(name, list(shape), dt).ap()

    def op(self, *a, **k):
        return self.P.op(*a, **k)

    def dma(self, q, out, in_, reads, writes, **kw):
        return self.P.op(q, lambda e: e.dma_start(out=out, in_=in_, **kw), reads=reads, writes=writes, dma=True)

    def bcast_load(self, q, dst, row_ap, nparts=128):
        return self.dma(q, dst.t[:], row_ap.partition_broadcast(nparts), [], [dst.b])

    def build(self):
        nc = self.nc
        L = self.nlayers
        n_ssd = (L + 1) // 2
        n_na = L // 2
        I = {}
        self.moeh = [self.dscr(f"moeh{cq}", [T, 512], F32) for cq in range(4)]
        self.hn = self.dscr("hn", [T, D], BF16)
        I["x"] = self.din("x", [T, D])
        I["mem"] = self.din("mem", [MEM, D])
        I["norm_mix_g"] = self.din("norm_mix_g", [L, D])
        I["norm_ffn_g"] = self.din("norm_ffn_g", [L, D])
        I["norm_final_g"] = self.din("norm_final_g", [1, D])
        I["mem_norm_g"] = self.din("mem_norm_g", [1, D])
        I["ssd_w_in"] = self.din("ssd_w_in", [n_ssd, D, SSD_IN])
        I["ssd_conv_w"] = self.din("ssd_conv_w", [n_ssd, 5, SSD_CONV_DIM])
        I["ssd_conv_b"] = self.din("ssd_conv_b", [n_ssd, SSD_CONV_DIM])
        I["ssd_dt_bias"] = self.din("ssd_dt_bias", [n_ssd, 48])
        I["ssd_a_log"] = self.din("ssd_a_log", [n_ssd, 48])
        I["ssd_d"] = self.din("ssd_d", [n_ssd, SSD_H])
        I["ssd_gate_norm_g"] = self.din("ssd_gate_norm_g", [n_ssd, D_SSD])
        I["ssd_w_out"] = self.din("ssd_w_out", [n_ssd, D, D])
        if n_na:
            I["na_w_in"] = self.din("na_w_in", [n_na, D, NA_IN])
            I["na_bias"] = self.din("na_bias", [n_na, NA_H, 128, 25, 128])
            I["na_w_out"] = self.din("na_w_out", [n_na, D, D])
        I["xa_w_kv"] = self.din("xa_w_kv", [L, D, 2 * D_XA])
        I["moe_w_router"] = self.din("moe_w_router", [L, D, NE])
        I["moe_w1"] = self.din("moe_w1", [L, NE, D, DFF])
        I["moe_w3"] = self.din("moe_w3", [L, NE, D, DFF])
        I["moe_w2"] = self.din("moe_w2", [L, NE, DFF, D])
        self.I = I
        self.out = nc.dram_tensor("out", [T, D], F32, kind="ExternalOutput").ap()
        self.xres = self.dscr("xres", [T, D], F32)
        self.ycat = self.dscr("ycat", [T, D], BF16)
        self.szd = self.dscr("szd", [T, D_SSD], BF16)
        self.Gd = self.dscr("Gd", [NT, 128, D_SSD], BF16)
        self.ABd = self.dscr("ABd", [NT, 2 * SSD_H * 128], F32)
        self.TOTd = self.dscr("TOTd", [1, NT * 48], F32)
        self.xsBd = self.dscr("xsBd", [T, 2048], BF16)
        self.BCTd = self.dscr("BCTd", [8, 128, T], BF16)
        self.TOKd = self.dscr("TOKd", [T, 4 * 48], F32)

        with self.outer:
            with Scope(self) as cs:
                self.consts(cs)
                for i in range(L):
                    if i % 2 == 0:
                        self.ssd_layer(cs, i)
                    else:
                        self.na_layer(cs, i)
                    if self.dbg == ("mix", i):
                        self.dump_xres(cs)
                        return self.nc
                    self.moe_layer(cs, i)
                    if self.dbg == ("moe", i):
                        self.dump_xres(cs)
                        return self.nc
                self.final_norm(cs)
        return self.nc

    def consts(self, cs):
        k = self
        self.identf = cs.sb("identf", [128, 128], F32)
        self.ident = cs.sb("ident", [128, 128], BF16)
        self.ones_bf = cs.sb("ones_bf", [128, 128], BF16)
        self.maskf = cs.sb("maskf", [128, 128], F32)
        self.maskb = cs.sb("maskb", [128, 128], F32)
        self.iota256 = cs.sb("iota256", [128, 256], F32)
        self.memT = cs.sb("memT", [128, ND, MEM], BF16)
        self.epsc = cs.sb("epsc", [128, 1], F32)
        idf, idb = self.identf, self.ident
        k.op("pool", lambda e: e.memset(idf.t[:], 0.0), writes=[idf.b])
        k.op("pool", lambda e: e.affine_select(out=idf.t[:], in_=idf.t[:], pattern=[[-1, 128]], compare_op=ALU.not_equal,
                                               fill=1.0, base=0, channel_multiplier=1), reads=[idf.b], writes=[idf.b])
        k.op("dve", lambda e: e.tensor_copy(out=idb.t[:], in_=idf.t[:]), reads=[idf.b], writes=[idb.b])
        k.op("pool", lambda e: e.memset(self.ones_bf.t[:], 1.0), writes=[self.ones_bf.b])
        k.op("pool", lambda e: e.memset(self.epsc.t[:], EPS), writes=[self.epsc.b])
        k.op("pool", lambda e: e.memset(self.maskf.t[:], 1.0), writes=[self.maskf.b])
        k.op("pool", lambda e: e.affine_select(out=self.maskf.t[:], in_=self.maskf.t[:], pattern=[[1, 128]], compare_op=ALU.is_ge,
                                               fill=0.0, base=0, channel_multiplier=-1), reads=[self.maskf.b], writes=[self.maskf.b])
        k.op("pool", lambda e: e.memset(self.maskb.t[:], 1.0), writes=[self.maskb.b])
        k.op("pool", lambda e: e.affine_select(out=self.maskb.t[:], in_=self.maskb.t[:], pattern=[[-1, 128]], compare_op=ALU.is_ge,
                                               fill=0.0, base=0, channel_multiplier=1), reads=[self.maskb.b], writes=[self.maskb.b])
        k.op("pool", lambda e: e.iota(self.iota256.t[:], pattern=[[1, 256]], base=0, channel_multiplier=0,
                                      allow_small_or_imprecise_dtypes=True), writes=[self.iota256.b])
        xb = self.P.buf("xres_all")
        k.dma("sp", self.xres[:, :], self.I["x"][:, :], [], [xb])
        self.P.flush()
        with Scope(self) as s:
            g = s.sb("g", [128, D], F32)
            k.bcast_load("sp", g, self.I["mem_norm_g"][0:1, :])
            for mt in range(2):
                self.norm_tile(s, self.I["mem"][mt * 128:(mt + 1) * 128, :], g, dstT=self.memT, tcol=mt * 128, tag=f"m{mt}")

    def norm_tile(self, s, src_ap, g, dstT=None, tcol=0, tag="", hn_dst=None, want_f32=None, q="sp"):
        k = self
        if not hasattr(s, "_nt"):
            s._nt = {}
            for j in range(3):
                s._nt[j] = dict(
                    xt=s.sb("n_xt", [128, D], F32), ss=s.sb("n_ss", [128, 1], F32),
                    hb=s.sb("n_hb", [128, D], BF16), pt=s.ps("n_pt", [128, 8, 128], BF16) if j < 2 else None)
            s._ntc = 0
        R = s._nt[s._ntc % 3]
        pt = s._nt[s._ntc % 2]["pt"]
        s._ntc += 1
        xt, ss, hb = R["xt"], R["ss"], R["hb"]
        k.dma(q, xt.t[:], src_ap, [], [xt.b])
        k.op("act", lambda e: e.activation(out=hb.t[:], in_=xt.t[:], func=AF.Square, accum_out=ss.t[:]), reads=[xt.b], writes=[hb.b, ss.b])
        k.op("act", lambda e: e.activation(out=ss.t[:], in_=ss.t[:], func=AF.Sqrt, scale=1.0 / D, bias=self.epsc.t[:]), reads=[ss.b, self.epsc.b], writes=[ss.b])
        k.op("dve", lambda e: e.reciprocal(out=ss.t[:], in_=ss.t[:]), reads=[ss.b], writes=[ss.b])
        if want_f32 is not None:
            hf = want_f32
            k.op("dve", lambda e: e.scalar_tensor_tensor(out=hf.t[:], in0=xt.t[:], scalar=ss.t[:, 0:1], in1=g.t[:], op0=ALU.mult, op1=ALU.mult),
                 reads=[xt.b, ss.b, g.b], writes=[hf.b])
            k.op("act", lambda e: e.activation(out=hb.t[:], in_=hf.t[:], func=AF.Copy), reads=[hf.b], writes=[hb.b])
        else:
            k.op("dve", lambda e: e.scalar_tensor_tensor(out=hb.t[:], in0=xt.t[:], scalar=ss.t[:, 0:1], in1=g.t[:], op0=ALU.mult, op1=ALU.mult),
                 reads=[xt.b, ss.b, g.b], writes=[hb.b])
        if hn_dst is not None:
            k.dma("sp", hn_dst[0], hb.t[:], [hb.b], [hn_dst[1]])
        if dstT is not None:
            for half in range(2):
                for j in range(8):
                    kk = half * 8 + j
                    k.op("pe", lambda e, kk=kk, j=j: e.transpose(out=pt.t[:, j, :], in_=hb.t[:, kk * 128:(kk + 1) * 128], identity=self.ident.t[:]),
                         reads=[hb.b, self.ident.b], writes=[pt.b])
                eng = "dve" if half == 0 else "act"
                if eng == "dve":
                    k.op("dve", lambda e, half=half: e.tensor_copy(out=dstT.t[:, half * 8:(half + 1) * 8, tcol:tcol + 128], in_=pt.t[:]),
                         reads=[pt.b], writes=[dstT.b])
                else:
                    k.op("act", lambda e, half=half: e.activation(out=dstT.t[:, half * 8:(half + 1) * 8, tcol:tcol + 128], in_=pt.t[:], func=AF.Copy),
                         reads=[pt.b], writes=[dstT.b])

    def dump_xres(self, cs):
        self.P.flush()
        ob = self.P.buf("out")
        self.dma("sp", self.out[:, :], self.xres[:, :], [], [ob])
        self.P.flush()

    def final_norm(self, cs):
        k = self
        with Scope(self) as s:
            g = s.sb("g", [128, D], F32)
            k.bcast_load("sp", g, self.I["norm_final_g"][0:1, :])
            outs = [s.sb("fo", [128, D], F32) for _ in range(2)]
            ob = self.P.buf("out")
            for tt in range(NT):
                o = outs[tt % 2]
                self.norm_tile(s, self.xres[tt * 128:(tt + 1) * 128, :], g, want_f32=o, tag=f"f{tt}")
                k.dma("sp", self.out[tt * 128:(tt + 1) * 128, :], o.t[:], [o.b], [ob])

    def wblock(self, s, w_ap, c0, ncols, nk=ND):
        if not hasattr(s, "_wb"):
            s._wb = [s.sb("wblk", [128, ND, 512], BF16) for _ in range(2)]
            s._wbc = 0
        w = s._wb[s._wbc % 2]
        s._wbc += 1
        self.dma("pool", w.t[:, 0:nk, 0:ncols], w_ap[:, c0:c0 + ncols].rearrange("(k p) n -> p k n", p=128), [], [w.b])
        return w

    def proj_fm(self, s, hT, w, ncols, ps_bank_tiles, et):
        m0 = et * 128
        m = min(128, ncols - m0)
        for tc in range(4):
            pb = ps_bank_tiles[tc]
            for kk in range(ND):
                self.op("pe", lambda e, kk=kk, tc=tc, pb=pb: e.matmul(pb.t[0:m, :], lhsT=w.t[:, kk, m0:m0 + m], rhs=hT.t[:, kk, tc * 512:(tc + 1) * 512],
                                                                     start=(kk == 0), stop=(kk == ND - 1)),
                        reads=[w.b, hT.b], writes=[pb.b])

    def proj_tm(self, hT, w, ncols, pb, tt):
        for kk in range(ND):
            self.op("pe", lambda e, kk=kk: e.matmul(pb.t[:, 0:ncols], lhsT=hT.t[:, kk, tt * 128:(tt + 1) * 128], rhs=w.t[:, kk, 0:ncols],
                                                    start=(kk == 0), stop=(kk == ND - 1)),
                    reads=[w.b, hT.b], writes=[pb.b])

    def norm_to_hT(self, s, hT, gain_row):
        g = s.sb("g", [128, D], F32)
        self.bcast_load("sp", g, gain_row)
        for tt in range(NT):
            self.norm_tile(s, self.xres[tt * 128:(tt + 1) * 128, :], g, dstT=hT, tcol=tt * 128)

    def xa_block(self, s, li, hT, wq_ap, c0):
        k = self
        qT = s.sb("xa_qT", [128, 4, T], BF16)
        kT = s.sb("xa_kT", [128, 4, MEM], BF16)
        va = s.sb("xa_va", [128, 2, 4, 129], BF16)
        pbs = [s.ps("xa_pb", [128, 512]) for _ in range(4)]
        w = self.wblock(s, wq_ap, c0, 512)
        for et in range(4):
            self.proj_fm(s, hT, w, 512, pbs, et)
            for tc in range(4):
                eng = "act" if tc % 2 == 0 else "dve"
                if eng == "act":
                    k.op("act", lambda e, et=et, tc=tc: e.activation(out=qT.t[:, et, tc * 512:(tc + 1) * 512], in_=pbs[tc].t[:], func=AF.Copy),
                         reads=[pbs[tc].b], writes=[qT.b])
                else:
                    k.op("dve", lambda e, et=et, tc=tc: e.tensor_copy(out=qT.t[:, et, tc * 512:(tc + 1) * 512], in_=pbs[tc].t[:]),
                         reads=[pbs[tc].b], writes=[qT.b])
        wkv = self.I["xa_w_kv"][li]
        w = self.wblock(s, wkv, 0, 512)
        for et in range(4):
            pb = pbs[et]
            for kk in range(ND):
                k.op("pe", lambda e, kk=kk, et=et, pb=pb: e.matmul(pb.t[:, 0:MEM], lhsT=w.t[:, kk, et * 128:(et + 1) * 128], rhs=self.memT.t[:, kk, :],
                                                                  start=(kk == 0), stop=(kk == ND - 1)), reads=[w.b, self.memT.b], writes=[pb.b])
            k.op("act", lambda e, et=et, pb=pb: e.activation(out=kT.t[:, et, :], in_=pb.t[:, 0:MEM], func=AF.Copy), reads=[pb.b], writes=[kT.b])
        w = self.wblock(s, wkv, 512, 512)
        k.op("pool", lambda e: e.memset(va.t[:], 1.0), writes=[va.b])
        for mt in range(2):
            pb = pbs[mt]
            for kk in range(ND):
                k.op("pe", lambda e, kk=kk, mt=mt, pb=pb: e.matmul(pb.t[:, :], lhsT=self.memT.t[:, kk, mt * 128:(mt + 1) * 128], rhs=w.t[:, kk, :],
                                                                  start=(kk == 0), stop=(kk == ND - 1)), reads=[w.b, self.memT.b], writes=[pb.b])
            k.op("dve", lambda e, mt=mt, pb=pb: e.tensor_copy(out=va.t[:, mt, :, 0:128], in_=pb.t[:].rearrange("p (h d) -> p h d", h=4)),
                 reads=[pb.b], writes=[va.b])
        sps = [s.ps("xa_s", [128, 2, 128]) for _ in range(2)]
        ops_ = [s.ps("xa_o", [128, 4, 256]) for _ in range(1)]
        pT = [s.sb("xa_p", [128, 2, 128], BF16) for _ in range(2)]
        rc = [s.sb("xa_rc", [128, 4], F32) for _ in range(2)]
        ot = [s.sb("xa_ot", [128, 512], BF16) for _ in range(2)]
        yb = self.P.buf("ycat_xa")
        scale = 128.0 ** -0.5
        it = 0
        for tt in range(NT):
            o_ps, o_t, r_c = ops_[0], ot[tt % 2], rc[tt % 2]
            for h in range(4):
                sp_, p_ = sps[it % 2], pT[it % 2]
                it += 1
                for mt in range(2):
                    k.op("pe", lambda e, mt=mt, h=h, sp_=sp_: e.matmul(sp_.t[:, mt, :], lhsT=kT.t[:, h, mt * 128:(mt + 1) * 128], rhs=qT.t[:, h, tt * 128:(tt + 1) * 128],
                                                                      start=True, stop=True), reads=[kT.b, qT.b], writes=[sp_.b])
                k.op("act", lambda e, sp_=sp_, p_=p_: e.activation(out=p_.t[:], in_=sp_.t[:], func=AF.Exp, scale=scale), reads=[sp_.b], writes=[p_.b])
                for mt in range(2):
                    k.op("pe", lambda e, mt=mt, h=h, p_=p_, o_ps=o_ps: e.matmul(o_ps.t[:, h, 0:129], lhsT=p_.t[:, mt, :], rhs=va.t[:, mt, h, :],
                                                                               start=(mt == 0), stop=(mt == 1)), reads=[p_.b, va.b], writes=[o_ps.b])
            k.op("dve", lambda e, o_ps=o_ps, r_c=r_c: e.reciprocal(out=r_c.t[:], in_=o_ps.t[:, :, 128]), reads=[o_ps.b], writes=[r_c.b])
            k.op("dve", lambda e, o_ps=o_ps, r_c=r_c, o_t=o_t: e.tensor_tensor(out=o_t.t[:].rearrange("p (h d) -> p h d", h=4), in0=o_ps.t[:, :, 0:128],
                                                                               in1=r_c.t[:].unsqueeze(2).to_broadcast([128, 4, 128]), op=ALU.mult),
                 reads=[o_ps.b, r_c.b], writes=[o_t.b])
            k.dma("sp", self.ycat[tt * 128:(tt + 1) * 128, D_SSD:D], o_t.t[:], [o_t.b], [yb])

    def out_proj(self, w_ap):
        k = self
        with Scope(self) as s:
            W = s.sb("wout", [128, ND, D], BF16)
            for c in range(4):
                k.dma("pool", W.t[:, :, c * 512:(c + 1) * 512], w_ap[:, c * 512:(c + 1) * 512].rearrange("(k p) n -> p k n", p=128), [], [W.b])
            yt = [s.sb("op_y", [128, D], BF16) for _ in range(2)]
            yT = [s.sb("op_yT", [128, ND, 128], BF16) for _ in range(2)]
            pt = [s.ps("op_pt", [128, 8, 128], BF16) for _ in range(2)]
            po = [s.ps("op_po", [128, 512]) for _ in range(4)]
            mix = [s.sb("op_mix", [128, D], F32) for _ in range(2)]
            xb = self.P.buf("xres_w")
            for tt in range(NT):
                y_, yT_, mx = yt[tt % 2], yT[tt % 2], mix[tt % 2]
                k.dma("sp", y_.t[:], self.ycat[tt * 128:(tt + 1) * 128, :], [], [y_.b])
                for half in range(2):
                    p_ = pt[half]
                    for j in range(8):
                        kk = half * 8 + j
                        k.op("pe", lambda e, kk=kk, j=j, p_=p_, y_=y_: e.transpose(out=p_.t[:, j, :], in_=y_.t[:, kk * 128:(kk + 1) * 128], identity=self.ident.t[:]),
                             reads=[y_.b, self.ident.b], writes=[p_.b])
                    if half == 0:
                        k.op("dve", lambda e, p_=p_, yT_=yT_: e.tensor_copy(out=yT_.t[:, 0:8, :], in_=p_.t[:]), reads=[p_.b], writes=[yT_.b])
                    else:
                        k.op("act", lambda e, p_=p_, yT_=yT_: e.activation(out=yT_.t[:, 8:16, :], in_=p_.t[:], func=AF.Copy), reads=[p_.b], writes=[yT_.b])
                for dc in range(4):
                    pb = po[dc]
                    for kk in range(ND):
                        k.op("pe", lambda e, kk=kk, dc=dc, pb=pb, yT_=yT_: e.matmul(pb.t[:], lhsT=yT_.t[:, kk, :], rhs=W.t[:, kk, dc * 512:(dc + 1) * 512],
                                                                                   start=(kk == 0), stop=(kk == ND - 1)), reads=[yT_.b, W.b], writes=[pb.b])
                    if dc % 2 == 0:
                        k.op("act", lambda e, dc=dc, pb=pb, mx=mx: e.activation(out=mx.t[:, dc * 512:(dc + 1) * 512], in_=pb.t[:], func=AF.Copy), reads=[pb.b], writes=[mx.b])
                    else:
                        k.op("dve", lambda e, dc=dc, pb=pb, mx=mx: e.tensor_copy(out=mx.t[:, dc * 512:(dc + 1) * 512], in_=pb.t[:]), reads=[pb.b], writes=[mx.b])
                k.dma("pool", self.xres[tt * 128:(tt + 1) * 128, :], mx.t[:], [mx.b], [xb], accum_op=ALU.add)

    def ssd_layer(self, cs, li):
        j = li // 2
        Win = self.I["ssd_w_in"][j]
        with Scope(self) as AS:
            hT = AS.sb("hT", [128, ND, T], BF16)
            with Scope(self) as s:
                self.norm_to_hT(s, hT, self.I["norm_mix_g"][li:li + 1, :])
            with Scope(self) as s:
                self.ssd_proj_z(s, Win, hT)
            with Scope(self) as s:
                self.ssd_proj_xbc(s, j, Win, hT)
            with Scope(self) as s:
                self.ssd_proj_dt(s, j, Win, hT)
            with Scope(self) as s:
                self.xa_block(s, li, hT, Win, 4144)
        with Scope(self) as s:
            self.ssd_scan(s, j)
        self.out_proj(self.I["ssd_w_out"][j])

    def ssd_proj_z(self, s, Win, hT):
        k = self
        pbs = [s.ps("z_pb", [128, 512]) for _ in range(4)]
        zs = [s.sb("z_sb", [128, 512], BF16) for _ in range(4)]
        zb = self.P.buf("szd")
        it = 0
        for blk in range(3):
            w = self.wblock(s, Win, blk * 512, 512)
            for tt in range(NT):
                pb, z_ = pbs[it % 4], zs[it % 4]
                it += 1
                self.proj_tm(hT, w, 512, pb, tt)
                k.op("act", lambda e, pb=pb, z_=z_: e.activation(out=z_.t[:], in_=pb.t[:], func=AF.Silu), reads=[pb.b], writes=[z_.b])
                k.dma("sp", self.szd[tt * 128:(tt + 1) * 128, blk * 512:(blk + 1) * 512], z_.t[:], [z_.b], [zb])

    def ssd_proj_xbc(self, s, j, Win, hT):
        k = self
        raw = s.sb("cw_raw", [120, 128], F32)
        cwT = s.sb("cwT", [128, 120], F32)
        ptr = s.ps("cw_pt", [128, 120], F32)
        k.dma("sp", raw.t[0:100, :], self.I["ssd_conv_w"][j].rearrange("k (n p) -> (k n) p", p=128), [], [raw.b])
        k.dma("sp", raw.t[100:120, :], self.I["ssd_conv_b"][j:j + 1, :].rearrange("o (n p) -> (o n) p", p=128), [], [raw.b])
        k.op("pe", lambda e: e.transpose(out=ptr.t[:], in_=raw.t[:], identity=self.identf.t[0:120, 0:120]), reads=[raw.b, self.identf.b], writes=[ptr.b])
        k.op("dve", lambda e: e.tensor_copy(out=cwT.t[:], in_=ptr.t[:]), reads=[ptr.b], writes=[cwT.b])
        pbs = [s.ps("x_pb", [128, 512]) for _ in range(4)]
        pcs = [s.ps("x_pc", [128, 512]) for _ in range(2)]
        ptt = s.ps("x_ptt", [128, 8, 128], BF16)
        pre = [s.sb("x_pre", [128, T + 4], BF16) for _ in range(2)]
        post = [s.sb("x_post", [128, T], BF16) for _ in range(2)]
        dg = [s.sb("x_dg", [128, 5, 128], BF16) for _ in range(2)]
        tok = [s.sb("x_tok", [128, NT, 128], BF16) for _ in range(2)]
        xb = self.P.buf("xsBd")
        bb = self.P.buf("BCTd")
        for p_ in pre:
            k.op("pool", lambda e, p_=p_: e.memset(p_.t[:], 0.0), writes=[p_.b])
        cvi = 0
        for blk in range(5):
            w = self.wblock(s, Win, 1536 + blk * 512, 512)
            for et in range(4):
                ct = blk * 4 + et
                pr, po, dg_, tk = pre[ct % 2], post[ct % 2], dg[ct % 2], tok[ct % 2]
                for kk in range(5):
                    k.op("dve", lambda e, kk=kk, ct=ct, dg_=dg_: e.tensor_scalar(out=dg_.t[:, kk, :], in0=self.identf.t[:], scalar1=cwT.t[:, kk * 20 + ct:kk * 20 + ct + 1],
                                                                               scalar2=None, op0=ALU.mult), reads=[self.identf.b, cwT.b], writes=[dg_.b])
                self.proj_fm(s, hT, w, 512, pbs, et)
                for tc in range(4):
                    if tc % 2 == 0:
                        k.op("act", lambda e, tc=tc, pr=pr: e.activation(out=pr.t[:, 2 + tc * 512:2 + (tc + 1) * 512], in_=pbs[tc].t[:], func=AF.Copy), reads=[pbs[tc].b], writes=[pr.b])
                    else:
                        k.op("dve", lambda e, tc=tc, pr=pr: e.tensor_copy(out=pr.t[:, 2 + tc * 512:2 + (tc + 1) * 512], in_=pbs[tc].t[:]), reads=[pbs[tc].b], writes=[pr.b])
                for tc in range(4):
                    pc = pcs[cvi % 2]
                    cvi += 1
                    for kk in range(5):
                        k.op("pe", lambda e, kk=kk, tc=tc, pc=pc, dg_=dg_, pr=pr: e.matmul(pc.t[:], lhsT=dg_.t[:, kk, :], rhs=pr.t[:, tc * 512 + kk:tc * 512 + kk + 512],
                                                                                          start=(kk == 0), stop=(kk == 4)), reads=[dg_.b, pr.b], writes=[pc.b])
                    k.op("act", lambda e, tc=tc, pc=pc, po=po, ct=ct: e.activation(out=po.t[:, tc * 512:(tc + 1) * 512], in_=pc.t[:], func=AF.Silu, bias=cwT.t[:, 100 + ct:101 + ct]),
                         reads=[pc.b, cwT.b], writes=[po.b])
                if ct >= 12:
                    k.dma("sp", self.BCTd[ct - 12], po.t[:], [po.b], [bb])
                if ct < 16:
                    for half in range(2):
                        for jj in range(8):
                            tt = half * 8 + jj
                            k.op("pe", lambda e, jj=jj, tt=tt, po=po: e.transpose(out=ptt.t[:, jj, :], in_=po.t[:, tt * 128:(tt + 1) * 128], identity=self.ident.t[:]),
                                 reads=[po.b, self.ident.b], writes=[ptt.b])
                        k.op("dve", lambda e, half=half, tk=tk: e.tensor_copy(out=tk.t[:, half * 8:(half + 1) * 8, :], in_=ptt.t[:]), reads=[ptt.b], writes=[tk.b])
                    k.dma("sp", self.xsBd.rearrange("(n p) c -> p n c", p=128)[:, :, ct * 128:(ct + 1) * 128], tk.t[:], [tk.b], [xb])

    def ssd_proj_dt(self, s, j, Win, hT):
        k = self
        pbs = [s.ps("d_pb", [128, 512]) for _ in range(4)]
        ptk = s.ps("d_ptk", [128, 4, 48], F32)
        dtb = s.sb("d_dtb", [48, 1], F32)
        nA = s.sb("d_nA", [48, 1], F32)
        v = s.sb("d_v", [48, T], F32)
        a = s.sb("d_a", [48, T], F32)
        dt = s.sb("d_dt", [48, T], F32)
        dA = s.sb("d_dA", [48, T], F32)
        pre = s.sb("d_pre", [48, T], F32)
        suf = s.sb("d_suf", [48, T], F32)
        tk = [s.sb("d_tk", [128, 4, 48], F32) for _ in range(2)]
        k.dma("sp", dtb.t[:], self.I["ssd_dt_bias"][j:j + 1, :].rearrange("o h -> h o"), [], [dtb.b])
        k.dma("sp", nA.t[:], self.I["ssd_a_log"][j:j + 1, :].rearrange("o h -> h o"), [], [nA.b])
        k.op("act", lambda e: e.activation(out=nA.t[:], in_=nA.t[:], func=AF.Exp), reads=[nA.b], writes=[nA.b])
        k.op("dve", lambda e: e.tensor_scalar(out=nA.t[:], in0=nA.t[:], scalar1=-1.0, scalar2=None, op0=ALU.mult), reads=[nA.b], writes=[nA.b])
        w = self.wblock(s, Win, 4096, 48)
        self.proj_fm(s, hT, w, 48, pbs, 0)
        for tc in range(4):
            k.op("act", lambda e, tc=tc: e.activation(out=v.t[:, tc * 512:(tc + 1) * 512], in_=pbs[tc].t[0:48, :], func=AF.Identity, bias=dtb.t[:]),
                 reads=[pbs[tc].b, dtb.b], writes=[v.b])
        k.op("dve", lambda e: e.scalar_tensor_tensor(out=a.t[:], in0=v.t[:], scalar=-1.0, in1=v.t[:], op0=ALU.mult, op1=ALU.max), reads=[v.b], writes=[a.b])
        k.op("act", lambda e: e.activation(out=a.t[:], in_=a.t[:], func=AF.Exp, scale=-1.0), reads=[a.b], writes=[a.b])
        k.op("act", lambda e: e.activation(out=a.t[:], in_=a.t[:], func=AF.Ln, bias=1.0), reads=[a.b], writes=[a.b])
        k.op("dve", lambda e: e.scalar_tensor_tensor(out=dt.t[:], in0=v.t[:], scalar=0.0, in1=a.t[:], op0=ALU.max, op1=ALU.add), reads=[v.b, a.b], writes=[dt.b])
        k.op("dve", lambda e: e.tensor_scalar(out=dA.t[:], in0=dt.t[:], scalar1=nA.t[:, 0:1], scalar2=None, op0=ALU.mult), reads=[dt.b, nA.b], writes=[dA.b])
        for c in range(NT):
            sl = slice(c * 128, (c + 1) * 128)
            k.op("dve", lambda e, sl=sl: e.tensor_tensor_scan(out=pre.t[:, sl], data0=dA.t[:, sl], data1=dA.t[:, sl], initial=0.0, op0=ALU.add, op1=ALU.bypass),
                 reads=[dA.b], writes=[pre.b])
        for c in range(NT):
            sl = slice(c * 128, (c + 1) * 128)
            k.op("dve", lambda e, sl=sl, c=c: e.tensor_scalar(out=suf.t[:, sl], in0=pre.t[:, sl], scalar1=-1.0, scalar2=pre.t[:, c * 128 + 127:c * 128 + 128],
                                                             op0=ALU.mult, op1=ALU.add), reads=[pre.b], writes=[suf.b])
        k.op("dve", lambda e: e.tensor_tensor(out=suf.t[:], in0=suf.t[:], in1=dA.t[:], op=ALU.add), reads=[suf.b, dA.b], writes=[suf.b])
        ab = self.P.buf("ABd")
        tb = self.P.buf("TOTd")
        kb = self.P.buf("TOKd")
        ABv = self.ABd.rearrange("c (d h l) -> d h c l", d=2, h=SSD_H)
        k.dma("sp", ABv[0], pre.t[0:24, :].rearrange("h (c l) -> h c l", l=128), [pre.b], [ab])
        k.dma("sp", ABv[1], suf.t[24:48, :].rearrange("h (c l) -> h c l", l=128), [suf.b], [ab])
        k.dma("sp", self.TOTd.rearrange("o (c h) -> h (o c)", h=48), pre.t[:, 127::128], [pre.b], [tb], allow_slow_non_contiguous=True)
        srcs = [dt, pre, suf, dA]
        for c in range(NT):
            t_ = tk[c % 2]
            for qi, src in enumerate(srcs):
                k.op("pe", lambda e, qi=qi, src=src, c=c: e.transpose(out=ptk.t[:, qi, :], in_=src.t[:, c * 128:(c + 1) * 128], identity=self.identf.t[0:48, 0:48]),
                     reads=[src.b, self.identf.b], writes=[ptk.b])
            k.op("dve", lambda e, t_=t_: e.tensor_copy(out=t_.t[:], in_=ptk.t[:]), reads=[ptk.b], writes=[t_.b])
            k.dma("sp", self.TOKd[c * 128:(c + 1) * 128, :], t_.t[:].rearrange("p a h -> p (a h)"), [t_.b], [kb])

    def ssd_scan(self, s, j):
        k = self
        H, Pd = SSD_H, SSD_P
        Dful = s.sb("sc_D", [128, D_SSD], F32)
        d24 = s.sb("sc_d24", [128, H], F32)
        gn = s.sb("sc_gn", [128, D_SSD], F32)
        CD = s.sb("sc_CD", [128, NT, 48], F32)
        k.bcast_load("sp", d24, self.I["ssd_d"][j:j + 1, :])
        k.bcast_load("sp", gn, self.I["ssd_gate_norm_g"][j:j + 1, :])
        k.bcast_load("sp", CD_flat := Tl(CD.t, CD.b), self.TOTd[0:1, :]) if False else k.dma("sp", CD.t[:].rearrange("p c h -> p (c h)"), self.TOTd[0:1, :].partition_broadcast(128), [], [CD.b])
        k.op("act", lambda e: e.activation(out=CD.t[:], in_=CD.t[:], func=AF.Exp), reads=[CD.b], writes=[CD.b])
        k.op("dve", lambda e: e.tensor_copy(out=Dful.t[:].rearrange("p (h q) -> p h q", h=H), in_=d24.t[:].unsqueeze(2).to_broadcast([128, H, Pd])), reads=[d24.b], writes=[Dful.b])
        xs = [s.sb("sc_xs", [128, 2048], BF16) for _ in range(2)]
        tok = [s.sb("sc_tok", [128, 4, 48], F32) for _ in range(2)]
        Hs = s.sb("sc_H", [128, D_SSD], F32)
        Hbf = [s.sb("sc_Hbf", [128, D_SSD], BF16) for _ in range(2)]
        w24 = [s.sb("sc_w24", [128, H], F32) for _ in range(2)]
        xdd = [s.sb("sc_xdd", [128, D_SSD], BF16) for _ in range(2)]
        psS = [s.ps("sc_psS", [128, 512])] * 2
        gb = self.P.buf("Gd")
        xview = lambda t_: t_.t[:, 0:D_SSD].rearrange("p (h q) -> p h q", h=H)

        def load_chunk(c, bi):
            k.dma("sp", xs[bi].t[:], self.xsBd[c * 128:(c + 1) * 128, :], [], [xs[bi].b])
            k.dma("sp", tok[bi].t[:].rearrange("p a h -> p (a h)"), self.TOKd[c * 128:(c + 1) * 128, :], [], [tok[bi].b])

        def states(c, bi, d, Hacc, psl):
            ho = 24 * d
            x_, t_, w_, xd = xs[bi], tok[bi], w24[bi], xdd[bi]
            src = 2 if d == 0 else 1
            k.op("dve", lambda e: e.tensor_tensor(out=w_.t[:], in0=t_.t[:, src, ho:ho + 24], in1=t_.t[:, 3, ho:ho + 24], op=ALU.subtract), reads=[t_.b], writes=[w_.b])
            k.op("act", lambda e: e.activation(out=w_.t[:], in_=w_.t[:], func=AF.Exp), reads=[w_.b], writes=[w_.b])
            k.op("dve", lambda e: e.tensor_tensor(out=w_.t[:], in0=w_.t[:], in1=t_.t[:, 0, ho:ho + 24], op=ALU.mult), reads=[w_.b, t_.b], writes=[w_.b])
            k.op("pool", lambda e: e.tensor_tensor(out=xd.t[:].rearrange("p (h q) -> p h q", h=H), in0=xview(x_), in1=w_.t[:].unsqueeze(2).to_broadcast([128, H, Pd]), op=ALU.mult),
                 reads=[x_.b, w_.b], writes=[xd.b])
            for g in range(SSD_G):
                ps_ = psl[g % 2]
                k.op("pe", lambda e, g=g, ps_=ps_: e.matmul(ps_.t[:, 0:384], lhsT=x_.t[:, D_SSD + g * 128:D_SSD + (g + 1) * 128], rhs=xd.t[:, g * 384:(g + 1) * 384], start=True, stop=True),
                     reads=[x_.b, xd.b], writes=[ps_.b])
                hv = Hacc.t[:, g * 384:(g + 1) * 384]
                k.op("dve", lambda e, g=g, hv=hv: e.tensor_tensor(out=hv.rearrange("p (h q) -> p h q", h=6), in0=hv.rearrange("p (h q) -> p h q", h=6),
                                                                 in1=CD.t[:, c, ho + g * 6:ho + g * 6 + 6].unsqueeze(2).to_broadcast([128, 6, Pd]), op=ALU.mult),
                     reads=[Hacc.b, CD.b], writes=[Hacc.b])
                k.op("dve", lambda e, g=g, hv=hv, ps_=ps_: e.tensor_tensor(out=hv, in0=hv, in1=ps_.t[:, 0:384], op=ALU.add), reads=[Hacc.b, ps_.b], writes=[Hacc.b])

        k.op("pool", lambda e: e.memset(Hs.t[:], 0.0), writes=[Hs.b])
        load_chunk(NT - 1, (NT - 1) % 2)
        for c in range(NT - 1, -1, -1):
            bi = c % 2
            if c > 0:
                load_chunk(c - 1, (c - 1) % 2)
            hb_ = Hbf[bi]
            k.op("act", lambda e, hb_=hb_: e.activation(out=hb_.t[:], in_=Hs.t[:], func=AF.Copy), reads=[Hs.b], writes=[hb_.b])
            k.dma("sp", self.Gd[c], hb_.t[:], [hb_.b], [gb])
            if c > 0:
                states(c, bi, 1, Hs, psS)
        self.P.flush()
        bct = [s.sb("sc_bct", [128, 8, 128], BF16) for _ in range(2)]
        bc = [s.sb("sc_bc", [128, 2, H, 128], F32) for _ in range(2)]
        Gc = [s.sb("sc_G", [128, D_SSD], BF16) for _ in range(2)]
        sz = [s.sb("sc_sz", [128, D_SSD], BF16) for _ in range(2)]
        eA = [s.sb("sc_eA", [128, 48], F32) for _ in range(2)]
        ntk = [s.sb("sc_ntk", [128, 2, 48], F32) for _ in range(2)]
        xdt = [[s.sb("sc_xdt", [128, D_SSD], BF16) for _ in range(2)] for _ in range(2)]
        mcb = [[s.sb("sc_mcb", [128, 4, 128], F32) for _ in range(2)] for _ in range(2)]
        seg = [[s.sb("sc_seg", [128, 6, 128], F32) for _ in range(2)] for _ in range(2)]
        MT = [[s.sb("sc_MT", [128, 6, 128], BF16) for _ in range(2)] for _ in range(2)]
        t1 = [s.sb("sc_t1", [128, 384], F32) for _ in range(2)]
        t2 = [s.sb("sc_t2", [128, 384], F32) for _ in range(2)]
        yall = [s.sb("sc_yall", [128, D_SSD], F32) for _ in range(2)]
        junk = s.sb("sc_junk", [128, 384], BF16)
        ss = [s.sb("sc_ss", [128, 4], F32) for _ in range(2)]
        yn = [s.sb("sc_yn", [128, D_SSD], BF16) for _ in range(2)]
        psCB = s.ps("sc_psCB", [128, 4, 128])
        psY = [[s.ps("sc_psY", [128, 512]) for _ in range(3)] for _ in range(2)]
        yb_ = self.P.buf("ycat_ssd")

        def load2(c, bi):
            load_chunk(c, bi)
            k.dma("sp", bct[bi].t[:], self.BCTd.rearrange("g n t -> n g t")[:, :, c * 128:(c + 1) * 128], [], [bct[bi].b])
            k.dma("sp", bc[bi].t[:].rearrange("p d h l -> p (d h l)"), self.ABd[c:c + 1, :].partition_broadcast(128), [], [bc[bi].b])
            k.dma("sp", Gc[bi].t[:], self.Gd[c], [], [Gc[bi].b])
            k.dma("sp", sz[bi].t[:], self.szd[c * 128:(c + 1) * 128, :], [], [sz[bi].b])

        k.op("pool", lambda e: e.memset(Hs.t[:], 0.0), writes=[Hs.b])
        k.op("pool", lambda e: e.memset(Hbf[0].t[:], 0.0), writes=[Hbf[0].b])
        load2(0, 0)
        gi = 0
        for c in range(NT):
            bi = c % 2
            if c + 1 < NT:
                load2(c + 1, (c + 1) % 2)
            x_, t_, b_, bc_, G_, sz_, eA_, ya = xs[bi], tok[bi], bct[bi], bc[bi], Gc[bi], sz[bi], eA[bi], yall[bi]
            hf_ = Hbf[bi]
            ntk_ = ntk[bi]
            k.op("dve", lambda e, t_=t_, ntk_=ntk_: e.tensor_scalar(out=ntk_.t[:], in0=t_.t[:, 1:3, :], scalar1=-1.0, scalar2=None, op0=ALU.mult), reads=[t_.b], writes=[ntk_.b])
            k.op("act", lambda e, t_=t_, eA_=eA_: e.activation(out=eA_.t[:, 0:24], in_=t_.t[:, 1, 0:24], func=AF.Exp), reads=[t_.b], writes=[eA_.b])
            k.op("act", lambda e, t_=t_, eA_=eA_: e.activation(out=eA_.t[:, 24:48], in_=t_.t[:, 2, 24:48], func=AF.Exp), reads=[t_.b], writes=[eA_.b])
            for d in range(2):
                k.op("pool", lambda e, d=d, x_=x_, t_=t_: e.tensor_tensor(out=xdt[d][bi].t[:].rearrange("p (h q) -> p h q", h=H), in0=xview(x_),
                                                                         in1=t_.t[:, 0, 24 * d:24 * d + 24].unsqueeze(2).to_broadcast([128, H, Pd]), op=ALU.mult),
                     reads=[x_.b, t_.b], writes=[xdt[d][bi].b])
            for g in range(SSD_G):
                k.op("pe", lambda e, g=g, b_=b_: e.matmul(psCB.t[:, g, :], lhsT=b_.t[:, g, :], rhs=b_.t[:, 4 + g, :], start=True, stop=True), reads=[b_.b], writes=[psCB.b])
            k.op("dve", lambda e: e.tensor_tensor(out=mcb[0][bi].t[:], in0=psCB.t[:], in1=self.maskf.t[:].unsqueeze(1).to_broadcast([128, 4, 128]), op=ALU.mult),
                 reads=[psCB.b, self.maskf.b], writes=[mcb[0][bi].b])
            k.op("dve", lambda e: e.tensor_tensor(out=mcb[1][bi].t[:], in0=psCB.t[:], in1=self.maskb.t[:].unsqueeze(1).to_broadcast([128, 4, 128]), op=ALU.mult),
                 reads=[psCB.b, self.maskb.b], writes=[mcb[1][bi].b])
            for g in range(SSD_G):
                pY = psY[gi % 2]
                sgi = gi % 2
                gi += 1
                for d in range(2):
                    sg, mt_ = seg[d][sgi], MT[d][sgi]
                    for jh in range(6):
                        h = g * 6 + jh
                        k.op("act", lambda e, d=d, jh=jh, h=h, sg=sg: e.activation(out=sg.t[:, jh, :], in_=bc_.t[:, d, h, :], func=AF.Exp, bias=ntk_.t[:, d, 24 * d + h:24 * d + h + 1]),
                             reads=[bc_.b, ntk_.b], writes=[sg.b])
                    k.op("dve", lambda e, d=d, g=g, sg=sg, mt_=mt_: e.scalar_tensor_tensor(out=mt_.t[:], in0=sg.t[:], scalar=1.0, in1=mcb[d][bi].t[:, g, :].unsqueeze(1).to_broadcast([128, 6, 128]),
                                                                                        op0=ALU.min, op1=ALU.mult), reads=[sg.b, mcb[d][bi].b], writes=[mt_.b])
                for jh in range(6):
                    h = g * 6 + jh
                    for d in range(2):
                        k.op("pe", lambda e, d=d, jh=jh, h=h, sgi=sgi, pY=pY: e.matmul(pY[0].t[:, jh * 64:(jh + 1) * 64], lhsT=MT[d][sgi].t[:, jh, :], rhs=xdt[d][bi].t[:, h * 64:(h + 1) * 64],
                                                                                      start=(d == 0), stop=(d == 1)), reads=[MT[d][sgi].b, xdt[d][bi].b], writes=[pY[0].b])
                k.op("pe", lambda e, g=g, pY=pY, hf_=hf_: e.matmul(pY[1].t[:, 0:384], lhsT=b_.t[:, 4 + g, :], rhs=hf_.t[:, g * 384:(g + 1) * 384], start=True, stop=True),
                     reads=[b_.b, hf_.b], writes=[pY[1].b])
                k.op("pe", lambda e, g=g, pY=pY: e.matmul(pY[2].t[:, 0:384], lhsT=b_.t[:, 4 + g, :], rhs=G_.t[:, g * 384:(g + 1) * 384], start=True, stop=True),
                     reads=[b_.b, G_.b], writes=[pY[2].b])
                a1, a2 = t1[sgi], t2[sgi]
                v3 = lambda ap: ap.rearrange("p (h q) -> p h q", h=6)
                k.op("dve", lambda e, g=g, pY=pY, a1=a1: e.tensor_tensor(out=v3(a1.t[:]), in0=v3(pY[1].t[:, 0:384]), in1=eA_.t[:, g * 6:g * 6 + 6].unsqueeze(2).to_broadcast([128, 6, Pd]), op=ALU.mult),
                     reads=[pY[1].b, eA_.b], writes=[a1.b])
                k.op("dve", lambda e, g=g, pY=pY, a2=a2: e.tensor_tensor(out=v3(a2.t[:]), in0=v3(pY[2].t[:, 0:384]), in1=eA_.t[:, 24 + g * 6:24 + g * 6 + 6].unsqueeze(2).to_broadcast([128, 6, Pd]), op=ALU.mult),
                     reads=[pY[2].b, eA_.b], writes=[a2.b])
                k.op("pool", lambda e, a1=a1, a2=a2: e.tensor_tensor(out=a1.t[:], in0=a1.t[:], in1=a2.t[:], op=ALU.add), reads=[a1.b, a2.b], writes=[a1.b])
                k.op("dve", lambda e, pY=pY, a1=a1: e.tensor_tensor(out=a1.t[:], in0=pY[0].t[:, 0:384], in1=a1.t[:], op=ALU.add), reads=[pY[0].b, a1.b], writes=[a1.b])
                k.op("pool", lambda e, g=g, a2=a2: e.tensor_tensor(out=a2.t[:], in0=x_.t[:, g * 384:(g + 1) * 384], in1=Dful.t[:, g * 384:(g + 1) * 384], op=ALU.mult),
                     reads=[x_.b, Dful.b], writes=[a2.b])
                k.op("pool", lambda e, g=g, a1=a1, a2=a2: e.tensor_tensor(out=ya.t[:, g * 384:(g + 1) * 384], in0=a1.t[:], in1=a2.t[:], op=ALU.add), reads=[a1.b, a2.b], writes=[ya.b])
            if c + 1 < NT:
                states(c, bi, 0, Hs, psS)
                hn_ = Hbf[(c + 1) % 2]
                k.op("act", lambda e, hn_=hn_: e.activation(out=hn_.t[:], in_=Hs.t[:], func=AF.Copy), reads=[Hs.b], writes=[hn_.b])
            s_, yn_ = ss[bi], yn[bi]
            k.op("dve", lambda e: e.tensor_tensor(out=ya.t[:], in0=ya.t[:], in1=sz_.t[:], op=ALU.mult), reads=[ya.b, sz_.b], writes=[ya.b])
            for g in range(SSD_G):
                k.op("act", lambda e, g=g: e.activation(out=junk.t[:], in_=ya.t[:, g * 384:(g + 1) * 384], func=AF.Square, accum_out=s_.t[:, g:g + 1]), reads=[ya.b], writes=[junk.b, s_.b])
            k.op("act", lambda e: e.activation(out=s_.t[:], in_=s_.t[:], func=AF.Sqrt, scale=1.0 / 384, bias=self.epsc.t[:]), reads=[s_.b, self.epsc.b], writes=[s_.b])
            k.op("dve", lambda e: e.reciprocal(out=s_.t[:], in_=s_.t[:]), reads=[s_.b], writes=[s_.b])
            k.op("dve", lambda e: e.tensor_tensor(out=ya.t[:].rearrange("p (g q) -> p g q", g=4), in0=ya.t[:].rearrange("p (g q) -> p g q", g=4),
                                                  in1=s_.t[:].unsqueeze(2).to_broadcast([128, 4, 384]), op=ALU.mult), reads=[ya.b, s_.b], writes=[ya.b])
            k.op("pool", lambda e: e.tensor_tensor(out=yn_.t[:], in0=ya.t[:], in1=gn.t[:], op=ALU.mult), reads=[ya.b, gn.b], writes=[yn_.b])
            k.dma("sp", self.ycat[c * 128:(c + 1) * 128, 0:D_SSD], yn_.t[:], [yn_.b], [yb_])

    def na_layer(self, cs, li):
        j = li // 2
        Win = self.I["na_w_in"][j]
        with Scope(self) as AS:
            hT = AS.sb("hT", [128, ND, T], BF16)
            with Scope(self) as s:
                self.norm_to_hT(s, hT, self.I["norm_mix_g"][li:li + 1, :])
            for hg in range(6):
                with Scope(self) as s:
                    self.na_group(s, j, hg, Win, hT)
            with Scope(self) as s:
                self.xa_block(s, li, hT, Win, 4608)
        self.out_proj(self.I["na_w_out"][j])

    def na_group(self, s, j, hg, Win, hT):
        k = self
        HG = 2
        W = HG * 128
        QT = s.sb("na_QT", [128, HG, T], BF16)
        KT = s.sb("na_KT", [128, HG, T], BF16)
        Va = s.sb("na_Va", [128, NT, HG, 129], BF16)
        oall = s.sb("na_oall", [128, NT, W], BF16)
        pbs = [s.ps("na_pb", [128, 512]) for _ in range(4)]
        k.op("pool", lambda e: e.memset(Va.t[:], 1.0), writes=[Va.b])
        for which, dst in ((0, QT), (1, KT)):
            w = self.wblock(s, Win, which * 1536 + hg * W, W)
            for et in range(HG):
                self.proj_fm(s, hT, w, W, pbs, et)
                for tc in range(4):
                    if tc % 2 == 0:
                        k.op("act", lambda e, et=et, tc=tc, dst=dst: e.activation(out=dst.t[:, et, tc * 512:(tc + 1) * 512], in_=pbs[tc].t[:], func=AF.Copy), reads=[pbs[tc].b], writes=[dst.b])
                    else:
                        k.op("dve", lambda e, et=et, tc=tc, dst=dst: e.tensor_copy(out=dst.t[:, et, tc * 512:(tc + 1) * 512], in_=pbs[tc].t[:]), reads=[pbs[tc].b], writes=[dst.b])
        w = self.wblock(s, Win, 3072 + hg * W, W)
        for tt in range(NT):
            pb = pbs[tt % 4]
            self.proj_tm(hT, w, W, pb, tt)
            k.op("act", lambda e, tt=tt, pb=pb: e.activation(out=Va.t[:, tt, :, 0:128], in_=pb.t[:, 0:W].rearrange("p (h d) -> p h d", h=HG), func=AF.Copy), reads=[pb.b], writes=[Va.b])
        self.P.flush()
        bias = [s.sb("na_bias", [128, 25, 128], F32) for _ in range(2)]
        psS = [s.ps("na_psS", [128, 5, 128]) for _ in range(1)]
        psO = [s.ps("na_psO", [128, 256]) for _ in range(2)]
        tmp = [s.sb("na_tmp", [128, 5, 128], F32) for _ in range(2)]
        Pm = [s.sb("na_P", [128, 5, 128], BF16) for _ in range(2)]
        rc = [s.sb("na_rc", [128, 1], F32) for _ in range(2)]
        scale = 128.0 ** -0.5
        it = 0
        for hh in range(HG):
            h = hg * HG + hh
            bt = bias[hh % 2]
            k.dma("sp", bt.t[:], self.I["na_bias"][j, h], [], [bt.b])
            for i in range(NT):
                kb = min(max(2 * i - 4, 0), 22)
                kt0 = kb // 2
                case = 0 if i == 0 else 1 if i == 1 else 3 if i == 14 else 4 if i == 15 else 2
                pS, pO, tm, P_, r_ = psS[0], psO[it % 2], tmp[it % 2], Pm[it % 2], rc[it % 2]
                it += 1
                for jt in range(5):
                    k.op("pe", lambda e, jt=jt, hh=hh, i=i, kt0=kt0, pS=pS: e.matmul(pS.t[:, jt, :], lhsT=KT.t[:, hh, (kt0 + jt) * 128:(kt0 + jt + 1) * 128], rhs=QT.t[:, hh, i * 128:(i + 1) * 128],
                                                                                  start=True, stop=True), reads=[KT.b, QT.b], writes=[pS.b])
                k.op("dve", lambda e, case=case, pS=pS, tm=tm, bt=bt: e.scalar_tensor_tensor(out=tm.t[:], in0=pS.t[:], scalar=scale, in1=bt.t[:, case * 5:(case + 1) * 5, :], op0=ALU.mult, op1=ALU.add),
                     reads=[pS.b, bt.b], writes=[tm.b])
                k.op("act", lambda e, tm=tm, P_=P_: e.activation(out=P_.t[:], in_=tm.t[:], func=AF.Exp), reads=[tm.b], writes=[P_.b])
                for jt in range(5):
                    k.op("pe", lambda e, jt=jt, hh=hh, kt0=kt0, pO=pO, P_=P_: e.matmul(pO.t[:, 0:129], lhsT=P_.t[:, jt, :], rhs=Va.t[:, kt0 + jt, hh, :], start=(jt == 0), stop=(jt == 4)),
                         reads=[P_.b, Va.b], writes=[pO.b])
                k.op("dve", lambda e, pO=pO, r_=r_: e.reciprocal(out=r_.t[:], in_=pO.t[:, 128:129]), reads=[pO.b], writes=[r_.b])
                k.op("act", lambda e, i=i, hh=hh, pO=pO, r_=r_: e.activation(out=oall.t[:, i, hh * 128:(hh + 1) * 128], in_=pO.t[:, 0:128], func=AF.Copy, scale=r_.t[:, 0:1]),
                     reads=[pO.b, r_.b], writes=[oall.b])
        yb = self.P.buf("ycat_na")
        k.dma("sp", self.ycat.rearrange("(n p) c -> p n c", p=128)[:, :, hg * W:(hg + 1) * W], oall.t[:], [oall.b], [yb])

    def moe_layer(self, cs, li):
        k = self
        with Scope(self) as MS:
            AFF = MS.sb("m_AFF", [128, NT, NE], F32)
            IDX = MS.sb("m_IDX", [128, 2 * NE], I32)
            GATE = MS.sb("m_GATE", [128, 2 * NE], F32)
            with Scope(self) as s:
                self.moe_router(s, li, AFF)
            with Scope(self) as s:
                self.moe_topk(s, AFF, IDX, GATE)
            with Scope(self) as s:
                self.moe_experts(s, li, IDX, GATE)

    def moe_router(self, s, li, AFF):
        k = self
        g = s.sb("g", [128, D], F32)
        k.bcast_load("sp", g, self.I["norm_ffn_g"][li:li + 1, :])
        Wr = s.sb("m_Wr", [128, ND, NE], F32)
        k.dma("sp", Wr.t[:], self.I["moe_w_router"][li].rearrange("(k p) e -> p k e", p=128), [], [Wr.b])
        hf = [s.sb("m_hf", [128, D], F32) for _ in range(2)]
        hTf = [s.sb("m_hTf", [128, ND, 128], F32) for _ in range(2)]
        ptf = [s.ps("m_ptf", [128, 4, 128], F32) for _ in range(4)]
        pl = [s.ps("m_pl", [128, NE], F32) for _ in range(2)]
        sm = [s.sb("m_sm", [128, 4], F32) for _ in range(2)]
        ex = [s.sb("m_ex", [128, NE], F32) for _ in range(2)]
        hb = self.P.buf("hn")
        for tt in range(NT):
            h_, hT_, pl_, sm_, ex_ = hf[tt % 2], hTf[tt % 2], pl[tt % 2], sm[tt % 2], ex[tt % 2]
            self.norm_tile(s, self.xres[tt * 128:(tt + 1) * 128, :], g, want_f32=h_, hn_dst=(self.hn[tt * 128:(tt + 1) * 128, :], hb))
            for q4 in range(4):
                p_ = ptf[q4]
                for jj in range(4):
                    kk = q4 * 4 + jj
                    k.op("pe", lambda e, kk=kk, jj=jj, p_=p_, h_=h_: e.transpose(out=p_.t[:, jj, :], in_=h_.t[:, kk * 128:(kk + 1) * 128], identity=self.identf.t[:]),
                         reads=[h_.b, self.identf.b], writes=[p_.b])
                if q4 % 2 == 0:
                    k.op("dve", lambda e, q4=q4, p_=p_, hT_=hT_: e.tensor_copy(out=hT_.t[:, q4 * 4:(q4 + 1) * 4, :], in_=p_.t[:]), reads=[p_.b], writes=[hT_.b])
                else:
                    k.op("act", lambda e, q4=q4, p_=p_, hT_=hT_: e.activation(out=hT_.t[:, q4 * 4:(q4 + 1) * 4, :], in_=p_.t[:], func=AF.Copy), reads=[p_.b], writes=[hT_.b])
            for kk in range(ND):
                k.op("pe", lambda e, kk=kk, hT_=hT_, pl_=pl_: e.matmul(pl_.t[:], lhsT=hT_.t[:, kk, :], rhs=Wr.t[:, kk, :], start=(kk == 0), stop=(kk == ND - 1)),
                     reads=[hT_.b, Wr.b], writes=[pl_.b])
            k.op("dve", lambda e, pl_=pl_, sm_=sm_: e.reduce_max(out=sm_.t[:, 0:1], in_=pl_.t[:], axis=AX.X), reads=[pl_.b], writes=[sm_.b])
            k.op("dve", lambda e, sm_=sm_: e.tensor_scalar(out=sm_.t[:, 1:2], in0=sm_.t[:, 0:1], scalar1=-1.0, scalar2=None, op0=ALU.mult), reads=[sm_.b], writes=[sm_.b])
            k.op("act", lambda e, pl_=pl_, sm_=sm_, ex_=ex_: e.activation(out=ex_.t[:], in_=pl_.t[:], func=AF.Exp, bias=sm_.t[:, 1:2], accum_out=sm_.t[:, 2:3]),
                 reads=[pl_.b, sm_.b], writes=[ex_.b, sm_.b])
            k.op("dve", lambda e, sm_=sm_: e.reciprocal(out=sm_.t[:, 3:4], in_=sm_.t[:, 2:3]), reads=[sm_.b], writes=[sm_.b])
            k.op("dve", lambda e, tt=tt, ex_=ex_, sm_=sm_: e.tensor_scalar(out=AFF.t[:, tt, :], in0=ex_.t[:], scalar1=sm_.t[:, 3:4], scalar2=None, op0=ALU.mult),
                 reads=[ex_.b, sm_.b], writes=[AFF.b])

    def moe_topk(self, s, AFF, IDX, GATE):
        k = self
        affT = s.sb("k_affT", [NE, T], F32)
        work = s.sb("k_work", [NE, T], F32)
        mx8 = s.sb("k_mx8", [NE, 8], F32)
        maskT = s.sb("k_mask", [NE, T], F32)
        pos = s.sb("k_pos", [NE, T], F32)
        POSM = s.sb("k_POSM", [128, NT, NE], F32)
        pA = [s.ps("k_pA", [128, 512]) for _ in range(4)]
        pP = s.ps("k_pP", [128, NT, NE], F32)
        for tt in range(NT):
            k.op("pe", lambda e, tt=tt: e.transpose(out=pA[tt // 4].t[0:NE, (tt % 4) * 128:(tt % 4 + 1) * 128], in_=AFF.t[:, tt, :], identity=self.identf.t[:]),
                 reads=[AFF.b, self.identf.b], writes=[pA[tt // 4].b])
        for q4 in range(4):
            k.op("dve", lambda e, q4=q4: e.tensor_copy(out=affT.t[:, q4 * 512:(q4 + 1) * 512], in_=pA[q4].t[0:NE, :]), reads=[pA[q4].b], writes=[affT.b])
        src = affT
        for it in range(CAP // 8):
            k.op("dve", lambda e, src=src: e.max(out=mx8.t[:], in_=src.t[:]), reads=[src.b], writes=[mx8.b])
            if it < CAP // 8 - 1:
                k.op("dve", lambda e, src=src: e.match_replace(out=work.t[:], in_to_replace=mx8.t[:], in_values=src.t[:], imm_value=-1.0), reads=[src.b, mx8.b], writes=[work.b])
                src = work
        k.op("dve", lambda e: e.tensor_scalar(out=maskT.t[:], in0=affT.t[:], scalar1=mx8.t[:, 7:8], scalar2=None, op0=ALU.is_ge), reads=[affT.b, mx8.b], writes=[maskT.b])
        k.op("dve", lambda e: e.tensor_tensor_scan(out=pos.t[:], data0=maskT.t[:], data1=maskT.t[:], initial=0.0, op0=ALU.add, op1=ALU.bypass), reads=[maskT.b], writes=[pos.b])
        k.op("dve", lambda e: e.tensor_tensor(out=pos.t[:], in0=pos.t[:], in1=maskT.t[:], op=ALU.mult), reads=[pos.b, maskT.b], writes=[pos.b])
        k.op("dve", lambda e: e.tensor_scalar(out=pos.t[:], in0=pos.t[:], scalar1=-1.0, scalar2=None, op0=ALU.add), reads=[pos.b], writes=[pos.b])
        for tt in range(NT):
            k.op("pe", lambda e, tt=tt: e.transpose(out=pP.t[:, tt, :], in_=pos.t[:, tt * 128:(tt + 1) * 128], identity=self.identf.t[0:NE, 0:NE]),
                 reads=[pos.b, self.identf.b], writes=[pP.b])
        k.op("dve", lambda e: e.tensor_copy(out=POSM.t[:], in_=pP.t[:]), reads=[pP.b], writes=[POSM.b])
        RV = s.sb("k_RV", [128, NT, NE, 5], BF16)
        r1 = s.sb("k_r1", [128, NT, NE], F32)
        gh = s.sb("k_gh", [128, NT, NE], BF16)
        pcol = s.sb("k_pcol", [128, 1], F32)
        k.op("pool", lambda e: e.iota(pcol.t[:], pattern=[[0, 1]], base=0, channel_multiplier=1, allow_small_or_imprecise_dtypes=True), writes=[pcol.b])
        for tt in range(NT):
            k.op("pool", lambda e, tt=tt: e.memset(RV.t[:, tt, :, 0], float(tt)), writes=[RV.b])
        k.op("dve", lambda e: e.tensor_copy(out=RV.t[:, :, :, 1].rearrange("p a b -> p (a b)"), in_=pcol.t[:].to_broadcast([128, NT * NE])), reads=[pcol.b], writes=[RV.b])
        k.op("dve", lambda e: e.tensor_copy(out=gh.t[:], in_=AFF.t[:]), reads=[AFF.b], writes=[gh.b])
        k.op("dve", lambda e: e.tensor_copy(out=RV.t[:, :, :, 2], in_=gh.t[:]), reads=[gh.b], writes=[RV.b])
        k.op("dve", lambda e: e.tensor_tensor(out=r1.t[:], in0=AFF.t[:], in1=gh.t[:], op=ALU.subtract), reads=[AFF.b, gh.b], writes=[r1.b])
        k.op("dve", lambda e: e.tensor_copy(out=gh.t[:], in_=r1.t[:]), reads=[r1.b], writes=[gh.b])
        k.op("dve", lambda e: e.tensor_copy(out=RV.t[:, :, :, 3], in_=gh.t[:]), reads=[gh.b], writes=[RV.b])
        k.op("dve", lambda e: e.tensor_tensor(out=r1.t[:], in0=r1.t[:], in1=gh.t[:], op=ALU.subtract), reads=[r1.b, gh.b], writes=[r1.b])
        k.op("dve", lambda e: e.tensor_copy(out=RV.t[:, :, :, 4], in_=r1.t[:]), reads=[r1.b], writes=[RV.b])
        O = [s.sb("k_O", [128, NT, CAP], BF16) for _ in range(2)]
        pz = [s.ps("k_pz", [128, 8], F32) for _ in range(2)]
        idf = s.sb("k_idf", [128, 2 * NE], F32)
        pzs = [s.sb("k_pzs", [128, 8], F32) for _ in range(2)]
        zi = 0
        for ex in range(NE):
            O_ = O[ex % 2]
            for tt in range(NT):
                k.op("dve", lambda e, tt=tt, ex=ex, O_=O_: e.tensor_scalar(out=O_.t[:, tt, :], in0=self.iota256.t[:], scalar1=POSM.t[:, tt, ex:ex + 1], scalar2=None, op0=ALU.is_equal),
                     reads=[self.iota256.b, POSM.b], writes=[O_.b])
            for half in range(2):
                pz_ = pz[zi % 2]
                zi += 1
                col = ex * 2 + half
                for tt in range(NT):
                    k.op("pe", lambda e, tt=tt, ex=ex, half=half, O_=O_, pz_=pz_: e.matmul(pz_.t[:, 0:5], lhsT=O_.t[:, tt, half * 128:(half + 1) * 128], rhs=RV.t[:, tt, ex, :], start=(tt == 0), stop=(tt == NT - 1)),
                         reads=[O_.b, RV.b], writes=[pz_.b])
                zs_ = pzs[zi % 2]
                k.op("dve", lambda e, pz_=pz_, zs_=zs_: e.tensor_copy(out=zs_.t[:, 0:5], in_=pz_.t[:, 0:5]), reads=[pz_.b], writes=[zs_.b])
                k.op("dve", lambda e, col=col, zs_=zs_: e.scalar_tensor_tensor(out=idf.t[:, col:col + 1], in0=zs_.t[:, 0:1], scalar=128.0, in1=zs_.t[:, 1:2], op0=ALU.mult, op1=ALU.add),
                     reads=[zs_.b], writes=[idf.b])
                k.op("dve", lambda e, col=col, zs_=zs_: e.tensor_reduce(out=GATE.t[:, col:col + 1], in_=zs_.t[:, 2:5], axis=AX.X, op=ALU.add), reads=[zs_.b], writes=[GATE.b])
        k.op("dve", lambda e: e.tensor_copy(out=IDX.t[:], in_=idf.t[:]), reads=[idf.b], writes=[IDX.b])

    def moe_experts(self, s, li, IDX, GATE):
        k = self
        ring = [s.sb("e_ring", [128, 8192], BF16) for _ in range(6)]
        xg = [s.sb("e_xg", [128, D], BF16) for _ in range(4)]
        xsT = [s.sb("e_xsT", [128, ND, CAP], BF16) for _ in range(2)]
        gT = [s.sb("e_gT", [128, 8, CAP], BF16) for _ in range(2)]
        sa = [s.sb("e_sa", [128, 512], F32) for _ in range(2)]
        gtok = [s.sb("e_gtok", [128, 512], BF16) for _ in range(2)]
        ysb = [s.sb("e_y", [128, D], F32) for _ in range(4)]
        ptr = [s.ps("e_ptr", [128, 8, 128], BF16) for _ in range(2)]
        pab = [s.ps("e_pab", [128, 512]) for _ in range(4)]
        py = [s.ps("e_py", [128, 512]) for _ in range(2)]
        xb = [self.P.buf("xres_moe") for _ in range(4)]
        hnb = self.P.buf("hn_r")
        cnt = dict(ti=0, ai=0, yi=0)
        for cq in range(4):
            k.dma("sp", self.moeh[cq][:, :], self.xres[:, cq * 512:(cq + 1) * 512], [], [xb[cq]])

        def gathers(ex):
            for half in range(2):
                col = ex * 2 + half
                x_ = xg[col % 4]
                k.op("pool", lambda e: e.indirect_dma_start(out=x_.t[:, :], out_offset=None, in_=self.hn[:, :],
                                                            in_offset=bass.IndirectOffsetOnAxis(ap=IDX.t[:, col:col + 1], axis=0)),
                     reads=[hnb, IDX.b], writes=[x_.b], dma=True)

        def wload(ex, ci):
            r_ = ring[ci]
            if ci < 4:
                wname, fh = ("moe_w1", "moe_w3")[ci % 2], ci // 2
                k.dma("pool", r_.t[:].rearrange("p (k n) -> p k n", k=ND), self.I[wname][li, ex][:, fh * 512:(fh + 1) * 512].rearrange("(k p) n -> p k n", p=128), [], [r_.b])
            else:
                dh = ci - 4
                k.dma("pool", r_.t[:].rearrange("p (k n) -> p k n", k=8), self.I["moe_w2"][li, ex][:, dh * 1024:(dh + 1) * 1024].rearrange("(k p) n -> p k n", p=128), [], [r_.b])

        gathers(0)
        for ci in range(6):
            wload(0, ci)
        for ex in range(NE):
            xT, g_ = xsT[ex % 2], gT[ex % 2]
            nxt = ex + 1 < NE
            if nxt:
                gathers(ex + 1)
            for half in range(2):
                x_ = xg[(ex * 2 + half) % 4]
                for kg in range(2):
                    p_ = ptr[cnt["ti"] % 2]
                    cnt["ti"] += 1
                    for jj in range(8):
                        kk = kg * 8 + jj
                        k.op("pe", lambda e: e.transpose(out=p_.t[:, jj, :], in_=x_.t[:, kk * 128:(kk + 1) * 128], identity=self.ident.t[:]),
                             reads=[x_.b, self.ident.b], writes=[p_.b])
                    if kg == 0:
                        k.op("dve", lambda e: e.tensor_copy(out=xT.t[:, kg * 8:(kg + 1) * 8, half * 128:(half + 1) * 128], in_=p_.t[:]), reads=[p_.b], writes=[xT.b])
                    else:
                        k.op("act", lambda e: e.activation(out=xT.t[:, kg * 8:(kg + 1) * 8, half * 128:(half + 1) * 128], in_=p_.t[:], func=AF.Copy), reads=[p_.b], writes=[xT.b])
            for fh in range(2):
                w1, w3 = ring[2 * fh], ring[2 * fh + 1]
                rv1 = w1.t[:].rearrange("p (k n) -> p k n", k=ND)
                rv3 = w3.t[:].rearrange("p (k n) -> p k n", k=ND)
                for half in range(2):
                    pa, pb = pab[cnt["ai"] % 4], pab[(cnt["ai"] + 1) % 4]
                    cnt["ai"] += 2
                    for kk in range(ND):
                        k.op("pe", lambda e: e.matmul(pa.t[:], lhsT=xT.t[:, kk, half * 128:(half + 1) * 128], rhs=rv1[:, kk, :], start=(kk == 0), stop=(kk == ND - 1)),
                             reads=[w1.b, xT.b], writes=[pa.b])
                        k.op("pe", lambda e: e.matmul(pb.t[:], lhsT=xT.t[:, kk, half * 128:(half + 1) * 128], rhs=rv3[:, kk, :], start=(kk == 0), stop=(kk == ND - 1)),
                             reads=[w3.b, xT.b], writes=[pb.b])
                    s_, gk = sa[cnt["ai"] // 2 % 2], gtok[cnt["ai"] // 2 % 2]
                    k.op("act", lambda e: e.activation(out=s_.t[:], in_=pa.t[:], func=AF.Silu), reads=[pa.b], writes=[s_.b])
                    k.op("dve", lambda e: e.tensor_tensor(out=gk.t[:], in0=s_.t[:], in1=pb.t[:], op=ALU.mult), reads=[s_.b, pb.b], writes=[gk.b])
                    p_ = ptr[cnt["ti"] % 2]
                    cnt["ti"] += 1
                    for jj in range(4):
                        k.op("pe", lambda e: e.transpose(out=p_.t[:, jj, :], in_=gk.t[:, jj * 128:(jj + 1) * 128], identity=self.ident.t[:]),
                             reads=[gk.b, self.ident.b], writes=[p_.b])
                    k.op("act", lambda e: e.activation(out=g_.t[:, fh * 4:(fh + 1) * 4, half * 128:(half + 1) * 128], in_=p_.t[:, 0:4, :], func=AF.Copy),
                         reads=[p_.b], writes=[g_.b])
                if nxt:
                    wload(ex + 1, 2 * fh)
                    wload(ex + 1, 2 * fh + 1)
            ys = [ysb[(ex * 2) % 4], ysb[(ex * 2 + 1) % 4]]
            for dh in range(2):
                r_ = ring[4 + dh]
                rv = r_.t[:].rearrange("p (k n) -> p k n", k=8)
                for half in range(2):
                    col = ex * 2 + half
                    for dc in range(2):
                        p_ = py[cnt["yi"] % 2]
                        cnt["yi"] += 1
                        for ft in range(8):
                            k.op("pe", lambda e: e.matmul(p_.t[:], lhsT=g_.t[:, ft, half * 128:(half + 1) * 128], rhs=rv[:, ft, dc * 512:(dc + 1) * 512], start=(ft == 0), stop=(ft == 7)),
                                 reads=[g_.b, r_.b], writes=[p_.b])
                        o0 = dh * 1024 + dc * 512
                        y_ = ys[half]
                        if dc == 0:
                            k.op("act", lambda e: e.activation(out=y_.t[:, o0:o0 + 512], in_=p_.t[:], func=AF.Copy, scale=GATE.t[:, col:col + 1]),
                                 reads=[p_.b, GATE.b], writes=[y_.b])
                        else:
                            k.op("dve", lambda e: e.tensor_scalar(out=y_.t[:, o0:o0 + 512], in0=p_.t[:], scalar1=GATE.t[:, col:col + 1], scalar2=None, op0=ALU.mult),
                                 reads=[p_.b, GATE.b], writes=[y_.b])
                if nxt:
                    wload(ex + 1, 4 + dh)
            for half in range(2):
                col = ex * 2 + half
                y_ = ys[half]
                for cq in range(4):
                    k.op("pool", lambda e: e.indirect_dma_start(out=self.moeh[cq][:, :], out_offset=bass.IndirectOffsetOnAxis(ap=IDX.t[:, col:col + 1], axis=0),
                                                                in_=y_.t[:, cq * 512:(cq + 1) * 512], in_offset=None, compute_op=ALU.add),
                         reads=[y_.b, IDX.b, xb[cq]], writes=[xb[cq]], dma=True)
        for cq in range(4):
            k.dma("sp", self.xres[:, cq * 512:(cq + 1) * 512], self.moeh[cq][:, :], [xb[cq]], [xb[cq]])


def _na_bias_tiles(rpb):
    n, Hh = rpb.shape[0], rpb.shape[1]
    out = np.full((n, Hh, 128, 25, 128), NEG, dtype=np.float32)
    a = np.arange(2)[:, None]; wk = np.arange(64)[None, :]
    for case, i in enumerate((0, 1, 2, 14, 15)):
        kb = min(max(2 * i - 4, 0), 22)
        for jt in range(5):
            for aa in range(2):
                kr = kb + 2 * jt + aa
                for bb in range(2):
                    r = 2 * i + bb
                    rs = min(max(r - 4, 0), 24)
                    if not (rs <= kr < rs + 8):
                        continue
                    dr = kr - r + 7
                    wq = np.arange(64)
                    ws = np.clip(wq - 8, 0, 48)
                    wkk = np.arange(64)[:, None]
                    valid = (wkk >= ws[None, :]) & (wkk < ws[None, :] + 16)
                    dc = np.clip(wkk - wq[None, :] + 15, 0, 30)
                    vals = rpb[:, :, dr, :][:, :, dc]
                    blk = out[:, :, aa * 64:(aa + 1) * 64, case * 5 + jt, bb * 64:(bb + 1) * 64]
                    blk[...] = np.where(valid[None, None], vals, NEG)
    return out


def _prep(inputs, nlayers=DEPTH):
    n_ssd = (nlayers + 1) // 2
    n_na = nlayers // 2
    f = lambda a: np.ascontiguousarray(a, dtype=np.float32)
    common = {
        "norm_mix_g": f(inputs["norm_mix_g"][:nlayers]), "norm_ffn_g": f(inputs["norm_ffn_g"][:nlayers]),
        "norm_final_g": f(inputs["norm_final_g"]).reshape(1, D), "mem_norm_g": f(inputs["mem_norm_g"]).reshape(1, D),
        "ssd_w_in": f(inputs["ssd_w_in"][:n_ssd]), "ssd_conv_w": f(inputs["ssd_conv_w"][:n_ssd]), "ssd_conv_b": f(inputs["ssd_conv_b"][:n_ssd]),
        "ssd_dt_bias": f(inputs["ssd_dt_bias"][:n_ssd]).reshape(n_ssd, 48), "ssd_a_log": f(inputs["ssd_a_log"][:n_ssd]).reshape(n_ssd, 48),
        "ssd_d": f(inputs["ssd_d"][:n_ssd]), "ssd_gate_norm_g": f(inputs["ssd_gate_norm_g"][:n_ssd]), "ssd_w_out": f(inputs["ssd_w_out"][:n_ssd]),
        "xa_w_kv": f(inputs["xa_w_kv"][:nlayers]), "moe_w_router": f(inputs["moe_w_router"][:nlayers]),
        "moe_w1": f(inputs["moe_w1"][:nlayers]), "moe_w3": f(inputs["moe_w3"][:nlayers]), "moe_w2": f(inputs["moe_w2"][:nlayers]),
    }
    if n_na:
        common["na_w_in"] = f(inputs["na_w_in"][:n_na])
        common["na_bias"] = _na_bias_tiles(f(inputs["na_rpb"][:n_na]))
        common["na_w_out"] = f(inputs["na_w_out"][:n_na])
    return common


_NC_CACHE = {}


def kernel(**inputs):
    x = np.asarray(inputs["x"], dtype=np.float32)
    mem = np.asarray(inputs["mem"], dtype=np.float32)
    common = _prep(inputs)
    if "nc" not in _NC_CACHE:
        _NC_CACHE["nc"] = KB().build()
    nc = _NC_CACHE["nc"]
    B = x.shape[0]
    in_maps = []
    for c in range(8):
        b = c % B
        m = dict(common)
        m["x"] = np.ascontiguousarray(x[b])
        m["mem"] = np.ascontiguousarray(mem[b])
        in_maps.append(m)
    res = run_bass_kernel_spmd(nc, in_maps, core_ids=list(range(8)))
    return np.stack([np.asarray(res.results[b]["out"], dtype=np.float32) for b in range(B)], axis=0)
```

```python
import contextlib
import numpy as np
import concourse.bass as bass
import concourse.mybir as mybir
from concourse.bass_utils import run_bass_kernel_spmd

F32 = mybir.dt.float32
BF16 = mybir.dt.bfloat16
I32 = mybir.dt.int32
AF = mybir.ActivationFunctionType
ALU = mybir.AluOpType
AX = mybir.AxisListType

T = 2048
D = 2048
NT = 16
ND = 16
DEPTH = 4
MEM = 256
EPS = 1e-6
D_XA = 512
D_SSD = 1536
SSD_H = 24
SSD_P = 64
SSD_G = 4
SSD_N = 128
SSD_CONV_DIM = 2560
SSD_IN = 4656
NA_H = 12
NA_IN = 5120
NE = 16
CAP = 256
DFF = 1024
NEG = -30000.0

ENGS = ("pe", "act", "dve", "pool", "sp")
ENGOBJ = {"pe": "tensor", "act": "scalar", "dve": "vector", "pool": "gpsimd", "sp": "sync"}
SEM_BLOCK = 30000


class Buf:
    __slots__ = ("name", "last_w", "readers", "sem", "base", "ndma")

    def __init__(self, name):
        self.name = name
        self.last_w = None
        self.readers = []
        self.sem = None
        self.base = 0
        self.ndma = 0


class Op:
    __slots__ = ("eng", "fn", "is_dma", "deps", "need_sig", "sig", "dst", "k")


class _Rec:
    def __init__(self):
        self.call = None

    def __getattr__(self, name):
        def f(*a, **kw):
            self.call = (name, a, kw)
            return self
        return f


class Prog:
    def __init__(self, nc, stack):
        self.nc = nc
        self.stack = stack
        self.q = {e: [] for e in ENGS}
        self.bufs = []
        self.eng_cnt = {e: 0 for e in ENGS}
        self.eng_sems = {}
        self.dma_pool = []
        self.n_sems = 0
        self.n_ops = 0

    def buf(self, name="b"):
        b = Buf(name)
        self.bufs.append(b)
        return b

    def _newsem(self, name):
        self.n_sems += 1
        return self.stack.enter_context(self.nc.semaphore(f"{name}_{self.n_sems}"))

    def op(self, eng, fn, reads=(), writes=(), dma=False):
        o = Op()
        rec = _Rec()
        fn(rec)
        assert rec.call is not None
        o.eng, o.fn, o.is_dma = eng, rec.call, dma
        o.need_sig, o.sig, o.dst, o.k = False, None, None, 0
        deps, seen = [], set()
        for r in reads:
            if r.last_w is not None:
                deps.append(r.last_w)
        for w in writes:
            if w.last_w is not None:
                deps.append(w.last_w)
            deps.extend(w.readers)
        dd = []
        for d in deps:
            if id(d) in seen or d is o:
                continue
            seen.add(id(d))
            if d.eng == "pe" and eng == "pe" and not d.is_dma and not dma:
                continue
            d.need_sig = True
            dd.append(d)
        o.deps = dd
        if dma:
            o.dst = writes[0]
            o.dst.ndma += 1
            o.k = o.dst.ndma
        for r in reads:
            r.readers.append(o)
        for w in writes:
            w.last_w = o
            w.readers = []
        self.q[eng].append(o)
        self.n_ops += 1
        return o

    def flush(self):
        nc = self.nc
        if not any(self.q[e] for e in ENGS):
            return
        lasts = []
        for e in ENGS:
            comp = [o for o in self.q[e] if not o.is_dma]
            if comp:
                comp[-1].need_sig = True
                lasts.append(comp[-1])
        dma_bufs = []
        for e in ENGS:
            for o in self.q[e]:
                if o.is_dma:
                    b = o.dst
                    if b.sem is None:
                        if self.dma_pool:
                            b.sem, b.base = self.dma_pool.pop(0)
                        else:
                            b.sem, b.base = self._newsem("d"), 0
                        dma_bufs.append(b)
                    o.sig = (b.sem, b.base + 16 * o.k)
                elif o.need_sig:
                    c = self.eng_cnt[e]
                    blk = c // SEM_BLOCK
                    key = (e, blk)
                    if key not in self.eng_sems:
                        self.eng_sems[key] = self._newsem(e)
                    o.sig = (self.eng_sems[key], c % SEM_BLOCK + 1)
                    self.eng_cnt[e] = c + 1
        finals = [o.sig for o in lasts] + [(b.sem, b.base + 16 * b.ndma) for b in dma_bufs]

        def run_queue(e, eng):
            waited = {}
            for o in self.q[e]:
                for d in o.deps:
                    sem, val = d.sig
                    if waited.get(id(sem), 0) >= val:
                        continue
                    waited[id(sem)] = val
                    eng.wait_ge(sem, val)
                nm, a_, kw_ = o.fn
                try:
                    ins = getattr(eng, nm)(*a_, **kw_)
                except Exception:
                    print("EMIT FAIL", e, nm, [getattr(x, "shape", x) for x in a_], {k_: getattr(v_, "shape", v_) for k_, v_ in kw_.items()}, flush=True)
                    for k_, v_ in kw_.items():
                        print("   ARG", k_, repr(v_)[:300], repr(getattr(v_, "ap", None))[:300], flush=True)
                    raise
                if o.sig is not None:
                    ins.then_inc(o.sig[0], 16 if o.is_dma else 1)
            for sem, val in finals:
                if waited.get(id(sem), 0) >= val:
                    continue
                eng.wait_ge(sem, val)

        with nc.Block() as block:
            for e in ENGS:
                def mk(e):
                    return lambda eng: run_queue(e, eng)
                getattr(block, ENGOBJ[e])(mk(e))
        for b in dma_bufs:
            cnt = b.base + 16 * b.ndma
            assert cnt < 32000, "dma semaphore count too large"
            self.dma_pool.append((b.sem, cnt))
        for b in self.bufs:
            b.last_w, b.readers, b.sem, b.base, b.ndma = None, [], None, 0, 0
        self.q = {e: [] for e in ENGS}


class Tl:
    __slots__ = ("t", "b")

    def __init__(self, t, b):
        self.t, self.b = t, b


class Scope:
    def __init__(self, k):
        self.k = k
        self.st = contextlib.ExitStack()

    def __enter__(self):
        self.st.__enter__()
        return self

    def __exit__(self, *a):
        self.k.P.flush()
        return self.st.__exit__(*a)

    def sb(self, name, shape, dt):
        self.k.uid += 1
        t = self.st.enter_context(self.k.nc.sbuf_tensor(f"{name}_{self.k.uid}", list(shape), dt))
        return Tl(t, self.k.P.buf(name))

    def ps(self, name, shape, dt=F32):
        self.k.uid += 1
        t = self.st.enter_context(self.k.nc.psum_tensor(f"{name}_{self.k.uid}", list(shape), dt))
        return Tl(t, self.k.P.buf(name))


class KB:
    def __init__(self, nlayers=DEPTH, dbg=None):
        self.nlayers = nlayers
        self.dbg = dbg
        self.uid = 0
        self.nc = bass.Bass("TRN2", target_bir_lowering=False)
        self.outer = contextlib.ExitStack()
        self.P = Prog(self.nc, self.outer)

    def din(self, name, shape, dt=F32):
        return self.nc.dram_tensor(name, list(shape), dt, kind="ExternalInput").ap()

    def dscr(self, name, shape, dt):
        return self.nc.dram_tensor(name, list(shape), dt).ap()

    def op(self, *a, **k):
        return self.P.op(*a, **k)

    def dma(self, q, out, in_, reads, writes, **kw):
        return self.P.op(q, lambda e: e.dma_start(out=out, in_=in_, **kw), reads=reads, writes=writes, dma=True)

    def bcast_load(self, q, dst, row_ap, nparts=128):
        return self.dma(q, dst.t[:], row_ap.partition_broadcast(nparts), [], [dst.b])

    def build(self):
        nc = self.nc
        L = self.nlayers
        n_ssd = (L + 1) // 2
        n_na = L // 2
        I = {}
        self.moeh = [self.dscr(f"moeh{cq}", [T, 512], F32) for cq in range(4)]
        self.hn = self.dscr("hn", [T, D], BF16)
        I["x"] = self.din("x", [T, D])
        I["mem"] = self.din("mem", [MEM, D])
        I["norm_mix_g"] = self.din("norm_mix_g", [L, D])
        I["norm_ffn_g"] = self.din("norm_ffn_g", [L, D])
        I["norm_final_g"] = self.din("norm_final_g", [1, D])
        I["mem_norm_g"] = self.din("mem_norm_g", [1, D])
        I["ssd_w_in"] = self.din("ssd_w_in", [n_ssd, D, SSD_IN])
        I["ssd_conv_w"] = self.din("ssd_conv_w", [n_ssd, 5, SSD_CONV_DIM])
        I["ssd_conv_b"] = self.din("ssd_conv_b", [n_ssd, SSD_CONV_DIM])
        I["ssd_dt_bias"] = self.din("ssd_dt_bias", [n_ssd, 48])
        I["ssd_a_log"] = self.din("ssd_a_log", [n_ssd, 48])
        I["ssd_d"] = self.din("ssd_d", [n_ssd, SSD_H])
        I["ssd_gate_norm_g"] = self.din("ssd_gate_norm_g", [n_ssd, D_SSD])
        I["ssd_w_out"] = self.din("ssd_w_out", [n_ssd, D, D])
        if n_na:
            I["na_w_in"] = self.din("na_w_in", [n_na, D, NA_IN])
            I["na_bias"] = self.din("na_bias", [n_na, NA_H, 128, 25, 128])
            I["na_w_out"] = self.din("na_w_out", [n_na, D, D])
        I["xa_w_kv"] = self.din("xa_w_kv", [L, D, 2 * D_XA])
        I["moe_w_router"] = self.din("moe_w_router", [L, D, NE])
        I["moe_w1"] = self.din("moe_w1", [L, NE, D, DFF])
        I["moe_w3"] = self.din("moe_w3", [L, NE, D, DFF])
        I["moe_w2"] = self.din("moe_w2", [L, NE, DFF, D])
        self.I = I
        self.out = nc.dram_tensor("out", [T, D], F32, kind="ExternalOutput").ap()
        self.xres = self.dscr("xres", [T, D], F32)
        self.ycat = self.dscr("ycat", [T, D], BF16)
        self.szd = self.dscr("szd", [T, D_SSD], BF16)
        self.Gd = self.dscr("Gd", [NT, 128, D_SSD], BF16)
        self.ABd = self.dscr("ABd", [NT, 2 * SSD_H * 128], F32)
        self.TOTd = self.dscr("TOTd", [1, NT * 48], F32)
        self.xsBd = self.dscr("xsBd", [T, 2048], BF16)
        self.BCTd = self.dscr("BCTd", [8, 128, T], BF16)
        self.TOKd = self.dscr("TOKd", [T, 4 * 48], F32)

        with self.outer:
            with Scope(self) as cs:
                self.consts(cs)
                for i in range(L):
                    if i % 2 == 0:
                        self.ssd_layer(cs, i)
                    else:
                        self.na_layer(cs, i)
                    if self.dbg == ("mix", i):
                        self.dump_xres(cs)
                        return self.nc
                    self.moe_layer(cs, i)
                    if self.dbg == ("moe", i):
                        self.dump_xres(cs)
                        return self.nc
                self.final_norm(cs)
        return self.nc

    def consts(self, cs):
        k = self
        self.identf = cs.sb("identf", [128, 128], F32)
        self.ident = cs.sb("ident", [128, 128], BF16)
        self.ones_bf = cs.sb("ones_bf", [128, 128], BF16)
        self.maskf = cs.sb("maskf", [128, 128], F32)
        self.maskb = cs.sb("maskb", [128, 128], F32)
        self.iota256 = cs.sb("iota256", [128, 256], F32)
        self.memT = cs.sb("memT", [128, ND, MEM], BF16)
        self.epsc = cs.sb("epsc", [128, 1], F32)
        idf, idb = self.identf, self.ident
        k.op("pool", lambda e: e.memset(idf.t[:], 0.0), writes=[idf.b])
        k.op("pool", lambda e: e.affine_select(out=idf.t[:], in_=idf.t[:], pattern=[[-1, 128]], compare_op=ALU.not_equal,
                                               fill=1.0, base=0, channel_multiplier=1), reads=[idf.b], writes=[idf.b])
        k.op("dve", lambda e: e.tensor_copy(out=idb.t[:], in_=idf.t[:]), reads=[idf.b], writes=[idb.b])
        k.op("pool", lambda e: e.memset(self.ones_bf.t[:], 1.0), writes=[self.ones_bf.b])
        k.op("pool", lambda e: e.memset(self.epsc.t[:], EPS), writes=[self.epsc.b])
        k.op("pool", lambda e: e.memset(self.maskf.t[:], 1.0), writes=[self.maskf.b])
        k.op("pool", lambda e: e.affine_select(out=self.maskf.t[:], in_=self.maskf.t[:], pattern=[[1, 128]], compare_op=ALU.is_ge,
                                               fill=0.0, base=0, channel_multiplier=-1), reads=[self.maskf.b], writes=[self.maskf.b])
        k.op("pool", lambda e: e.memset(self.maskb.t[:], 1.0), writes=[self.maskb.b])
        k.op("pool", lambda e: e.affine_select(out=self.maskb.t[:], in_=self.maskb.t[:], pattern=[[-1, 128]], compare_op=ALU.is_ge,
                                               fill=0.0, base=0, channel_multiplier=1), reads=[self.maskb.b], writes=[self.maskb.b])
        k.op("pool", lambda e: e.iota(self.iota256.t[:], pattern=[[1, 256]], base=0, channel_multiplier=0,
                                      allow_small_or_imprecise_dtypes=True), writes=[self.iota256.b])
        xb = self.P.buf("xres_all")
        k.dma("sp", self.xres[:, :], self.I["x"][:, :], [], [xb])
        self.P.flush()
        with Scope(self) as s:
            g = s.sb("g", [128, D], F32)
            k.bcast_load("sp", g, self.I["mem_norm_g"][0:1, :])
            for mt in range(2):
                self.norm_tile(s, self.I["mem"][mt * 128:(mt + 1) * 128, :], g, dstT=self.memT, tcol=mt * 128, tag=f"m{mt}")

    def norm_tile(self, s, src_ap, g, dstT=None, tcol=0, tag="", hn_dst=None, want_f32=None, q="sp"):
        k = self
        if not hasattr(s, "_nt"):
            s._nt = {}
            for j in range(3):
                s._nt[j] = dict(
                    xt=s.sb("n_xt", [128, D], F32), ss=s.sb("n_ss", [128, 1], F32),
                    hb=s.sb("n_hb", [128, D], BF16), pt=s.ps("n_pt", [128, 8, 128], BF16) if j < 2 else None)
            s._ntc = 0
        R = s._nt[s._ntc % 3]
        pt = s._nt[s._ntc % 2]["pt"]
        s._ntc += 1
        xt, ss, hb = R["xt"], R["ss"], R["hb"]
        k.dma(q, xt.t[:], src_ap, [], [xt.b])
        k.op("act", lambda e: e.activation(out=hb.t[:], in_=xt.t[:], func=AF.Square, accum_out=ss.t[:]), reads=[xt.b], writes=[hb.b, ss.b])
        k.op("act", lambda e: e.activation(out=ss.t[:], in_=ss.t[:], func=AF.Sqrt, scale=1.0 / D, bias=self.epsc.t[:]), reads=[ss.b, self.epsc.b], writes=[ss.b])
        k.op("dve", lambda e: e.reciprocal(out=ss.t[:], in_=ss.t[:]), reads=[ss.b], writes=[ss.b])
        if want_f32 is not None:
            hf = want_f32
            k.op("dve", lambda e: e.scalar_tensor_tensor(out=hf.t[:], in0=xt.t[:], scalar=ss.t[:, 0:1], in1=g.t[:], op0=ALU.mult, op1=ALU.mult),
                 reads=[xt.b, ss.b, g.b], writes=[hf.b])
            k.op("act", lambda e: e.activation(out=hb.t[:], in_=hf.t[:], func=AF.Copy), reads=[hf.b], writes=[hb.b])
        else:
            k.op("dve", lambda e: e.scalar_tensor_tensor(out=hb.t[:], in0=xt.t[:], scalar=ss.t[:, 0:1], in1=g.t[:], op0=ALU.mult, op1=ALU.mult),
                 reads=[xt.b, ss.b, g.b], writes=[hb.b])
        if hn_dst is not None:
            k.dma("sp", hn_dst[0], hb.t[:], [hb.b], [hn_dst[1]])
        if dstT is not None:
            for half in range(2):
                for j in range(8):
                    kk = half * 8 + j
                    k.op("pe", lambda e, kk=kk, j=j: e.transpose(out=pt.t[:, j, :], in_=hb.t[:, kk * 128:(kk + 1) * 128], identity=self.ident.t[:]),
                         reads=[hb.b, self.ident.b], writes=[pt.b])
                eng = "dve" if half == 0 else "act"
                if eng == "dve":
                    k.op("dve", lambda e, half=half: e.tensor_copy(out=dstT.t[:, half * 8:(half + 1) * 8, tcol:tcol + 128], in_=pt.t[:]),
                         reads=[pt.b], writes=[dstT.b])
                else:
                    k.op("act", lambda e, half=half: e.activation(out=dstT.t[:, half * 8:(half + 1) * 8, tcol:tcol + 128], in_=pt.t[:], func=AF.Copy),
                         reads=[pt.b], writes=[dstT.b])

    def dump_xres(self, cs):
        self.P.flush()
        ob = self.P.buf("out")
        self.dma("sp", self.out[:, :], self.xres[:, :], [], [ob])
        self.P.flush()

    def final_norm(self, cs):
        k = self
        with Scope(self) as s:
            g = s.sb("g", [128, D], F32)
            k.bcast_load("sp", g, self.I["norm_final_g"][0:1, :])
            outs = [s.sb("fo", [128, D], F32) for _ in range(2)]
            ob = self.P.buf("out")
            for tt in range(NT):
                o = outs[tt % 2]
                self.norm_tile(s, self.xres[tt * 128:(tt + 1) * 128, :], g, want_f32=o, tag=f"f{tt}")
                k.dma("sp", self.out[tt * 128:(tt + 1) * 128, :], o.t[:], [o.b], [ob])

    def wblock(self, s, w_ap, c0, ncols, nk=ND):
        if not hasattr(s, "_wb"):
            s._wb = [s.sb("wblk", [128, ND, 512], BF16) for _ in range(2)]
            s._wbc = 0
        w = s._wb[s._wbc % 2]
        s._wbc += 1
        self.dma("pool", w.t[:, 0:nk, 0:ncols], w_ap[:, c0:c0 + ncols].rearrange("(k p) n -> p k n", p=128), [], [w.b])
        return w

    def proj_fm(self, s, hT, w, ncols, ps_bank_tiles, et):
        m0 = et * 128
        m = min(128, ncols - m0)
        for tc in range(4):
            pb = ps_bank_tiles[tc]
            for kk in range(ND):
                self.op("pe", lambda e, kk=kk, tc=tc, pb=pb: e.matmul(pb.t[0:m, :], lhsT=w.t[:, kk, m0:m0 + m], rhs=hT.t[:, kk, tc * 512:(tc + 1) * 512],
                                                                     start=(kk == 0), stop=(kk == ND - 1)),
                        reads=[w.b, hT.b], writes=[pb.b])

    def proj_tm(self, hT, w, ncols, pb, tt):
        for kk in range(ND):
            self.op("pe", lambda e, kk=kk: e.matmul(pb.t[:, 0:ncols], lhsT=hT.t[:, kk, tt * 128:(tt + 1) * 128], rhs=w.t[:, kk, 0:ncols],
                                                    start=(kk == 0), stop=(kk == ND - 1)),
                    reads=[w.b, hT.b], writes=[pb.b])

    def norm_to_hT(self, s, hT, gain_row):
        g = s.sb("g", [128, D], F32)
        self.bcast_load("sp", g, gain_row)
        for tt in range(NT):
            self.norm_tile(s, self.xres[tt * 128:(tt + 1) * 128, :], g, dstT=hT, tcol=tt * 128)

    def xa_block(self, s, li, hT, wq_ap, c0):
        k = self
        qT = s.sb("xa_qT", [128, 4, T], BF16)
        kT = s.sb("xa_kT", [128, 4, MEM], BF16)
        va = s.sb("xa_va", [128, 2, 4, 129], BF16)
        pbs = [s.ps("xa_pb", [128, 512]) for _ in range(4)]
        w = self.wblock(s, wq_ap, c0, 512)
        for et in range(4):
            self.proj_fm(s, hT, w, 512, pbs, et)
            for tc in range(4):
                eng = "act" if tc % 2 == 0 else "dve"
                if eng == "act":
                    k.op("act", lambda e, et=et, tc=tc: e.activation(out=qT.t[:, et, tc * 512:(tc + 1) * 512], in_=pbs[tc].t[:], func=AF.Copy),
                         reads=[pbs[tc].b], writes=[qT.b])
                else:
                    k.op("dve", lambda e, et=et, tc=tc: e.tensor_copy(out=qT.t[:, et, tc * 512:(tc + 1) * 512], in_=pbs[tc].t[:]),
                         reads=[pbs[tc].b], writes=[qT.b])
        wkv = self.I["xa_w_kv"][li]
        w = self.wblock(s, wkv, 0, 512)
        for et in range(4):
            pb = pbs[et]
            for kk in range(ND):
                k.op("pe", lambda e, kk=kk, et=et, pb=pb: e.matmul(pb.t[:, 0:MEM], lhsT=w.t[:, kk, et * 128:(et + 1) * 128], rhs=self.memT.t[:, kk, :],
                                                                  start=(kk == 0), stop=(kk == ND - 1)), reads=[w.b, self.memT.b], writes=[pb.b])
            k.op("act", lambda e, et=et, pb=pb: e.activation(out=kT.t[:, et, :], in_=pb.t[:, 0:MEM], func=AF.Copy), reads=[pb.b], writes=[kT.b])
        w = self.wblock(s, wkv, 512, 512)
        k.op("pool", lambda e: e.memset(va.t[:], 1.0), writes=[va.b])
        for mt in range(2):
            pb = pbs[mt]
            for kk in range(ND):
                k.op("pe", lambda e, kk=kk, mt=mt, pb=pb: e.matmul(pb.t[:, :], lhsT=self.memT.t[:, kk, mt * 128:(mt + 1) * 128], rhs=w.t[:, kk, :],
                                                                  start=(kk == 0), stop=(kk == ND - 1)), reads=[w.b, self.memT.b], writes=[pb.b])
            k.op("dve", lambda e, mt=mt, pb=pb: e.tensor_copy(out=va.t[:, mt, :, 0:128], in_=pb.t[:].rearrange("p (h d) -> p h d", h=4)),
                 reads=[pb.b], writes=[va.b])
        sps = [s.ps("xa_s", [128, 2, 128]) for _ in range(2)]
        ops_ = [s.ps("xa_o", [128, 4, 256]) for _ in range(1)]
        pT = [s.sb("xa_p", [128, 2, 128], BF16) for _ in range(2)]
        rc = [s.sb("xa_rc", [128, 4], F32) for _ in range(2)]
        ot = [s.sb("xa_ot", [128, 512], BF16) for _ in range(2)]
        yb = self.P.buf("ycat_xa")
        scale = 128.0 ** -0.5
        it = 0
        for tt in range(NT):
            o_ps, o_t, r_c = ops_[0], ot[tt % 2], rc[tt % 2]
            for h in range(4):
                sp_, p_ = sps[it % 2], pT[it % 2]
                it += 1
                for mt in range(2):
                    k.op("pe", lambda e, mt=mt, h=h, sp_=sp_: e.matmul(sp_.t[:, mt, :], lhsT=kT.t[:, h, mt * 128:(mt + 1) * 128], rhs=qT.t[:, h, tt * 128:(tt + 1) * 128],
                                                                      start=True, stop=True), reads=[kT.b, qT.b], writes=[sp_.b])
                k.op("act", lambda e, sp_=sp_, p_=p_: e.activation(out=p_.t[:], in_=sp_.t[:], func=AF.Exp, scale=scale), reads=[sp_.b], writes=[p_.b])
                for mt in range(2):
                    k.op("pe", lambda e, mt=mt, h=h, p_=p_, o_ps=o_ps: e.matmul(o_ps.t[:, h, 0:129], lhsT=p_.t[:, mt, :], rhs=va.t[:, mt, h, :],
                                                                               start=(mt == 0), stop=(mt == 1)), reads=[p_.b, va.b], writes=[o_ps.b])
            k.op("dve", lambda e, o_ps=o_ps, r_c=r_c: e.reciprocal(out=r_c.t[:], in_=o_ps.t[:, :, 128]), reads=[o_ps.b], writes=[r_c.b])
            k.op("dve", lambda e, o_ps=o_ps, r_c=r_c, o_t=o_t: e.tensor_tensor(out=o_t.t[:].rearrange("p (h d) -> p h d", h=4), in0=o_ps.t[:, :, 0:128],
                                                                               in1=r_c.t[:].unsqueeze(2).to_broadcast([128, 4, 128]), op=ALU.mult),
                 reads=[o_ps.b, r_c.b], writes=[o_t.b])
            k.dma("sp", self.ycat[tt * 128:(tt + 1) * 128, D_SSD:D], o_t.t[:], [o_t.b], [yb])

    def out_proj(self, w_ap):
        k = self
        with Scope(self) as s:
            W = s.sb("wout", [128, ND, D], BF16)
            for c in range(4):
                k.dma("pool", W.t[:, :, c * 512:(c + 1) * 512], w_ap[:, c * 512:(c + 1) * 512].rearrange("(k p) n -> p k n", p=128), [], [W.b])
            yt = [s.sb("op_y", [128, D], BF16) for _ in range(2)]
            yT = [s.sb("op_yT", [128, ND, 128], BF16) for _ in range(2)]
            pt = [s.ps("op_pt", [128, 8, 128], BF16) for _ in range(2)]
            po = [s.ps("op_po", [128, 512]) for _ in range(4)]
            mix = [s.sb("op_mix", [128, D], F32) for _ in range(2)]
            xb = self.P.buf("xres_w")
            for tt in range(NT):
                y_, yT_, mx = yt[tt % 2], yT[tt % 2], mix[tt % 2]
                k.dma("sp", y_.t[:], self.ycat[tt * 128:(tt + 1) * 128, :], [], [y_.b])
                for half in range(2):
                    p_ = pt[half]
                    for j in range(8):
                        kk = half * 8 + j
                        k.op("pe", lambda e, kk=kk, j=j, p_=p_, y_=y_: e.transpose(out=p_.t[:, j, :], in_=y_.t[:, kk * 128:(kk + 1) * 128], identity=self.ident.t[:]),
                             reads=[y_.b, self.ident.b], writes=[p_.b])
                    if half == 0:
                        k.op("dve", lambda e, p_=p_, yT_=yT_: e.tensor_copy(out=yT_.t[:, 0:8, :], in_=p_.t[:]), reads=[p_.b], writes=[yT_.b])
                    else:
                        k.op("act", lambda e, p_=p_, yT_=yT_: e.activation(out=yT_.t[:, 8:16, :], in_=p_.t[:], func=AF.Copy), reads=[p_.b], writes=[yT_.b])
                for dc in range(4):
                    pb = po[dc]
                    for kk in range(ND):
                        k.op("pe", lambda e, kk=kk, dc=dc, pb=pb, yT_=yT_: e.matmul(pb.t[:], lhsT=yT_.t[:, kk, :], rhs=W.t[:, kk, dc * 512:(dc + 1) * 512],
                                                                                   start=(kk == 0), stop=(kk == ND - 1)), reads=[yT_.b, W.b], writes=[pb.b])
                    if dc % 2 == 0:
                        k.op("act", lambda e, dc=dc, pb=pb, mx=mx: e.activation(out=mx.t[:, dc * 512:(dc + 1) * 512], in_=pb.t[:], func=AF.Copy), reads=[pb.b], writes=[mx.b])
                    else:
                        k.op("dve", lambda e, dc=dc, pb=pb, mx=mx: e.tensor_copy(out=mx.t[:, dc * 512:(dc + 1) * 512], in_=pb.t[:]), reads=[pb.b], writes=[mx.b])
                k.dma("pool", self.xres[tt * 128:(tt + 1) * 128, :], mx.t[:], [mx.b], [xb], accum_op=ALU.add)

    def ssd_layer(self, cs, li):
        j = li // 2
        Win = self.I["ssd_w_in"][j]
        with Scope(self) as AS:
            hT = AS.sb("hT", [128, ND, T], BF16)
            with Scope(self) as s:
                self.norm_to_hT(s, hT, self.I["norm_mix_g"][li:li + 1, :])
            with Scope(self) as s:
                self.ssd_proj_z(s, Win, hT)
            with Scope(self) as s:
                self.ssd_proj_xbc(s, j, Win, hT)
            with Scope(self) as s:
                self.ssd_proj_dt(s, j, Win, hT)
            with Scope(self) as s:
                self.xa_block(s, li, hT, Win, 4144)
        with Scope(self) as s:
            self.ssd_scan(s, j)
        self.out_proj(self.I["ssd_w_out"][j])

    def ssd_proj_z(self, s, Win, hT):
        k = self
        pbs = [s.ps("z_pb", [128, 512]) for _ in range(4)]
        zs = [s.sb("z_sb", [128, 512], BF16) for _ in range(4)]
        zb = self.P.buf("szd")
        it = 0
        for blk in range(3):
            w = self.wblock(s, Win, blk * 512, 512)
            for tt in range(NT):
                pb, z_ = pbs[it % 4], zs[it % 4]
                it += 1
                self.proj_tm(hT, w, 512, pb, tt)
                k.op("act", lambda e, pb=pb, z_=z_: e.activation(out=z_.t[:], in_=pb.t[:], func=AF.Silu), reads=[pb.b], writes=[z_.b])
                k.dma("sp", self.szd[tt * 128:(tt + 1) * 128, blk * 512:(blk + 1) * 512], z_.t[:], [z_.b], [zb])

    def ssd_proj_xbc(self, s, j, Win, hT):
        k = self
        raw = s.sb("cw_raw", [120, 128], F32)
        cwT = s.sb("cwT", [128, 120], F32)
        ptr = s.ps("cw_pt", [128, 120], F32)
        k.dma("sp", raw.t[0:100, :], self.I["ssd_conv_w"][j].rearrange("k (n p) -> (k n) p", p=128), [], [raw.b])
        k.dma("sp", raw.t[100:120, :], self.I["ssd_conv_b"][j:j + 1, :].rearrange("o (n p) -> (o n) p", p=128), [], [raw.b])
        k.op("pe", lambda e: e.transpose(out=ptr.t[:], in_=raw.t[:], identity=self.identf.t[0:120, 0:120]), reads=[raw.b, self.identf.b], writes=[ptr.b])
        k.op("dve", lambda e: e.tensor_copy(out=cwT.t[:], in_=ptr.t[:]), reads=[ptr.b], writes=[cwT.b])
        pbs = [s.ps("x_pb", [128, 512]) for _ in range(4)]
        pcs = [s.ps("x_pc", [128, 512]) for _ in range(2)]
        ptt = s.ps("x_ptt", [128, 8, 128], BF16)
        pre = [s.sb("x_pre", [128, T + 4], BF16) for _ in range(2)]
        post = [s.sb("x_post", [128, T], BF16) for _ in range(2)]
        dg = [s.sb("x_dg", [128, 5, 128], BF16) for _ in range(2)]
        tok = [s.sb("x_tok", [128, NT, 128], BF16) for _ in range(2)]
        xb = self.P.buf("xsBd")
        bb = self.P.buf("BCTd")
        for p_ in pre:
            k.op("pool", lambda e, p_=p_: e.memset(p_.t[:], 0.0), writes=[p_.b])
        cvi = 0
        for blk in range(5):
            w = self.wblock(s, Win, 1536 + blk * 512, 512)
            for et in range(4):
                ct = blk * 4 + et
                pr, po, dg_, tk = pre[ct % 2], post[ct % 2], dg[ct % 2], tok[ct % 2]
                for kk in range(5):
                    k.op("dve", lambda e, kk=kk, ct=ct, dg_=dg_: e.tensor_scalar(out=dg_.t[:, kk, :], in0=self.identf.t[:], scalar1=cwT.t[:, kk * 20 + ct:kk * 20 + ct + 1],
                                                                               scalar2=None, op0=ALU.mult), reads=[self.identf.b, cwT.b], writes=[dg_.b])
                self.proj_fm(s, hT, w, 512, pbs, et)
                for tc in range(4):
                    if tc % 2 == 0:
                        k.op("act", lambda e, tc=tc, pr=pr: e.activation(out=pr.t[:, 2 + tc * 512:2 + (tc + 1) * 512], in_=pbs[tc].t[:], func=AF.Copy), reads=[pbs[tc].b], writes=[pr.b])
                    else:
                        k.op("dve", lambda e, tc=tc, pr=pr: e.tensor_copy(out=pr.t[:, 2 + tc * 512:2 + (tc + 1) * 512], in_=pbs[tc].t[:]), reads=[pbs[tc].b], writes=[pr.b])
                for tc in range(4):
                    pc = pcs[cvi % 2]
                    cvi += 1
                    for kk in range(5):
                        k.op("pe", lambda e, kk=kk, tc=tc, pc=pc, dg_=dg_, pr=pr: e.matmul(pc.t[:], lhsT=dg_.t[:, kk, :], rhs=pr.t[:, tc * 512 + kk:tc * 512 + kk + 512],
                                                                                          start=(kk == 0), stop=(kk == 4)), reads=[dg_.b, pr.b], writes=[pc.b])
                    k.op("act", lambda e, tc=tc, pc=pc, po=po, ct=ct: e.activation(out=po.t[:, tc * 512:(tc + 1) * 512], in_=pc.t[:], func=AF.Silu, bias=cwT.t[:, 100 + ct:101 + ct]),
                         reads=[pc.b, cwT.b], writes=[po.b])
                if ct >= 12:
                    k.dma("sp", self.BCTd[ct - 12], po.t[:], [po.b], [bb])
                if ct < 16:
                    for half in range(2):
                        for jj in range(8):
                            tt = half * 8 + jj
                            k.op("pe", lambda e, jj=jj, tt=tt, po=po: e.transpose(out=ptt.t[:, jj, :], in_=po.t[:, tt * 128:(tt + 1) * 128], identity=self.ident.t[:]),
                                 reads=[po.b, self.ident.b], writes=[ptt.b])
                        k.op("dve", lambda e, half=half, tk=tk: e.tensor_copy(out=tk.t[:, half * 8:(half + 1) * 8, :], in_=ptt.t[:]), reads=[ptt.b], writes=[tk.b])
                    k.dma("sp", self.xsBd.rearrange("(n p) c -> p n c", p=128)[:, :, ct * 128:(ct + 1) * 128], tk.t[:], [tk.b], [xb])

    def ssd_proj_dt(self, s, j, Win, hT):
        k = self
        pbs = [s.ps("d_pb", [128, 512]) for _ in range(4)]
        ptk = s.ps("d_ptk", [128, 4, 48], F32)
        dtb = s.sb("d_dtb", [48, 1], F32)
        nA = s.sb("d_nA", [48, 1], F32)
        v = s.sb("d_v", [48, T], F32)
        a = s.sb("d_a", [48, T], F32)
        dt = s.sb("d_dt", [48, T], F32)
        dA = s.sb("d_dA", [48, T], F32)
        pre = s.sb("d_pre", [48, T], F32)
        suf = s.sb("d_suf", [48, T], F32)
        tk = [s.sb("d_tk", [128, 4, 48], F32) for _ in range(2)]
        k.dma("sp", dtb.t[:], self.I["ssd_dt_bias"][j:j + 1, :].rearrange("o h -> h o"), [], [dtb.b])
        k.dma("sp", nA.t[:], self.I["ssd_a_log"][j:j + 1, :].rearrange("o h -> h o"), [], [nA.b])
        k.op("act", lambda e: e.activation(out=nA.t[:], in_=nA.t[:], func=AF.Exp), reads=[nA.b], writes=[nA.b])
        k.op("dve", lambda e: e.tensor_scalar(out=nA.t[:], in0=nA.t[:], scalar1=-1.0, scalar2=None, op0=ALU.mult), reads=[nA.b], writes=[nA.b])
        w = self.wblock(s, Win, 4096, 48)
        self.proj_fm(s, hT, w, 48, pbs, 0)
        for tc in range(4):
            k.op("act", lambda e, tc=tc: e.activation(out=v.t[:, tc * 512:(tc + 1) * 512], in_=pbs[tc].t[0:48, :], func=AF.Identity, bias=dtb.t[:]),
                 reads=[pbs[tc].b, dtb.b], writes=[v.b])
        k.op("dve", lambda e: e.scalar_tensor_tensor(out=a.t[:], in0=v.t[:], scalar=-1.0, in1=v.t[:], op0=ALU.mult, op1=ALU.max), reads=[v.b], writes=[a.b])
        k.op("act", lambda e: e.activation(out=a.t[:], in_=a.t[:], func=AF.Exp, scale=-1.0), reads=[a.b], writes=[a.b])
        k.op("act", lambda e: e.activation(out=a.t[:], in_=a.t[:], func=AF.Ln, bias=1.0), reads=[a.b], writes=[a.b])
        k.op("dve", lambda e: e.scalar_tensor_tensor(out=dt.t[:], in0=v.t[:], scalar=0.0, in1=a.t[:], op0=ALU.max, op1=ALU.add), reads=[v.b, a.b], writes=[dt.b])
        k.op("dve", lambda e: e.tensor_scalar(out=dA.t[:], in0=dt.t[:], scalar1=nA.t[:, 0:1], scalar2=None, op0=ALU.mult), reads=[dt.b, nA.b], writes=[dA.b])
        for c in range(NT):
            sl = slice(c * 128, (c + 1) * 128)
            k.op("dve", lambda e, sl=sl: e.tensor_tensor_scan(out=pre.t[:, sl], data0=dA.t[:, sl], data1=dA.t[:, sl], initial=0.0, op0=ALU.add, op1=ALU.bypass),
                 reads=[dA.b], writes=[pre.b])
        for c in range(NT):
            sl = slice(c * 128, (c + 1) * 128)
            k.op("dve", lambda e, sl=sl, c=c: e.tensor_scalar(out=suf.t[:, sl], in0=pre.t[:, sl], scalar1=-1.0, scalar2=pre.t[:, c * 128 + 127:c * 128 + 128],
                                                             op0=ALU.mult, op1=ALU.add), reads=[pre.b], writes=[suf.b])
        k.op("dve", lambda e: e.tensor_tensor(out=suf.t[:], in0=suf.t[:], in1=dA.t[:], op=ALU.add), reads=[suf.b, dA.b], writes=[suf.b])
        ab = self.P.buf("ABd")
        tb = self.P.buf("TOTd")
        kb = self.P.buf("TOKd")
        ABv = self.ABd.rearrange("c (d h l) -> d h c l", d=2, h=SSD_H)
        k.dma("sp", ABv[0], pre.t[0:24, :].rearrange("h (c l) -> h c l", l=128), [pre.b], [ab])
        k.dma("sp", ABv[1], suf.t[24:48, :].rearrange("h (c l) -> h c l", l=128), [suf.b], [ab])
        k.dma("sp", self.TOTd.rearrange("o (c h) -> h (o c)", h=48), pre.t[:, 127::128], [pre.b], [tb], allow_slow_non_contiguous=True)
        srcs = [dt, pre, suf, dA]
        for c in range(NT):
            t_ = tk[c % 2]
            for qi, src in enumerate(srcs):
                k.op("pe", lambda e, qi=qi, src=src, c=c: e.transpose(out=ptk.t[:, qi, :], in_=src.t[:, c * 128:(c + 1) * 128], identity=self.identf.t[0:48, 0:48]),
                     reads=[src.b, self.identf.b], writes=[ptk.b])
            k.op("dve", lambda e, t_=t_: e.tensor_copy(out=t_.t[:], in_=ptk.t[:]), reads=[ptk.b], writes=[t_.b])
            k.dma("sp", self.TOKd[c * 128:(c + 1) * 128, :], t_.t[:].rearrange("p a h -> p (a h)"), [t_.b], [kb])

    def ssd_scan(self, s, j):
        k = self
        H, Pd = SSD_H, SSD_P
        Dful = s.sb("sc_D", [128, D_SSD], F32)
        d24 = s.sb("sc_d24", [128, H], F32)
        gn = s.sb("sc_gn", [128, D_SSD], F32)
        CD = s.sb("sc_CD", [128, NT, 48], F32)
        k.bcast_load("sp", d24, self.I["ssd_d"][j:j + 1, :])
        k.bcast_load("sp", gn, self.I["ssd_gate_norm_g"][j:j + 1, :])
        k.bcast_load("sp", CD_flat := Tl(CD.t, CD.b), self.TOTd[0:1, :]) if False else k.dma("sp", CD.t[:].rearrange("p c h -> p (c h)"), self.TOTd[0:1, :].partition_broadcast(128), [], [CD.b])
        k.op("act", lambda e: e.activation(out=CD.t[:], in_=CD.t[:], func=AF.Exp), reads=[CD.b], writes=[CD.b])
        k.op("dve", lambda e: e.tensor_copy(out=Dful.t[:].rearrange("p (h q) -> p h q", h=H), in_=d24.t[:].unsqueeze(2).to_broadcast([128, H, Pd])), reads=[d24.b], writes=[Dful.b])
        xs = [s.sb("sc_xs", [128, 2048], BF16) for _ in range(2)]
        tok = [s.sb("sc_tok", [128, 4, 48], F32) for _ in range(2)]
        Hs = s.sb("sc_H", [128, D_SSD], F32)
        Hbf = [s.sb("sc_Hbf", [128, D_SSD], BF16) for _ in range(2)]
        w24 = [s.sb("sc_w24", [128, H], F32) for _ in range(2)]
        xdd = [s.sb("sc_xdd", [128, D_SSD], BF16) for _ in range(2)]
        psS = [s.ps("sc_psS", [128, 512])] * 2
        gb = self.P.buf("Gd")
        xview = lambda t_: t_.t[:, 0:D_SSD].rearrange("p (h q) -> p h q", h=H)

        def load_chunk(c, bi):
            k.dma("sp", xs[bi].t[:], self.xsBd[c * 128:(c + 1) * 128, :], [], [xs[bi].b])
            k.dma("sp", tok[bi].t[:].rearrange("p a h -> p (a h)"), self.TOKd[c * 128:(c + 1) * 128, :], [], [tok[bi].b])

        def states(c, bi, d, Hacc, psl):
            ho = 24 * d
            x_, t_, w_, xd = xs[bi], tok[bi], w24[bi], xdd[bi]
            src = 2 if d == 0 else 1
            k.op("dve", lambda e: e.tensor_tensor(out=w_.t[:], in0=t_.t[:, src, ho:ho + 24], in1=t_.t[:, 3, ho:ho + 24], op=ALU.subtract), reads=[t_.b], writes=[w_.b])
            k.op("act", lambda e: e.activation(out=w_.t[:], in_=w_.t[:], func=AF.Exp), reads=[w_.b], writes=[w_.b])
            k.op("dve", lambda e: e.tensor_tensor(out=w_.t[:], in0=w_.t[:], in1=t_.t[:, 0, ho:ho + 24], op=ALU.mult), reads=[w_.b, t_.b], writes=[w_.b])
            k.op("pool", lambda e: e.tensor_tensor(out=xd.t[:].rearrange("p (h q) -> p h q", h=H), in0=xview(x_), in1=w_.t[:].unsqueeze(2).to_broadcast([128, H, Pd]), op=ALU.mult),
                 reads=[x_.b, w_.b], writes=[xd.b])
            for g in range(SSD_G):
                ps_ = psl[g % 2]
                k.op("pe", lambda e, g=g, ps_=ps_: e.matmul(ps_.t[:, 0:384], lhsT=x_.t[:, D_SSD + g * 128:D_SSD + (g + 1) * 128], rhs=xd.t[:, g * 384:(g + 1) * 384], start=True, stop=True),
                     reads=[x_.b, xd.b], writes=[ps_.b])
                hv = Hacc.t[:, g * 384:(g + 1) * 384]
                k.op("dve", lambda e, g=g, hv=hv: e.tensor_tensor(out=hv.rearrange("p (h q) -> p h q", h=6), in0=hv.rearrange("p (h q) -> p h q", h=6),
                                                                 in1=CD.t[:, c, ho + g * 6:ho + g * 6 + 6].unsqueeze(2).to_broadcast([128, 6, Pd]), op=ALU.mult),
                     reads=[Hacc.b, CD.b], writes=[Hacc.b])
                k.op("dve", lambda e, g=g, hv=hv, ps_=ps_: e.tensor_tensor(out=hv, in0=hv, in1=ps_.t[:, 0:384], op=ALU.add), reads=[Hacc.b, ps_.b], writes=[Hacc.b])

        k.op("pool", lambda e: e.memset(Hs.t[:], 0.0), writes=[Hs.b])
        load_chunk(NT - 1, (NT - 1) % 2)
        for c in range(NT - 1, -1, -1):
            bi = c % 2
            if c > 0:
                load_chunk(c - 1, (c - 1) % 2)
            hb_ = Hbf[bi]
            k.op("act", lambda e, hb_=hb_: e.activation(out=hb_.t[:], in_=Hs.t[:], func=AF.Copy), reads=[Hs.b], writes=[hb_.b])
            k.dma("sp", self.Gd[c], hb_.t[:], [hb_.b], [gb])
            if c > 0:
                states(c, bi, 1, Hs, psS)
        self.P.flush()
        bct = [s.sb("sc_bct", [128, 8, 128], BF16) for _ in range(2)]
        bc = [s.sb("sc_bc", [128, 2, H, 128], F32) for _ in range(2)]
        Gc = [s.sb("sc_G", [128, D_SSD], BF16) for _ in range(2)]
        sz = [s.sb("sc_sz", [128, D_SSD], BF16) for _ in range(2)]
        eA = [s.sb("sc_eA", [128, 48], F32) for _ in range(2)]
        ntk = [s.sb("sc_ntk", [128, 2, 48], F32) for _ in range(2)]
        xdt = [[s.sb("sc_xdt", [128, D_SSD], BF16) for _ in range(2)] for _ in range(2)]
        mcb = [[s.sb("sc_mcb", [128, 4, 128], F32) for _ in range(2)] for _ in range(2)]
        seg = [[s.sb("sc_seg", [128, 6, 128], F32) for _ in range(2)] for _ in range(2)]
        MT = [[s.sb("sc_MT", [128, 6, 128], BF16) for _ in range(2)] for _ in range(2)]
        t1 = [s.sb("sc_t1", [128, 384], F32) for _ in range(2)]
        t2 = [s.sb("sc_t2", [128, 384], F32) for _ in range(2)]
        yall = [s.sb("sc_yall", [128, D_SSD], F32) for _ in range(2)]
        junk = s.sb("sc_junk", [128, 384], BF16)
        ss = [s.sb("sc_ss", [128, 4], F32) for _ in range(2)]
        yn = [s.sb("sc_yn", [128, D_SSD], BF16) for _ in range(2)]
        psCB = s.ps("sc_psCB", [128, 4, 128])
        psY = [[s.ps("sc_psY", [128, 512]) for _ in range(3)] for _ in range(2)]
        yb_ = self.P.buf("ycat_ssd")

        def load2(c, bi):
            load_chunk(c, bi)
            k.dma("sp", bct[bi].t[:], self.BCTd.rearrange("g n t -> n g t")[:, :, c * 128:(c + 1) * 128], [], [bct[bi].b])
            k.dma("sp", bc[bi].t[:].rearrange("p d h l -> p (d h l)"), self.ABd[c:c + 1, :].partition_broadcast(128), [], [bc[bi].b])
            k.dma("sp", Gc[bi].t[:], self.Gd[c], [], [Gc[bi].b])
            k.dma("sp", sz[bi].t[:], self.szd[c * 128:(c + 1) * 128, :], [], [sz[bi].b])

        k.op("pool", lambda e: e.memset(Hs.t[:], 0.0), writes=[Hs.b])
        k.op("pool", lambda e: e.memset(Hbf[0].t[:], 0.0), writes=[Hbf[0].b])
        load2(0, 0)
        gi = 0
        for c in range(NT):
            bi = c % 2
            if c + 1 < NT:
                load2(c + 1, (c + 1) % 2)
            x_, t_, b_, bc_, G_, sz_, eA_, ya = xs[bi], tok[bi], bct[bi], bc[bi], Gc[bi], sz[bi], eA[bi], yall[bi]
            hf_ = Hbf[bi]
            ntk_ = ntk[bi]
            k.op("dve", lambda e, t_=t_, ntk_=ntk_: e.tensor_scalar(out=ntk_.t[:], in0=t_.t[:, 1:3, :], scalar1=-1.0, scalar2=None, op0=ALU.mult), reads=[t_.b], writes=[ntk_.b])
            k.op("act", lambda e, t_=t_, eA_=eA_: e.activation(out=eA_.t[:, 0:24], in_=t_.t[:, 1, 0:24], func=AF.Exp), reads=[t_.b], writes=[eA_.b])
            k.op("act", lambda e, t_=t_, eA_=eA_: e.activation(out=eA_.t[:, 24:48], in_=t_.t[:, 2, 24:48], func=AF.Exp), reads=[t_.b], writes=[eA_.b])
            for d in range(2):
                k.op("pool", lambda e, d=d, x_=x_, t_=t_: e.tensor_tensor(out=xdt[d][bi].t[:].rearrange("p (h q) -> p h q", h=H), in0=xview(x_),
                                                                         in1=t_.t[:, 0, 24 * d:24 * d + 24].unsqueeze(2).to_broadcast([128, H, Pd]), op=ALU.mult),
                     reads=[x_.b, t_.b], writes=[xdt[d][bi].b])
            for g in range(SSD_G):
                k.op("pe", lambda e, g=g, b_=b_: e.matmul(psCB.t[:, g, :], lhsT=b_.t[:, g, :], rhs=b_.t[:, 4 + g, :], start=True, stop=True), reads=[b_.b], writes=[psCB.b])
            k.op("dve", lambda e: e.tensor_tensor(out=mcb[0][bi].t[:], in0=psCB.t[:], in1=self.maskf.t[:].unsqueeze(1).to_broadcast([128, 4, 128]), op=ALU.mult),
                 reads=[psCB.b, self.maskf.b], writes=[mcb[0][bi].b])
            k.op("dve", lambda e: e.tensor_tensor(out=mcb[1][bi].t[:], in0=psCB.t[:], in1=self.maskb.t[:].unsqueeze(1).to_broadcast([128, 4, 128]), op=ALU.mult),
                 reads=[psCB.b, self.maskb.b], writes=[mcb[1][bi].b])
            def prep(g, sgi):
                for d in range(2):
                    sg, mt_ = seg[d][sgi], MT[d][sgi]
                    for jh in range(6):
                        h = g * 6 + jh
                        k.op("act", lambda e: e.activation(out=sg.t[:, jh, :], in_=bc_.t[:, d, h, :], func=AF.Exp, bias=ntk_.t[:, d, 24 * d + h:24 * d + h + 1]),
                             reads=[bc_.b, ntk_.b], writes=[sg.b])
                    k.op("dve", lambda e: e.scalar_tensor_tensor(out=mt_.t[:], in0=sg.t[:], scalar=1.0, in1=mcb[d][bi].t[:, g, :].unsqueeze(1).to_broadcast([128, 6, 128]),
                                                                 op0=ALU.min, op1=ALU.mult), reads=[sg.b, mcb[d][bi].b], writes=[mt_.b])

            prep(0, gi % 2)
            for g in range(SSD_G):
                pY = psY[gi % 2]
                sgi = gi % 2
                gi += 1
                if g + 1 < SSD_G:
                    prep(g + 1, gi % 2)
                for jh in range(6):
                    h = g * 6 + jh
                    for d in range(2):
                        k.op("pe", lambda e: e.matmul(pY[0].t[:, jh * 64:(jh + 1) * 64], lhsT=MT[d][sgi].t[:, jh, :], rhs=xdt[d][bi].t[:, h * 64:(h + 1) * 64],
                                                      start=(d == 0), stop=(d == 1)), reads=[MT[d][sgi].b, xdt[d][bi].b], writes=[pY[0].b])
                k.op("pe", lambda e: e.matmul(pY[1].t[:, 0:384], lhsT=b_.t[:, 4 + g, :], rhs=hf_.t[:, g * 384:(g + 1) * 384], start=True, stop=True),
                     reads=[b_.b, hf_.b], writes=[pY[1].b])
                k.op("pe", lambda e: e.matmul(pY[2].t[:, 0:384], lhsT=b_.t[:, 4 + g, :], rhs=G_.t[:, g * 384:(g + 1) * 384], start=True, stop=True),
                     reads=[b_.b, G_.b], writes=[pY[2].b])
                a1, a2 = t1[sgi], t2[sgi]
                v3 = lambda ap: ap.rearrange("p (h q) -> p h q", h=6)
                k.op("dve", lambda e: e.tensor_tensor(out=v3(a1.t[:]), in0=v3(pY[1].t[:, 0:384]), in1=eA_.t[:, g * 6:g * 6 + 6].unsqueeze(2).to_broadcast([128, 6, Pd]), op=ALU.mult),
                     reads=[pY[1].b, eA_.b], writes=[a1.b])
                k.op("dve", lambda e: e.tensor_tensor(out=v3(a2.t[:]), in0=v3(pY[2].t[:, 0:384]), in1=eA_.t[:, 24 + g * 6:24 + g * 6 + 6].unsqueeze(2).to_broadcast([128, 6, Pd]), op=ALU.mult),
                     reads=[pY[2].b, eA_.b], writes=[a2.b])
                k.op("pool", lambda e: e.tensor_tensor(out=a1.t[:], in0=a1.t[:], in1=a2.t[:], op=ALU.add), reads=[a1.b, a2.b], writes=[a1.b])
                k.op("dve", lambda e: e.tensor_tensor(out=a1.t[:], in0=pY[0].t[:, 0:384], in1=a1.t[:], op=ALU.add), reads=[pY[0].b, a1.b], writes=[a1.b])
                k.op("pool", lambda e: e.tensor_tensor(out=a2.t[:], in0=x_.t[:, g * 384:(g + 1) * 384], in1=Dful.t[:, g * 384:(g + 1) * 384], op=ALU.mult),
                     reads=[x_.b, Dful.b], writes=[a2.b])
                k.op("pool", lambda e: e.tensor_tensor(out=ya.t[:, g * 384:(g + 1) * 384], in0=a1.t[:], in1=a2.t[:], op=ALU.add), reads=[a1.b, a2.b], writes=[ya.b])
            if c + 1 < NT:
                states(c, bi, 0, Hs, psS)
                hn_ = Hbf[(c + 1) % 2]
                k.op("act", lambda e, hn_=hn_: e.activation(out=hn_.t[:], in_=Hs.t[:], func=AF.Copy), reads=[Hs.b], writes=[hn_.b])
            s_, yn_ = ss[bi], yn[bi]
            k.op("dve", lambda e: e.tensor_tensor(out=ya.t[:], in0=ya.t[:], in1=sz_.t[:], op=ALU.mult), reads=[ya.b, sz_.b], writes=[ya.b])
            for g in range(SSD_G):
                k.op("act", lambda e, g=g: e.activation(out=junk.t[:], in_=ya.t[:, g * 384:(g + 1) * 384], func=AF.Square, accum_out=s_.t[:, g:g + 1]), reads=[ya.b], writes=[junk.b, s_.b])
            k.op("act", lambda e: e.activation(out=s_.t[:], in_=s_.t[:], func=AF.Sqrt, scale=1.0 / 384, bias=self.epsc.t[:]), reads=[s_.b, self.epsc.b], writes=[s_.b])
            k.op("dve", lambda e: e.reciprocal(out=s_.t[:], in_=s_.t[:]), reads=[s_.b], writes=[s_.b])
            k.op("dve", lambda e: e.tensor_tensor(out=ya.t[:].rearrange("p (g q) -> p g q", g=4), in0=ya.t[:].rearrange("p (g q) -> p g q", g=4),
                                                  in1=s_.t[:].unsqueeze(2).to_broadcast([128, 4, 384]), op=ALU.mult), reads=[ya.b, s_.b], writes=[ya.b])
            k.op("pool", lambda e: e.tensor_tensor(out=yn_.t[:], in0=ya.t[:], in1=gn.t[:], op=ALU.mult), reads=[ya.b, gn.b], writes=[yn_.b])
            k.dma("sp", self.ycat[c * 128:(c + 1) * 128, 0:D_SSD], yn_.t[:], [yn_.b], [yb_])

    def na_layer(self, cs, li):
        j = li // 2
        Win = self.I["na_w_in"][j]
        with Scope(self) as AS:
            hT = AS.sb("hT", [128, ND, T], BF16)
            with Scope(self) as s:
                self.norm_to_hT(s, hT, self.I["norm_mix_g"][li:li + 1, :])
            for hg in range(6):
                with Scope(self) as s:
                    self.na_group(s, j, hg, Win, hT)
            with Scope(self) as s:
                self.xa_block(s, li, hT, Win, 4608)
        self.out_proj(self.I["na_w_out"][j])

    def na_group(self, s, j, hg, Win, hT):
        k = self
        HG = 2
        W = HG * 128
        QT = s.sb("na_QT", [128, HG, T], BF16)
        KT = s.sb("na_KT", [128, HG, T], BF16)
        Va = s.sb("na_Va", [128, NT, HG, 129], BF16)
        oall = s.sb("na_oall", [128, NT, W], BF16)
        pbig = s.ps("na_pbig", [128, 4, 512])
        pbs = [Tl(pbig.t[:, i, :], self.P.buf("na_pb")) for i in range(4)]
        k.op("pool", lambda e: e.memset(Va.t[:], 1.0), writes=[Va.b])
        for which, dst in ((0, QT), (1, KT)):
            w = self.wblock(s, Win, which * 1536 + hg * W, W)
            for et in range(HG):
                self.proj_fm(s, hT, w, W, pbs, et)
                for tc in range(4):
                    if tc % 2 == 0:
                        k.op("act", lambda e: e.activation(out=dst.t[:, et, tc * 512:(tc + 1) * 512], in_=pbs[tc].t, func=AF.Copy), reads=[pbs[tc].b], writes=[dst.b])
                    else:
                        k.op("dve", lambda e: e.tensor_copy(out=dst.t[:, et, tc * 512:(tc + 1) * 512], in_=pbs[tc].t), reads=[pbs[tc].b], writes=[dst.b])
        w = self.wblock(s, Win, 3072 + hg * W, W)
        for tt in range(NT):
            pb = pbs[tt % 4]
            self.proj_tm(hT, w, W, pb, tt)
            k.op("act", lambda e: e.activation(out=Va.t[:, tt, :, 0:128], in_=pb.t[:, 0:W].rearrange("p (h d) -> p h d", h=HG), func=AF.Copy), reads=[pb.b], writes=[Va.b])
        self.P.flush()
        bias = [s.sb("na_bias", [128, 25, 128], F32) for _ in range(2)]
        psS = [Tl(pbig.t[:, 2 * i:2 * i + 2, :].rearrange("p a n -> p (a n)")[:, 0:640].rearrange("p (j q) -> p j q", j=5), self.P.buf("na_psS")) for i in range(2)]
        psO = [s.ps("na_psO", [128, 256]) for _ in range(2)]
        tmp = [s.sb("na_tmp", [128, 5, 128], F32) for _ in range(2)]
        Pm = [s.sb("na_P", [128, 5, 128], BF16) for _ in range(2)]
        rc = [s.sb("na_rc", [128, 1], F32) for _ in range(2)]
        scale = 128.0 ** -0.5
        iters = [(hh, i) for hh in range(HG) for i in range(NT)]

        def geo(i):
            kb = min(max(2 * i - 4, 0), 22)
            case = 0 if i == 0 else 1 if i == 1 else 3 if i == 14 else 4 if i == 15 else 2
            return kb // 2, case

        def s_mm(n):
            hh, i = iters[n]
            kt0, _ = geo(i)
            pS = psS[n % 2]
            for jt in range(5):
                k.op("pe", lambda e: e.matmul(pS.t[:, jt, :], lhsT=KT.t[:, hh, (kt0 + jt) * 128:(kt0 + jt + 1) * 128], rhs=QT.t[:, hh, i * 128:(i + 1) * 128], start=True, stop=True),
                     reads=[KT.b, QT.b], writes=[pS.b])

        for hh in range(HG):
            k.dma("sp", bias[hh % 2].t[:], self.I["na_bias"][j, hg * HG + hh], [], [bias[hh % 2].b])
        s_mm(0)
        for n, (hh, i) in enumerate(iters):
            kt0, case = geo(i)
            bt = bias[hh % 2]
            pS, pO, tm, P_, r_ = psS[n % 2], psO[n % 2], tmp[n % 2], Pm[n % 2], rc[n % 2]
            if n + 1 < len(iters):
                s_mm(n + 1)
            k.op("dve", lambda e: e.scalar_tensor_tensor(out=tm.t[:], in0=pS.t, scalar=scale, in1=bt.t[:, case * 5:(case + 1) * 5, :], op0=ALU.mult, op1=ALU.add),
                 reads=[pS.b, bt.b], writes=[tm.b])
            k.op("act", lambda e: e.activation(out=P_.t[:], in_=tm.t[:], func=AF.Exp), reads=[tm.b], writes=[P_.b])
            for jt in range(5):
                k.op("pe", lambda e: e.matmul(pO.t[:, 0:129], lhsT=P_.t[:, jt, :], rhs=Va.t[:, kt0 + jt, hh, :], start=(jt == 0), stop=(jt == 4)),
                     reads=[P_.b, Va.b], writes=[pO.b])
            k.op("dve", lambda e: e.reciprocal(out=r_.t[:], in_=pO.t[:, 128:129]), reads=[pO.b], writes=[r_.b])
            k.op("act", lambda e: e.activation(out=oall.t[:, i, hh * 128:(hh + 1) * 128], in_=pO.t[:, 0:128], func=AF.Copy, scale=r_.t[:, 0:1]),
                 reads=[pO.b, r_.b], writes=[oall.b])
        yb = self.P.buf("ycat_na")
        k.dma("sp", self.ycat.rearrange("(n p) c -> p n c", p=128)[:, :, hg * W:(hg + 1) * W], oall.t[:], [oall.b], [yb])

    def moe_layer(self, cs, li):
        k = self
        with Scope(self) as MS:
            AFF = MS.sb("m_AFF", [128, NT, NE], F32)
            IDX = MS.sb("m_IDX", [128, 2 * NE], I32)
            GATE = MS.sb("m_GATE", [128, 2 * NE], F32)
            with Scope(self) as s:
                self.moe_router(s, li, AFF)
            with Scope(self) as s:
                self.moe_topk(s, AFF, IDX, GATE)
            with Scope(self) as s:
                self.moe_experts(s, li, IDX, GATE)

    def moe_router(self, s, li, AFF):
        k = self
        g = s.sb("g", [128, D], F32)
        k.bcast_load("sp", g, self.I["norm_ffn_g"][li:li + 1, :])
        Wr = s.sb("m_Wr", [128, ND, NE], F32)
        k.dma("sp", Wr.t[:], self.I["moe_w_router"][li].rearrange("(k p) e -> p k e", p=128), [], [Wr.b])
        hf = [s.sb("m_hf", [128, D], F32) for _ in range(2)]
        hTf = [s.sb("m_hTf", [128, ND, 128], F32) for _ in range(2)]
        ptf = [s.ps("m_ptf", [128, 4, 128], F32) for _ in range(4)]
        pl = [s.ps("m_pl", [128, NE], F32) for _ in range(2)]
        sm = [s.sb("m_sm", [128, 4], F32) for _ in range(2)]
        ex = [s.sb("m_ex", [128, NE], F32) for _ in range(2)]
        hb = self.P.buf("hn")
        for tt in range(NT):
            h_, hT_, pl_, sm_, ex_ = hf[tt % 2], hTf[tt % 2], pl[tt % 2], sm[tt % 2], ex[tt % 2]
            self.norm_tile(s, self.xres[tt * 128:(tt + 1) * 128, :], g, want_f32=h_, hn_dst=(self.hn[tt * 128:(tt + 1) * 128, :], hb))
            for q4 in range(4):
                p_ = ptf[q4]
                for jj in range(4):
                    kk = q4 * 4 + jj
                    k.op("pe", lambda e, kk=kk, jj=jj, p_=p_, h_=h_: e.transpose(out=p_.t[:, jj, :], in_=h_.t[:, kk * 128:(kk + 1) * 128], identity=self.identf.t[:]),
                         reads=[h_.b, self.identf.b], writes=[p_.b])
                if q4 % 2 == 0:
                    k.op("dve", lambda e, q4=q4, p_=p_, hT_=hT_: e.tensor_copy(out=hT_.t[:, q4 * 4:(q4 + 1) * 4, :], in_=p_.t[:]), reads=[p_.b], writes=[hT_.b])
                else:
                    k.op("act", lambda e, q4=q4, p_=p_, hT_=hT_: e.activation(out=hT_.t[:, q4 * 4:(q4 + 1) * 4, :], in_=p_.t[:], func=AF.Copy), reads=[p_.b], writes=[hT_.b])
            for kk in range(ND):
                k.op("pe", lambda e, kk=kk, hT_=hT_, pl_=pl_: e.matmul(pl_.t[:], lhsT=hT_.t[:, kk, :], rhs=Wr.t[:, kk, :], start=(kk == 0), stop=(kk == ND - 1)),
                     reads=[hT_.b, Wr.b], writes=[pl_.b])
            k.op("dve", lambda e, pl_=pl_, sm_=sm_: e.reduce_max(out=sm_.t[:, 0:1], in_=pl_.t[:], axis=AX.X), reads=[pl_.b], writes=[sm_.b])
            k.op("dve", lambda e, sm_=sm_: e.tensor_scalar(out=sm_.t[:, 1:2], in0=sm_.t[:, 0:1], scalar1=-1.0, scalar2=None, op0=ALU.mult), reads=[sm_.b], writes=[sm_.b])
            k.op("act", lambda e, pl_=pl_, sm_=sm_, ex_=ex_: e.activation(out=ex_.t[:], in_=pl_.t[:], func=AF.Exp, bias=sm_.t[:, 1:2], accum_out=sm_.t[:, 2:3]),
                 reads=[pl_.b, sm_.b], writes=[ex_.b, sm_.b])
            k.op("dve", lambda e, sm_=sm_: e.reciprocal(out=sm_.t[:, 3:4], in_=sm_.t[:, 2:3]), reads=[sm_.b], writes=[sm_.b])
            k.op("dve", lambda e, tt=tt, ex_=ex_, sm_=sm_: e.tensor_scalar(out=AFF.t[:, tt, :], in0=ex_.t[:], scalar1=sm_.t[:, 3:4], scalar2=None, op0=ALU.mult),
                 reads=[ex_.b, sm_.b], writes=[AFF.b])

    def moe_topk(self, s, AFF, IDX, GATE):
        k = self
        affT = s.sb("k_affT", [NE, T], F32)
        work = s.sb("k_work", [NE, T], F32)
        mx8 = s.sb("k_mx8", [NE, 8], F32)
        maskT = s.sb("k_mask", [NE, T], F32)
        pos = s.sb("k_pos", [NE, T], F32)
        POSM = s.sb("k_POSM", [128, NT, NE], F32)
        pA = [s.ps("k_pA", [128, 512]) for _ in range(4)]
        pP = s.ps("k_pP", [128, NT, NE], F32)
        for tt in range(NT):
            k.op("pe", lambda e, tt=tt: e.transpose(out=pA[tt // 4].t[0:NE, (tt % 4) * 128:(tt % 4 + 1) * 128], in_=AFF.t[:, tt, :], identity=self.identf.t[:]),
                 reads=[AFF.b, self.identf.b], writes=[pA[tt // 4].b])
        for q4 in range(4):
            k.op("dve", lambda e, q4=q4: e.tensor_copy(out=affT.t[:, q4 * 512:(q4 + 1) * 512], in_=pA[q4].t[0:NE, :]), reads=[pA[q4].b], writes=[affT.b])
        src = affT
        for it in range(CAP // 8):
            k.op("dve", lambda e, src=src: e.max(out=mx8.t[:], in_=src.t[:]), reads=[src.b], writes=[mx8.b])
            if it < CAP // 8 - 1:
                k.op("dve", lambda e, src=src: e.match_replace(out=work.t[:], in_to_replace=mx8.t[:], in_values=src.t[:], imm_value=-1.0), reads=[src.b, mx8.b], writes=[work.b])
                src = work
        k.op("dve", lambda e: e.tensor_scalar(out=maskT.t[:], in0=affT.t[:], scalar1=mx8.t[:, 7:8], scalar2=None, op0=ALU.is_ge), reads=[affT.b, mx8.b], writes=[maskT.b])
        k.op("dve", lambda e: e.tensor_tensor_scan(out=pos.t[:], data0=maskT.t[:], data1=maskT.t[:], initial=0.0, op0=ALU.add, op1=ALU.bypass), reads=[maskT.b], writes=[pos.b])
        k.op("dve", lambda e: e.tensor_tensor(out=pos.t[:], in0=pos.t[:], in1=maskT.t[:], op=ALU.mult), reads=[pos.b, maskT.b], writes=[pos.b])
        k.op("dve", lambda e: e.tensor_scalar(out=pos.t[:], in0=pos.t[:], scalar1=-1.0, scalar2=None, op0=ALU.add), reads=[pos.b], writes=[pos.b])
        for tt in range(NT):
            k.op("pe", lambda e, tt=tt: e.transpose(out=pP.t[:, tt, :], in_=pos.t[:, tt * 128:(tt + 1) * 128], identity=self.identf.t[0:NE, 0:NE]),
                 reads=[pos.b, self.identf.b], writes=[pP.b])
        k.op("dve", lambda e: e.tensor_copy(out=POSM.t[:], in_=pP.t[:]), reads=[pP.b], writes=[POSM.b])
        RV = s.sb("k_RV", [128, NT, NE, 5], BF16)
        r1 = s.sb("k_r1", [128, NT, NE], F32)
        gh = s.sb("k_gh", [128, NT, NE], BF16)
        pcol = s.sb("k_pcol", [128, 1], F32)
        k.op("pool", lambda e: e.iota(pcol.t[:], pattern=[[0, 1]], base=0, channel_multiplier=1, allow_small_or_imprecise_dtypes=True), writes=[pcol.b])
        for tt in range(NT):
            k.op("pool", lambda e, tt=tt: e.memset(RV.t[:, tt, :, 0], float(tt)), writes=[RV.b])
        k.op("dve", lambda e: e.tensor_copy(out=RV.t[:, :, :, 1].rearrange("p a b -> p (a b)"), in_=pcol.t[:].to_broadcast([128, NT * NE])), reads=[pcol.b], writes=[RV.b])
        k.op("dve", lambda e: e.tensor_copy(out=gh.t[:], in_=AFF.t[:]), reads=[AFF.b], writes=[gh.b])
        k.op("dve", lambda e: e.tensor_copy(out=RV.t[:, :, :, 2], in_=gh.t[:]), reads=[gh.b], writes=[RV.b])
        k.op("dve", lambda e: e.tensor_tensor(out=r1.t[:], in0=AFF.t[:], in1=gh.t[:], op=ALU.subtract), reads=[AFF.b, gh.b], writes=[r1.b])
        k.op("dve", lambda e: e.tensor_copy(out=gh.t[:], in_=r1.t[:]), reads=[r1.b], writes=[gh.b])
        k.op("dve", lambda e: e.tensor_copy(out=RV.t[:, :, :, 3], in_=gh.t[:]), reads=[gh.b], writes=[RV.b])
        k.op("dve", lambda e: e.tensor_tensor(out=r1.t[:], in0=r1.t[:], in1=gh.t[:], op=ALU.subtract), reads=[r1.b, gh.b], writes=[r1.b])
        k.op("dve", lambda e: e.tensor_copy(out=RV.t[:, :, :, 4], in_=r1.t[:]), reads=[r1.b], writes=[RV.b])
        O = [s.sb("k_O", [128, NT, CAP], BF16) for _ in range(2)]
        pz = [s.ps("k_pz", [128, 8], F32) for _ in range(2)]
        idf = s.sb("k_idf", [128, 2 * NE], F32)
        pzs = [s.sb("k_pzs", [128, 8], F32) for _ in range(2)]
        zi = 0
        for ex in range(NE):
            O_ = O[ex % 2]
            for tt in range(NT):
                k.op("dve", lambda e, tt=tt, ex=ex, O_=O_: e.tensor_scalar(out=O_.t[:, tt, :], in0=self.iota256.t[:], scalar1=POSM.t[:, tt, ex:ex + 1], scalar2=None, op0=ALU.is_equal),
                     reads=[self.iota256.b, POSM.b], writes=[O_.b])
            for half in range(2):
                pz_ = pz[zi % 2]
                zi += 1
                col = ex * 2 + half
                for tt in range(NT):
                    k.op("pe", lambda e, tt=tt, ex=ex, half=half, O_=O_, pz_=pz_: e.matmul(pz_.t[:, 0:5], lhsT=O_.t[:, tt, half * 128:(half + 1) * 128], rhs=RV.t[:, tt, ex, :], start=(tt == 0), stop=(tt == NT - 1)),
                         reads=[O_.b, RV.b], writes=[pz_.b])
                zs_ = pzs[zi % 2]
                k.op("dve", lambda e, pz_=pz_, zs_=zs_: e.tensor_copy(out=zs_.t[:, 0:5], in_=pz_.t[:, 0:5]), reads=[pz_.b], writes=[zs_.b])
                k.op("dve", lambda e, col=col, zs_=zs_: e.scalar_tensor_tensor(out=idf.t[:, col:col + 1], in0=zs_.t[:, 0:1], scalar=128.0, in1=zs_.t[:, 1:2], op0=ALU.mult, op1=ALU.add),
                     reads=[zs_.b], writes=[idf.b])
                k.op("dve", lambda e, col=col, zs_=zs_: e.tensor_reduce(out=GATE.t[:, col:col + 1], in_=zs_.t[:, 2:5], axis=AX.X, op=ALU.add), reads=[zs_.b], writes=[GATE.b])
        k.op("dve", lambda e: e.tensor_copy(out=IDX.t[:], in_=idf.t[:]), reads=[idf.b], writes=[IDX.b])

    def moe_experts(self, s, li, IDX, GATE):
        k = self
        ring = [s.sb("e_ring", [128, 8192], BF16) for _ in range(6)]
        xg = [s.sb("e_xg", [128, D], BF16) for _ in range(4)]
        xsT = [s.sb("e_xsT", [128, ND, CAP], BF16) for _ in range(2)]
        gT = [s.sb("e_gT", [128, 8, CAP], BF16) for _ in range(2)]
        sa = [s.sb("e_sa", [128, 512], F32) for _ in range(2)]
        gtok = [s.sb("e_gtok", [128, 512], BF16) for _ in range(2)]
        ysb = [s.sb("e_y", [128, D], F32) for _ in range(4)]
        ptr = [s.ps("e_ptr", [128, 8, 128], BF16) for _ in range(2)]
        pab = [s.ps("e_pab", [128, 512]) for _ in range(4)]
        py = [s.ps("e_py", [128, 512]) for _ in range(2)]
        xb = [self.P.buf("xres_moe") for _ in range(4)]
        hnb = self.P.buf("hn_r")
        cnt = dict(ti=0, ai=0, yi=0)
        for cq in range(4):
            k.dma("sp", self.moeh[cq][:, :], self.xres[:, cq * 512:(cq + 1) * 512], [], [xb[cq]])

        def gathers(ex):
            for half in range(2):
                col = ex * 2 + half
                x_ = xg[col % 4]
                k.op("pool", lambda e: e.indirect_dma_start(out=x_.t[:, :], out_offset=None, in_=self.hn[:, :],
                                                            in_offset=bass.IndirectOffsetOnAxis(ap=IDX.t[:, col:col + 1], axis=0)),
                     reads=[hnb, IDX.b], writes=[x_.b], dma=True)

        def wload(ex, ci):
            r_ = ring[ci]
            if ci < 4:
                wname, fh = ("moe_w1", "moe_w3")[ci % 2], ci // 2
                k.dma("pool", r_.t[:].rearrange("p (k n) -> p k n", k=ND), self.I[wname][li, ex][:, fh * 512:(fh + 1) * 512].rearrange("(k p) n -> p k n", p=128), [], [r_.b])
            else:
                dh = ci - 4
                k.dma("pool", r_.t[:].rearrange("p (k n) -> p k n", k=8), self.I["moe_w2"][li, ex][:, dh * 1024:(dh + 1) * 1024].rearrange("(k p) n -> p k n", p=128), [], [r_.b])

        gathers(0)
        for ci in range(6):
            wload(0, ci)
        for ex in range(NE):
            xT, g_ = xsT[ex % 2], gT[ex % 2]
            nxt = ex + 1 < NE
            if nxt:
                gathers(ex + 1)
            for half in range(2):
                x_ = xg[(ex * 2 + half) % 4]
                for kg in range(2):
                    p_ = ptr[cnt["ti"] % 2]
                    cnt["ti"] += 1
                    for jj in range(8):
                        kk = kg * 8 + jj
                        k.op("pe", lambda e: e.transpose(out=p_.t[:, jj, :], in_=x_.t[:, kk * 128:(kk + 1) * 128], identity=self.ident.t[:]),
                             reads=[x_.b, self.ident.b], writes=[p_.b])
                    if kg == 0:
                        k.op("dve", lambda e: e.tensor_copy(out=xT.t[:, kg * 8:(kg + 1) * 8, half * 128:(half + 1) * 128], in_=p_.t[:]), reads=[p_.b], writes=[xT.b])
                    else:
                        k.op("act", lambda e: e.activation(out=xT.t[:, kg * 8:(kg + 1) * 8, half * 128:(half + 1) * 128], in_=p_.t[:], func=AF.Copy), reads=[p_.b], writes=[xT.b])
            for fh in range(2):
                w1, w3 = ring[2 * fh], ring[2 * fh + 1]
                rv1 = w1.t[:].rearrange("p (k n) -> p k n", k=ND)
                rv3 = w3.t[:].rearrange("p (k n) -> p k n", k=ND)
                for half in range(2):
                    pa, pb = pab[cnt["ai"] % 4], pab[(cnt["ai"] + 1) % 4]
                    cnt["ai"] += 2
                    for kk in range(ND):
                        k.op("pe", lambda e: e.matmul(pa.t[:], lhsT=xT.t[:, kk, half * 128:(half + 1) * 128], rhs=rv1[:, kk, :], start=(kk == 0), stop=(kk == ND - 1)),
                             reads=[w1.b, xT.b], writes=[pa.b])
                        k.op("pe", lambda e: e.matmul(pb.t[:], lhsT=xT.t[:, kk, half * 128:(half + 1) * 128], rhs=rv3[:, kk, :], start=(kk == 0), stop=(kk == ND - 1)),
                             reads=[w3.b, xT.b], writes=[pb.b])
                    s_, gk = sa[cnt["ai"] // 2 % 2], gtok[cnt["ai"] // 2 % 2]
                    k.op("act", lambda e: e.activation(out=s_.t[:], in_=pa.t[:], func=AF.Silu), reads=[pa.b], writes=[s_.b])
                    k.op("dve", lambda e: e.tensor_tensor(out=gk.t[:], in0=s_.t[:], in1=pb.t[:], op=ALU.mult), reads=[s_.b, pb.b], writes=[gk.b])
                    p_ = ptr[cnt["ti"] % 2]
                    cnt["ti"] += 1
                    for jj in range(4):
                        k.op("pe", lambda e: e.transpose(out=p_.t[:, jj, :], in_=gk.t[:, jj * 128:(jj + 1) * 128], identity=self.ident.t[:]),
                             reads=[gk.b, self.ident.b], writes=[p_.b])
                    k.op("act", lambda e: e.activation(out=g_.t[:, fh * 4:(fh + 1) * 4, half * 128:(half + 1) * 128], in_=p_.t[:, 0:4, :], func=AF.Copy),
                         reads=[p_.b], writes=[g_.b])
                if nxt:
                    wload(ex + 1, 2 * fh)
                    wload(ex + 1, 2 * fh + 1)
            ys = [ysb[(ex * 2) % 4], ysb[(ex * 2 + 1) % 4]]
            for dh in range(2):
                r_ = ring[4 + dh]
                rv = r_.t[:].rearrange("p (k n) -> p k n", k=8)
                for half in range(2):
                    col = ex * 2 + half
                    for dc in range(2):
                        p_ = py[cnt["yi"] % 2]
                        cnt["yi"] += 1
                        for ft in range(8):
                            k.op("pe", lambda e: e.matmul(p_.t[:], lhsT=g_.t[:, ft, half * 128:(half + 1) * 128], rhs=rv[:, ft, dc * 512:(dc + 1) * 512], start=(ft == 0), stop=(ft == 7)),
                                 reads=[g_.b, r_.b], writes=[p_.b])
                        o0 = dh * 1024 + dc * 512
                        y_ = ys[half]
                        if dc == 0:
                            k.op("act", lambda e: e.activation(out=y_.t[:, o0:o0 + 512], in_=p_.t[:], func=AF.Copy, scale=GATE.t[:, col:col + 1]),
                                 reads=[p_.b, GATE.b], writes=[y_.b])
                        else:
                            k.op("dve", lambda e: e.tensor_scalar(out=y_.t[:, o0:o0 + 512], in0=p_.t[:], scalar1=GATE.t[:, col:col + 1], scalar2=None, op0=ALU.mult),
                                 reads=[p_.b, GATE.b], writes=[y_.b])
                if nxt:
                    wload(ex + 1, 4 + dh)
            for half in range(2):
                col = ex * 2 + half
                y_ = ys[half]
                for cq in range(4):
                    k.op("pool", lambda e: e.indirect_dma_start(out=self.moeh[cq][:, :], out_offset=bass.IndirectOffsetOnAxis(ap=IDX.t[:, col:col + 1], axis=0),
                                                                in_=y_.t[:, cq * 512:(cq + 1) * 512], in_offset=None, compute_op=ALU.add),
                         reads=[y_.b, IDX.b, xb[cq]], writes=[xb[cq]], dma=True)
        for cq in range(4):
            k.dma("sp", self.xres[:, cq * 512:(cq + 1) * 512], self.moeh[cq][:, :], [xb[cq]], [xb[cq]])


def _na_bias_tiles(rpb):
    n, Hh = rpb.shape[0], rpb.shape[1]
    out = np.full((n, Hh, 128, 25, 128), NEG, dtype=np.float32)
    a = np.arange(2)[:, None]; wk = np.arange(64)[None, :]
    for case, i in enumerate((0, 1, 2, 14, 15)):
        kb = min(max(2 * i - 4, 0), 22)
        for jt in range(5):
            for aa in range(2):
                kr = kb + 2 * jt + aa
                for bb in range(2):
                    r = 2 * i + bb
                    rs = min(max(r - 4, 0), 24)
                    if not (rs <= kr < rs + 8):
                        continue
                    dr = kr - r + 7
                    wq = np.arange(64)
                    ws = np.clip(wq - 8, 0, 48)
                    wkk = np.arange(64)[:, None]
                    valid = (wkk >= ws[None, :]) & (wkk < ws[None, :] + 16)
                    dc = np.clip(wkk - wq[None, :] + 15, 0, 30)
                    vals = rpb[:, :, dr, :][:, :, dc]
                    blk = out[:, :, aa * 64:(aa + 1) * 64, case * 5 + jt, bb * 64:(bb + 1) * 64]
                    blk[...] = np.where(valid[None, None], vals, NEG)
    return out


def _prep(inputs, nlayers=DEPTH):
    n_ssd = (nlayers + 1) // 2
    n_na = nlayers // 2
    f = lambda a: np.ascontiguousarray(a, dtype=np.float32)
    common = {
        "norm_mix_g": f(inputs["norm_mix_g"][:nlayers]), "norm_ffn_g": f(inputs["norm_ffn_g"][:nlayers]),
        "norm_final_g": f(inputs["norm_final_g"]).reshape(1, D), "mem_norm_g": f(inputs["mem_norm_g"]).reshape(1, D),
        "ssd_w_in": f(inputs["ssd_w_in"][:n_ssd]), "ssd_conv_w": f(inputs["ssd_conv_w"][:n_ssd]), "ssd_conv_b": f(inputs["ssd_conv_b"][:n_ssd]),
        "ssd_dt_bias": f(inputs["ssd_dt_bias"][:n_ssd]).reshape(n_ssd, 48), "ssd_a_log": f(inputs["ssd_a_log"][:n_ssd]).reshape(n_ssd, 48),
        "ssd_d": f(inputs["ssd_d"][:n_ssd]), "ssd_gate_norm_g": f(inputs["ssd_gate_norm_g"][:n_ssd]), "ssd_w_out": f(inputs["ssd_w_out"][:n_ssd]),
        "xa_w_kv": f(inputs["xa_w_kv"][:nlayers]), "moe_w_router": f(inputs["moe_w_router"][:nlayers]),
        "moe_w1": f(inputs["moe_w1"][:nlayers]), "moe_w3": f(inputs["moe_w3"][:nlayers]), "moe_w2": f(inputs["moe_w2"][:nlayers]),
    }
    if n_na:
        common["na_w_in"] = f(inputs["na_w_in"][:n_na])
        common["na_bias"] = _na_bias_tiles(f(inputs["na_rpb"][:n_na]))
        common["na_w_out"] = f(inputs["na_w_out"][:n_na])
    return common


_NC_CACHE = {}


def kernel(**inputs):
    x = np.asarray(inputs["x"], dtype=np.float32)
    mem = np.asarray(inputs["mem"], dtype=np.float32)
    common = _prep(inputs)
    if "nc" not in _NC_CACHE:
        _NC_CACHE["nc"] = KB().build()
    nc = _NC_CACHE["nc"]
    B = x.shape[0]
    in_maps = []
    for c in range(8):
        b = c % B
        m = dict(common)
        m["x"] = np.ascontiguousarray(x[b])
        m["mem"] = np.ascontiguousarray(mem[b])
        in_maps.append(m)
    res = run_bass_kernel_spmd(nc, in_maps, core_ids=list(range(8)))
    return np.stack([np.asarray(res.results[b]["out"], dtype=np.float32) for b in range(B)], axis=0)
```

```python
import contextlib
import numpy as np
import concourse.bass as bass
import concourse.mybir as mybir
from concourse.bass_utils import run_bass_kernel_spmd

F32 = mybir.dt.float32
BF16 = mybir.dt.bfloat16
I32 = mybir.dt.int32
AF = mybir.ActivationFunctionType
ALU = mybir.AluOpType
AX = mybir.AxisListType

T = 2048
D = 2048
NT = 16
ND = 16
DEPTH = 4
MEM = 256
EPS = 1e-6
D_XA = 512
D_SSD = 1536
SSD_H = 24
SSD_P = 64
SSD_G = 4
SSD_N = 128
SSD_CONV_DIM = 2560
SSD_IN = 4656
NA_H = 12
NA_IN = 5120
NE = 16
CAP = 256
DFF = 1024
NEG = -30000.0

ENGS = ("pe", "act", "dve", "pool", "sp")
ENGOBJ = {"pe": "tensor", "act": "scalar", "dve": "vector", "pool": "gpsimd", "sp": "sync"}
SEM_BLOCK = 30000


class Buf:
    __slots__ = ("name", "last_w", "readers", "sem", "base", "ndma")

    def __init__(self, name):
        self.name = name
        self.last_w = None
        self.readers = []
        self.sem = None
        self.base = 0
        self.ndma = 0


class Op:
    __slots__ = ("eng", "fn", "is_dma", "deps", "need_sig", "sig", "dst", "k")


class _Rec:
    def __init__(self):
        self.call = None

    def __getattr__(self, name):
        def f(*a, **kw):
            self.call = (name, a, kw)
            return self
        return f


class Prog:
    def __init__(self, nc, stack):
        self.nc = nc
        self.stack = stack
        self.q = {e: [] for e in ENGS}
        self.bufs = []
        self.eng_cnt = {e: 0 for e in ENGS}
        self.eng_sems = {}
        self.dma_pool = []
        self.n_sems = 0
        self.n_ops = 0

    def buf(self, name="b"):
        b = Buf(name)
        self.bufs.append(b)
        return b

    def _newsem(self, name):
        self.n_sems += 1
        return self.stack.enter_context(self.nc.semaphore(f"{name}_{self.n_sems}"))

    def op(self, eng, fn, reads=(), writes=(), dma=False):
        o = Op()
        rec = _Rec()
        fn(rec)
        assert rec.call is not None
        o.eng, o.fn, o.is_dma = eng, rec.call, dma
        o.need_sig, o.sig, o.dst, o.k = False, None, None, 0
        deps, seen = [], set()
        for r in reads:
            if r.last_w is not None:
                deps.append(r.last_w)
        for w in writes:
            if w.last_w is not None:
                deps.append(w.last_w)
            deps.extend(w.readers)
        dd = []
        for d in deps:
            if id(d) in seen or d is o:
                continue
            seen.add(id(d))
            if d.eng == "pe" and eng == "pe" and not d.is_dma and not dma:
                continue
            d.need_sig = True
            dd.append(d)
        o.deps = dd
        if dma:
            o.dst = writes[0]
            o.dst.ndma += 1
            o.k = o.dst.ndma
        for r in reads:
            r.readers.append(o)
        for w in writes:
            w.last_w = o
            w.readers = []
        self.q[eng].append(o)
        self.n_ops += 1
        return o

    def flush(self):
        nc = self.nc
        if not any(self.q[e] for e in ENGS):
            return
        lasts = []
        for e in ENGS:
            comp = [o for o in self.q[e] if not o.is_dma]
            if comp:
                comp[-1].need_sig = True
                lasts.append(comp[-1])
        dma_bufs = []
        for e in ENGS:
            for o in self.q[e]:
                if o.is_dma:
                    b = o.dst
                    if b.sem is None:
                        if self.dma_pool:
                            b.sem, b.base = self.dma_pool.pop(0)
                        else:
                            b.sem, b.base = self._newsem("d"), 0
                        dma_bufs.append(b)
                    o.sig = (b.sem, b.base + 16 * o.k)
                elif o.need_sig:
                    c = self.eng_cnt[e]
                    blk = c // SEM_BLOCK
                    key = (e, blk)
                    if key not in self.eng_sems:
                        self.eng_sems[key] = self._newsem(e)
                    o.sig = (self.eng_sems[key], c % SEM_BLOCK + 1)
                    self.eng_cnt[e] = c + 1
        finals = [o.sig for o in lasts] + [(b.sem, b.base + 16 * b.ndma) for b in dma_bufs]

        def run_queue(e, eng):
            waited = {}
            for o in self.q[e]:
                for d in o.deps:
                    sem, val = d.sig
                    if waited.get(id(sem), 0) >= val:
                        continue
                    waited[id(sem)] = val
                    eng.wait_ge(sem, val)
                nm, a_, kw_ = o.fn
                try:
                    ins = getattr(eng, nm)(*a_, **kw_)
                except Exception:
                    print("EMIT FAIL", e, nm, [getattr(x, "shape", x) for x in a_], {k_: getattr(v_, "shape", v_) for k_, v_ in kw_.items()}, flush=True)
                    for k_, v_ in kw_.items():
                        print("   ARG", k_, repr(v_)[:300], repr(getattr(v_, "ap", None))[:300], flush=True)
                    raise
                if o.sig is not None:
                    ins.then_inc(o.sig[0], 16 if o.is_dma else 1)
            for sem, val in finals:
                if waited.get(id(sem), 0) >= val:
                    continue
                eng.wait_ge(sem, val)

        with nc.Block() as block:
            for e in ENGS:
                def mk(e):
                    return lambda eng: run_queue(e, eng)
                getattr(block, ENGOBJ[e])(mk(e))
        for b in dma_bufs:
            cnt = b.base + 16 * b.ndma
            assert cnt < 32000, "dma semaphore count too large"
            self.dma_pool.append((b.sem, cnt))
        for b in self.bufs:
            b.last_w, b.readers, b.sem, b.base, b.ndma = None, [], None, 0, 0
        self.q = {e: [] for e in ENGS}


class Tl:
    __slots__ = ("t", "b")

    def __init__(self, t, b):
        self.t, self.b = t, b


class Scope:
    def __init__(self, k):
        self.k = k
        self.st = contextlib.ExitStack()

    def __enter__(self):
        self.st.__enter__()
        return self

    def __exit__(self, *a):
        self.k.P.flush()
        return self.st.__exit__(*a)

    def sb(self, name, shape, dt):
        self.k.uid += 1
        t = self.st.enter_context(self.k.nc.sbuf_tensor(f"{name}_{self.k.uid}", list(shape), dt))
        return Tl(t, self.k.P.buf(name))

    def ps(self, name, shape, dt=F32):
        self.k.uid += 1
        t = self.st.enter_context(self.k.nc.psum_tensor(f"{name}_{self.k.uid}", list(shape), dt))
        return Tl(t, self.k.P.buf(name))


class KB:
    def __init__(self, nlayers=DEPTH, dbg=None):
        self.nlayers = nlayers
        self.dbg = dbg
        self.uid = 0
        self.nc = bass.Bass("TRN2", target_bir_lowering=False)
        self.outer = contextlib.ExitStack()
        self.P = Prog(self.nc, self.outer)

    def din(self, name, shape, dt=F32):
        return self.nc.dram_tensor(name, list(shape), dt, kind="ExternalInput").ap()

    def dscr(self, name, shape, dt):
        return self.nc.dram_tensor(name, list(shape), dt).ap()

    def op(self, *a, **k):
        return self.P.op(*a, **k)

    def dma(self, q, out, in_, reads, writes, **kw):
        return self.P.op(q, lambda e: e.dma_start(out=out, in_=in_, **kw), reads=reads, writes=writes, dma=True)

    def bcast_load(self, q, dst, row_ap, nparts=128):
        return self.dma(q, dst.t[:], row_ap.partition_broadcast(nparts), [], [dst.b])

    def build(self):
        nc = self.nc
        L = self.nlayers
        n_ssd = (L + 1) // 2
        n_na = L // 2
        I = {}
        self.moeh = [self.dscr(f"moeh{cq}", [T, 512], F32) for cq in range(4)]
        self.hn = self.dscr("hn", [T, D], BF16)
        I["x"] = self.din("x", [T, D])
        I["mem"] = self.din("mem", [MEM, D])
        I["norm_mix_g"] = self.din("norm_mix_g", [L, D])
        I["norm_ffn_g"] = self.din("norm_ffn_g", [L, D])
        I["norm_final_g"] = self.din("norm_final_g", [1, D])
        I["mem_norm_g"] = self.din("mem_norm_g", [1, D])
        I["ssd_w_in"] = self.din("ssd_w_in", [n_ssd, D, SSD_IN])
        I["ssd_conv_w"] = self.din("ssd_conv_w", [n_ssd, 5, SSD_CONV_DIM])
        I["ssd_conv_b"] = self.din("ssd_conv_b", [n_ssd, SSD_CONV_DIM])
        I["ssd_dt_bias"] = self.din("ssd_dt_bias", [n_ssd, 48])
        I["ssd_a_log"] = self.din("ssd_a_log", [n_ssd, 48])
        I["ssd_d"] = self.din("ssd_d", [n_ssd, SSD_H])
        I["ssd_gate_norm_g"] = self.din("ssd_gate_norm_g", [n_ssd, D_SSD])
        I["ssd_w_out"] = self.din("ssd_w_out", [n_ssd, D, D])
        if n_na:
            I["na_w_in"] = self.din("na_w_in", [n_na, D, NA_IN])
            I["na_bias"] = self.din("na_bias", [n_na, NA_H, 128, 25, 128])
            I["na_w_out"] = self.din("na_w_out", [n_na, D, D])
        I["xa_w_kv"] = self.din("xa_w_kv", [L, D, 2 * D_XA])
        I["moe_w_router"] = self.din("moe_w_router", [L, D, NE])
        I["moe_w1"] = self.din("moe_w1", [L, NE, D, DFF])
        I["moe_w3"] = self.din("moe_w3", [L, NE, D, DFF])
        I["moe_w2"] = self.din("moe_w2", [L, NE, DFF, D])
        self.I = I
        self.out = nc.dram_tensor("out", [T, D], F32, kind="ExternalOutput").ap()
        self.xres = self.dscr("xres", [T, D], F32)
        self.ycat = self.dscr("ycat", [T, D], BF16)
        self.szd = self.dscr("szd", [T, D_SSD], BF16)
        self.Gd = self.dscr("Gd", [NT, 128, D_SSD], BF16)
        self.ABd = self.dscr("ABd", [NT, 2 * SSD_H * 128], F32)
        self.TOTd = self.dscr("TOTd", [1, NT * 48], F32)
        self.xsBd = self.dscr("xsBd", [T, 2048], BF16)
        self.BCTd = self.dscr("BCTd", [8, 128, T], BF16)
        self.TOKd = self.dscr("TOKd", [T, 4 * 48], F32)

        with self.outer:
            with Scope(self) as cs:
                self.consts(cs)
                for i in range(L):
                    if i % 2 == 0:
                        self.ssd_layer(cs, i)
                    else:
                        self.na_layer(cs, i)
                    if self.dbg == ("mix", i):
                        self.dump_xres(cs)
                        return self.nc
                    self.moe_layer(cs, i)
                    if self.dbg == ("moe", i):
                        self.dump_xres(cs)
                        return self.nc
                self.final_norm(cs)
        return self.nc

    def consts(self, cs):
        k = self
        self.identf = cs.sb("identf", [128, 128], F32)
        self.ident = cs.sb("ident", [128, 128], BF16)
        self.ones_bf = cs.sb("ones_bf", [128, 128], BF16)
        self.maskf = cs.sb("maskf", [128, 128], F32)
        self.maskb = cs.sb("maskb", [128, 128], F32)
        self.iota256 = cs.sb("iota256", [128, 256], F32)
        self.memT = cs.sb("memT", [128, ND, MEM], BF16)
        self.epsc = cs.sb("epsc", [128, 1], F32)
        idf, idb = self.identf, self.ident
        k.op("pool", lambda e: e.memset(idf.t[:], 0.0), writes=[idf.b])
        k.op("pool", lambda e: e.affine_select(out=idf.t[:], in_=idf.t[:], pattern=[[-1, 128]], compare_op=ALU.not_equal,
                                               fill=1.0, base=0, channel_multiplier=1), reads=[idf.b], writes=[idf.b])
        k.op("dve", lambda e: e.tensor_copy(out=idb.t[:], in_=idf.t[:]), reads=[idf.b], writes=[idb.b])
        k.op("pool", lambda e: e.memset(self.ones_bf.t[:], 1.0), writes=[self.ones_bf.b])
        k.op("pool", lambda e: e.memset(self.epsc.t[:], EPS), writes=[self.epsc.b])
        k.op("pool", lambda e: e.memset(self.maskf.t[:], 1.0), writes=[self.maskf.b])
        k.op("pool", lambda e: e.affine_select(out=self.maskf.t[:], in_=self.maskf.t[:], pattern=[[1, 128]], compare_op=ALU.is_ge,
                                               fill=0.0, base=0, channel_multiplier=-1), reads=[self.maskf.b], writes=[self.maskf.b])
        k.op("pool", lambda e: e.memset(self.maskb.t[:], 1.0), writes=[self.maskb.b])
        k.op("pool", lambda e: e.affine_select(out=self.maskb.t[:], in_=self.maskb.t[:], pattern=[[-1, 128]], compare_op=ALU.is_ge,
                                               fill=0.0, base=0, channel_multiplier=1), reads=[self.maskb.b], writes=[self.maskb.b])
        k.op("pool", lambda e: e.iota(self.iota256.t[:], pattern=[[1, 256]], base=0, channel_multiplier=0,
                                      allow_small_or_imprecise_dtypes=True), writes=[self.iota256.b])
        xb = self.P.buf("xres_all")
        k.dma("sp", self.xres[:, :], self.I["x"][:, :], [], [xb])
        self.P.flush()
        with Scope(self) as s:
            g = s.sb("g", [128, D], F32)
            k.bcast_load("sp", g, self.I["mem_norm_g"][0:1, :])
            for mt in range(2):
                self.norm_tile(s, self.I["mem"][mt * 128:(mt + 1) * 128, :], g, dstT=self.memT, tcol=mt * 128, tag=f"m{mt}")

    def norm_tile(self, s, src_ap, g, dstT=None, tcol=0, tag="", hn_dst=None, want_f32=None, q="sp", defer=False, need_bf16=True):
        k = self
        if not hasattr(s, "_nt"):
            s._nt = {}
            for j in range(3):
                s._nt[j] = dict(
                    xt=s.sb("n_xt", [128, D], F32), ss=s.sb("n_ss", [128, 1], F32),
                    hb=s.sb("n_hb", [128, D], BF16), pt=s.ps("n_pt", [128, 8, 128], BF16) if j < 2 else None)
            s._ntc = 0
        R = s._nt[s._ntc % 3]
        pt = s._nt[s._ntc % 2]["pt"]
        s._ntc += 1
        xt, ss, hb = R["xt"], R["ss"], R["hb"]
        k.dma(q, xt.t[:], src_ap, [], [xt.b])
        k.op("act", lambda e: e.activation(out=hb.t[:], in_=xt.t[:], func=AF.Square, accum_out=ss.t[:]), reads=[xt.b], writes=[hb.b, ss.b])
        k.op("act", lambda e: e.activation(out=ss.t[:], in_=ss.t[:], func=AF.Sqrt, scale=1.0 / D, bias=self.epsc.t[:]), reads=[ss.b, self.epsc.b], writes=[ss.b])
        k.op("dve", lambda e: e.reciprocal(out=ss.t[:], in_=ss.t[:]), reads=[ss.b], writes=[ss.b])
        if want_f32 is not None:
            hf = want_f32
            k.op("dve", lambda e: e.scalar_tensor_tensor(out=hf.t[:], in0=xt.t[:], scalar=ss.t[:, 0:1], in1=g.t[:], op0=ALU.mult, op1=ALU.mult),
                 reads=[xt.b, ss.b, g.b], writes=[hf.b])
            if need_bf16:
                k.op("act", lambda e: e.activation(out=hb.t[:], in_=hf.t[:], func=AF.Copy), reads=[hf.b], writes=[hb.b])
        else:
            k.op("dve", lambda e: e.scalar_tensor_tensor(out=hb.t[:], in0=xt.t[:], scalar=ss.t[:, 0:1], in1=g.t[:], op0=ALU.mult, op1=ALU.mult),
                 reads=[xt.b, ss.b, g.b], writes=[hb.b])
        if hn_dst is not None:
            k.dma("sp", hn_dst[0], hb.t[:], [hb.b], [hn_dst[1]])
        def back():
            if dstT is None:
                return
            for half in range(2):
                for j in range(8):
                    kk = half * 8 + j
                    k.op("pe", lambda e: e.transpose(out=pt.t[:, j, :], in_=hb.t[:, kk * 128:(kk + 1) * 128], identity=self.ident.t[:]),
                         reads=[hb.b, self.ident.b], writes=[pt.b])
                if half == 0:
                    k.op("dve", lambda e: e.tensor_copy(out=dstT.t[:, half * 8:(half + 1) * 8, tcol:tcol + 128], in_=pt.t[:]),
                         reads=[pt.b], writes=[dstT.b])
                else:
                    k.op("act", lambda e: e.activation(out=dstT.t[:, half * 8:(half + 1) * 8, tcol:tcol + 128], in_=pt.t[:], func=AF.Copy),
                         reads=[pt.b], writes=[dstT.b])
        if defer:
            return back
        back()

    def dump_xres(self, cs):
        self.P.flush()
        ob = self.P.buf("out")
        self.dma("sp", self.out[:, :], self.xres[:, :], [], [ob])
        self.P.flush()

    def final_norm(self, cs):
        k = self
        with Scope(self) as s:
            g = s.sb("g", [128, D], F32)
            k.bcast_load("sp", g, self.I["norm_final_g"][0:1, :])
            outs = [s.sb("fo", [128, D], F32) for _ in range(2)]
            ob = self.P.buf("out")
            for tt in range(NT):
                o = outs[tt % 2]
                self.norm_tile(s, self.xres[tt * 128:(tt + 1) * 128, :], g, want_f32=o, tag=f"f{tt}", need_bf16=False)
                k.dma("sp", self.out[tt * 128:(tt + 1) * 128, :], o.t[:], [o.b], [ob])

    def wblock(self, s, w_ap, c0, ncols, nk=ND):
        if not hasattr(s, "_wb"):
            s._wb = [s.sb("wblk", [128, ND, 512], BF16) for _ in range(2)]
            s._wbc = 0
        w = s._wb[s._wbc % 2]
        s._wbc += 1
        self.dma("pool", w.t[:, 0:nk, 0:ncols], w_ap[:, c0:c0 + ncols].rearrange("(k p) n -> p k n", p=128), [], [w.b])
        return w

    def proj_fm(self, s, hT, w, ncols, ps_bank_tiles, et):
        m0 = et * 128
        m = min(128, ncols - m0)
        for tc in range(4):
            pb = ps_bank_tiles[tc]
            for kk in range(ND):
                self.op("pe", lambda e, kk=kk, tc=tc, pb=pb: e.matmul(pb.t[0:m, :], lhsT=w.t[:, kk, m0:m0 + m], rhs=hT.t[:, kk, tc * 512:(tc + 1) * 512],
                                                                     start=(kk == 0), stop=(kk == ND - 1)),
                        reads=[w.b, hT.b], writes=[pb.b])

    def proj_tm(self, hT, w, ncols, pb, tt):
        for kk in range(ND):
            self.op("pe", lambda e, kk=kk: e.matmul(pb.t[:, 0:ncols], lhsT=hT.t[:, kk, tt * 128:(tt + 1) * 128], rhs=w.t[:, kk, 0:ncols],
                                                    start=(kk == 0), stop=(kk == ND - 1)),
                    reads=[w.b, hT.b], writes=[pb.b])

    def norm_to_hT(self, s, hT, gain_row):
        g = s.sb("g", [128, D], F32)
        self.bcast_load("sp", g, gain_row)
        pend = None
        for tt in range(NT):
            b = self.norm_tile(s, self.xres[tt * 128:(tt + 1) * 128, :], g, dstT=hT, tcol=tt * 128, defer=True)
            if pend is not None:
                pend()
            pend = b
        pend()

    def xa_block(self, s, li, hT, wq_ap, c0):
        k = self
        qT = s.sb("xa_qT", [128, 4, T], BF16)
        kT = s.sb("xa_kT", [128, 4, MEM], BF16)
        va = s.sb("xa_va", [128, 2, 4, 129], BF16)
        pbs = [s.ps("xa_pb", [128, 512]) for _ in range(4)]
        w = self.wblock(s, wq_ap, c0, 512)
        for et in range(4):
            self.proj_fm(s, hT, w, 512, pbs, et)
            for tc in range(4):
                eng = "act" if tc % 2 == 0 else "dve"
                if eng == "act":
                    k.op("act", lambda e, et=et, tc=tc: e.activation(out=qT.t[:, et, tc * 512:(tc + 1) * 512], in_=pbs[tc].t[:], func=AF.Copy),
                         reads=[pbs[tc].b], writes=[qT.b])
                else:
                    k.op("dve", lambda e, et=et, tc=tc: e.tensor_copy(out=qT.t[:, et, tc * 512:(tc + 1) * 512], in_=pbs[tc].t[:]),
                         reads=[pbs[tc].b], writes=[qT.b])
        wkv = self.I["xa_w_kv"][li]
        w = self.wblock(s, wkv, 0, 512)
        for et in range(4):
            pb = pbs[et]
            for kk in range(ND):
                k.op("pe", lambda e, kk=kk, et=et, pb=pb: e.matmul(pb.t[:, 0:MEM], lhsT=w.t[:, kk, et * 128:(et + 1) * 128], rhs=self.memT.t[:, kk, :],
                                                                  start=(kk == 0), stop=(kk == ND - 1)), reads=[w.b, self.memT.b], writes=[pb.b])
            k.op("act", lambda e, et=et, pb=pb: e.activation(out=kT.t[:, et, :], in_=pb.t[:, 0:MEM], func=AF.Copy), reads=[pb.b], writes=[kT.b])
        w = self.wblock(s, wkv, 512, 512)
        k.op("pool", lambda e: e.memset(va.t[:], 1.0), writes=[va.b])
        for mt in range(2):
            pb = pbs[mt]
            for kk in range(ND):
                k.op("pe", lambda e, kk=kk, mt=mt, pb=pb: e.matmul(pb.t[:, :], lhsT=self.memT.t[:, kk, mt * 128:(mt + 1) * 128], rhs=w.t[:, kk, :],
                                                                  start=(kk == 0), stop=(kk == ND - 1)), reads=[w.b, self.memT.b], writes=[pb.b])
            k.op("dve", lambda e, mt=mt, pb=pb: e.tensor_copy(out=va.t[:, mt, :, 0:128], in_=pb.t[:].rearrange("p (h d) -> p h d", h=4)),
                 reads=[pb.b], writes=[va.b])
        sps = [s.ps("xa_s", [128, 2, 128]) for _ in range(2)]
        ops_ = [s.ps("xa_o", [128, 4, 256]) for _ in range(1)]
        pT = [s.sb("xa_p", [128, 2, 128], BF16) for _ in range(2)]
        rc = [s.sb("xa_rc", [128, 4], F32) for _ in range(2)]
        ot = [s.sb("xa_ot", [128, 512], BF16) for _ in range(2)]
        yb = self.P.buf("ycat_xa")
        scale = 128.0 ** -0.5
        it = 0
        for tt in range(NT):
            o_ps, o_t, r_c = ops_[0], ot[tt % 2], rc[tt % 2]
            for h in range(4):
                sp_, p_ = sps[it % 2], pT[it % 2]
                it += 1
                for mt in range(2):
                    k.op("pe", lambda e, mt=mt, h=h, sp_=sp_: e.matmul(sp_.t[:, mt, :], lhsT=kT.t[:, h, mt * 128:(mt + 1) * 128], rhs=qT.t[:, h, tt * 128:(tt + 1) * 128],
                                                                      start=True, stop=True), reads=[kT.b, qT.b], writes=[sp_.b])
                k.op("act", lambda e, sp_=sp_, p_=p_: e.activation(out=p_.t[:], in_=sp_.t[:], func=AF.Exp, scale=scale), reads=[sp_.b], writes=[p_.b])
                for mt in range(2):
                    k.op("pe", lambda e, mt=mt, h=h, p_=p_, o_ps=o_ps: e.matmul(o_ps.t[:, h, 0:129], lhsT=p_.t[:, mt, :], rhs=va.t[:, mt, h, :],
                                                                               start=(mt == 0), stop=(mt == 1)), reads=[p_.b, va.b], writes=[o_ps.b])
            k.op("dve", lambda e, o_ps=o_ps, r_c=r_c: e.reciprocal(out=r_c.t[:], in_=o_ps.t[:, :, 128]), reads=[o_ps.b], writes=[r_c.b])
            k.op("dve", lambda e, o_ps=o_ps, r_c=r_c, o_t=o_t: e.tensor_tensor(out=o_t.t[:].rearrange("p (h d) -> p h d", h=4), in0=o_ps.t[:, :, 0:128],
                                                                               in1=r_c.t[:].unsqueeze(2).to_broadcast([128, 4, 128]), op=ALU.mult),
                 reads=[o_ps.b, r_c.b], writes=[o_t.b])
            k.dma("sp", self.ycat[tt * 128:(tt + 1) * 128, D_SSD:D], o_t.t[:], [o_t.b], [yb])

    def out_proj(self, w_ap):
        k = self
        with Scope(self) as s:
            W = s.sb("wout", [128, ND, D], BF16)
            for c in range(4):
                k.dma("pool", W.t[:, :, c * 512:(c + 1) * 512], w_ap[:, c * 512:(c + 1) * 512].rearrange("(k p) n -> p k n", p=128), [], [W.b])
            yt = [s.sb("op_y", [128, D], BF16) for _ in range(2)]
            yT = [s.sb("op_yT", [128, ND, 128], BF16) for _ in range(2)]
            pt = [s.ps("op_pt", [128, 8, 128], BF16) for _ in range(2)]
            po = [s.ps("op_po", [128, 512]) for _ in range(4)]
            mix = [s.sb("op_mix", [128, D], F32) for _ in range(2)]
            xb = self.P.buf("xres_w")
            for tt in range(NT):
                y_, yT_, mx = yt[tt % 2], yT[tt % 2], mix[tt % 2]
                k.dma("sp", y_.t[:], self.ycat[tt * 128:(tt + 1) * 128, :], [], [y_.b])
                for half in range(2):
                    p_ = pt[half]
                    for j in range(8):
                        kk = half * 8 + j
                        k.op("pe", lambda e, kk=kk, j=j, p_=p_, y_=y_: e.transpose(out=p_.t[:, j, :], in_=y_.t[:, kk * 128:(kk + 1) * 128], identity=self.ident.t[:]),
                             reads=[y_.b, self.ident.b], writes=[p_.b])
                    if half == 0:
                        k.op("dve", lambda e, p_=p_, yT_=yT_: e.tensor_copy(out=yT_.t[:, 0:8, :], in_=p_.t[:]), reads=[p_.b], writes=[yT_.b])
                    else:
                        k.op("act", lambda e, p_=p_, yT_=yT_: e.activation(out=yT_.t[:, 8:16, :], in_=p_.t[:], func=AF.Copy), reads=[p_.b], writes=[yT_.b])
                for dc in range(4):
                    pb = po[dc]
                    for kk in range(ND):
                        k.op("pe", lambda e, kk=kk, dc=dc, pb=pb, yT_=yT_: e.matmul(pb.t[:], lhsT=yT_.t[:, kk, :], rhs=W.t[:, kk, dc * 512:(dc + 1) * 512],
                                                                                   start=(kk == 0), stop=(kk == ND - 1)), reads=[yT_.b, W.b], writes=[pb.b])
                    if dc % 2 == 0:
                        k.op("act", lambda e, dc=dc, pb=pb, mx=mx: e.activation(out=mx.t[:, dc * 512:(dc + 1) * 512], in_=pb.t[:], func=AF.Copy), reads=[pb.b], writes=[mx.b])
                    else:
                        k.op("dve", lambda e, dc=dc, pb=pb, mx=mx: e.tensor_copy(out=mx.t[:, dc * 512:(dc + 1) * 512], in_=pb.t[:]), reads=[pb.b], writes=[mx.b])
                k.dma("pool", self.xres[tt * 128:(tt + 1) * 128, :], mx.t[:], [mx.b], [xb], accum_op=ALU.add)

    def ssd_layer(self, cs, li):
        j = li // 2
        Win = self.I["ssd_w_in"][j]
        with Scope(self) as AS:
            hT = AS.sb("hT", [128, ND, T], BF16)
            with Scope(self) as s:
                self.norm_to_hT(s, hT, self.I["norm_mix_g"][li:li + 1, :])
            with Scope(self) as s:
                self.ssd_proj_z(s, Win, hT)
            with Scope(self) as s:
                self.ssd_proj_xbc(s, j, Win, hT)
            with Scope(self) as s:
                self.ssd_proj_dt(s, j, Win, hT)
            with Scope(self) as s:
                self.xa_block(s, li, hT, Win, 4144)
        with Scope(self) as s:
            self.ssd_scan(s, j)
        self.out_proj(self.I["ssd_w_out"][j])

    def ssd_proj_z(self, s, Win, hT):
        k = self
        pbs = [s.ps("z_pb", [128, 512]) for _ in range(4)]
        zs = [s.sb("z_sb", [128, 512], BF16) for _ in range(4)]
        zb = self.P.buf("szd")
        it = 0
        for blk in range(3):
            w = self.wblock(s, Win, blk * 512, 512)
            for tt in range(NT):
                pb, z_ = pbs[it % 4], zs[it % 4]
                it += 1
                self.proj_tm(hT, w, 512, pb, tt)
                k.op("act", lambda e, pb=pb, z_=z_: e.activation(out=z_.t[:], in_=pb.t[:], func=AF.Silu), reads=[pb.b], writes=[z_.b])
                k.dma("sp", self.szd[tt * 128:(tt + 1) * 128, blk * 512:(blk + 1) * 512], z_.t[:], [z_.b], [zb])

    def ssd_proj_xbc(self, s, j, Win, hT):
        k = self
        raw = s.sb("cw_raw", [120, 128], F32)
        cwT = s.sb("cwT", [128, 120], F32)
        ptr = s.ps("cw_pt", [128, 120], F32)
        k.dma("sp", raw.t[0:100, :], self.I["ssd_conv_w"][j].rearrange("k (n p) -> (k n) p", p=128), [], [raw.b])
        k.dma("sp", raw.t[100:120, :], self.I["ssd_conv_b"][j:j + 1, :].rearrange("o (n p) -> (o n) p", p=128), [], [raw.b])
        k.op("pe", lambda e: e.transpose(out=ptr.t[:], in_=raw.t[:], identity=self.identf.t[0:120, 0:120]), reads=[raw.b, self.identf.b], writes=[ptr.b])
        k.op("dve", lambda e: e.tensor_copy(out=cwT.t[:], in_=ptr.t[:]), reads=[ptr.b], writes=[cwT.b])
        pbs = [s.ps("x_pb", [128, 512]) for _ in range(4)]
        pcs = [s.ps("x_pc", [128, 512]) for _ in range(2)]
        ptt = s.ps("x_ptt", [128, 8, 128], BF16)
        pre = [s.sb("x_pre", [128, T + 4], BF16) for _ in range(2)]
        post = [s.sb("x_post", [128, T], BF16) for _ in range(2)]
        dg = [s.sb("x_dg", [128, 5, 128], BF16) for _ in range(2)]
        tok = [s.sb("x_tok", [128, NT, 128], BF16) for _ in range(2)]
        xb = self.P.buf("xsBd")
        bb = self.P.buf("BCTd")
        for p_ in pre:
            k.op("pool", lambda e, p_=p_: e.memset(p_.t[:], 0.0), writes=[p_.b])
        cvi = 0
        for blk in range(5):
            w = self.wblock(s, Win, 1536 + blk * 512, 512)
            for et in range(4):
                ct = blk * 4 + et
                pr, po, dg_, tk = pre[ct % 2], post[ct % 2], dg[ct % 2], tok[ct % 2]
                for kk in range(5):
                    k.op("dve", lambda e, kk=kk, ct=ct, dg_=dg_: e.tensor_scalar(out=dg_.t[:, kk, :], in0=self.identf.t[:], scalar1=cwT.t[:, kk * 20 + ct:kk * 20 + ct + 1],
                                                                               scalar2=None, op0=ALU.mult), reads=[self.identf.b, cwT.b], writes=[dg_.b])
                self.proj_fm(s, hT, w, 512, pbs, et)
                for tc in range(4):
                    if tc % 2 == 0:
                        k.op("act", lambda e, tc=tc, pr=pr: e.activation(out=pr.t[:, 2 + tc * 512:2 + (tc + 1) * 512], in_=pbs[tc].t[:], func=AF.Copy), reads=[pbs[tc].b], writes=[pr.b])
                    else:
                        k.op("dve", lambda e, tc=tc, pr=pr: e.tensor_copy(out=pr.t[:, 2 + tc * 512:2 + (tc + 1) * 512], in_=pbs[tc].t[:]), reads=[pbs[tc].b], writes=[pr.b])
                for tc in range(4):
                    pc = pcs[cvi % 2]
                    cvi += 1
                    for kk in range(5):
                        k.op("pe", lambda e, kk=kk, tc=tc, pc=pc, dg_=dg_, pr=pr: e.matmul(pc.t[:], lhsT=dg_.t[:, kk, :], rhs=pr.t[:, tc * 512 + kk:tc * 512 + kk + 512],
                                                                                          start=(kk == 0), stop=(kk == 4)), reads=[dg_.b, pr.b], writes=[pc.b])
                    k.op("act", lambda e, tc=tc, pc=pc, po=po, ct=ct: e.activation(out=po.t[:, tc * 512:(tc + 1) * 512], in_=pc.t[:], func=AF.Silu, bias=cwT.t[:, 100 + ct:101 + ct]),
                         reads=[pc.b, cwT.b], writes=[po.b])
                if ct >= 12:
                    k.dma("sp", self.BCTd[ct - 12], po.t[:], [po.b], [bb])
                if ct < 16:
                    for half in range(2):
                        for jj in range(8):
                            tt = half * 8 + jj
                            k.op("pe", lambda e, jj=jj, tt=tt, po=po: e.transpose(out=ptt.t[:, jj, :], in_=po.t[:, tt * 128:(tt + 1) * 128], identity=self.ident.t[:]),
                                 reads=[po.b, self.ident.b], writes=[ptt.b])
                        k.op("dve", lambda e, half=half, tk=tk: e.tensor_copy(out=tk.t[:, half * 8:(half + 1) * 8, :], in_=ptt.t[:]), reads=[ptt.b], writes=[tk.b])
                    k.dma("sp", self.xsBd.rearrange("(n p) c -> p n c", p=128)[:, :, ct * 128:(ct + 1) * 128], tk.t[:], [tk.b], [xb])

    def ssd_proj_dt(self, s, j, Win, hT):
        k = self
        pbs = [s.ps("d_pb", [128, 512]) for _ in range(4)]
        ptk = s.ps("d_ptk", [128, 4, 48], F32)
        dtb = s.sb("d_dtb", [48, 1], F32)
        nA = s.sb("d_nA", [48, 1], F32)
        v = s.sb("d_v", [48, T], F32)
        a = s.sb("d_a", [48, T], F32)
        dt = s.sb("d_dt", [48, T], F32)
        dA = s.sb("d_dA", [48, T], F32)
        pre = s.sb("d_pre", [48, T], F32)
        suf = s.sb("d_suf", [48, T], F32)
        tk = [s.sb("d_tk", [128, 4, 48], F32) for _ in range(2)]
        k.dma("sp", dtb.t[:], self.I["ssd_dt_bias"][j:j + 1, :].rearrange("o h -> h o"), [], [dtb.b])
        k.dma("sp", nA.t[:], self.I["ssd_a_log"][j:j + 1, :].rearrange("o h -> h o"), [], [nA.b])
        k.op("act", lambda e: e.activation(out=nA.t[:], in_=nA.t[:], func=AF.Exp), reads=[nA.b], writes=[nA.b])
        k.op("dve", lambda e: e.tensor_scalar(out=nA.t[:], in0=nA.t[:], scalar1=-1.0, scalar2=None, op0=ALU.mult), reads=[nA.b], writes=[nA.b])
        w = self.wblock(s, Win, 4096, 48)
        self.proj_fm(s, hT, w, 48, pbs, 0)
        for tc in range(4):
            k.op("act", lambda e, tc=tc: e.activation(out=v.t[:, tc * 512:(tc + 1) * 512], in_=pbs[tc].t[0:48, :], func=AF.Identity, bias=dtb.t[:]),
                 reads=[pbs[tc].b, dtb.b], writes=[v.b])
        k.op("dve", lambda e: e.scalar_tensor_tensor(out=a.t[:], in0=v.t[:], scalar=-1.0, in1=v.t[:], op0=ALU.mult, op1=ALU.max), reads=[v.b], writes=[a.b])
        k.op("act", lambda e: e.activation(out=a.t[:], in_=a.t[:], func=AF.Exp, scale=-1.0), reads=[a.b], writes=[a.b])
        k.op("act", lambda e: e.activation(out=a.t[:], in_=a.t[:], func=AF.Ln, bias=1.0), reads=[a.b], writes=[a.b])
        k.op("dve", lambda e: e.scalar_tensor_tensor(out=dt.t[:], in0=v.t[:], scalar=0.0, in1=a.t[:], op0=ALU.max, op1=ALU.add), reads=[v.b, a.b], writes=[dt.b])
        k.op("dve", lambda e: e.tensor_scalar(out=dA.t[:], in0=dt.t[:], scalar1=nA.t[:, 0:1], scalar2=None, op0=ALU.mult), reads=[dt.b, nA.b], writes=[dA.b])
        for c in range(NT):
            sl = slice(c * 128, (c + 1) * 128)
            k.op("dve", lambda e, sl=sl: e.tensor_tensor_scan(out=pre.t[:, sl], data0=dA.t[:, sl], data1=dA.t[:, sl], initial=0.0, op0=ALU.add, op1=ALU.bypass),
                 reads=[dA.b], writes=[pre.b])
        for c in range(NT):
            sl = slice(c * 128, (c + 1) * 128)
            k.op("dve", lambda e, sl=sl, c=c: e.tensor_scalar(out=suf.t[:, sl], in0=pre.t[:, sl], scalar1=-1.0, scalar2=pre.t[:, c * 128 + 127:c * 128 + 128],
                                                             op0=ALU.mult, op1=ALU.add), reads=[pre.b], writes=[suf.b])
        k.op("dve", lambda e: e.tensor_tensor(out=suf.t[:], in0=suf.t[:], in1=dA.t[:], op=ALU.add), reads=[suf.b, dA.b], writes=[suf.b])
        ab = self.P.buf("ABd")
        tb = self.P.buf("TOTd")
        kb = self.P.buf("TOKd")
        ABv = self.ABd.rearrange("c (d h l) -> d h c l", d=2, h=SSD_H)
        k.dma("sp", ABv[0], pre.t[0:24, :].rearrange("h (c l) -> h c l", l=128), [pre.b], [ab])
        k.dma("sp", ABv[1], suf.t[24:48, :].rearrange("h (c l) -> h c l", l=128), [suf.b], [ab])
        k.dma("sp", self.TOTd.rearrange("o (c h) -> h (o c)", h=48), pre.t[:, 127::128], [pre.b], [tb], allow_slow_non_contiguous=True)
        srcs = [dt, pre, suf, dA]
        for c in range(NT):
            t_ = tk[c % 2]
            for qi, src in enumerate(srcs):
                k.op("pe", lambda e, qi=qi, src=src, c=c: e.transpose(out=ptk.t[:, qi, :], in_=src.t[:, c * 128:(c + 1) * 128], identity=self.identf.t[0:48, 0:48]),
                     reads=[src.b, self.identf.b], writes=[ptk.b])
            k.op("dve", lambda e, t_=t_: e.tensor_copy(out=t_.t[:], in_=ptk.t[:]), reads=[ptk.b], writes=[t_.b])
            k.dma("sp", self.TOKd[c * 128:(c + 1) * 128, :], t_.t[:].rearrange("p a h -> p (a h)"), [t_.b], [kb])

    def ssd_scan(self, s, j):
        k = self
        H, Pd = SSD_H, SSD_P
        Dful = s.sb("sc_D", [128, D_SSD], F32)
        d24 = s.sb("sc_d24", [128, H], F32)
        gn = s.sb("sc_gn", [128, D_SSD], F32)
        CD = s.sb("sc_CD", [128, NT, 48], F32)
        k.bcast_load("sp", d24, self.I["ssd_d"][j:j + 1, :])
        k.bcast_load("sp", gn, self.I["ssd_gate_norm_g"][j:j + 1, :])
        k.bcast_load("sp", CD_flat := Tl(CD.t, CD.b), self.TOTd[0:1, :]) if False else k.dma("sp", CD.t[:].rearrange("p c h -> p (c h)"), self.TOTd[0:1, :].partition_broadcast(128), [], [CD.b])
        k.op("act", lambda e: e.activation(out=CD.t[:], in_=CD.t[:], func=AF.Exp), reads=[CD.b], writes=[CD.b])
        k.op("dve", lambda e: e.tensor_copy(out=Dful.t[:].rearrange("p (h q) -> p h q", h=H), in_=d24.t[:].unsqueeze(2).to_broadcast([128, H, Pd])), reads=[d24.b], writes=[Dful.b])
        xs = [s.sb("sc_xs", [128, 2048], BF16) for _ in range(2)]
        tok = [s.sb("sc_tok", [128, 4, 48], F32) for _ in range(2)]
        Hs = s.sb("sc_H", [128, D_SSD], F32)
        Hbf = [s.sb("sc_Hbf", [128, D_SSD], BF16) for _ in range(2)]
        w24 = [s.sb("sc_w24", [128, H], F32) for _ in range(2)]
        xdd = [s.sb("sc_xdd", [128, D_SSD], BF16) for _ in range(2)]
        psS = [s.ps("sc_psS", [128, 512])] * 2
        gb = self.P.buf("Gd")
        xview = lambda t_: t_.t[:, 0:D_SSD].rearrange("p (h q) -> p h q", h=H)

        def load_chunk(c, bi):
            k.dma("sp", xs[bi].t[:], self.xsBd[c * 128:(c + 1) * 128, :], [], [xs[bi].b])
            k.dma("sp", tok[bi].t[:].rearrange("p a h -> p (a h)"), self.TOKd[c * 128:(c + 1) * 128, :], [], [tok[bi].b])

        def states(c, bi, d, Hacc, psl):
            ho = 24 * d
            x_, t_, w_, xd = xs[bi], tok[bi], w24[bi], xdd[bi]
            src = 2 if d == 0 else 1
            k.op("dve", lambda e: e.tensor_tensor(out=w_.t[:], in0=t_.t[:, src, ho:ho + 24], in1=t_.t[:, 3, ho:ho + 24], op=ALU.subtract), reads=[t_.b], writes=[w_.b])
            k.op("act", lambda e: e.activation(out=w_.t[:], in_=w_.t[:], func=AF.Exp), reads=[w_.b], writes=[w_.b])
            k.op("dve", lambda e: e.tensor_tensor(out=w_.t[:], in0=w_.t[:], in1=t_.t[:, 0, ho:ho + 24], op=ALU.mult), reads=[w_.b, t_.b], writes=[w_.b])
            k.op("pool", lambda e: e.tensor_tensor(out=xd.t[:].rearrange("p (h q) -> p h q", h=H), in0=xview(x_), in1=w_.t[:].unsqueeze(2).to_broadcast([128, H, Pd]), op=ALU.mult),
                 reads=[x_.b, w_.b], writes=[xd.b])
            for g in range(SSD_G):
                ps_ = psl[g % 2]
                k.op("pe", lambda e, g=g, ps_=ps_: e.matmul(ps_.t[:, 0:384], lhsT=x_.t[:, D_SSD + g * 128:D_SSD + (g + 1) * 128], rhs=xd.t[:, g * 384:(g + 1) * 384], start=True, stop=True),
                     reads=[x_.b, xd.b], writes=[ps_.b])
                hv = Hacc.t[:, g * 384:(g + 1) * 384]
                k.op("dve", lambda e, g=g, hv=hv: e.tensor_tensor(out=hv.rearrange("p (h q) -> p h q", h=6), in0=hv.rearrange("p (h q) -> p h q", h=6),
                                                                 in1=CD.t[:, c, ho + g * 6:ho + g * 6 + 6].unsqueeze(2).to_broadcast([128, 6, Pd]), op=ALU.mult),
                     reads=[Hacc.b, CD.b], writes=[Hacc.b])
                k.op("dve", lambda e, g=g, hv=hv, ps_=ps_: e.tensor_tensor(out=hv, in0=hv, in1=ps_.t[:, 0:384], op=ALU.add), reads=[Hacc.b, ps_.b], writes=[Hacc.b])

        k.op("pool", lambda e: e.memset(Hs.t[:], 0.0), writes=[Hs.b])
        load_chunk(NT - 1, (NT - 1) % 2)
        for c in range(NT - 1, -1, -1):
            bi = c % 2
            if c > 0:
                load_chunk(c - 1, (c - 1) % 2)
            hb_ = Hbf[bi]
            k.op("act", lambda e, hb_=hb_: e.activation(out=hb_.t[:], in_=Hs.t[:], func=AF.Copy), reads=[Hs.b], writes=[hb_.b])
            k.dma("sp", self.Gd[c], hb_.t[:], [hb_.b], [gb])
            if c > 0:
                states(c, bi, 1, Hs, psS)
        self.P.flush()
        bct = [s.sb("sc_bct", [128, 8, 128], BF16) for _ in range(2)]
        bc = [s.sb("sc_bc", [128, 2, H, 128], F32) for _ in range(2)]
        Gc = [s.sb("sc_G", [128, D_SSD], BF16) for _ in range(2)]
        sz = [s.sb("sc_sz", [128, D_SSD], BF16) for _ in range(2)]
        eA = [s.sb("sc_eA", [128, 48], F32) for _ in range(2)]
        ntk = [s.sb("sc_ntk", [128, 2, 48], F32) for _ in range(2)]
        xdt = [[s.sb("sc_xdt", [128, D_SSD], BF16) for _ in range(2)] for _ in range(2)]
        mcb = [[s.sb("sc_mcb", [128, 4, 128], F32) for _ in range(2)] for _ in range(2)]
        seg = [[s.sb("sc_seg", [128, 6, 128], F32) for _ in range(2)] for _ in range(2)]
        MT = [[s.sb("sc_MT", [128, 6, 128], BF16) for _ in range(2)] for _ in range(2)]
        t1 = [s.sb("sc_t1", [128, 384], F32) for _ in range(2)]
        t2 = [s.sb("sc_t2", [128, 384], F32) for _ in range(2)]
        yall = [s.sb("sc_yall", [128, D_SSD], F32) for _ in range(2)]
        junk = s.sb("sc_junk", [128, 384], BF16)
        ss = [s.sb("sc_ss", [128, 4], F32) for _ in range(2)]
        yn = [s.sb("sc_yn", [128, D_SSD], BF16) for _ in range(2)]
        psCB = s.ps("sc_psCB", [128, 4, 128])
        psY = [[s.ps("sc_psY", [128, 512]) for _ in range(3)] for _ in range(2)]
        yb_ = self.P.buf("ycat_ssd")

        def load2(c, bi):
            load_chunk(c, bi)
            k.dma("sp", bct[bi].t[:], self.BCTd.rearrange("g n t -> n g t")[:, :, c * 128:(c + 1) * 128], [], [bct[bi].b])
            k.dma("sp", bc[bi].t[:].rearrange("p d h l -> p (d h l)"), self.ABd[c:c + 1, :].partition_broadcast(128), [], [bc[bi].b])
            k.dma("sp", Gc[bi].t[:], self.Gd[c], [], [Gc[bi].b])
            k.dma("sp", sz[bi].t[:], self.szd[c * 128:(c + 1) * 128, :], [], [sz[bi].b])

        k.op("pool", lambda e: e.memset(Hs.t[:], 0.0), writes=[Hs.b])
        k.op("pool", lambda e: e.memset(Hbf[0].t[:], 0.0), writes=[Hbf[0].b])
        v3 = lambda ap: ap.rearrange("p (h q) -> p h q", h=6)

        def prep(c, g):
            bi, sgi = c % 2, g % 2
            bc_, ntk_ = bc[bi], ntk[bi]
            for d in range(2):
                sg, mt_ = seg[d][sgi], MT[d][sgi]
                for jh in range(6):
                    h = g * 6 + jh
                    k.op("act", lambda e: e.activation(out=sg.t[:, jh, :], in_=bc_.t[:, d, h, :], func=AF.Exp, bias=ntk_.t[:, d, 24 * d + h:24 * d + h + 1]),
                         reads=[bc_.b, ntk_.b], writes=[sg.b])
                k.op("dve", lambda e: e.scalar_tensor_tensor(out=mt_.t[:], in0=sg.t[:], scalar=1.0, in1=mcb[d][bi].t[:, g, :].unsqueeze(1).to_broadcast([128, 6, 128]),
                                                             op0=ALU.min, op1=ALU.mult), reads=[sg.b, mcb[d][bi].b], writes=[mt_.b])

        def head(c):
            bi = c % 2
            x_, t_, b_, eA_, ntk_ = xs[bi], tok[bi], bct[bi], eA[bi], ntk[bi]
            k.op("dve", lambda e: e.tensor_scalar(out=ntk_.t[:], in0=t_.t[:, 1:3, :], scalar1=-1.0, scalar2=None, op0=ALU.mult), reads=[t_.b], writes=[ntk_.b])
            k.op("act", lambda e: e.activation(out=eA_.t[:, 0:24], in_=t_.t[:, 1, 0:24], func=AF.Exp), reads=[t_.b], writes=[eA_.b])
            k.op("act", lambda e: e.activation(out=eA_.t[:, 24:48], in_=t_.t[:, 2, 24:48], func=AF.Exp), reads=[t_.b], writes=[eA_.b])
            for d in range(2):
                k.op("pool", lambda e: e.tensor_tensor(out=xdt[d][bi].t[:].rearrange("p (h q) -> p h q", h=H), in0=xview(x_),
                                                       in1=t_.t[:, 0, 24 * d:24 * d + 24].unsqueeze(2).to_broadcast([128, H, Pd]), op=ALU.mult),
                     reads=[x_.b, t_.b], writes=[xdt[d][bi].b])
            for g in range(SSD_G):
                k.op("pe", lambda e: e.matmul(psCB.t[:, g, :], lhsT=b_.t[:, g, :], rhs=b_.t[:, 4 + g, :], start=True, stop=True), reads=[b_.b], writes=[psCB.b])
            k.op("dve", lambda e: e.tensor_tensor(out=mcb[0][bi].t[:], in0=psCB.t[:], in1=self.maskf.t[:].unsqueeze(1).to_broadcast([128, 4, 128]), op=ALU.mult),
                 reads=[psCB.b, self.maskf.b], writes=[mcb[0][bi].b])
            k.op("dve", lambda e: e.tensor_tensor(out=mcb[1][bi].t[:], in0=psCB.t[:], in1=self.maskb.t[:].unsqueeze(1).to_broadcast([128, 4, 128]), op=ALU.mult),
                 reads=[psCB.b, self.maskb.b], writes=[mcb[1][bi].b])
            prep(c, 0)

        def body(c):
            bi = c % 2
            x_, b_, G_, eA_, ya, hf_ = xs[bi], bct[bi], Gc[bi], eA[bi], yall[bi], Hbf[bi]
            for g in range(SSD_G):
                pY, sgi = psY[g % 2], g % 2
                if g + 1 < SSD_G:
                    prep(c, g + 1)
                for jh in range(6):
                    h = g * 6 + jh
                    for d in range(2):
                        k.op("pe", lambda e: e.matmul(pY[0].t[:, jh * 64:(jh + 1) * 64], lhsT=MT[d][sgi].t[:, jh, :], rhs=xdt[d][bi].t[:, h * 64:(h + 1) * 64],
                                                      start=(d == 0), stop=(d == 1)), reads=[MT[d][sgi].b, xdt[d][bi].b], writes=[pY[0].b])
                k.op("pe", lambda e: e.matmul(pY[1].t[:, 0:384], lhsT=b_.t[:, 4 + g, :], rhs=hf_.t[:, g * 384:(g + 1) * 384], start=True, stop=True),
                     reads=[b_.b, hf_.b], writes=[pY[1].b])
                k.op("pe", lambda e: e.matmul(pY[2].t[:, 0:384], lhsT=b_.t[:, 4 + g, :], rhs=G_.t[:, g * 384:(g + 1) * 384], start=True, stop=True),
                     reads=[b_.b, G_.b], writes=[pY[2].b])
                a1, a2 = t1[sgi], t2[sgi]
                k.op("dve", lambda e: e.tensor_tensor(out=v3(a1.t[:]), in0=v3(pY[1].t[:, 0:384]), in1=eA_.t[:, g * 6:g * 6 + 6].unsqueeze(2).to_broadcast([128, 6, Pd]), op=ALU.mult),
                     reads=[pY[1].b, eA_.b], writes=[a1.b])
                k.op("dve", lambda e: e.tensor_tensor(out=v3(a2.t[:]), in0=v3(pY[2].t[:, 0:384]), in1=eA_.t[:, 24 + g * 6:24 + g * 6 + 6].unsqueeze(2).to_broadcast([128, 6, Pd]), op=ALU.mult),
                     reads=[pY[2].b, eA_.b], writes=[a2.b])
                k.op("pool", lambda e: e.tensor_tensor(out=a1.t[:], in0=a1.t[:], in1=a2.t[:], op=ALU.add), reads=[a1.b, a2.b], writes=[a1.b])
                k.op("dve", lambda e: e.tensor_tensor(out=a1.t[:], in0=pY[0].t[:, 0:384], in1=a1.t[:], op=ALU.add), reads=[pY[0].b, a1.b], writes=[a1.b])
                k.op("pool", lambda e: e.tensor_tensor(out=a2.t[:], in0=x_.t[:, g * 384:(g + 1) * 384], in1=Dful.t[:, g * 384:(g + 1) * 384], op=ALU.mult),
                     reads=[x_.b, Dful.b], writes=[a2.b])
                k.op("pool", lambda e: e.tensor_tensor(out=ya.t[:, g * 384:(g + 1) * 384], in0=a1.t[:], in1=a2.t[:], op=ALU.add), reads=[a1.b, a2.b], writes=[ya.b])

        def tail(c):
            bi = c % 2
            sz_, ya = sz[bi], yall[bi]
            if c + 1 < NT:
                states(c, bi, 0, Hs, psS)
                hn_ = Hbf[(c + 1) % 2]
                k.op("act", lambda e: e.activation(out=hn_.t[:], in_=Hs.t[:], func=AF.Copy), reads=[Hs.b], writes=[hn_.b])
            s_, yn_ = ss[bi], yn[bi]
            k.op("dve", lambda e: e.tensor_tensor(out=ya.t[:], in0=ya.t[:], in1=sz_.t[:], op=ALU.mult), reads=[ya.b, sz_.b], writes=[ya.b])
            for g in range(SSD_G):
                k.op("act", lambda e: e.activation(out=junk.t[:], in_=ya.t[:, g * 384:(g + 1) * 384], func=AF.Square, accum_out=s_.t[:, g:g + 1]), reads=[ya.b], writes=[junk.b, s_.b])
            k.op("act", lambda e: e.activation(out=s_.t[:], in_=s_.t[:], func=AF.Sqrt, scale=1.0 / 384, bias=self.epsc.t[:]), reads=[s_.b, self.epsc.b], writes=[s_.b])
            k.op("dve", lambda e: e.reciprocal(out=s_.t[:], in_=s_.t[:]), reads=[s_.b], writes=[s_.b])
            k.op("dve", lambda e: e.tensor_tensor(out=ya.t[:].rearrange("p (g q) -> p g q", g=4), in0=ya.t[:].rearrange("p (g q) -> p g q", g=4),
                                                  in1=s_.t[:].unsqueeze(2).to_broadcast([128, 4, 384]), op=ALU.mult), reads=[ya.b, s_.b], writes=[ya.b])
            k.op("pool", lambda e: e.tensor_tensor(out=yn_.t[:], in0=ya.t[:], in1=gn.t[:], op=ALU.mult), reads=[ya.b, gn.b], writes=[yn_.b])
            k.dma("sp", self.ycat[c * 128:(c + 1) * 128, 0:D_SSD], yn_.t[:], [yn_.b], [yb_])

        load2(0, 0)
        head(0)
        for c in range(NT):
            if c + 1 < NT:
                load2(c + 1, (c + 1) % 2)
            body(c)
            if c + 1 < NT:
                head(c + 1)
            tail(c)

    def na_layer(self, cs, li):
        j = li // 2
        Win = self.I["na_w_in"][j]
        with Scope(self) as AS:
            hT = AS.sb("hT", [128, ND, T], BF16)
            with Scope(self) as s:
                self.norm_to_hT(s, hT, self.I["norm_mix_g"][li:li + 1, :])
            for hg in range(6):
                with Scope(self) as s:
                    self.na_group(s, j, hg, Win, hT)
            with Scope(self) as s:
                self.xa_block(s, li, hT, Win, 4608)
        self.out_proj(self.I["na_w_out"][j])

    def na_group(self, s, j, hg, Win, hT):
        k = self
        HG = 2
        W = HG * 128
        QT = s.sb("na_QT", [128, HG, T], BF16)
        KT = s.sb("na_KT", [128, HG, T], BF16)
        Va = s.sb("na_Va", [128, NT, HG, 129], BF16)
        oall = s.sb("na_oall", [128, NT, W], BF16)
        pbig = s.ps("na_pbig", [128, 4, 512])
        pbs = [Tl(pbig.t[:, i, :], self.P.buf("na_pb")) for i in range(4)]
        k.op("pool", lambda e: e.memset(Va.t[:], 1.0), writes=[Va.b])
        for which, dst in ((0, QT), (1, KT)):
            w = self.wblock(s, Win, which * 1536 + hg * W, W)
            for et in range(HG):
                self.proj_fm(s, hT, w, W, pbs, et)
                for tc in range(4):
                    if tc % 2 == 0:
                        k.op("act", lambda e: e.activation(out=dst.t[:, et, tc * 512:(tc + 1) * 512], in_=pbs[tc].t, func=AF.Copy), reads=[pbs[tc].b], writes=[dst.b])
                    else:
                        k.op("dve", lambda e: e.tensor_copy(out=dst.t[:, et, tc * 512:(tc + 1) * 512], in_=pbs[tc].t), reads=[pbs[tc].b], writes=[dst.b])
        w = self.wblock(s, Win, 3072 + hg * W, W)
        for tt in range(NT):
            pb = pbs[tt % 4]
            self.proj_tm(hT, w, W, pb, tt)
            k.op("act", lambda e: e.activation(out=Va.t[:, tt, :, 0:128], in_=pb.t[:, 0:W].rearrange("p (h d) -> p h d", h=HG), func=AF.Copy), reads=[pb.b], writes=[Va.b])
        self.P.flush()
        bias = [s.sb("na_bias", [128, 25, 128], F32) for _ in range(2)]
        psS = [Tl(pbig.t[:, 2 * i:2 * i + 2, :].rearrange("p a n -> p (a n)")[:, 0:640].rearrange("p (j q) -> p j q", j=5), self.P.buf("na_psS")) for i in range(2)]
        psO = [s.ps("na_psO", [128, 256]) for _ in range(2)]
        tmp = [s.sb("na_tmp", [128, 5, 128], F32) for _ in range(2)]
        Pm = [s.sb("na_P", [128, 5, 128], BF16) for _ in range(2)]
        rc = [s.sb("na_rc", [128, 1], F32) for _ in range(2)]
        scale = 128.0 ** -0.5
        iters = [(hh, i) for hh in range(HG) for i in range(NT)]

        def geo(i):
            kb = min(max(2 * i - 4, 0), 22)
            case = 0 if i == 0 else 1 if i == 1 else 3 if i == 14 else 4 if i == 15 else 2
            return kb // 2, case

        def s_mm(n):
            hh, i = iters[n]
            kt0, _ = geo(i)
            pS = psS[n % 2]
            for jt in range(5):
                k.op("pe", lambda e: e.matmul(pS.t[:, jt, :], lhsT=KT.t[:, hh, (kt0 + jt) * 128:(kt0 + jt + 1) * 128], rhs=QT.t[:, hh, i * 128:(i + 1) * 128], start=True, stop=True),
                     reads=[KT.b, QT.b], writes=[pS.b])

        for hh in range(HG):
            k.dma("sp", bias[hh % 2].t[:], self.I["na_bias"][j, hg * HG + hh], [], [bias[hh % 2].b])
        s_mm(0)
        for n, (hh, i) in enumerate(iters):
            kt0, case = geo(i)
            bt = bias[hh % 2]
            pS, pO, tm, P_, r_ = psS[n % 2], psO[n % 2], tmp[n % 2], Pm[n % 2], rc[n % 2]
            if n + 1 < len(iters):
                s_mm(n + 1)
            k.op("dve", lambda e: e.scalar_tensor_tensor(out=tm.t[:], in0=pS.t, scalar=scale, in1=bt.t[:, case * 5:(case + 1) * 5, :], op0=ALU.mult, op1=ALU.add),
                 reads=[pS.b, bt.b], writes=[tm.b])
            k.op("act", lambda e: e.activation(out=P_.t[:], in_=tm.t[:], func=AF.Exp), reads=[tm.b], writes=[P_.b])
            for jt in range(5):
                k.op("pe", lambda e: e.matmul(pO.t[:, 0:129], lhsT=P_.t[:, jt, :], rhs=Va.t[:, kt0 + jt, hh, :], start=(jt == 0), stop=(jt == 4)),
                     reads=[P_.b, Va.b], writes=[pO.b])
            k.op("dve", lambda e: e.reciprocal(out=r_.t[:], in_=pO.t[:, 128:129]), reads=[pO.b], writes=[r_.b])
            k.op("act", lambda e: e.activation(out=oall.t[:, i, hh * 128:(hh + 1) * 128], in_=pO.t[:, 0:128], func=AF.Copy, scale=r_.t[:, 0:1]),
                 reads=[pO.b, r_.b], writes=[oall.b])
        yb = self.P.buf("ycat_na")
        k.dma("sp", self.ycat.rearrange("(n p) c -> p n c", p=128)[:, :, hg * W:(hg + 1) * W], oall.t[:], [oall.b], [yb])

    def moe_layer(self, cs, li):
        k = self
        with Scope(self) as MS:
            AFF = MS.sb("m_AFF", [128, NT, NE], F32)
            IDX = MS.sb("m_IDX", [128, 2 * NE], I32)
            GATE = MS.sb("m_GATE", [128, 2 * NE], F32)
            ring = [MS.sb("e_ring", [128, 8192], BF16) for _ in range(6)]
            for ci in range(6):
                self.exp_wload(ring, li, 0, ci)
            with Scope(self) as s:
                self.moe_router(s, li, AFF)
            with Scope(self) as s:
                self.moe_topk(s, AFF, IDX, GATE)
            with Scope(self) as s:
                self.moe_experts(s, li, IDX, GATE, ring)

    def exp_wload(self, ring, li, ex, ci):
        r_ = ring[ci]
        if ci < 4:
            wname, fh = ("moe_w1", "moe_w3")[ci % 2], ci // 2
            self.dma("pool", r_.t[:].rearrange("p (k n) -> p k n", k=ND), self.I[wname][li, ex][:, fh * 512:(fh + 1) * 512].rearrange("(k p) n -> p k n", p=128), [], [r_.b])
        else:
            dh = ci - 4
            self.dma("pool", r_.t[:].rearrange("p (k n) -> p k n", k=8), self.I["moe_w2"][li, ex][:, dh * 1024:(dh + 1) * 1024].rearrange("(k p) n -> p k n", p=128), [], [r_.b])

    def moe_router(self, s, li, AFF):
        k = self
        g = s.sb("g", [128, D], F32)
        k.bcast_load("sp", g, self.I["norm_ffn_g"][li:li + 1, :])
        Wr = s.sb("m_Wr", [128, ND, NE], F32)
        k.dma("sp", Wr.t[:], self.I["moe_w_router"][li].rearrange("(k p) e -> p k e", p=128), [], [Wr.b])
        hf = [s.sb("m_hf", [128, D], F32) for _ in range(2)]
        hTf = [s.sb("m_hTf", [128, ND, 128], F32) for _ in range(2)]
        ptf = [s.ps("m_ptf", [128, 4, 128], F32) for _ in range(4)]
        pl = [s.ps("m_pl", [128, NE], F32) for _ in range(2)]
        sm = [s.sb("m_sm", [128, 4], F32) for _ in range(2)]
        ex = [s.sb("m_ex", [128, NE], F32) for _ in range(2)]
        hb = self.P.buf("hn")
        def front(tt):
            self.norm_tile(s, self.xres[tt * 128:(tt + 1) * 128, :], g, want_f32=hf[tt % 2], hn_dst=(self.hn[tt * 128:(tt + 1) * 128, :], hb))

        def tail(tt):
            h_, hT_, pl_, sm_, ex_ = hf[tt % 2], hTf[tt % 2], pl[tt % 2], sm[tt % 2], ex[tt % 2]
            for q4 in range(4):
                p_ = ptf[q4]
                for jj in range(4):
                    kk = q4 * 4 + jj
                    k.op("pe", lambda e: e.transpose(out=p_.t[:, jj, :], in_=h_.t[:, kk * 128:(kk + 1) * 128], identity=self.identf.t[:]),
                         reads=[h_.b, self.identf.b], writes=[p_.b])
                if q4 % 2 == 0:
                    k.op("dve", lambda e: e.tensor_copy(out=hT_.t[:, q4 * 4:(q4 + 1) * 4, :], in_=p_.t[:]), reads=[p_.b], writes=[hT_.b])
                else:
                    k.op("act", lambda e: e.activation(out=hT_.t[:, q4 * 4:(q4 + 1) * 4, :], in_=p_.t[:], func=AF.Copy), reads=[p_.b], writes=[hT_.b])
            for kk in range(ND):
                k.op("pe", lambda e: e.matmul(pl_.t[:], lhsT=hT_.t[:, kk, :], rhs=Wr.t[:, kk, :], start=(kk == 0), stop=(kk == ND - 1)),
                     reads=[hT_.b, Wr.b], writes=[pl_.b])
            k.op("dve", lambda e: e.reduce_max(out=sm_.t[:, 0:1], in_=pl_.t[:], axis=AX.X), reads=[pl_.b], writes=[sm_.b])
            k.op("dve", lambda e: e.tensor_scalar(out=sm_.t[:, 1:2], in0=sm_.t[:, 0:1], scalar1=-1.0, scalar2=None, op0=ALU.mult), reads=[sm_.b], writes=[sm_.b])
            k.op("act", lambda e: e.activation(out=ex_.t[:], in_=pl_.t[:], func=AF.Exp, bias=sm_.t[:, 1:2], accum_out=sm_.t[:, 2:3]),
                 reads=[pl_.b, sm_.b], writes=[ex_.b, sm_.b])
            k.op("dve", lambda e: e.reciprocal(out=sm_.t[:, 3:4], in_=sm_.t[:, 2:3]), reads=[sm_.b], writes=[sm_.b])
            k.op("dve", lambda e: e.tensor_scalar(out=AFF.t[:, tt, :], in0=ex_.t[:], scalar1=sm_.t[:, 3:4], scalar2=None, op0=ALU.mult),
                 reads=[ex_.b, sm_.b], writes=[AFF.b])

        front(0)
        for tt in range(NT):
            if tt + 1 < NT:
                front(tt + 1)
            tail(tt)

    def moe_topk(self, s, AFF, IDX, GATE):
        k = self
        affT = s.sb("k_affT", [NE, T], F32)
        work = s.sb("k_work", [NE, T], F32)
        mx8 = s.sb("k_mx8", [NE, 8], F32)
        maskT = s.sb("k_mask", [NE, T], F32)
        pos = s.sb("k_pos", [NE, T], F32)
        POSM = s.sb("k_POSM", [128, NT, NE], F32)
        pA = [s.ps("k_pA", [128, 512]) for _ in range(4)]
        pP = s.ps("k_pP", [128, NT, NE], F32)
        for tt in range(NT):
            k.op("pe", lambda e, tt=tt: e.transpose(out=pA[tt // 4].t[0:NE, (tt % 4) * 128:(tt % 4 + 1) * 128], in_=AFF.t[:, tt, :], identity=self.identf.t[:]),
                 reads=[AFF.b, self.identf.b], writes=[pA[tt // 4].b])
        for q4 in range(4):
            k.op("dve", lambda e, q4=q4: e.tensor_copy(out=affT.t[:, q4 * 512:(q4 + 1) * 512], in_=pA[q4].t[0:NE, :]), reads=[pA[q4].b], writes=[affT.b])
        src = affT
        for it in range(CAP // 8):
            k.op("dve", lambda e, src=src: e.max(out=mx8.t[:], in_=src.t[:]), reads=[src.b], writes=[mx8.b])
            if it < CAP // 8 - 1:
                k.op("dve", lambda e, src=src: e.match_replace(out=work.t[:], in_to_replace=mx8.t[:], in_values=src.t[:], imm_value=-1.0), reads=[src.b, mx8.b], writes=[work.b])
                src = work
        k.op("dve", lambda e: e.tensor_scalar(out=maskT.t[:], in0=affT.t[:], scalar1=mx8.t[:, 7:8], scalar2=None, op0=ALU.is_ge), reads=[affT.b, mx8.b], writes=[maskT.b])
        k.op("dve", lambda e: e.tensor_tensor_scan(out=pos.t[:], data0=maskT.t[:], data1=maskT.t[:], initial=0.0, op0=ALU.add, op1=ALU.bypass), reads=[maskT.b], writes=[pos.b])
        k.op("dve", lambda e: e.tensor_tensor(out=pos.t[:], in0=pos.t[:], in1=maskT.t[:], op=ALU.mult), reads=[pos.b, maskT.b], writes=[pos.b])
        k.op("dve", lambda e: e.tensor_scalar(out=pos.t[:], in0=pos.t[:], scalar1=-1.0, scalar2=None, op0=ALU.add), reads=[pos.b], writes=[pos.b])
        for tt in range(NT):
            k.op("pe", lambda e, tt=tt: e.transpose(out=pP.t[:, tt, :], in_=pos.t[:, tt * 128:(tt + 1) * 128], identity=self.identf.t[0:NE, 0:NE]),
                 reads=[pos.b, self.identf.b], writes=[pP.b])
        k.op("dve", lambda e: e.tensor_copy(out=POSM.t[:], in_=pP.t[:]), reads=[pP.b], writes=[POSM.b])
        RV = s.sb("k_RV", [128, NT, NE, 5], BF16)
        r1 = s.sb("k_r1", [128, NT, NE], F32)
        gh = s.sb("k_gh", [128, NT, NE], BF16)
        pcol = s.sb("k_pcol", [128, 1], F32)
        k.op("pool", lambda e: e.iota(pcol.t[:], pattern=[[0, 1]], base=0, channel_multiplier=1, allow_small_or_imprecise_dtypes=True), writes=[pcol.b])
        for tt in range(NT):
            k.op("pool", lambda e, tt=tt: e.memset(RV.t[:, tt, :, 0], float(tt)), writes=[RV.b])
        k.op("dve", lambda e: e.tensor_copy(out=RV.t[:, :, :, 1].rearrange("p a b -> p (a b)"), in_=pcol.t[:].to_broadcast([128, NT * NE])), reads=[pcol.b], writes=[RV.b])
        k.op("dve", lambda e: e.tensor_copy(out=gh.t[:], in_=AFF.t[:]), reads=[AFF.b], writes=[gh.b])
        k.op("dve", lambda e: e.tensor_copy(out=RV.t[:, :, :, 2], in_=gh.t[:]), reads=[gh.b], writes=[RV.b])
        k.op("dve", lambda e: e.tensor_tensor(out=r1.t[:], in0=AFF.t[:], in1=gh.t[:], op=ALU.subtract), reads=[AFF.b, gh.b], writes=[r1.b])
        k.op("dve", lambda e: e.tensor_copy(out=gh.t[:], in_=r1.t[:]), reads=[r1.b], writes=[gh.b])
        k.op("dve", lambda e: e.tensor_copy(out=RV.t[:, :, :, 3], in_=gh.t[:]), reads=[gh.b], writes=[RV.b])
        k.op("dve", lambda e: e.tensor_tensor(out=r1.t[:], in0=r1.t[:], in1=gh.t[:], op=ALU.subtract), reads=[r1.b, gh.b], writes=[r1.b])
        k.op("dve", lambda e: e.tensor_copy(out=RV.t[:, :, :, 4], in_=r1.t[:]), reads=[r1.b], writes=[RV.b])
        O = [s.sb("k_O", [128, NT, CAP], BF16) for _ in range(2)]
        pz = [s.ps("k_pz", [128, 8], F32) for _ in range(2)]
        idf = s.sb("k_idf", [128, 2 * NE], F32)
        pzs = [s.sb("k_pzs", [128, 8], F32) for _ in range(2)]
        zi = 0
        for ex in range(NE):
            O_ = O[ex % 2]
            for tt in range(NT):
                k.op("dve", lambda e, tt=tt, ex=ex, O_=O_: e.tensor_scalar(out=O_.t[:, tt, :], in0=self.iota256.t[:], scalar1=POSM.t[:, tt, ex:ex + 1], scalar2=None, op0=ALU.is_equal),
                     reads=[self.iota256.b, POSM.b], writes=[O_.b])
            for half in range(2):
                pz_ = pz[zi % 2]
                zi += 1
                col = ex * 2 + half
                for tt in range(NT):
                    k.op("pe", lambda e, tt=tt, ex=ex, half=half, O_=O_, pz_=pz_: e.matmul(pz_.t[:, 0:5], lhsT=O_.t[:, tt, half * 128:(half + 1) * 128], rhs=RV.t[:, tt, ex, :], start=(tt == 0), stop=(tt == NT - 1)),
                         reads=[O_.b, RV.b], writes=[pz_.b])
                zs_ = pzs[zi % 2]
                k.op("dve", lambda e, pz_=pz_, zs_=zs_: e.tensor_copy(out=zs_.t[:, 0:5], in_=pz_.t[:, 0:5]), reads=[pz_.b], writes=[zs_.b])
                k.op("dve", lambda e, col=col, zs_=zs_: e.scalar_tensor_tensor(out=idf.t[:, col:col + 1], in0=zs_.t[:, 0:1], scalar=128.0, in1=zs_.t[:, 1:2], op0=ALU.mult, op1=ALU.add),
                     reads=[zs_.b], writes=[idf.b])
                k.op("dve", lambda e, col=col, zs_=zs_: e.tensor_reduce(out=GATE.t[:, col:col + 1], in_=zs_.t[:, 2:5], axis=AX.X, op=ALU.add), reads=[zs_.b], writes=[GATE.b])
        k.op("dve", lambda e: e.tensor_copy(out=IDX.t[:], in_=idf.t[:]), reads=[idf.b], writes=[IDX.b])

    def moe_experts(self, s, li, IDX, GATE, ring):
        k = self
        xg = [s.sb("e_xg", [128, D], BF16) for _ in range(4)]
        xsT = [s.sb("e_xsT", [128, ND, CAP], BF16) for _ in range(2)]
        gT = [s.sb("e_gT", [128, 8, CAP], BF16) for _ in range(2)]
        sa = [s.sb("e_sa", [128, 512], F32) for _ in range(2)]
        gtok = [s.sb("e_gtok", [128, 512], BF16) for _ in range(2)]
        ysb = [s.sb("e_y", [128, D], F32) for _ in range(4)]
        ptr = [s.ps("e_ptr", [128, 8, 128], BF16) for _ in range(2)]
        pab = [s.ps("e_pab", [128, 512]) for _ in range(4)]
        py = [s.ps("e_py", [128, 512]) for _ in range(2)]
        xb = [self.P.buf("xres_moe") for _ in range(4)]
        hnb = self.P.buf("hn_r")
        cnt = dict(ti=0, ai=0, yi=0)
        for cq in range(4):
            k.dma("sp", self.moeh[cq][:, :], self.xres[:, cq * 512:(cq + 1) * 512], [], [xb[cq]])

        def gathers(ex):
            for half in range(2):
                col = ex * 2 + half
                x_ = xg[col % 4]
                k.op("pool", lambda e: e.indirect_dma_start(out=x_.t[:, :], out_offset=None, in_=self.hn[:, :],
                                                            in_offset=bass.IndirectOffsetOnAxis(ap=IDX.t[:, col:col + 1], axis=0)),
                     reads=[hnb, IDX.b], writes=[x_.b], dma=True)

        def wload(ex, ci):
            self.exp_wload(ring, li, ex, ci)

        gathers(0)
        for ex in range(NE):
            xT, g_ = xsT[ex % 2], gT[ex % 2]
            nxt = ex + 1 < NE
            if nxt:
                gathers(ex + 1)
            for half in range(2):
                x_ = xg[(ex * 2 + half) % 4]
                for kg in range(2):
                    p_ = ptr[cnt["ti"] % 2]
                    cnt["ti"] += 1
                    for jj in range(8):
                        kk = kg * 8 + jj
                        k.op("pe", lambda e: e.transpose(out=p_.t[:, jj, :], in_=x_.t[:, kk * 128:(kk + 1) * 128], identity=self.ident.t[:]),
                             reads=[x_.b, self.ident.b], writes=[p_.b])
                    if kg == 0:
                        k.op("dve", lambda e: e.tensor_copy(out=xT.t[:, kg * 8:(kg + 1) * 8, half * 128:(half + 1) * 128], in_=p_.t[:]), reads=[p_.b], writes=[xT.b])
                    else:
                        k.op("act", lambda e: e.activation(out=xT.t[:, kg * 8:(kg + 1) * 8, half * 128:(half + 1) * 128], in_=p_.t[:], func=AF.Copy), reads=[p_.b], writes=[xT.b])
            for fh in range(2):
                w1, w3 = ring[2 * fh], ring[2 * fh + 1]
                rv1 = w1.t[:].rearrange("p (k n) -> p k n", k=ND)
                rv3 = w3.t[:].rearrange("p (k n) -> p k n", k=ND)
                for half in range(2):
                    pa, pb = pab[cnt["ai"] % 4], pab[(cnt["ai"] + 1) % 4]
                    cnt["ai"] += 2
                    for kk in range(ND):
                        k.op("pe", lambda e: e.matmul(pa.t[:], lhsT=xT.t[:, kk, half * 128:(half + 1) * 128], rhs=rv1[:, kk, :], start=(kk == 0), stop=(kk == ND - 1)),
                             reads=[w1.b, xT.b], writes=[pa.b])
                        k.op("pe", lambda e: e.matmul(pb.t[:], lhsT=xT.t[:, kk, half * 128:(half + 1) * 128], rhs=rv3[:, kk, :], start=(kk == 0), stop=(kk == ND - 1)),
                             reads=[w3.b, xT.b], writes=[pb.b])
                    s_, gk = sa[cnt["ai"] // 2 % 2], gtok[cnt["ai"] // 2 % 2]
                    k.op("act", lambda e: e.activation(out=s_.t[:], in_=pa.t[:], func=AF.Silu), reads=[pa.b], writes=[s_.b])
                    k.op("dve", lambda e: e.tensor_tensor(out=gk.t[:], in0=s_.t[:], in1=pb.t[:], op=ALU.mult), reads=[s_.b, pb.b], writes=[gk.b])
                    p_ = ptr[cnt["ti"] % 2]
                    cnt["ti"] += 1
                    for jj in range(4):
                        k.op("pe", lambda e: e.transpose(out=p_.t[:, jj, :], in_=gk.t[:, jj * 128:(jj + 1) * 128], identity=self.ident.t[:]),
                             reads=[gk.b, self.ident.b], writes=[p_.b])
                    k.op("act", lambda e: e.activation(out=g_.t[:, fh * 4:(fh + 1) * 4, half * 128:(half + 1) * 128], in_=p_.t[:, 0:4, :], func=AF.Copy),
                         reads=[p_.b], writes=[g_.b])
                if nxt:
                    wload(ex + 1, 2 * fh)
                    wload(ex + 1, 2 * fh + 1)
            ys = [ysb[(ex * 2) % 4], ysb[(ex * 2 + 1) % 4]]
            for dh in range(2):
                r_ = ring[4 + dh]
                rv = r_.t[:].rearrange("p (k n) -> p k n", k=8)
                for half in range(2):
                    col = ex * 2 + half
                    for dc in range(2):
                        p_ = py[cnt["yi"] % 2]
                        cnt["yi"] += 1
                        for ft in range(8):
                            k.op("pe", lambda e: e.matmul(p_.t[:], lhsT=g_.t[:, ft, half * 128:(half + 1) * 128], rhs=rv[:, ft, dc * 512:(dc + 1) * 512], start=(ft == 0), stop=(ft == 7)),
                                 reads=[g_.b, r_.b], writes=[p_.b])
                        o0 = dh * 1024 + dc * 512
                        y_ = ys[half]
                        if dc == 0:
                            k.op("act", lambda e: e.activation(out=y_.t[:, o0:o0 + 512], in_=p_.t[:], func=AF.Copy, scale=GATE.t[:, col:col + 1]),
                                 reads=[p_.b, GATE.b], writes=[y_.b])
                        else:
                            k.op("dve", lambda e: e.tensor_scalar(out=y_.t[:, o0:o0 + 512], in0=p_.t[:], scalar1=GATE.t[:, col:col + 1], scalar2=None, op0=ALU.mult),
                                 reads=[p_.b, GATE.b], writes=[y_.b])
                if nxt:
                    wload(ex + 1, 4 + dh)
            for half in range(2):
                col = ex * 2 + half
                y_ = ys[half]
                for cq in range(4):
                    k.op("pool", lambda e: e.indirect_dma_start(out=self.moeh[cq][:, :], out_offset=bass.IndirectOffsetOnAxis(ap=IDX.t[:, col:col + 1], axis=0),
                                                                in_=y_.t[:, cq * 512:(cq + 1) * 512], in_offset=None, compute_op=ALU.add),
                         reads=[y_.b, IDX.b, xb[cq]], writes=[xb[cq]], dma=True)
        for cq in range(4):
            k.dma("sp", self.xres[:, cq * 512:(cq + 1) * 512], self.moeh[cq][:, :], [xb[cq]], [xb[cq]])


def _na_bias_tiles(rpb):
    n, Hh = rpb.shape[0], rpb.shape[1]
    out = np.full((n, Hh, 128, 25, 128), NEG, dtype=np.float32)
    a = np.arange(2)[:, None]; wk = np.arange(64)[None, :]
    for case, i in enumerate((0, 1, 2, 14, 15)):
        kb = min(max(2 * i - 4, 0), 22)
        for jt in range(5):
            for aa in range(2):
                kr = kb + 2 * jt + aa
                for bb in range(2):
                    r = 2 * i + bb
                    rs = min(max(r - 4, 0), 24)
                    if not (rs <= kr < rs + 8):
                        continue
                    dr = kr - r + 7
                    wq = np.arange(64)
                    ws = np.clip(wq - 8, 0, 48)
                    wkk = np.arange(64)[:, None]
                    valid = (wkk >= ws[None, :]) & (wkk < ws[None, :] + 16)
                    dc = np.clip(wkk - wq[None, :] + 15, 0, 30)
                    vals = rpb[:, :, dr, :][:, :, dc]
                    blk = out[:, :, aa * 64:(aa + 1) * 64, case * 5 + jt, bb * 64:(bb + 1) * 64]
                    blk[...] = np.where(valid[None, None], vals, NEG)
    return out


def _prep(inputs, nlayers=DEPTH):
    n_ssd = (nlayers + 1) // 2
    n_na = nlayers // 2
    f = lambda a: np.ascontiguousarray(a, dtype=np.float32)
    common = {
        "norm_mix_g": f(inputs["norm_mix_g"][:nlayers]), "norm_ffn_g": f(inputs["norm_ffn_g"][:nlayers]),
        "norm_final_g": f(inputs["norm_final_g"]).reshape(1, D), "mem_norm_g": f(inputs["mem_norm_g"]).reshape(1, D),
        "ssd_w_in": f(inputs["ssd_w_in"][:n_ssd]), "ssd_conv_w": f(inputs["ssd_conv_w"][:n_ssd]), "ssd_conv_b": f(inputs["ssd_conv_b"][:n_ssd]),
        "ssd_dt_bias": f(inputs["ssd_dt_bias"][:n_ssd]).reshape(n_ssd, 48), "ssd_a_log": f(inputs["ssd_a_log"][:n_ssd]).reshape(n_ssd, 48),
        "ssd_d": f(inputs["ssd_d"][:n_ssd]), "ssd_gate_norm_g": f(inputs["ssd_gate_norm_g"][:n_ssd]), "ssd_w_out": f(inputs["ssd_w_out"][:n_ssd]),
        "xa_w_kv": f(inputs["xa_w_kv"][:nlayers]), "moe_w_router": f(inputs["moe_w_router"][:nlayers]),
        "moe_w1": f(inputs["moe_w1"][:nlayers]), "moe_w3": f(inputs["moe_w3"][:nlayers]), "moe_w2": f(inputs["moe_w2"][:nlayers]),
    }
    if n_na:
        common["na_w_in"] = f(inputs["na_w_in"][:n_na])
        common["na_bias"] = _na_bias_tiles(f(inputs["na_rpb"][:n_na]))
        common["na_w_out"] = f(inputs["na_w_out"][:n_na])
    return common


_NC_CACHE = {}


def kernel(**inputs):
    x = np.asarray(inputs["x"], dtype=np.float32)
    mem = np.asarray(inputs["mem"], dtype=np.float32)
    common = _prep(inputs)
    if "nc" not in _NC_CACHE:
        _NC_CACHE["nc"] = KB().build()
    nc = _NC_CACHE["nc"]
    B = x.shape[0]
    in_maps = []
    for c in range(8):
        b = c % B
        m = dict(common)
        m["x"] = np.ascontiguousarray(x[b])
        m["mem"] = np.ascontiguousarray(mem[b])
        in_maps.append(m)
    res = run_bass_kernel_spmd(nc, in_maps, core_ids=list(range(8)))
    return np.stack([np.asarray(res.results[b]["out"], dtype=np.float32) for b in range(B)], axis=0)
```

```python
import contextlib
import numpy as np
import concourse.bass as bass
import concourse.mybir as mybir
from concourse.bass_utils import run_bass_kernel_spmd

F32 = mybir.dt.float32
BF16 = mybir.dt.bfloat16
I32 = mybir.dt.int32
AF = mybir.ActivationFunctionType
ALU = mybir.AluOpType
AX = mybir.AxisListType

T = 2048
D = 2048
NT = 16
ND = 16
DEPTH = 4
MEM = 256
EPS = 1e-6
D_XA = 512
D_SSD = 1536
SSD_H = 24
SSD_P = 64
SSD_G = 4
SSD_N = 128
SSD_CONV_DIM = 2560
SSD_IN = 4656
NA_H = 12
NA_IN = 5120
NE = 16
CAP = 256
DFF = 1024
NEG = -30000.0

ENGS = ("pe", "act", "dve", "pool", "sp")
ENGOBJ = {"pe": "tensor", "act": "scalar", "dve": "vector", "pool": "gpsimd", "sp": "sync"}
SEM_BLOCK = 30000


class Buf:
    __slots__ = ("name", "last_w", "readers", "sem", "base", "ndma")

    def __init__(self, name):
        self.name = name
        self.last_w = None
        self.readers = []
        self.sem = None
        self.base = 0
        self.ndma = 0


class Op:
    __slots__ = ("eng", "fn", "is_dma", "deps", "need_sig", "sig", "dst", "k", "qi")


class _Rec:
    def __init__(self):
        self.call = None

    def __getattr__(self, name):
        def f(*a, **kw):
            self.call = (name, a, kw)
            return self
        return f


class Prog:
    def __init__(self, nc, stack):
        self.nc = nc
        self.stack = stack
        self.q = {e: [] for e in ENGS}
        self.bufs = []
        self.eng_cnt = {e: 0 for e in ENGS}
        self.eng_sems = {}
        self.dma_pool = []
        self.n_sems = 0
        self.n_ops = 0

    def buf(self, name="b"):
        b = Buf(name)
        self.bufs.append(b)
        return b

    def _newsem(self, name):
        self.n_sems += 1
        return self.stack.enter_context(self.nc.semaphore(f"{name}_{self.n_sems}"))

    def op(self, eng, fn, reads=(), writes=(), dma=False, extra=()):
        o = Op()
        rec = _Rec()
        fn(rec)
        assert rec.call is not None
        o.eng, o.fn, o.is_dma = eng, rec.call, dma
        o.need_sig, o.sig, o.dst, o.k = False, None, None, 0
        deps, seen = [], set()
        for r in reads:
            if r.last_w is not None:
                deps.append(r.last_w)
        for w in writes:
            if w.last_w is not None:
                deps.append(w.last_w)
            deps.extend(w.readers)
        deps.extend(extra)
        dd = []
        last = {}
        for d in deps:
            if id(d) in seen or d is o:
                continue
            seen.add(id(d))
            if d.eng == "pe" and eng == "pe" and not d.is_dma and not dma:
                continue
            if d.is_dma:
                d.need_sig = True
                dd.append(d)
            elif d.eng not in last or d.qi > last[d.eng].qi:
                last[d.eng] = d
        for d in last.values():
            d.need_sig = True
            dd.append(d)
        o.deps = dd
        o.qi = len(self.q[eng])
        if dma:
            o.dst = writes[0]
            o.dst.ndma += 1
            o.k = o.dst.ndma
        for r in reads:
            r.readers.append(o)
        for w in writes:
            w.last_w = o
            w.readers = []
        self.q[eng].append(o)
        self.n_ops += 1
        return o

    def flush(self):
        nc = self.nc
        if not any(self.q[e] for e in ENGS):
            return
        lasts = []
        for e in ENGS:
            comp = [o for o in self.q[e] if not o.is_dma]
            if comp:
                comp[-1].need_sig = True
                lasts.append(comp[-1])
        dma_bufs = []
        for e in ENGS:
            for o in self.q[e]:
                if o.is_dma:
                    b = o.dst
                    if b.sem is None:
                        if self.dma_pool:
                            b.sem, b.base = self.dma_pool.pop(0)
                        else:
                            b.sem, b.base = self._newsem("d"), 0
                        dma_bufs.append(b)
                    o.sig = (b.sem, b.base + 16 * o.k)
                elif o.need_sig:
                    c = self.eng_cnt[e]
                    blk = c // SEM_BLOCK
                    key = (e, blk)
                    if key not in self.eng_sems:
                        self.eng_sems[key] = self._newsem(e)
                    o.sig = (self.eng_sems[key], c % SEM_BLOCK + 1)
                    self.eng_cnt[e] = c + 1
        finals = [o.sig for o in lasts] + [(b.sem, b.base + 16 * b.ndma) for b in dma_bufs]

        def run_queue(e, eng):
            waited = {}
            for o in self.q[e]:
                for d in o.deps:
                    sem, val = d.sig
                    if waited.get(id(sem), 0) >= val:
                        continue
                    waited[id(sem)] = val
                    eng.wait_ge(sem, val)
                nm, a_, kw_ = o.fn
                try:
                    ins = getattr(eng, nm)(*a_, **kw_)
                except Exception:
                    print("EMIT FAIL", e, nm, [getattr(x, "shape", x) for x in a_], {k_: getattr(v_, "shape", v_) for k_, v_ in kw_.items()}, flush=True)
                    for k_, v_ in kw_.items():
                        print("   ARG", k_, repr(v_)[:300], repr(getattr(v_, "ap", None))[:300], flush=True)
                    raise
                if o.sig is not None:
                    ins.then_inc(o.sig[0], 16 if o.is_dma else 1)
            for sem, val in finals:
                if waited.get(id(sem), 0) >= val:
                    continue
                eng.wait_ge(sem, val)

        with nc.Block() as block:
            for e in ENGS:
                def mk(e):
                    return lambda eng: run_queue(e, eng)
                getattr(block, ENGOBJ[e])(mk(e))
        for b in dma_bufs:
            cnt = b.base + 16 * b.ndma
            assert cnt < 32000, "dma semaphore count too large"
            self.dma_pool.append((b.sem, cnt))
        for b in self.bufs:
            b.last_w, b.readers, b.sem, b.base, b.ndma = None, [], None, 0, 0
        self.q = {e: [] for e in ENGS}


class Tl:
    __slots__ = ("t", "b")

    def __init__(self, t, b):
        self.t, self.b = t, b


class Scope:
    def __init__(self, k):
        self.k = k
        self.st = contextlib.ExitStack()

    def __enter__(self):
        self.st.__enter__()
        return self

    def __exit__(self, *a):
        self.k.P.flush()
        return self.st.__exit__(*a)

    def sb(self, name, shape, dt):
        self.k.uid += 1
        t = self.st.enter_context(self.k.nc.sbuf_tensor(f"{name}_{self.k.uid}", list(shape), dt))
        return Tl(t, self.k.P.buf(name))

    def ps(self, name, shape, dt=F32):
        self.k.uid += 1
        t = self.st.enter_context(self.k.nc.psum_tensor(f"{name}_{self.k.uid}", list(shape), dt))
        return Tl(t, self.k.P.buf(name))


class KB:
    def __init__(self, nlayers=DEPTH, dbg=None):
        self.nlayers = nlayers
        self.dbg = dbg
        self.uid = 0
        self.nc = bass.Bass("TRN2", target_bir_lowering=False)
        self.outer = contextlib.ExitStack()
        self.P = Prog(self.nc, self.outer)

    def din(self, name, shape, dt=F32):
        return self.nc.dram_tensor(name, list(shape), dt, kind="ExternalInput").ap()

    def dscr(self, name, shape, dt):
        return self.nc.dram_tensor(name, list(shape), dt).ap()

    def op(self, *a, **k):
        return self.P.op(*a, **k)

    def dma(self, q, out, in_, reads, writes, **kw):
        return self.P.op(q, lambda e: e.dma_start(out=out, in_=in_, **kw), reads=reads, writes=writes, dma=True)

    def bcast_load(self, q, dst, row_ap, nparts=128):
        return self.dma(q, dst.t[:], row_ap.partition_broadcast(nparts), [], [dst.b])

    def build(self):
        nc = self.nc
        L = self.nlayers
        n_ssd = (L + 1) // 2
        n_na = L // 2
        I = {}
        self.moeh = [self.dscr(f"moeh{cq}", [T, 512], F32) for cq in range(4)]
        self.hn = self.dscr("hn", [T, D], BF16)
        I["x"] = self.din("x", [T, D])
        I["mem"] = self.din("mem", [MEM, D])
        I["norm_mix_g"] = self.din("norm_mix_g", [L, D])
        I["norm_ffn_g"] = self.din("norm_ffn_g", [L, D])
        I["norm_final_g"] = self.din("norm_final_g", [1, D])
        I["mem_norm_g"] = self.din("mem_norm_g", [1, D])
        I["ssd_w_in"] = self.din("ssd_w_in", [n_ssd, D, SSD_IN])
        I["ssd_conv_w"] = self.din("ssd_conv_w", [n_ssd, 5, SSD_CONV_DIM])
        I["ssd_conv_b"] = self.din("ssd_conv_b", [n_ssd, SSD_CONV_DIM])
        I["ssd_dt_bias"] = self.din("ssd_dt_bias", [n_ssd, 48])
        I["ssd_a_log"] = self.din("ssd_a_log", [n_ssd, 48])
        I["ssd_d"] = self.din("ssd_d", [n_ssd, SSD_H])
        I["ssd_gate_norm_g"] = self.din("ssd_gate_norm_g", [n_ssd, D_SSD])
        I["ssd_w_out"] = self.din("ssd_w_out", [n_ssd, D, D])
        if n_na:
            I["na_w_in"] = self.din("na_w_in", [n_na, D, NA_IN])
            I["na_bias"] = self.din("na_bias", [n_na, NA_H, 128, 25, 128])
            I["na_w_out"] = self.din("na_w_out", [n_na, D, D])
        I["xa_w_kv"] = self.din("xa_w_kv", [L, D, 2 * D_XA])
        I["moe_w_router"] = self.din("moe_w_router", [L, D, NE])
        I["moe_w1"] = self.din("moe_w1", [L, NE, D, DFF])
        I["moe_w3"] = self.din("moe_w3", [L, NE, D, DFF])
        I["moe_w2"] = self.din("moe_w2", [L, NE, DFF, D])
        self.I = I
        self.out = nc.dram_tensor("out", [T, D], F32, kind="ExternalOutput").ap()
        self.xres = self.dscr("xres", [T, D], F32)
        self.ycat = self.dscr("ycat", [T, D], BF16)
        self.szd = self.dscr("szd", [T, D_SSD], BF16)
        self.Gd = self.dscr("Gd", [NT, 128, D_SSD], BF16)
        self.ABd = self.dscr("ABd", [NT, 2 * SSD_H * 128], F32)
        self.TOTd = self.dscr("TOTd", [1, NT * 48], F32)
        self.xsBd = self.dscr("xsBd", [T, 2048], BF16)
        self.BCTd = self.dscr("BCTd", [8, 128, T], BF16)
        self.TOKd = self.dscr("TOKd", [T, 4 * 48], F32)

        with self.outer:
            with Scope(self) as cs:
                self.consts(cs)
                for i in range(L):
                    if i % 2 == 0:
                        self.ssd_layer(cs, i)
                    else:
                        self.na_layer(cs, i)
                    if self.dbg == ("mix", i):
                        self.dump_xres(cs)
                        return self.nc
                    self.moe_layer(cs, i)
                    if self.dbg == ("moe", i):
                        self.dump_xres(cs)
                        return self.nc
                self.final_norm(cs)
        return self.nc

    def consts(self, cs):
        k = self
        self.identf = cs.sb("identf", [128, 128], F32)
        self.ident = cs.sb("ident", [128, 128], BF16)
        self.ones_bf = cs.sb("ones_bf", [128, 128], BF16)
        self.maskf = cs.sb("maskf", [128, 128], F32)
        self.maskb = cs.sb("maskb", [128, 128], F32)
        self.iota256 = cs.sb("iota256", [128, 256], F32)
        self.memT = cs.sb("memT", [128, ND, MEM], BF16)
        self.epsc = cs.sb("epsc", [128, 1], F32)
        idf, idb = self.identf, self.ident
        k.op("pool", lambda e: e.memset(idf.t[:], 0.0), writes=[idf.b])
        k.op("pool", lambda e: e.affine_select(out=idf.t[:], in_=idf.t[:], pattern=[[-1, 128]], compare_op=ALU.not_equal,
                                               fill=1.0, base=0, channel_multiplier=1), reads=[idf.b], writes=[idf.b])
        k.op("dve", lambda e: e.tensor_copy(out=idb.t[:], in_=idf.t[:]), reads=[idf.b], writes=[idb.b])
        k.op("pool", lambda e: e.memset(self.ones_bf.t[:], 1.0), writes=[self.ones_bf.b])
        k.op("pool", lambda e: e.memset(self.epsc.t[:], EPS), writes=[self.epsc.b])
        k.op("pool", lambda e: e.memset(self.maskf.t[:], 1.0), writes=[self.maskf.b])
        k.op("pool", lambda e: e.affine_select(out=self.maskf.t[:], in_=self.maskf.t[:], pattern=[[1, 128]], compare_op=ALU.is_ge,
                                               fill=0.0, base=0, channel_multiplier=-1), reads=[self.maskf.b], writes=[self.maskf.b])
        k.op("pool", lambda e: e.memset(self.maskb.t[:], 1.0), writes=[self.maskb.b])
        k.op("pool", lambda e: e.affine_select(out=self.maskb.t[:], in_=self.maskb.t[:], pattern=[[-1, 128]], compare_op=ALU.is_ge,
                                               fill=0.0, base=0, channel_multiplier=1), reads=[self.maskb.b], writes=[self.maskb.b])
        k.op("pool", lambda e: e.iota(self.iota256.t[:], pattern=[[1, 256]], base=0, channel_multiplier=0,
                                      allow_small_or_imprecise_dtypes=True), writes=[self.iota256.b])
        xb = self.P.buf("xres_all")
        k.dma("sp", self.xres[:, :], self.I["x"][:, :], [], [xb])
        self.P.flush()
        with Scope(self) as s:
            g = s.sb("g", [128, D], F32)
            k.bcast_load("sp", g, self.I["mem_norm_g"][0:1, :])
            for mt in range(2):
                self.norm_tile(s, self.I["mem"][mt * 128:(mt + 1) * 128, :], g, dstT=self.memT, tcol=mt * 128, tag=f"m{mt}")

    def norm_tile(self, s, src_ap, g, dstT=None, tcol=0, tag="", hn_dst=None, want_f32=None, q="sp", defer=False, need_bf16=True):
        k = self
        if not hasattr(s, "_nt"):
            s._nt = {}
            for j in range(3):
                s._nt[j] = dict(
                    xt=s.sb("n_xt", [128, D], F32), ss=s.sb("n_ss", [128, 1], F32),
                    hb=s.sb("n_hb", [128, D], BF16), pt=s.ps("n_pt", [128, 8, 128], BF16) if j < 2 else None)
            s._ntc = 0
        R = s._nt[s._ntc % 3]
        pt = s._nt[s._ntc % 2]["pt"]
        s._ntc += 1
        xt, ss, hb = R["xt"], R["ss"], R["hb"]
        k.dma(q, xt.t[:], src_ap, [], [xt.b])
        k.op("act", lambda e: e.activation(out=hb.t[:], in_=xt.t[:], func=AF.Square, accum_out=ss.t[:]), reads=[xt.b], writes=[hb.b, ss.b])
        k.op("act", lambda e: e.activation(out=ss.t[:], in_=ss.t[:], func=AF.Sqrt, scale=1.0 / D, bias=self.epsc.t[:]), reads=[ss.b, self.epsc.b], writes=[ss.b])
        k.op("dve", lambda e: e.reciprocal(out=ss.t[:], in_=ss.t[:]), reads=[ss.b], writes=[ss.b])
        if want_f32 is not None:
            hf = want_f32
            k.op("dve", lambda e: e.scalar_tensor_tensor(out=hf.t[:], in0=xt.t[:], scalar=ss.t[:, 0:1], in1=g.t[:], op0=ALU.mult, op1=ALU.mult),
                 reads=[xt.b, ss.b, g.b], writes=[hf.b])
            if need_bf16:
                k.op("act", lambda e: e.activation(out=hb.t[:], in_=hf.t[:], func=AF.Copy), reads=[hf.b], writes=[hb.b])
        else:
            k.op("dve", lambda e: e.scalar_tensor_tensor(out=hb.t[:], in0=xt.t[:], scalar=ss.t[:, 0:1], in1=g.t[:], op0=ALU.mult, op1=ALU.mult),
                 reads=[xt.b, ss.b, g.b], writes=[hb.b])
        if hn_dst is not None:
            k.dma("sp", hn_dst[0], hb.t[:], [hb.b], [hn_dst[1]])
        def back():
            if dstT is None:
                return
            for half in range(2):
                for j in range(8):
                    kk = half * 8 + j
                    k.op("pe", lambda e: e.transpose(out=pt.t[:, j, :], in_=hb.t[:, kk * 128:(kk + 1) * 128], identity=self.ident.t[:]),
                         reads=[hb.b, self.ident.b], writes=[pt.b])
                if half == 0:
                    k.op("dve", lambda e: e.tensor_copy(out=dstT.t[:, half * 8:(half + 1) * 8, tcol:tcol + 128], in_=pt.t[:]),
                         reads=[pt.b], writes=[dstT.b])
                else:
                    k.op("act", lambda e: e.activation(out=dstT.t[:, half * 8:(half + 1) * 8, tcol:tcol + 128], in_=pt.t[:], func=AF.Copy),
                         reads=[pt.b], writes=[dstT.b])
        if defer:
            return back
        back()

    def dump_xres(self, cs):
        self.P.flush()
        ob = self.P.buf("out")
        self.dma("sp", self.out[:, :], self.xres[:, :], [], [ob])
        self.P.flush()

    def final_norm(self, cs):
        k = self
        with Scope(self) as s:
            g = s.sb("g", [128, D], F32)
            k.bcast_load("sp", g, self.I["norm_final_g"][0:1, :])
            outs = [s.sb("fo", [128, D], F32) for _ in range(2)]
            ob = self.P.buf("out")
            for tt in range(NT):
                o = outs[tt % 2]
                self.norm_tile(s, self.xres[tt * 128:(tt + 1) * 128, :], g, want_f32=o, tag=f"f{tt}", need_bf16=False)
                k.dma("sp", self.out[tt * 128:(tt + 1) * 128, :], o.t[:], [o.b], [ob])

    def wblock(self, s, w_ap, c0, ncols, nk=ND):
        if not hasattr(s, "_wb"):
            s._wb = [s.sb("wblk", [128, ND, 512], BF16) for _ in range(2)]
            s._wbc = 0
        w = s._wb[s._wbc % 2]
        s._wbc += 1
        self.dma("pool", w.t[:, 0:nk, 0:ncols], w_ap[:, c0:c0 + ncols].rearrange("(k p) n -> p k n", p=128), [], [w.b])
        return w

    def proj_fm(self, s, hT, w, ncols, ps_bank_tiles, et):
        m0 = et * 128
        m = min(128, ncols - m0)
        for tc in range(4):
            pb = ps_bank_tiles[tc]
            for kk in range(ND):
                self.op("pe", lambda e, kk=kk, tc=tc, pb=pb: e.matmul(pb.t[0:m, :], lhsT=w.t[:, kk, m0:m0 + m], rhs=hT.t[:, kk, tc * 512:(tc + 1) * 512],
                                                                     start=(kk == 0), stop=(kk == ND - 1)),
                        reads=[w.b, hT.b], writes=[pb.b])

    def proj_tm(self, hT, w, ncols, pb, tt):
        for kk in range(ND):
            self.op("pe", lambda e, kk=kk: e.matmul(pb.t[:, 0:ncols], lhsT=hT.t[:, kk, tt * 128:(tt + 1) * 128], rhs=w.t[:, kk, 0:ncols],
                                                    start=(kk == 0), stop=(kk == ND - 1)),
                    reads=[w.b, hT.b], writes=[pb.b])

    def norm_to_hT(self, s, hT, gain_row):
        g = s.sb("g", [128, D], F32)
        self.bcast_load("sp", g, gain_row)
        pend = None
        for tt in range(NT):
            b = self.norm_tile(s, self.xres[tt * 128:(tt + 1) * 128, :], g, dstT=hT, tcol=tt * 128, defer=True)
            if pend is not None:
                pend()
            pend = b
        pend()

    def xa_block(self, s, li, hT, wq_ap, c0):
        k = self
        qT = s.sb("xa_qT", [128, 4, T], BF16)
        kT = s.sb("xa_kT", [128, 4, MEM], BF16)
        va = s.sb("xa_va", [128, 2, 4, 129], BF16)
        pbs = [s.ps("xa_pb", [128, 512]) for _ in range(4)]
        w = self.wblock(s, wq_ap, c0, 512)
        for et in range(4):
            self.proj_fm(s, hT, w, 512, pbs, et)
            for tc in range(4):
                eng = "act" if tc % 2 == 0 else "dve"
                if eng == "act":
                    k.op("act", lambda e, et=et, tc=tc: e.activation(out=qT.t[:, et, tc * 512:(tc + 1) * 512], in_=pbs[tc].t[:], func=AF.Copy),
                         reads=[pbs[tc].b], writes=[qT.b])
                else:
                    k.op("dve", lambda e, et=et, tc=tc: e.tensor_copy(out=qT.t[:, et, tc * 512:(tc + 1) * 512], in_=pbs[tc].t[:]),
                         reads=[pbs[tc].b], writes=[qT.b])
        wkv = self.I["xa_w_kv"][li]
        w = self.wblock(s, wkv, 0, 512)
        for et in range(4):
            pb = pbs[et]
            for kk in range(ND):
                k.op("pe", lambda e, kk=kk, et=et, pb=pb: e.matmul(pb.t[:, 0:MEM], lhsT=w.t[:, kk, et * 128:(et + 1) * 128], rhs=self.memT.t[:, kk, :],
                                                                  start=(kk == 0), stop=(kk == ND - 1)), reads=[w.b, self.memT.b], writes=[pb.b])
            k.op("act", lambda e, et=et, pb=pb: e.activation(out=kT.t[:, et, :], in_=pb.t[:, 0:MEM], func=AF.Copy), reads=[pb.b], writes=[kT.b])
        w = self.wblock(s, wkv, 512, 512)
        k.op("pool", lambda e: e.memset(va.t[:], 1.0), writes=[va.b])
        for mt in range(2):
            pb = pbs[mt]
            for kk in range(ND):
                k.op("pe", lambda e, kk=kk, mt=mt, pb=pb: e.matmul(pb.t[:, :], lhsT=self.memT.t[:, kk, mt * 128:(mt + 1) * 128], rhs=w.t[:, kk, :],
                                                                  start=(kk == 0), stop=(kk == ND - 1)), reads=[w.b, self.memT.b], writes=[pb.b])
            k.op("dve", lambda e, mt=mt, pb=pb: e.tensor_copy(out=va.t[:, mt, :, 0:128], in_=pb.t[:].rearrange("p (h d) -> p h d", h=4)),
                 reads=[pb.b], writes=[va.b])
        sps = [s.ps("xa_s", [128, 2, 128]) for _ in range(2)]
        ops_ = [s.ps("xa_o", [128, 4, 256]) for _ in range(1)]
        pT = [s.sb("xa_p", [128, 2, 128], BF16) for _ in range(2)]
        rc = [s.sb("xa_rc", [128, 4], F32) for _ in range(2)]
        ot = [s.sb("xa_ot", [128, 512], BF16) for _ in range(2)]
        yb = self.P.buf("ycat_xa")
        scale = 128.0 ** -0.5
        it = 0
        for tt in range(NT):
            o_ps, o_t, r_c = ops_[0], ot[tt % 2], rc[tt % 2]
            for h in range(4):
                sp_, p_ = sps[it % 2], pT[it % 2]
                it += 1
                for mt in range(2):
                    k.op("pe", lambda e, mt=mt, h=h, sp_=sp_: e.matmul(sp_.t[:, mt, :], lhsT=kT.t[:, h, mt * 128:(mt + 1) * 128], rhs=qT.t[:, h, tt * 128:(tt + 1) * 128],
                                                                      start=True, stop=True), reads=[kT.b, qT.b], writes=[sp_.b])
                k.op("act", lambda e, sp_=sp_, p_=p_: e.activation(out=p_.t[:], in_=sp_.t[:], func=AF.Exp, scale=scale), reads=[sp_.b], writes=[p_.b])
                for mt in range(2):
                    k.op("pe", lambda e, mt=mt, h=h, p_=p_, o_ps=o_ps: e.matmul(o_ps.t[:, h, 0:129], lhsT=p_.t[:, mt, :], rhs=va.t[:, mt, h, :],
                                                                               start=(mt == 0), stop=(mt == 1)), reads=[p_.b, va.b], writes=[o_ps.b])
            k.op("dve", lambda e, o_ps=o_ps, r_c=r_c: e.reciprocal(out=r_c.t[:], in_=o_ps.t[:, :, 128]), reads=[o_ps.b], writes=[r_c.b])
            k.op("dve", lambda e, o_ps=o_ps, r_c=r_c, o_t=o_t: e.tensor_tensor(out=o_t.t[:].rearrange("p (h d) -> p h d", h=4), in0=o_ps.t[:, :, 0:128],
                                                                               in1=r_c.t[:].unsqueeze(2).to_broadcast([128, 4, 128]), op=ALU.mult),
                 reads=[o_ps.b, r_c.b], writes=[o_t.b])
            k.dma("sp", self.ycat[tt * 128:(tt + 1) * 128, D_SSD:D], o_t.t[:], [o_t.b], [yb])

    def out_proj(self, w_ap):
        k = self
        with Scope(self) as s:
            W = s.sb("wout", [128, ND, D], BF16)
            for c in range(4):
                k.dma("pool", W.t[:, :, c * 512:(c + 1) * 512], w_ap[:, c * 512:(c + 1) * 512].rearrange("(k p) n -> p k n", p=128), [], [W.b])
            yt = [s.sb("op_y", [128, D], BF16) for _ in range(2)]
            yT = [s.sb("op_yT", [128, ND, 128], BF16) for _ in range(2)]
            pt = [s.ps("op_pt", [128, 8, 128], BF16) for _ in range(2)]
            po = [s.ps("op_po", [128, 512]) for _ in range(4)]
            mix = [s.sb("op_mix", [128, D], F32) for _ in range(2)]
            xb = self.P.buf("xres_w")
            for tt in range(NT):
                y_, yT_, mx = yt[tt % 2], yT[tt % 2], mix[tt % 2]
                k.dma("sp", y_.t[:], self.ycat[tt * 128:(tt + 1) * 128, :], [], [y_.b])
                for half in range(2):
                    p_ = pt[half]
                    for j in range(8):
                        kk = half * 8 + j
                        k.op("pe", lambda e, kk=kk, j=j, p_=p_, y_=y_: e.transpose(out=p_.t[:, j, :], in_=y_.t[:, kk * 128:(kk + 1) * 128], identity=self.ident.t[:]),
                             reads=[y_.b, self.ident.b], writes=[p_.b])
                    if half == 0:
                        k.op("dve", lambda e, p_=p_, yT_=yT_: e.tensor_copy(out=yT_.t[:, 0:8, :], in_=p_.t[:]), reads=[p_.b], writes=[yT_.b])
                    else:
                        k.op("act", lambda e, p_=p_, yT_=yT_: e.activation(out=yT_.t[:, 8:16, :], in_=p_.t[:], func=AF.Copy), reads=[p_.b], writes=[yT_.b])
                for dc in range(4):
                    pb = po[dc]
                    for kk in range(ND):
                        k.op("pe", lambda e, kk=kk, dc=dc, pb=pb, yT_=yT_: e.matmul(pb.t[:], lhsT=yT_.t[:, kk, :], rhs=W.t[:, kk, dc * 512:(dc + 1) * 512],
                                                                                   start=(kk == 0), stop=(kk == ND - 1)), reads=[yT_.b, W.b], writes=[pb.b])
                    if dc % 2 == 0:
                        k.op("act", lambda e, dc=dc, pb=pb, mx=mx: e.activation(out=mx.t[:, dc * 512:(dc + 1) * 512], in_=pb.t[:], func=AF.Copy), reads=[pb.b], writes=[mx.b])
                    else:
                        k.op("dve", lambda e, dc=dc, pb=pb, mx=mx: e.tensor_copy(out=mx.t[:, dc * 512:(dc + 1) * 512], in_=pb.t[:]), reads=[pb.b], writes=[mx.b])
                k.dma("pool", self.xres[tt * 128:(tt + 1) * 128, :], mx.t[:], [mx.b], [xb], accum_op=ALU.add)

    def ssd_layer(self, cs, li):
        j = li // 2
        Win = self.I["ssd_w_in"][j]
        with Scope(self) as AS:
            hT = AS.sb("hT", [128, ND, T], BF16)
            with Scope(self) as s:
                self.norm_to_hT(s, hT, self.I["norm_mix_g"][li:li + 1, :])
            with Scope(self) as s:
                self.ssd_proj_z(s, Win, hT)
            with Scope(self) as s:
                self.ssd_proj_xbc(s, j, Win, hT)
            with Scope(self) as s:
                self.ssd_proj_dt(s, j, Win, hT)
            with Scope(self) as s:
                self.xa_block(s, li, hT, Win, 4144)
        with Scope(self) as s:
            self.ssd_scan(s, j)
        self.out_proj(self.I["ssd_w_out"][j])

    def ssd_proj_z(self, s, Win, hT):
        k = self
        pbs = [s.ps("z_pb", [128, 512]) for _ in range(4)]
        zs = [s.sb("z_sb", [128, 512], BF16) for _ in range(4)]
        zb = self.P.buf("szd")
        it = 0
        for blk in range(3):
            w = self.wblock(s, Win, blk * 512, 512)
            for tt in range(NT):
                pb, z_ = pbs[it % 4], zs[it % 4]
                it += 1
                self.proj_tm(hT, w, 512, pb, tt)
                k.op("act", lambda e, pb=pb, z_=z_: e.activation(out=z_.t[:], in_=pb.t[:], func=AF.Silu), reads=[pb.b], writes=[z_.b])
                k.dma("sp", self.szd[tt * 128:(tt + 1) * 128, blk * 512:(blk + 1) * 512], z_.t[:], [z_.b], [zb])

    def ssd_proj_xbc(self, s, j, Win, hT):
        k = self
        raw = s.sb("cw_raw", [120, 128], F32)
        cwT = s.sb("cwT", [128, 120], F32)
        ptr = s.ps("cw_pt", [128, 120], F32)
        k.dma("sp", raw.t[0:100, :], self.I["ssd_conv_w"][j].rearrange("k (n p) -> (k n) p", p=128), [], [raw.b])
        k.dma("sp", raw.t[100:120, :], self.I["ssd_conv_b"][j:j + 1, :].rearrange("o (n p) -> (o n) p", p=128), [], [raw.b])
        k.op("pe", lambda e: e.transpose(out=ptr.t[:], in_=raw.t[:], identity=self.identf.t[0:120, 0:120]), reads=[raw.b, self.identf.b], writes=[ptr.b])
        k.op("dve", lambda e: e.tensor_copy(out=cwT.t[:], in_=ptr.t[:]), reads=[ptr.b], writes=[cwT.b])
        pbs = [s.ps("x_pb", [128, 512]) for _ in range(4)]
        pcs = [s.ps("x_pc", [128, 512]) for _ in range(2)]
        ptt = s.ps("x_ptt", [128, 8, 128], BF16)
        pre = [s.sb("x_pre", [128, T + 4], BF16) for _ in range(2)]
        post = [s.sb("x_post", [128, T], BF16) for _ in range(2)]
        dg = [s.sb("x_dg", [128, 5, 128], BF16) for _ in range(2)]
        tok = [s.sb("x_tok", [128, NT, 128], BF16) for _ in range(2)]
        xb = self.P.buf("xsBd")
        bb = self.P.buf("BCTd")
        for p_ in pre:
            k.op("pool", lambda e, p_=p_: e.memset(p_.t[:], 0.0), writes=[p_.b])
        cvi = 0
        for blk in range(5):
            w = self.wblock(s, Win, 1536 + blk * 512, 512)
            for et in range(4):
                ct = blk * 4 + et
                pr, po, dg_, tk = pre[ct % 2], post[ct % 2], dg[ct % 2], tok[ct % 2]
                for kk in range(5):
                    k.op("dve", lambda e, kk=kk, ct=ct, dg_=dg_: e.tensor_scalar(out=dg_.t[:, kk, :], in0=self.identf.t[:], scalar1=cwT.t[:, kk * 20 + ct:kk * 20 + ct + 1],
                                                                               scalar2=None, op0=ALU.mult), reads=[self.identf.b, cwT.b], writes=[dg_.b])
                self.proj_fm(s, hT, w, 512, pbs, et)
                for tc in range(4):
                    if tc % 2 == 0:
                        k.op("act", lambda e, tc=tc, pr=pr: e.activation(out=pr.t[:, 2 + tc * 512:2 + (tc + 1) * 512], in_=pbs[tc].t[:], func=AF.Copy), reads=[pbs[tc].b], writes=[pr.b])
                    else:
                        k.op("dve", lambda e, tc=tc, pr=pr: e.tensor_copy(out=pr.t[:, 2 + tc * 512:2 + (tc + 1) * 512], in_=pbs[tc].t[:]), reads=[pbs[tc].b], writes=[pr.b])
                for tc in range(4):
                    pc = pcs[cvi % 2]
                    cvi += 1
                    for kk in range(5):
                        k.op("pe", lambda e, kk=kk, tc=tc, pc=pc, dg_=dg_, pr=pr: e.matmul(pc.t[:], lhsT=dg_.t[:, kk, :], rhs=pr.t[:, tc * 512 + kk:tc * 512 + kk + 512],
                                                                                          start=(kk == 0), stop=(kk == 4)), reads=[dg_.b, pr.b], writes=[pc.b])
                    k.op("act", lambda e, tc=tc, pc=pc, po=po, ct=ct: e.activation(out=po.t[:, tc * 512:(tc + 1) * 512], in_=pc.t[:], func=AF.Silu, bias=cwT.t[:, 100 + ct:101 + ct]),
                         reads=[pc.b, cwT.b], writes=[po.b])
                if ct >= 12:
                    k.dma("sp", self.BCTd[ct - 12], po.t[:], [po.b], [bb])
                if ct < 16:
                    for half in range(2):
                        for jj in range(8):
                            tt = half * 8 + jj
                            k.op("pe", lambda e, jj=jj, tt=tt, po=po: e.transpose(out=ptt.t[:, jj, :], in_=po.t[:, tt * 128:(tt + 1) * 128], identity=self.ident.t[:]),
                                 reads=[po.b, self.ident.b], writes=[ptt.b])
                        k.op("dve", lambda e, half=half, tk=tk: e.tensor_copy(out=tk.t[:, half * 8:(half + 1) * 8, :], in_=ptt.t[:]), reads=[ptt.b], writes=[tk.b])
                    k.dma("sp", self.xsBd.rearrange("(n p) c -> p n c", p=128)[:, :, ct * 128:(ct + 1) * 128], tk.t[:], [tk.b], [xb])

    def ssd_proj_dt(self, s, j, Win, hT):
        k = self
        pbs = [s.ps("d_pb", [128, 512]) for _ in range(4)]
        ptk = s.ps("d_ptk", [128, 4, 48], F32)
        dtb = s.sb("d_dtb", [48, 1], F32)
        nA = s.sb("d_nA", [48, 1], F32)
        v = s.sb("d_v", [48, T], F32)
        a = s.sb("d_a", [48, T], F32)
        dt = s.sb("d_dt", [48, T], F32)
        dA = s.sb("d_dA", [48, T], F32)
        pre = s.sb("d_pre", [48, T], F32)
        suf = s.sb("d_suf", [48, T], F32)
        tk = [s.sb("d_tk", [128, 4, 48], F32) for _ in range(2)]
        k.dma("sp", dtb.t[:], self.I["ssd_dt_bias"][j:j + 1, :].rearrange("o h -> h o"), [], [dtb.b])
        k.dma("sp", nA.t[:], self.I["ssd_a_log"][j:j + 1, :].rearrange("o h -> h o"), [], [nA.b])
        k.op("act", lambda e: e.activation(out=nA.t[:], in_=nA.t[:], func=AF.Exp), reads=[nA.b], writes=[nA.b])
        k.op("dve", lambda e: e.tensor_scalar(out=nA.t[:], in0=nA.t[:], scalar1=-1.0, scalar2=None, op0=ALU.mult), reads=[nA.b], writes=[nA.b])
        w = self.wblock(s, Win, 4096, 48)
        self.proj_fm(s, hT, w, 48, pbs, 0)
        for tc in range(4):
            k.op("act", lambda e, tc=tc: e.activation(out=v.t[:, tc * 512:(tc + 1) * 512], in_=pbs[tc].t[0:48, :], func=AF.Identity, bias=dtb.t[:]),
                 reads=[pbs[tc].b, dtb.b], writes=[v.b])
        k.op("dve", lambda e: e.scalar_tensor_tensor(out=a.t[:], in0=v.t[:], scalar=-1.0, in1=v.t[:], op0=ALU.mult, op1=ALU.max), reads=[v.b], writes=[a.b])
        k.op("act", lambda e: e.activation(out=a.t[:], in_=a.t[:], func=AF.Exp, scale=-1.0), reads=[a.b], writes=[a.b])
        k.op("act", lambda e: e.activation(out=a.t[:], in_=a.t[:], func=AF.Ln, bias=1.0), reads=[a.b], writes=[a.b])
        k.op("dve", lambda e: e.scalar_tensor_tensor(out=dt.t[:], in0=v.t[:], scalar=0.0, in1=a.t[:], op0=ALU.max, op1=ALU.add), reads=[v.b, a.b], writes=[dt.b])
        k.op("dve", lambda e: e.tensor_scalar(out=dA.t[:], in0=dt.t[:], scalar1=nA.t[:, 0:1], scalar2=None, op0=ALU.mult), reads=[dt.b, nA.b], writes=[dA.b])
        for c in range(NT):
            sl = slice(c * 128, (c + 1) * 128)
            k.op("dve", lambda e, sl=sl: e.tensor_tensor_scan(out=pre.t[:, sl], data0=dA.t[:, sl], data1=dA.t[:, sl], initial=0.0, op0=ALU.add, op1=ALU.bypass),
                 reads=[dA.b], writes=[pre.b])
        for c in range(NT):
            sl = slice(c * 128, (c + 1) * 128)
            k.op("dve", lambda e, sl=sl, c=c: e.tensor_scalar(out=suf.t[:, sl], in0=pre.t[:, sl], scalar1=-1.0, scalar2=pre.t[:, c * 128 + 127:c * 128 + 128],
                                                             op0=ALU.mult, op1=ALU.add), reads=[pre.b], writes=[suf.b])
        k.op("dve", lambda e: e.tensor_tensor(out=suf.t[:], in0=suf.t[:], in1=dA.t[:], op=ALU.add), reads=[suf.b, dA.b], writes=[suf.b])
        ab = self.P.buf("ABd")
        tb = self.P.buf("TOTd")
        kb = self.P.buf("TOKd")
        ABv = self.ABd.rearrange("c (d h l) -> d h c l", d=2, h=SSD_H)
        k.dma("sp", ABv[0], pre.t[0:24, :].rearrange("h (c l) -> h c l", l=128), [pre.b], [ab])
        k.dma("sp", ABv[1], suf.t[24:48, :].rearrange("h (c l) -> h c l", l=128), [suf.b], [ab])
        k.dma("sp", self.TOTd.rearrange("o (c h) -> h (o c)", h=48), pre.t[:, 127::128], [pre.b], [tb], allow_slow_non_contiguous=True)
        srcs = [dt, pre, suf, dA]
        for c in range(NT):
            t_ = tk[c % 2]
            for qi, src in enumerate(srcs):
                k.op("pe", lambda e, qi=qi, src=src, c=c: e.transpose(out=ptk.t[:, qi, :], in_=src.t[:, c * 128:(c + 1) * 128], identity=self.identf.t[0:48, 0:48]),
                     reads=[src.b, self.identf.b], writes=[ptk.b])
            k.op("dve", lambda e, t_=t_: e.tensor_copy(out=t_.t[:], in_=ptk.t[:]), reads=[ptk.b], writes=[t_.b])
            k.dma("sp", self.TOKd[c * 128:(c + 1) * 128, :], t_.t[:].rearrange("p a h -> p (a h)"), [t_.b], [kb])

    def ssd_scan(self, s, j):
        k = self
        H, Pd = SSD_H, SSD_P
        Dful = s.sb("sc_D", [128, D_SSD], F32)
        d24 = s.sb("sc_d24", [128, H], F32)
        gn = s.sb("sc_gn", [128, D_SSD], F32)
        CD = s.sb("sc_CD", [128, NT, 48], F32)
        k.bcast_load("sp", d24, self.I["ssd_d"][j:j + 1, :])
        k.bcast_load("sp", gn, self.I["ssd_gate_norm_g"][j:j + 1, :])
        k.bcast_load("sp", CD_flat := Tl(CD.t, CD.b), self.TOTd[0:1, :]) if False else k.dma("sp", CD.t[:].rearrange("p c h -> p (c h)"), self.TOTd[0:1, :].partition_broadcast(128), [], [CD.b])
        k.op("act", lambda e: e.activation(out=CD.t[:], in_=CD.t[:], func=AF.Exp), reads=[CD.b], writes=[CD.b])
        k.op("dve", lambda e: e.tensor_copy(out=Dful.t[:].rearrange("p (h q) -> p h q", h=H), in_=d24.t[:].unsqueeze(2).to_broadcast([128, H, Pd])), reads=[d24.b], writes=[Dful.b])
        xs = [s.sb("sc_xs", [128, 2048], BF16) for _ in range(2)]
        tok = [s.sb("sc_tok", [128, 4, 48], F32) for _ in range(2)]
        Hs = s.sb("sc_H", [128, D_SSD], F32)
        Hbf = [s.sb("sc_Hbf", [128, D_SSD], BF16) for _ in range(2)]
        w24 = [s.sb("sc_w24", [128, H], F32) for _ in range(2)]
        xdd = [s.sb("sc_xdd", [128, D_SSD], BF16) for _ in range(2)]
        psS = [s.ps("sc_psS", [128, 512])] * 2
        gb = self.P.buf("Gd")
        xview = lambda t_: t_.t[:, 0:D_SSD].rearrange("p (h q) -> p h q", h=H)

        def load_chunk(c, bi):
            k.dma("sp", xs[bi].t[:], self.xsBd[c * 128:(c + 1) * 128, :], [], [xs[bi].b])
            k.dma("sp", tok[bi].t[:].rearrange("p a h -> p (a h)"), self.TOKd[c * 128:(c + 1) * 128, :], [], [tok[bi].b])

        def states(c, bi, d, Hacc, psl):
            ho = 24 * d
            x_, t_, w_, xd = xs[bi], tok[bi], w24[bi], xdd[bi]
            src = 2 if d == 0 else 1
            k.op("dve", lambda e: e.tensor_tensor(out=w_.t[:], in0=t_.t[:, src, ho:ho + 24], in1=t_.t[:, 3, ho:ho + 24], op=ALU.subtract), reads=[t_.b], writes=[w_.b])
            k.op("act", lambda e: e.activation(out=w_.t[:], in_=w_.t[:], func=AF.Exp), reads=[w_.b], writes=[w_.b])
            k.op("dve", lambda e: e.tensor_tensor(out=w_.t[:], in0=w_.t[:], in1=t_.t[:, 0, ho:ho + 24], op=ALU.mult), reads=[w_.b, t_.b], writes=[w_.b])
            k.op("pool", lambda e: e.tensor_tensor(out=xd.t[:].rearrange("p (h q) -> p h q", h=H), in0=xview(x_), in1=w_.t[:].unsqueeze(2).to_broadcast([128, H, Pd]), op=ALU.mult),
                 reads=[x_.b, w_.b], writes=[xd.b])
            for g in range(SSD_G):
                ps_ = psl[g % 2]
                k.op("pe", lambda e, g=g, ps_=ps_: e.matmul(ps_.t[:, 0:384], lhsT=x_.t[:, D_SSD + g * 128:D_SSD + (g + 1) * 128], rhs=xd.t[:, g * 384:(g + 1) * 384], start=True, stop=True),
                     reads=[x_.b, xd.b], writes=[ps_.b])
                hv = Hacc.t[:, g * 384:(g + 1) * 384]
                k.op("dve", lambda e, g=g, hv=hv: e.tensor_tensor(out=hv.rearrange("p (h q) -> p h q", h=6), in0=hv.rearrange("p (h q) -> p h q", h=6),
                                                                 in1=CD.t[:, c, ho + g * 6:ho + g * 6 + 6].unsqueeze(2).to_broadcast([128, 6, Pd]), op=ALU.mult),
                     reads=[Hacc.b, CD.b], writes=[Hacc.b])
                k.op("dve", lambda e, g=g, hv=hv, ps_=ps_: e.tensor_tensor(out=hv, in0=hv, in1=ps_.t[:, 0:384], op=ALU.add), reads=[Hacc.b, ps_.b], writes=[Hacc.b])

        k.op("pool", lambda e: e.memset(Hs.t[:], 0.0), writes=[Hs.b])
        load_chunk(NT - 1, (NT - 1) % 2)
        for c in range(NT - 1, -1, -1):
            bi = c % 2
            if c > 0:
                load_chunk(c - 1, (c - 1) % 2)
            hb_ = Hbf[bi]
            k.op("act", lambda e, hb_=hb_: e.activation(out=hb_.t[:], in_=Hs.t[:], func=AF.Copy), reads=[Hs.b], writes=[hb_.b])
            k.dma("sp", self.Gd[c], hb_.t[:], [hb_.b], [gb])
            if c > 0:
                states(c, bi, 1, Hs, psS)
        self.P.flush()
        bct = [s.sb("sc_bct", [128, 8, 128], BF16) for _ in range(2)]
        bc = [s.sb("sc_bc", [128, 2, H, 128], F32) for _ in range(2)]
        Gc = [s.sb("sc_G", [128, D_SSD], BF16) for _ in range(2)]
        sz = [s.sb("sc_sz", [128, D_SSD], BF16) for _ in range(2)]
        eA = [s.sb("sc_eA", [128, 48], F32) for _ in range(2)]
        ntk = [s.sb("sc_ntk", [128, 2, 48], F32) for _ in range(2)]
        xdt = [[s.sb("sc_xdt", [128, D_SSD], BF16) for _ in range(2)] for _ in range(2)]
        mcb = [[s.sb("sc_mcb", [128, 4, 128], F32) for _ in range(2)] for _ in range(2)]
        seg = [[s.sb("sc_seg", [128, 6, 128], F32) for _ in range(2)] for _ in range(2)]
        MT = [[s.sb("sc_MT", [128, 6, 128], BF16) for _ in range(2)] for _ in range(2)]
        t1 = [s.sb("sc_t1", [128, 384], F32) for _ in range(2)]
        t2 = [s.sb("sc_t2", [128, 384], F32) for _ in range(2)]
        yall = [s.sb("sc_yall", [128, D_SSD], F32) for _ in range(2)]
        junk = s.sb("sc_junk", [128, 384], BF16)
        ss = [s.sb("sc_ss", [128, 4], F32) for _ in range(2)]
        yn = [s.sb("sc_yn", [128, D_SSD], BF16) for _ in range(2)]
        psCB = s.ps("sc_psCB", [128, 4, 128])
        psY = [[s.ps("sc_psY", [128, 512]) for _ in range(3)] for _ in range(2)]
        yb_ = self.P.buf("ycat_ssd")

        def load2(c, bi):
            load_chunk(c, bi)
            k.dma("sp", bct[bi].t[:], self.BCTd.rearrange("g n t -> n g t")[:, :, c * 128:(c + 1) * 128], [], [bct[bi].b])
            k.dma("sp", bc[bi].t[:].rearrange("p d h l -> p (d h l)"), self.ABd[c:c + 1, :].partition_broadcast(128), [], [bc[bi].b])
            k.dma("sp", Gc[bi].t[:], self.Gd[c], [], [Gc[bi].b])
            k.dma("sp", sz[bi].t[:], self.szd[c * 128:(c + 1) * 128, :], [], [sz[bi].b])

        k.op("pool", lambda e: e.memset(Hs.t[:], 0.0), writes=[Hs.b])
        k.op("pool", lambda e: e.memset(Hbf[0].t[:], 0.0), writes=[Hbf[0].b])
        v3 = lambda ap: ap.rearrange("p (h q) -> p h q", h=6)

        def prep(c, g):
            bi, sgi = c % 2, g % 2
            bc_, ntk_ = bc[bi], ntk[bi]
            for d in range(2):
                sg, mt_ = seg[d][sgi], MT[d][sgi]
                for jh in range(6):
                    h = g * 6 + jh
                    k.op("act", lambda e: e.activation(out=sg.t[:, jh, :], in_=bc_.t[:, d, h, :], func=AF.Exp, bias=ntk_.t[:, d, 24 * d + h:24 * d + h + 1]),
                         reads=[bc_.b, ntk_.b], writes=[sg.b])
                k.op("dve", lambda e: e.scalar_tensor_tensor(out=mt_.t[:], in0=sg.t[:], scalar=1.0, in1=mcb[d][bi].t[:, g, :].unsqueeze(1).to_broadcast([128, 6, 128]),
                                                             op0=ALU.min, op1=ALU.mult), reads=[sg.b, mcb[d][bi].b], writes=[mt_.b])

        def head(c):
            bi = c % 2
            x_, t_, b_, eA_, ntk_ = xs[bi], tok[bi], bct[bi], eA[bi], ntk[bi]
            k.op("dve", lambda e: e.tensor_scalar(out=ntk_.t[:], in0=t_.t[:, 1:3, :], scalar1=-1.0, scalar2=None, op0=ALU.mult), reads=[t_.b], writes=[ntk_.b])
            k.op("act", lambda e: e.activation(out=eA_.t[:, 0:24], in_=t_.t[:, 1, 0:24], func=AF.Exp), reads=[t_.b], writes=[eA_.b])
            k.op("act", lambda e: e.activation(out=eA_.t[:, 24:48], in_=t_.t[:, 2, 24:48], func=AF.Exp), reads=[t_.b], writes=[eA_.b])
            for d in range(2):
                k.op("pool", lambda e: e.tensor_tensor(out=xdt[d][bi].t[:].rearrange("p (h q) -> p h q", h=H), in0=xview(x_),
                                                       in1=t_.t[:, 0, 24 * d:24 * d + 24].unsqueeze(2).to_broadcast([128, H, Pd]), op=ALU.mult),
                     reads=[x_.b, t_.b], writes=[xdt[d][bi].b])
            for g in range(SSD_G):
                k.op("pe", lambda e: e.matmul(psCB.t[:, g, :], lhsT=b_.t[:, g, :], rhs=b_.t[:, 4 + g, :], start=True, stop=True), reads=[b_.b], writes=[psCB.b])
            k.op("dve", lambda e: e.tensor_tensor(out=mcb[0][bi].t[:], in0=psCB.t[:], in1=self.maskf.t[:].unsqueeze(1).to_broadcast([128, 4, 128]), op=ALU.mult),
                 reads=[psCB.b, self.maskf.b], writes=[mcb[0][bi].b])
            k.op("dve", lambda e: e.tensor_tensor(out=mcb[1][bi].t[:], in0=psCB.t[:], in1=self.maskb.t[:].unsqueeze(1).to_broadcast([128, 4, 128]), op=ALU.mult),
                 reads=[psCB.b, self.maskb.b], writes=[mcb[1][bi].b])
            prep(c, 0)

        def body(c):
            bi = c % 2
            x_, b_, G_, eA_, ya, hf_ = xs[bi], bct[bi], Gc[bi], eA[bi], yall[bi], Hbf[bi]
            for g in range(SSD_G):
                pY, sgi = psY[g % 2], g % 2
                if g + 1 < SSD_G:
                    prep(c, g + 1)
                for jh in range(6):
                    h = g * 6 + jh
                    for d in range(2):
                        k.op("pe", lambda e: e.matmul(pY[0].t[:, jh * 64:(jh + 1) * 64], lhsT=MT[d][sgi].t[:, jh, :], rhs=xdt[d][bi].t[:, h * 64:(h + 1) * 64],
                                                      start=(d == 0), stop=(d == 1)), reads=[MT[d][sgi].b, xdt[d][bi].b], writes=[pY[0].b])
                k.op("pe", lambda e: e.matmul(pY[1].t[:, 0:384], lhsT=b_.t[:, 4 + g, :], rhs=hf_.t[:, g * 384:(g + 1) * 384], start=True, stop=True),
                     reads=[b_.b, hf_.b], writes=[pY[1].b])
                k.op("pe", lambda e: e.matmul(pY[2].t[:, 0:384], lhsT=b_.t[:, 4 + g, :], rhs=G_.t[:, g * 384:(g + 1) * 384], start=True, stop=True),
                     reads=[b_.b, G_.b], writes=[pY[2].b])
                a1, a2 = t1[sgi], t2[sgi]
                k.op("dve", lambda e: e.tensor_tensor(out=v3(a1.t[:]), in0=v3(pY[1].t[:, 0:384]), in1=eA_.t[:, g * 6:g * 6 + 6].unsqueeze(2).to_broadcast([128, 6, Pd]), op=ALU.mult),
                     reads=[pY[1].b, eA_.b], writes=[a1.b])
                k.op("dve", lambda e: e.tensor_tensor(out=v3(a2.t[:]), in0=v3(pY[2].t[:, 0:384]), in1=eA_.t[:, 24 + g * 6:24 + g * 6 + 6].unsqueeze(2).to_broadcast([128, 6, Pd]), op=ALU.mult),
                     reads=[pY[2].b, eA_.b], writes=[a2.b])
                k.op("pool", lambda e: e.tensor_tensor(out=a1.t[:], in0=a1.t[:], in1=a2.t[:], op=ALU.add), reads=[a1.b, a2.b], writes=[a1.b])
                k.op("dve", lambda e: e.tensor_tensor(out=a1.t[:], in0=pY[0].t[:, 0:384], in1=a1.t[:], op=ALU.add), reads=[pY[0].b, a1.b], writes=[a1.b])
                k.op("pool", lambda e: e.tensor_tensor(out=a2.t[:], in0=x_.t[:, g * 384:(g + 1) * 384], in1=Dful.t[:, g * 384:(g + 1) * 384], op=ALU.mult),
                     reads=[x_.b, Dful.b], writes=[a2.b])
                k.op("pool", lambda e: e.tensor_tensor(out=ya.t[:, g * 384:(g + 1) * 384], in0=a1.t[:], in1=a2.t[:], op=ALU.add), reads=[a1.b, a2.b], writes=[ya.b])

        def tail(c):
            bi = c % 2
            sz_, ya = sz[bi], yall[bi]
            if c + 1 < NT:
                states(c, bi, 0, Hs, psS)
                hn_ = Hbf[(c + 1) % 2]
                k.op("act", lambda e: e.activation(out=hn_.t[:], in_=Hs.t[:], func=AF.Copy), reads=[Hs.b], writes=[hn_.b])
            s_, yn_ = ss[bi], yn[bi]
            k.op("dve", lambda e: e.tensor_tensor(out=ya.t[:], in0=ya.t[:], in1=sz_.t[:], op=ALU.mult), reads=[ya.b, sz_.b], writes=[ya.b])
            for g in range(SSD_G):
                k.op("act", lambda e: e.activation(out=junk.t[:], in_=ya.t[:, g * 384:(g + 1) * 384], func=AF.Square, accum_out=s_.t[:, g:g + 1]), reads=[ya.b], writes=[junk.b, s_.b])
            k.op("act", lambda e: e.activation(out=s_.t[:], in_=s_.t[:], func=AF.Sqrt, scale=1.0 / 384, bias=self.epsc.t[:]), reads=[s_.b, self.epsc.b], writes=[s_.b])
            k.op("dve", lambda e: e.reciprocal(out=s_.t[:], in_=s_.t[:]), reads=[s_.b], writes=[s_.b])
            k.op("dve", lambda e: e.tensor_tensor(out=ya.t[:].rearrange("p (g q) -> p g q", g=4), in0=ya.t[:].rearrange("p (g q) -> p g q", g=4),
                                                  in1=s_.t[:].unsqueeze(2).to_broadcast([128, 4, 384]), op=ALU.mult), reads=[ya.b, s_.b], writes=[ya.b])
            k.op("pool", lambda e: e.tensor_tensor(out=yn_.t[:], in0=ya.t[:], in1=gn.t[:], op=ALU.mult), reads=[ya.b, gn.b], writes=[yn_.b])
            k.dma("sp", self.ycat[c * 128:(c + 1) * 128, 0:D_SSD], yn_.t[:], [yn_.b], [yb_])

        load2(0, 0)
        head(0)
        for c in range(NT):
            if c + 1 < NT:
                load2(c + 1, (c + 1) % 2)
            body(c)
            if c + 1 < NT:
                head(c + 1)
            tail(c)

    def na_layer(self, cs, li):
        j = li // 2
        Win = self.I["na_w_in"][j]
        with Scope(self) as AS:
            hT = AS.sb("hT", [128, ND, T], BF16)
            with Scope(self) as s:
                self.norm_to_hT(s, hT, self.I["norm_mix_g"][li:li + 1, :])
            for hg in range(6):
                with Scope(self) as s:
                    self.na_group(s, j, hg, Win, hT)
            with Scope(self) as s:
                self.xa_block(s, li, hT, Win, 4608)
        self.out_proj(self.I["na_w_out"][j])

    def na_group(self, s, j, hg, Win, hT):
        k = self
        HG = 2
        W = HG * 128
        QT = s.sb("na_QT", [128, HG, T], BF16)
        KT = s.sb("na_KT", [128, HG, T], BF16)
        Va = s.sb("na_Va", [128, NT, HG, 129], BF16)
        oall = s.sb("na_oall", [128, NT, W], BF16)
        pbig = s.ps("na_pbig", [128, 4, 512])
        pbs = [Tl(pbig.t[:, i, :], self.P.buf("na_pb")) for i in range(4)]
        k.op("pool", lambda e: e.memset(Va.t[:], 1.0), writes=[Va.b])
        for which, dst in ((0, QT), (1, KT)):
            w = self.wblock(s, Win, which * 1536 + hg * W, W)
            for et in range(HG):
                self.proj_fm(s, hT, w, W, pbs, et)
                for tc in range(4):
                    if tc % 2 == 0:
                        k.op("act", lambda e: e.activation(out=dst.t[:, et, tc * 512:(tc + 1) * 512], in_=pbs[tc].t, func=AF.Copy), reads=[pbs[tc].b], writes=[dst.b])
                    else:
                        k.op("dve", lambda e: e.tensor_copy(out=dst.t[:, et, tc * 512:(tc + 1) * 512], in_=pbs[tc].t), reads=[pbs[tc].b], writes=[dst.b])
        w = self.wblock(s, Win, 3072 + hg * W, W)
        for tt in range(NT):
            pb = pbs[tt % 4]
            self.proj_tm(hT, w, W, pb, tt)
            k.op("act", lambda e: e.activation(out=Va.t[:, tt, :, 0:128], in_=pb.t[:, 0:W].rearrange("p (h d) -> p h d", h=HG), func=AF.Copy), reads=[pb.b], writes=[Va.b])
        self.P.flush()
        bias = [s.sb("na_bias", [128, 25, 128], F32) for _ in range(2)]
        psS = [Tl(pbig.t[:, 2 * i:2 * i + 2, :].rearrange("p a n -> p (a n)")[:, 0:640].rearrange("p (j q) -> p j q", j=5), self.P.buf("na_psS")) for i in range(2)]
        psO = [s.ps("na_psO", [128, 256]) for _ in range(2)]
        tmp = [s.sb("na_tmp", [128, 5, 128], F32) for _ in range(2)]
        Pm = [s.sb("na_P", [128, 5, 128], BF16) for _ in range(2)]
        rc = [s.sb("na_rc", [128, 1], F32) for _ in range(2)]
        scale = 128.0 ** -0.5
        iters = [(hh, i) for hh in range(HG) for i in range(NT)]

        def geo(i):
            kb = min(max(2 * i - 4, 0), 22)
            case = 0 if i == 0 else 1 if i == 1 else 3 if i == 14 else 4 if i == 15 else 2
            return kb // 2, case

        def s_mm(n):
            hh, i = iters[n]
            kt0, _ = geo(i)
            pS = psS[n % 2]
            for jt in range(5):
                k.op("pe", lambda e: e.matmul(pS.t[:, jt, :], lhsT=KT.t[:, hh, (kt0 + jt) * 128:(kt0 + jt + 1) * 128], rhs=QT.t[:, hh, i * 128:(i + 1) * 128], start=True, stop=True),
                     reads=[KT.b, QT.b], writes=[pS.b])

        for hh in range(HG):
            k.dma("sp", bias[hh % 2].t[:], self.I["na_bias"][j, hg * HG + hh], [], [bias[hh % 2].b])
        s_mm(0)
        for n, (hh, i) in enumerate(iters):
            kt0, case = geo(i)
            bt = bias[hh % 2]
            pS, pO, tm, P_, r_ = psS[n % 2], psO[n % 2], tmp[n % 2], Pm[n % 2], rc[n % 2]
            if n + 1 < len(iters):
                s_mm(n + 1)
            k.op("dve", lambda e: e.scalar_tensor_tensor(out=tm.t[:], in0=pS.t, scalar=scale, in1=bt.t[:, case * 5:(case + 1) * 5, :], op0=ALU.mult, op1=ALU.add),
                 reads=[pS.b, bt.b], writes=[tm.b])
            k.op("act", lambda e: e.activation(out=P_.t[:], in_=tm.t[:], func=AF.Exp), reads=[tm.b], writes=[P_.b])
            for jt in range(5):
                k.op("pe", lambda e: e.matmul(pO.t[:, 0:129], lhsT=P_.t[:, jt, :], rhs=Va.t[:, kt0 + jt, hh, :], start=(jt == 0), stop=(jt == 4)),
                     reads=[P_.b, Va.b], writes=[pO.b])
            k.op("dve", lambda e: e.reciprocal(out=r_.t[:], in_=pO.t[:, 128:129]), reads=[pO.b], writes=[r_.b])
            k.op("act", lambda e: e.activation(out=oall.t[:, i, hh * 128:(hh + 1) * 128], in_=pO.t[:, 0:128], func=AF.Copy, scale=r_.t[:, 0:1]),
                 reads=[pO.b, r_.b], writes=[oall.b])
        yb = self.P.buf("ycat_na")
        k.dma("sp", self.ycat.rearrange("(n p) c -> p n c", p=128)[:, :, hg * W:(hg + 1) * W], oall.t[:], [oall.b], [yb])

    def moe_layer(self, cs, li):
        k = self
        with Scope(self) as MS:
            AFF = MS.sb("m_AFF", [128, NT, NE], F32)
            IDX = MS.sb("m_IDX", [128, 2 * NE], I32)
            GATE = MS.sb("m_GATE", [128, 2 * NE], F32)
            ring = [MS.sb("e_ring", [128, 8192], BF16) for _ in range(6)]
            for ci in range(6):
                self.exp_wload(ring, li, 0, ci)
            with Scope(self) as s:
                self.moe_router(s, li, AFF)
            with Scope(self) as s:
                self.moe_topk(s, AFF, IDX, GATE)
            with Scope(self) as s:
                self.moe_experts(s, li, IDX, GATE, ring)

    def exp_wload(self, ring, li, ex, ci):
        r_ = ring[ci]
        if ci < 4:
            wname, fh = ("moe_w1", "moe_w3")[ci % 2], ci // 2
            self.dma("pool", r_.t[:].rearrange("p (k n) -> p k n", k=ND), self.I[wname][li, ex][:, fh * 512:(fh + 1) * 512].rearrange("(k p) n -> p k n", p=128), [], [r_.b])
        else:
            dh = ci - 4
            self.dma("pool", r_.t[:].rearrange("p (k n) -> p k n", k=8), self.I["moe_w2"][li, ex][:, dh * 1024:(dh + 1) * 1024].rearrange("(k p) n -> p k n", p=128), [], [r_.b])

    def moe_router(self, s, li, AFF):
        k = self
        g = s.sb("g", [128, D], F32)
        k.bcast_load("sp", g, self.I["norm_ffn_g"][li:li + 1, :])
        Wr = s.sb("m_Wr", [128, ND, NE], F32)
        k.dma("sp", Wr.t[:], self.I["moe_w_router"][li].rearrange("(k p) e -> p k e", p=128), [], [Wr.b])
        hf = [s.sb("m_hf", [128, D], F32) for _ in range(2)]
        hTf = [s.sb("m_hTf", [128, ND, 128], F32) for _ in range(2)]
        ptf = [s.ps("m_ptf", [128, 4, 128], F32) for _ in range(4)]
        pl = [s.ps("m_pl", [128, NE], F32) for _ in range(2)]
        sm = [s.sb("m_sm", [128, 4], F32) for _ in range(2)]
        ex = [s.sb("m_ex", [128, NE], F32) for _ in range(2)]
        hb = self.P.buf("hn")
        def front(tt):
            self.norm_tile(s, self.xres[tt * 128:(tt + 1) * 128, :], g, want_f32=hf[tt % 2], hn_dst=(self.hn[tt * 128:(tt + 1) * 128, :], hb))

        def tail(tt):
            h_, hT_, pl_, sm_, ex_ = hf[tt % 2], hTf[tt % 2], pl[tt % 2], sm[tt % 2], ex[tt % 2]
            for q4 in range(4):
                p_ = ptf[q4]
                for jj in range(4):
                    kk = q4 * 4 + jj
                    k.op("pe", lambda e: e.transpose(out=p_.t[:, jj, :], in_=h_.t[:, kk * 128:(kk + 1) * 128], identity=self.identf.t[:]),
                         reads=[h_.b, self.identf.b], writes=[p_.b])
                if q4 % 2 == 0:
                    k.op("dve", lambda e: e.tensor_copy(out=hT_.t[:, q4 * 4:(q4 + 1) * 4, :], in_=p_.t[:]), reads=[p_.b], writes=[hT_.b])
                else:
                    k.op("act", lambda e: e.activation(out=hT_.t[:, q4 * 4:(q4 + 1) * 4, :], in_=p_.t[:], func=AF.Copy), reads=[p_.b], writes=[hT_.b])
            for kk in range(ND):
                k.op("pe", lambda e: e.matmul(pl_.t[:], lhsT=hT_.t[:, kk, :], rhs=Wr.t[:, kk, :], start=(kk == 0), stop=(kk == ND - 1)),
                     reads=[hT_.b, Wr.b], writes=[pl_.b])
            k.op("dve", lambda e: e.reduce_max(out=sm_.t[:, 0:1], in_=pl_.t[:], axis=AX.X), reads=[pl_.b], writes=[sm_.b])
            k.op("dve", lambda e: e.tensor_scalar(out=sm_.t[:, 1:2], in0=sm_.t[:, 0:1], scalar1=-1.0, scalar2=None, op0=ALU.mult), reads=[sm_.b], writes=[sm_.b])
            k.op("act", lambda e: e.activation(out=ex_.t[:], in_=pl_.t[:], func=AF.Exp, bias=sm_.t[:, 1:2], accum_out=sm_.t[:, 2:3]),
                 reads=[pl_.b, sm_.b], writes=[ex_.b, sm_.b])
            k.op("dve", lambda e: e.reciprocal(out=sm_.t[:, 3:4], in_=sm_.t[:, 2:3]), reads=[sm_.b], writes=[sm_.b])
            k.op("dve", lambda e: e.tensor_scalar(out=AFF.t[:, tt, :], in0=ex_.t[:], scalar1=sm_.t[:, 3:4], scalar2=None, op0=ALU.mult),
                 reads=[ex_.b, sm_.b], writes=[AFF.b])

        front(0)
        for tt in range(NT):
            if tt + 1 < NT:
                front(tt + 1)
            tail(tt)

    def moe_topk(self, s, AFF, IDX, GATE):
        k = self
        affT = s.sb("k_affT", [NE, T], F32)
        work = s.sb("k_work", [NE, T], F32)
        mx8 = s.sb("k_mx8", [NE, 8], F32)
        maskT = s.sb("k_mask", [NE, T], F32)
        pos = s.sb("k_pos", [NE, T], F32)
        POSM = s.sb("k_POSM", [128, NT, NE], F32)
        pA = [s.ps("k_pA", [128, 512]) for _ in range(4)]
        pP = s.ps("k_pP", [128, NT, NE], F32)
        for tt in range(NT):
            k.op("pe", lambda e, tt=tt: e.transpose(out=pA[tt // 4].t[0:NE, (tt % 4) * 128:(tt % 4 + 1) * 128], in_=AFF.t[:, tt, :], identity=self.identf.t[:]),
                 reads=[AFF.b, self.identf.b], writes=[pA[tt // 4].b])
        for q4 in range(4):
            k.op("dve", lambda e, q4=q4: e.tensor_copy(out=affT.t[:, q4 * 512:(q4 + 1) * 512], in_=pA[q4].t[0:NE, :]), reads=[pA[q4].b], writes=[affT.b])
        src = affT
        for it in range(CAP // 8):
            k.op("dve", lambda e, src=src: e.max(out=mx8.t[:], in_=src.t[:]), reads=[src.b], writes=[mx8.b])
            if it < CAP // 8 - 1:
                k.op("dve", lambda e, src=src: e.match_replace(out=work.t[:], in_to_replace=mx8.t[:], in_values=src.t[:], imm_value=-1.0), reads=[src.b, mx8.b], writes=[work.b])
                src = work
        k.op("dve", lambda e: e.tensor_scalar(out=maskT.t[:], in0=affT.t[:], scalar1=mx8.t[:, 7:8], scalar2=None, op0=ALU.is_ge), reads=[affT.b, mx8.b], writes=[maskT.b])
        k.op("dve", lambda e: e.tensor_tensor_scan(out=pos.t[:], data0=maskT.t[:], data1=maskT.t[:], initial=0.0, op0=ALU.add, op1=ALU.bypass), reads=[maskT.b], writes=[pos.b])
        k.op("dve", lambda e: e.tensor_tensor(out=pos.t[:], in0=pos.t[:], in1=maskT.t[:], op=ALU.mult), reads=[pos.b, maskT.b], writes=[pos.b])
        k.op("dve", lambda e: e.tensor_scalar(out=pos.t[:], in0=pos.t[:], scalar1=-1.0, scalar2=None, op0=ALU.add), reads=[pos.b], writes=[pos.b])
        for tt in range(NT):
            k.op("pe", lambda e, tt=tt: e.transpose(out=pP.t[:, tt, :], in_=pos.t[:, tt * 128:(tt + 1) * 128], identity=self.identf.t[0:NE, 0:NE]),
                 reads=[pos.b, self.identf.b], writes=[pP.b])
        k.op("dve", lambda e: e.tensor_copy(out=POSM.t[:], in_=pP.t[:]), reads=[pP.b], writes=[POSM.b])
        RV = s.sb("k_RV", [128, NT, NE, 5], BF16)
        r1 = s.sb("k_r1", [128, NT, NE], F32)
        gh = s.sb("k_gh", [128, NT, NE], BF16)
        pcol = s.sb("k_pcol", [128, 1], F32)
        k.op("pool", lambda e: e.iota(pcol.t[:], pattern=[[0, 1]], base=0, channel_multiplier=1, allow_small_or_imprecise_dtypes=True), writes=[pcol.b])
        for tt in range(NT):
            k.op("pool", lambda e, tt=tt: e.memset(RV.t[:, tt, :, 0], float(tt)), writes=[RV.b])
        k.op("dve", lambda e: e.tensor_copy(out=RV.t[:, :, :, 1].rearrange("p a b -> p (a b)"), in_=pcol.t[:].to_broadcast([128, NT * NE])), reads=[pcol.b], writes=[RV.b])
        k.op("dve", lambda e: e.tensor_copy(out=gh.t[:], in_=AFF.t[:]), reads=[AFF.b], writes=[gh.b])
        k.op("dve", lambda e: e.tensor_copy(out=RV.t[:, :, :, 2], in_=gh.t[:]), reads=[gh.b], writes=[RV.b])
        k.op("dve", lambda e: e.tensor_tensor(out=r1.t[:], in0=AFF.t[:], in1=gh.t[:], op=ALU.subtract), reads=[AFF.b, gh.b], writes=[r1.b])
        k.op("dve", lambda e: e.tensor_copy(out=gh.t[:], in_=r1.t[:]), reads=[r1.b], writes=[gh.b])
        k.op("dve", lambda e: e.tensor_copy(out=RV.t[:, :, :, 3], in_=gh.t[:]), reads=[gh.b], writes=[RV.b])
        k.op("dve", lambda e: e.tensor_tensor(out=r1.t[:], in0=r1.t[:], in1=gh.t[:], op=ALU.subtract), reads=[r1.b, gh.b], writes=[r1.b])
        k.op("dve", lambda e: e.tensor_copy(out=RV.t[:, :, :, 4], in_=r1.t[:]), reads=[r1.b], writes=[RV.b])
        O = [s.sb("k_O", [128, NT, CAP], BF16) for _ in range(2)]
        pz = [s.ps("k_pz", [128, 8], F32) for _ in range(2)]
        idf = s.sb("k_idf", [128, 2 * NE], F32)
        pzs = [s.sb("k_pzs", [128, 8], F32) for _ in range(2)]
        zi = 0
        for ex in range(NE):
            O_ = O[ex % 2]
            for tt in range(NT):
                k.op("dve", lambda e, tt=tt, ex=ex, O_=O_: e.tensor_scalar(out=O_.t[:, tt, :], in0=self.iota256.t[:], scalar1=POSM.t[:, tt, ex:ex + 1], scalar2=None, op0=ALU.is_equal),
                     reads=[self.iota256.b, POSM.b], writes=[O_.b])
            for half in range(2):
                pz_ = pz[zi % 2]
                zi += 1
                col = ex * 2 + half
                for tt in range(NT):
                    k.op("pe", lambda e, tt=tt, ex=ex, half=half, O_=O_, pz_=pz_: e.matmul(pz_.t[:, 0:5], lhsT=O_.t[:, tt, half * 128:(half + 1) * 128], rhs=RV.t[:, tt, ex, :], start=(tt == 0), stop=(tt == NT - 1)),
                         reads=[O_.b, RV.b], writes=[pz_.b])
                zs_ = pzs[zi % 2]
                k.op("dve", lambda e, pz_=pz_, zs_=zs_: e.tensor_copy(out=zs_.t[:, 0:5], in_=pz_.t[:, 0:5]), reads=[pz_.b], writes=[zs_.b])
                k.op("dve", lambda e, col=col, zs_=zs_: e.scalar_tensor_tensor(out=idf.t[:, col:col + 1], in0=zs_.t[:, 0:1], scalar=128.0, in1=zs_.t[:, 1:2], op0=ALU.mult, op1=ALU.add),
                     reads=[zs_.b], writes=[idf.b])
                k.op("dve", lambda e, col=col, zs_=zs_: e.tensor_reduce(out=GATE.t[:, col:col + 1], in_=zs_.t[:, 2:5], axis=AX.X, op=ALU.add), reads=[zs_.b], writes=[GATE.b])
        k.op("dve", lambda e: e.tensor_copy(out=IDX.t[:], in_=idf.t[:]), reads=[idf.b], writes=[IDX.b])

    def moe_experts(self, s, li, IDX, GATE, ring):
        k = self
        xg = [s.sb("e_xg", [128, D], BF16) for _ in range(4)]
        xsT = [s.sb("e_xsT", [128, ND, CAP], BF16) for _ in range(2)]
        gT = [s.sb("e_gT", [128, 8, CAP], BF16) for _ in range(2)]
        sa = [s.sb("e_sa", [128, 512], F32) for _ in range(2)]
        gtok = [s.sb("e_gtok", [128, 512], BF16) for _ in range(2)]
        ysb = [s.sb("e_y", [128, D], F32) for _ in range(4)]
        ptr = [s.ps("e_ptr", [128, 8, 128], BF16) for _ in range(2)]
        pab = [s.ps("e_pab", [128, 512]) for _ in range(4)]
        py = [s.ps("e_py", [128, 512]) for _ in range(2)]
        xb = [self.P.buf("xres_moe") for _ in range(4)]
        hnb = self.P.buf("hn_r")
        cnt = dict(ti=0, ai=0, yi=0)
        chain = [[self.P.buf("moe_chain") for _ in range(4)] for _ in range(2)]
        prev_sc = {}
        for cq in range(4):
            o_ = k.dma("sp", self.moeh[cq][:, :], self.xres[:, cq * 512:(cq + 1) * 512], [], [xb[cq]])
            prev_sc[(0, cq)] = prev_sc[(1, cq)] = o_

        def gathers(ex):
            for half in range(2):
                col = ex * 2 + half
                x_ = xg[col % 4]
                k.op("pool", lambda e: e.indirect_dma_start(out=x_.t[:, :], out_offset=None, in_=self.hn[:, :],
                                                            in_offset=bass.IndirectOffsetOnAxis(ap=IDX.t[:, col:col + 1], axis=0)),
                     reads=[hnb, IDX.b], writes=[x_.b], dma=True)

        def wload(ex, ci):
            self.exp_wload(ring, li, ex, ci)

        gathers(0)
        for ex in range(NE):
            xT, g_ = xsT[ex % 2], gT[ex % 2]
            nxt = ex + 1 < NE
            if nxt:
                gathers(ex + 1)
            for half in range(2):
                x_ = xg[(ex * 2 + half) % 4]
                for kg in range(2):
                    p_ = ptr[cnt["ti"] % 2]
                    cnt["ti"] += 1
                    for jj in range(8):
                        kk = kg * 8 + jj
                        k.op("pe", lambda e: e.transpose(out=p_.t[:, jj, :], in_=x_.t[:, kk * 128:(kk + 1) * 128], identity=self.ident.t[:]),
                             reads=[x_.b, self.ident.b], writes=[p_.b])
                    if kg == 0:
                        k.op("dve", lambda e: e.tensor_copy(out=xT.t[:, kg * 8:(kg + 1) * 8, half * 128:(half + 1) * 128], in_=p_.t[:]), reads=[p_.b], writes=[xT.b])
                    else:
                        k.op("act", lambda e: e.activation(out=xT.t[:, kg * 8:(kg + 1) * 8, half * 128:(half + 1) * 128], in_=p_.t[:], func=AF.Copy), reads=[p_.b], writes=[xT.b])
            for fh in range(2):
                w1, w3 = ring[2 * fh], ring[2 * fh + 1]
                rv1 = w1.t[:].rearrange("p (k n) -> p k n", k=ND)
                rv3 = w3.t[:].rearrange("p (k n) -> p k n", k=ND)
                for half in range(2):
                    pa, pb = pab[cnt["ai"] % 4], pab[(cnt["ai"] + 1) % 4]
                    cnt["ai"] += 2
                    for kk in range(ND):
                        k.op("pe", lambda e: e.matmul(pa.t[:], lhsT=xT.t[:, kk, half * 128:(half + 1) * 128], rhs=rv1[:, kk, :], start=(kk == 0), stop=(kk == ND - 1)),
                             reads=[w1.b, xT.b], writes=[pa.b])
                        k.op("pe", lambda e: e.matmul(pb.t[:], lhsT=xT.t[:, kk, half * 128:(half + 1) * 128], rhs=rv3[:, kk, :], start=(kk == 0), stop=(kk == ND - 1)),
                             reads=[w3.b, xT.b], writes=[pb.b])
                    s_, gk = sa[cnt["ai"] // 2 % 2], gtok[cnt["ai"] // 2 % 2]
                    k.op("act", lambda e: e.activation(out=s_.t[:], in_=pa.t[:], func=AF.Silu), reads=[pa.b], writes=[s_.b])
                    k.op("dve", lambda e: e.tensor_tensor(out=gk.t[:], in0=s_.t[:], in1=pb.t[:], op=ALU.mult), reads=[s_.b, pb.b], writes=[gk.b])
                    p_ = ptr[cnt["ti"] % 2]
                    cnt["ti"] += 1
                    for jj in range(4):
                        k.op("pe", lambda e: e.transpose(out=p_.t[:, jj, :], in_=gk.t[:, jj * 128:(jj + 1) * 128], identity=self.ident.t[:]),
                             reads=[gk.b, self.ident.b], writes=[p_.b])
                    k.op("act", lambda e: e.activation(out=g_.t[:, fh * 4:(fh + 1) * 4, half * 128:(half + 1) * 128], in_=p_.t[:, 0:4, :], func=AF.Copy),
                         reads=[p_.b], writes=[g_.b])
                if nxt:
                    wload(ex + 1, 2 * fh)
                    wload(ex + 1, 2 * fh + 1)
            ys = [ysb[(ex * 2) % 4], ysb[(ex * 2 + 1) % 4]]
            for dh in range(2):
                r_ = ring[4 + dh]
                rv = r_.t[:].rearrange("p (k n) -> p k n", k=8)
                for half in range(2):
                    col = ex * 2 + half
                    for dc in range(2):
                        p_ = py[cnt["yi"] % 2]
                        cnt["yi"] += 1
                        for ft in range(8):
                            k.op("pe", lambda e: e.matmul(p_.t[:], lhsT=g_.t[:, ft, half * 128:(half + 1) * 128], rhs=rv[:, ft, dc * 512:(dc + 1) * 512], start=(ft == 0), stop=(ft == 7)),
                                 reads=[g_.b, r_.b], writes=[p_.b])
                        o0 = dh * 1024 + dc * 512
                        y_ = ys[half]
                        if dc == 0:
                            k.op("act", lambda e: e.activation(out=y_.t[:, o0:o0 + 512], in_=p_.t[:], func=AF.Copy, scale=GATE.t[:, col:col + 1]),
                                 reads=[p_.b, GATE.b], writes=[y_.b])
                        else:
                            k.op("dve", lambda e: e.tensor_scalar(out=y_.t[:, o0:o0 + 512], in0=p_.t[:], scalar1=GATE.t[:, col:col + 1], scalar2=None, op0=ALU.mult),
                                 reads=[p_.b, GATE.b], writes=[y_.b])
                if nxt:
                    wload(ex + 1, 4 + dh)
            cur = {}
            for half in range(2):
                col = ex * 2 + half
                y_ = ys[half]
                for cq in range(4):
                    cur[(half, cq)] = k.op("pool", lambda e: e.indirect_dma_start(out=self.moeh[cq][:, :], out_offset=bass.IndirectOffsetOnAxis(ap=IDX.t[:, col:col + 1], axis=0),
                                                                                  in_=y_.t[:, cq * 512:(cq + 1) * 512], in_offset=None, compute_op=ALU.add),
                                           reads=[y_.b, IDX.b], writes=[chain[half][cq]], dma=True, extra=[prev_sc[(1 - half, cq)]])
            prev_sc = cur
        for cq in range(4):
            k.dma("sp", self.xres[:, cq * 512:(cq + 1) * 512], self.moeh[cq][:, :], [chain[0][cq], chain[1][cq]], [xb[cq]])


def _na_bias_tiles(rpb):
    n, Hh = rpb.shape[0], rpb.shape[1]
    out = np.full((n, Hh, 128, 25, 128), NEG, dtype=np.float32)
    a = np.arange(2)[:, None]; wk = np.arange(64)[None, :]
    for case, i in enumerate((0, 1, 2, 14, 15)):
        kb = min(max(2 * i - 4, 0), 22)
        for jt in range(5):
            for aa in range(2):
                kr = kb + 2 * jt + aa
                for bb in range(2):
                    r = 2 * i + bb
                    rs = min(max(r - 4, 0), 24)
                    if not (rs <= kr < rs + 8):
                        continue
                    dr = kr - r + 7
                    wq = np.arange(64)
                    ws = np.clip(wq - 8, 0, 48)
                    wkk = np.arange(64)[:, None]
                    valid = (wkk >= ws[None, :]) & (wkk < ws[None, :] + 16)
                    dc = np.clip(wkk - wq[None, :] + 15, 0, 30)
                    vals = rpb[:, :, dr, :][:, :, dc]
                    blk = out[:, :, aa * 64:(aa + 1) * 64, case * 5 + jt, bb * 64:(bb + 1) * 64]
                    blk[...] = np.where(valid[None, None], vals, NEG)
    return out


def _prep(inputs, nlayers=DEPTH):
    n_ssd = (nlayers + 1) // 2
    n_na = nlayers // 2
    f = lambda a: np.ascontiguousarray(a, dtype=np.float32)
    common = {
        "norm_mix_g": f(inputs["norm_mix_g"][:nlayers]), "norm_ffn_g": f(inputs["norm_ffn_g"][:nlayers]),
        "norm_final_g": f(inputs["norm_final_g"]).reshape(1, D), "mem_norm_g": f(inputs["mem_norm_g"]).reshape(1, D),
        "ssd_w_in": f(inputs["ssd_w_in"][:n_ssd]), "ssd_conv_w": f(inputs["ssd_conv_w"][:n_ssd]), "ssd_conv_b": f(inputs["ssd_conv_b"][:n_ssd]),
        "ssd_dt_bias": f(inputs["ssd_dt_bias"][:n_ssd]).reshape(n_ssd, 48), "ssd_a_log": f(inputs["ssd_a_log"][:n_ssd]).reshape(n_ssd, 48),
        "ssd_d": f(inputs["ssd_d"][:n_ssd]), "ssd_gate_norm_g": f(inputs["ssd_gate_norm_g"][:n_ssd]), "ssd_w_out": f(inputs["ssd_w_out"][:n_ssd]),
        "xa_w_kv": f(inputs["xa_w_kv"][:nlayers]), "moe_w_router": f(inputs["moe_w_router"][:nlayers]),
        "moe_w1": f(inputs["moe_w1"][:nlayers]), "moe_w3": f(inputs["moe_w3"][:nlayers]), "moe_w2": f(inputs["moe_w2"][:nlayers]),
    }
    if n_na:
        common["na_w_in"] = f(inputs["na_w_in"][:n_na])
        common["na_bias"] = _na_bias_tiles(f(inputs["na_rpb"][:n_na]))
        common["na_w_out"] = f(inputs["na_w_out"][:n_na])
    return common


_NC_CACHE = {}


def kernel(**inputs):
    x = np.asarray(inputs["x"], dtype=np.float32)
    mem = np.asarray(inputs["mem"], dtype=np.float32)
    common = _prep(inputs)
    if "nc" not in _NC_CACHE:
        _NC_CACHE["nc"] = KB().build()
    nc = _NC_CACHE["nc"]
    B = x.shape[0]
    in_maps = []
    for c in range(8):
        b = c % B
        m = dict(common)
        m["x"] = np.ascontiguousarray(x[b])
        m["mem"] = np.ascontiguousarray(mem[b])
        in_maps.append(m)
    res = run_bass_kernel_spmd(nc, in_maps, core_ids=list(range(8)))
    return np.stack([np.asarray(res.results[b]["out"], dtype=np.float32) for b in range(B)], axis=0)
```
